# Optimizing a Trainium2 kernel written in Bass

```python
import math
import jax, jax.numpy as jnp
from jax import lax
import numpy as np

D_MODEL = 1024
BATCH = 32
SEQ = 2048
DEPTH = 2

CHUNK = 64
CONV_K = 4
EPS = 1e-6
A_HEADS = D_MODEL // 256
A_DK = 128
A_DV = 128
A_QK_W = A_HEADS * A_DK
A_V_W = A_HEADS * A_DV
B_HEADS = D_MODEL // 256
B_DK = 128
B_DV = 128
B_QK_W = B_HEADS * B_DK
B_V_W = B_HEADS * B_DV
ROPE_BASE = 10000.0
C_INNER = D_MODEL
C_HEAD_DIM = 64
C_HEADS = C_INNER // C_HEAD_DIM
C_GROUPS = 2
C_HEADS_PER_GROUP = C_HEADS // C_GROUPS
C_STATE = 128
C_XBC_W = C_INNER + 2 * C_GROUPS * C_STATE
N_BRANCH = 3
N_GROUPS = 4
EXPERTS_PER_GROUP = 8
N_EXPERTS = N_GROUPS * EXPERTS_PER_GROUP
TOP_K_IN_GROUP = 2
D_EXPERT = D_MODEL // 2
DEEPNORM_ALPHA = (2 * DEPTH) ** 0.25
DEEPNORM_BETA = (8 * DEPTH) ** -0.25
IN_SIZES = (2 * A_QK_W + A_V_W, A_V_W, A_HEADS, A_HEADS,
            B_QK_W, B_QK_W, B_V_W, B_V_W,
            C_INNER, C_XBC_W, C_HEADS,
            N_BRANCH * D_MODEL)
P_IN = sum(IN_SIZES)

kernel_name = "hybrid_deltanet_retention_ssd_hmoe_deepnorm"


def _layernorm(x, g, b):
    xf = x.astype(jnp.float32)
    mu = jnp.mean(xf, axis=-1, keepdims=True)
    var = jnp.mean(jnp.square(xf - mu), axis=-1, keepdims=True)
    return ((xf - mu) * lax.rsqrt(var + EPS) * g + b).astype(x.dtype)


def _rmsnorm(x, g):
    xf = x.astype(jnp.float32)
    return xf * lax.rsqrt(jnp.mean(jnp.square(xf), axis=-1, keepdims=True) + EPS) * g


def _l2norm(t):
    return t * lax.rsqrt(jnp.sum(jnp.square(t), axis=-1, keepdims=True) + EPS)


def _causal_conv(x, w):
    k = w.shape[0]
    return lax.conv_general_dilated(x, w[:, None, :], window_strides=(1,), padding=[(k - 1, 0)],
                                    dimension_numbers=('NWC', 'WIO', 'NWC'),
                                    feature_group_count=x.shape[-1])


def _rope(t, cos, sin):
    half = t.shape[-1] // 2
    t1, t2 = t[..., :half], t[..., half:]
    return jnp.concatenate([t1 * cos - t2 * sin, t1 * sin + t2 * cos], axis=-1)


def _gated_deltanet(qkv, z, a, b, conv_w, a_log, dt_bias, norm_w):
    f32 = jnp.float32
    bsz, s, _ = qkv.shape
    nc = s // CHUNK
    qkv = jax.nn.silu(_causal_conv(qkv, conv_w)).astype(f32)
    q, k, v = jnp.split(qkv, [A_QK_W, 2 * A_QK_W], axis=-1)

    def heads(t, d):
        return t.reshape(bsz, nc, CHUNK, A_HEADS, d).transpose(0, 3, 1, 2, 4)

    def per_head(t):
        return t.reshape(bsz, nc, CHUNK, A_HEADS).transpose(0, 3, 1, 2)

    q = _l2norm(heads(q, A_DK)) * (A_DK ** -0.5)
    k = _l2norm(heads(k, A_DK))
    v = heads(v, A_DV)
    beta = per_head(jax.nn.sigmoid(b.astype(f32)))
    g = per_head(-jnp.exp(a_log.astype(f32)) * jax.nn.softplus(a.astype(f32) + dt_bias))
    gc = jnp.cumsum(g, axis=-1)
    strict = jnp.tril(jnp.ones((CHUNK, CHUNK), bool), -1)
    diff = gc[..., :, None] - gc[..., None, :]
    decay = jnp.where(strict, jnp.exp(jnp.where(strict, diff, 0.0)), 0.0)
    amat = beta[..., :, None] * jnp.einsum('bhcid,bhcjd->bhcij', k, k) * decay
    lhs = amat + jnp.eye(CHUNK, dtype=f32)
    rhs = jnp.concatenate([v * beta[..., None], k * (beta * jnp.exp(gc))[..., None]], axis=-1)
    sol = lax.linalg.triangular_solve(lhs, rhs, left_side=True, lower=True, unit_diagonal=True)
    u, w = sol[..., :A_DV], sol[..., A_DV:]
    g_last = gc[..., -1]
    k_end = k * jnp.exp(g_last[..., None] - gc)[..., None]

    def step(state, inp):
        q_c, k_c, u_c, w_c, dec_c = inp
        delta = u_c - jnp.einsum('bhlk,bhkv->bhlv', w_c, state)
        state = dec_c[..., None, None] * state + jnp.einsum('bhlk,bhlv->bhkv', k_c, delta)
        return state, jnp.einsum('bhlk,bhkv->bhlv', q_c, state)

    xs = (jnp.moveaxis(q, 2, 0), jnp.moveaxis(k_end, 2, 0), jnp.moveaxis(u, 2, 0),
          jnp.moveaxis(w, 2, 0), jnp.moveaxis(jnp.exp(g_last), 2, 0))
    s0 = jnp.zeros((bsz, A_HEADS, A_DK, A_DV), f32)
    _, o = lax.scan(step, s0, xs)
    o = o.transpose(1, 0, 3, 2, 4).reshape(bsz, s, A_HEADS, A_DV)
    o = _rmsnorm(o, norm_w) * jax.nn.silu(z.astype(f32).reshape(bsz, s, A_HEADS, A_DV))
    return o.reshape(bsz, s, A_V_W)


def _retention(q, k, v, gate, norm_w, cos, sin):
    f32 = jnp.float32
    bsz, s, _ = q.shape
    nc = s // CHUNK
    q = _rope(q.astype(f32).reshape(bsz, s, B_HEADS, B_DK), cos, sin)
    k = _rope(k.astype(f32).reshape(bsz, s, B_HEADS, B_DK), cos, sin) * (B_DK ** -0.5)

    def heads(t, d):
        return t.reshape(bsz, nc, CHUNK, B_HEADS, d).transpose(0, 3, 1, 2, 4)

    qh, kh, vh = heads(q, B_DK), heads(k, B_DK), heads(v.astype(f32), B_DV)
    log_gamma = jnp.log1p(-jnp.exp2(-5.0 - jnp.arange(B_HEADS, dtype=f32)))
    idx = jnp.arange(CHUNK, dtype=f32)
    intra_decay = jnp.exp(log_gamma[:, None, None] * jnp.abs(idx[:, None] - idx[None, :]))
    read_decay = jnp.exp(log_gamma[:, None] * (idx + 1.0))
    write_decay = jnp.exp(log_gamma[:, None] * (CHUNK - 1.0 - idx))
    chunk_decay = jnp.exp(log_gamma * CHUNK)
    scores = jnp.einsum('bhcid,bhcjd->bhcij', qh, kh) * intra_decay[:, None]
    o_intra = jnp.einsum('bhcij,bhcjd->bhcid', scores, vh)
    kw = kh * write_decay[:, None, :, None]

    def step(state, inp):
        q_c, k_c, v_c = inp
        o = jnp.einsum('bhlk,bhkv->bhlv', q_c, state) * read_decay[:, :, None]
        state = chunk_decay[:, None, None] * state + jnp.einsum('bhlk,bhlv->bhkv', k_c, v_c)
        return state, o

    r0 = jnp.zeros((bsz, B_HEADS, B_DK, B_DV), f32)
    _, o_inter = lax.scan(step, r0, (jnp.moveaxis(qh, 2, 0), jnp.moveaxis(kw, 2, 0), jnp.moveaxis(vh, 2, 0)))
    o = o_intra + jnp.moveaxis(o_inter, 0, 2)
    o = o.transpose(0, 2, 3, 1, 4).reshape(bsz, s, B_HEADS, B_DV)
    mu = jnp.mean(o, axis=-1, keepdims=True)
    var = jnp.mean(jnp.square(o - mu), axis=-1, keepdims=True)
    o = (o - mu) * lax.rsqrt(var + EPS) * norm_w.reshape(B_HEADS, B_DV)
    o = o * jax.nn.silu(gate.astype(f32).reshape(bsz, s, B_HEADS, B_DV))
    return o.reshape(bsz, s, B_V_W)


def _ssd(z, xbc, dt, conv_w, conv_b, dt_bias, a_log, d_skip, norm_w):
    f32 = jnp.float32
    bsz, s, _ = xbc.shape
    nc = s // CHUNK
    xbc = jax.nn.silu(_causal_conv(xbc, conv_w) + conv_b).astype(f32)
    xs, bm, cm = jnp.split(xbc, [C_INNER, C_INNER + C_GROUPS * C_STATE], axis=-1)
    xh = xs.reshape(bsz, nc, CHUNK, C_GROUPS, C_HEADS_PER_GROUP, C_HEAD_DIM).transpose(0, 3, 4, 1, 2, 5)
    bm = bm.reshape(bsz, nc, CHUNK, C_GROUPS, C_STATE).transpose(0, 3, 1, 2, 4)
    cm = cm.reshape(bsz, nc, CHUNK, C_GROUPS, C_STATE).transpose(0, 3, 1, 2, 4)
    dt = jax.nn.softplus(dt.astype(f32) + dt_bias)
    dt = dt.reshape(bsz, nc, CHUNK, C_GROUPS, C_HEADS_PER_GROUP).transpose(0, 3, 4, 1, 2)
    a = -jnp.exp(a_log.astype(f32)).reshape(C_GROUPS, C_HEADS_PER_GROUP)
    lc = jnp.cumsum(dt * a[None, :, :, None, None], axis=-1)
    decay = jnp.exp(-jnp.abs(lc[..., :, None] - lc[..., None, :]))
    cb = jnp.einsum('bgcin,bgcjn->bgcij', cm, bm)
    wmat = cb[:, :, None] * decay * dt[..., None, :]
    y = jnp.einsum('bghcij,bghcjp->bghcip', wmat, xh)

    def step(state, inp):
        c_c, b_c, x_c, lc_c, dt_c = inp
        y_c = jnp.einsum('bgln,bghnp->bghlp', c_c, state) * jnp.exp(lc_c)[..., None]
        wr = jnp.exp(lc_c[..., -1:] - lc_c) * dt_c
        state = (jnp.exp(lc_c[..., -1])[..., None, None] * state
                 + jnp.einsum('bgln,bghlp->bghnp', b_c, x_c * wr[..., None]))
        return state, y_c

    h0 = jnp.zeros((bsz, C_GROUPS, C_HEADS_PER_GROUP, C_STATE, C_HEAD_DIM), f32)
    _, y_inter = lax.scan(step, h0, (jnp.moveaxis(cm, 2, 0), jnp.moveaxis(bm, 2, 0), jnp.moveaxis(xh, 3, 0),
                                     jnp.moveaxis(lc, 3, 0), jnp.moveaxis(dt, 3, 0)))
    y = y + jnp.moveaxis(y_inter, 0, 3) + d_skip.reshape(C_GROUPS, C_HEADS_PER_GROUP)[None, :, :, None, None, None] * xh
    y = y.transpose(0, 3, 4, 1, 2, 5).reshape(bsz, s, C_INNER)
    y = y * jax.nn.silu(z.astype(f32))
    y = _rmsnorm(y.reshape(bsz, s, C_GROUPS, C_INNER // C_GROUPS), norm_w.reshape(C_GROUPS, -1))
    return y.reshape(bsz, s, C_INNER)


def _token_mixers(x, w_in, conv_a, a_log_a, dt_bias_a, norm_a, norm_b, conv_c, conv_bias_c,
                  dt_bias_c, a_log_c, d_skip_c, norm_c, b_gate, w_branch_a, w_branch_b,
                  w_branch_c, w_out, cos, sin):
    bsz, s, d = x.shape
    proj = x @ w_in
    split_points = np.cumsum(IN_SIZES)[:-1].tolist()
    (a_qkv, a_z, a_a, a_b, b_q, b_k, b_v, b_g, c_z, c_xbc, c_dt, gate_in) = jnp.split(proj, split_points, axis=-1)
    y_a = _gated_deltanet(a_qkv, a_z, a_a, a_b, conv_a, a_log_a, dt_bias_a, norm_a).astype(x.dtype)
    y_b = _retention(b_q, b_k, b_v, b_g, norm_b, cos, sin).astype(x.dtype)
    y_c = _ssd(c_z, c_xbc, c_dt, conv_c, conv_bias_c, dt_bias_c, a_log_c, d_skip_c, norm_c).astype(x.dtype)
    gates = jax.nn.sigmoid(gate_in.reshape(bsz, s, N_BRANCH, d) + b_gate)
    merged = (gates[:, :, 0] * (y_a @ w_branch_a) + gates[:, :, 1] * (y_b @ w_branch_b)
              + gates[:, :, 2] * (y_c @ w_branch_c))
    return merged @ w_out


def _hier_moe(x, w_rg, b_rg, w_re, b_re, w_gate, w_up, w_down):
    f32 = jnp.float32
    bsz, s, d = x.shape
    xt = x.reshape(-1, d)
    g_logits = (xt @ w_rg).astype(f32) + b_rg
    g_prob = jax.nn.softmax(g_logits, axis=-1)
    g_idx = jnp.argmax(g_logits, axis=-1)
    g_p = jnp.take_along_axis(g_prob, g_idx[:, None], axis=-1)
    e_logits = ((xt @ w_re).astype(f32) + b_re).reshape(-1, N_GROUPS, EXPERTS_PER_GROUP)
    e_sel = jnp.take_along_axis(e_logits, g_idx[:, None, None], axis=1)[:, 0]
    top_v, top_i = lax.top_k(e_sel, TOP_K_IN_GROUP)
    top_w = jax.nn.softmax(top_v, axis=-1) * g_p
    eid = g_idx[:, None] * EXPERTS_PER_GROUP + top_i
    combine = jnp.sum(jax.nn.one_hot(eid, N_EXPERTS, dtype=f32) * top_w[..., None], axis=1).astype(x.dtype)
    y = jnp.zeros_like(xt)
    for e in range(N_EXPERTS):
        h = jax.nn.silu(xt @ w_gate[e]) * (xt @ w_up[e])
        y = y + combine[:, e:e + 1] * (h @ w_down[e])
    return y.reshape(bsz, s, d)


def setup_inputs(seed: int = 0) -> dict:
    key = jax.random.key(seed)
    ks = iter(jax.random.split(key, 40))
    f32 = jnp.float32

    def nrm(shape, scale):
        return jax.random.normal(next(ks), shape, f32) * scale

    def gain(shape):
        return 1.0 + nrm(shape, 0.02)

    def dt_bias(shape):
        dt = jnp.exp(jax.random.uniform(next(ks), shape, f32, math.log(1e-3), math.log(1e-1)))
        return dt + jnp.log(-jnp.expm1(-dt))

    def a_log(shape):
        return jnp.log(jax.random.uniform(next(ks), shape, f32, 1.0, 16.0))

    L = DEPTH
    return {
        "x": nrm((BATCH, SEQ, D_MODEL), 1.0),
        "w_in": nrm((L, D_MODEL, P_IN), D_MODEL ** -0.5),
        "conv_a": nrm((L, CONV_K, 2 * A_QK_W + A_V_W), CONV_K ** -0.5),
        "a_log_a": a_log((L, A_HEADS)),
        "dt_bias_a": dt_bias((L, A_HEADS)),
        "norm_a": gain((L, A_DV)),
        "norm_b": gain((L, B_V_W)),
        "conv_c": nrm((L, CONV_K, C_XBC_W), CONV_K ** -0.5),
        "conv_bias_c": nrm((L, C_XBC_W), 0.01),
        "dt_bias_c": dt_bias((L, C_HEADS)),
        "a_log_c": a_log((L, C_HEADS)),
        "d_skip_c": gain((L, C_HEADS)),
        "norm_c": gain((L, C_INNER)),
        "b_gate": nrm((L, N_BRANCH, D_MODEL), 0.01),
        "w_branch_a": nrm((L, A_V_W, D_MODEL), A_V_W ** -0.5),
        "w_branch_b": nrm((L, B_V_W, D_MODEL), B_V_W ** -0.5),
        "w_branch_c": nrm((L, C_INNER, D_MODEL), C_INNER ** -0.5),
        "w_out": nrm((L, D_MODEL, D_MODEL), D_MODEL ** -0.5 * DEEPNORM_BETA),
        "ln1_g": gain((L, D_MODEL)),
        "ln1_b": nrm((L, D_MODEL), 0.01),
        "w_router_group": nrm((L, D_MODEL, N_GROUPS), D_MODEL ** -0.5),
        "b_router_group": nrm((L, N_GROUPS), 0.01),
        "w_router_expert": nrm((L, D_MODEL, N_EXPERTS), D_MODEL ** -0.5),
        "b_router_expert": nrm((L, N_EXPERTS), 0.01),
        "w_gate_e": nrm((L, N_EXPERTS, D_MODEL, D_EXPERT), D_MODEL ** -0.5),
        "w_up_e": nrm((L, N_EXPERTS, D_MODEL, D_EXPERT), D_MODEL ** -0.5),
        "w_down_e": nrm((L, N_EXPERTS, D_EXPERT, D_MODEL), D_EXPERT ** -0.5 * DEEPNORM_BETA),
        "ln2_g": gain((L, D_MODEL)),
        "ln2_b": nrm((L, D_MODEL), 0.01),
    }


def reference(x, w_in, conv_a, a_log_a, dt_bias_a, norm_a, norm_b, conv_c, conv_bias_c, dt_bias_c,
              a_log_c, d_skip_c, norm_c, b_gate, w_branch_a, w_branch_b, w_branch_c, w_out,
              ln1_g, ln1_b, w_router_group, b_router_group, w_router_expert, b_router_expert,
              w_gate_e, w_up_e, w_down_e, ln2_g, ln2_b):
    s = x.shape[1]
    pos = jnp.arange(s, dtype=jnp.float32)
    inv_freq = ROPE_BASE ** (-jnp.arange(0, B_DK, 2, dtype=jnp.float32) / B_DK)
    ang = pos[:, None] * inv_freq[None, :]
    cos, sin = jnp.cos(ang)[:, None, :], jnp.sin(ang)[:, None, :]
    for l in range(DEPTH):
        mix = _token_mixers(x, w_in[l], conv_a[l], a_log_a[l], dt_bias_a[l], norm_a[l], norm_b[l],
                            conv_c[l], conv_bias_c[l], dt_bias_c[l], a_log_c[l], d_skip_c[l], norm_c[l],
                            b_gate[l], w_branch_a[l], w_branch_b[l], w_branch_c[l], w_out[l], cos, sin)
        x = _layernorm(DEEPNORM_ALPHA * x + mix, ln1_g[l], ln1_b[l])
        ffn = _hier_moe(x, w_router_group[l], b_router_group[l], w_router_expert[l], b_router_expert[l],
                        w_gate_e[l], w_up_e[l], w_down_e[l])
        x = _layernorm(DEEPNORM_ALPHA * x + ffn, ln2_g[l], ln2_b[l])
    return x
```

```python
import contextlib
import numpy as np
import concourse.bass as bass
import concourse.mybir as mybir
from concourse.bass_utils import run_bass_kernel_spmd

F32 = mybir.dt.float32
BF16 = mybir.dt.bfloat16
AF = mybir.ActivationFunctionType
ALU = mybir.AluOpType
AX = mybir.AxisListType

D = 1024
KC = 8
P_IN = 9752
O_AQKV, O_AZ, O_AA, O_AB = 0, 1536, 2048, 2052
O_BQ, O_BK, O_BV, O_BG = 2056, 2568, 3080, 3592
O_CZ, O_CXBC, O_CDT, O_GATE = 4104, 5128, 6664, 6680
EPS = 1e-6
ALPHA = 4.0 ** 0.25
NE = 32
DE = 512
BIGM = 30000.0
ENGS = ("pe", "act", "dve", "pool", "sp")


class _Rec:
    def __getattr__(self, name):
        def f(*a, **k):
            self.__dict__["call"] = (name, a, k)
            return self
        return f


class Prog:
    def __init__(self, nc):
        self.nc = nc
        self.ops = []
        self.last_w = {}
        self.readers = {}

    def _add(self, eng, fn, reads, writes, chan=None):
        rec = _Rec()
        fn(rec)
        call = rec.call
        fn = lambda e, call=call: getattr(e, call[0])(*call[1], **call[2])
        idx = len(self.ops)
        deps = set()
        for r in reads:
            w = self.last_w.get(r)
            if w is not None:
                deps.add(w)
            if isinstance(r, str) and r.startswith("pb"):
                for rd in self.readers.get(r, ()):
                    if self.ops[rd]["eng"] != eng:
                        deps.add(rd)
        for r in writes:
            w = self.last_w.get(r)
            if w is not None and not (chan is not None and self.ops[w]["chan"] == chan and self.ops[w]["eng"] == eng):
                deps.add(w)
            for rd in self.readers.get(r, ()):
                deps.add(rd)
        for r in reads:
            self.readers.setdefault(r, []).append(idx)
        for r in writes:
            self.last_w[r] = idx
            self.readers[r] = []
        deps.discard(idx)
        self.ops.append(dict(eng=eng, fn=fn, deps=deps, chan=chan, has_dep=False))
        return idx

    def op(self, eng, fn, reads=(), writes=()):
        return self._add(eng, fn, tuple(reads), tuple(writes))

    def dma(self, eng, fn, reads=(), writes=(), chan="d0"):
        return self._add(eng, fn, tuple(reads), tuple(writes), chan=chan)

    def emit(self, final_wait_ops=()):
        nc = self.nc
        ops = self.ops
        for i, o in enumerate(ops):
            nd = set()
            for d in o["deps"]:
                p = ops[d]
                if p["chan"] is None and o["chan"] is None and p["eng"] == "pe" and o["eng"] == "pe":
                    continue
                nd.add(d)
            o["deps"] = nd
            for d in nd:
                ops[d]["has_dep"] = True
        for d in final_wait_ops:
            ops[d]["has_dep"] = True
        eng_cnt = {e: 0 for e in ENGS}
        chan_cnt = {}
        chans = []
        for o in ops:
            if o["chan"] is not None:
                c = o["chan"]
                if c not in chan_cnt:
                    chan_cnt[c] = 0
                    chans.append(c)
                chan_cnt[c] += 16
                o["tok"] = (("chan", c), chan_cnt[c])
            elif o["has_dep"]:
                eng_cnt[o["eng"]] += 1
                o["tok"] = (("eng", o["eng"]), eng_cnt[o["eng"]])
            else:
                o["tok"] = None
        sem_keys = [("eng", e) for e in ENGS] + [("chan", c) for c in chans]
        with contextlib.ExitStack() as st:
            sems = {}
            for k in sem_keys:
                sems[k] = st.enter_context(nc.semaphore("s_%s_%s" % k))
            blk = st.enter_context(nc.Block())

            def run_engine(eng_name, eng_obj):
                waited = {}
                for i, o in enumerate(ops):
                    if o["eng"] != eng_name:
                        continue
                    need = {}
                    for d in o["deps"]:
                        k, v = ops[d]["tok"]
                        if need.get(k, 0) < v:
                            need[k] = v
                    for k, v in need.items():
                        if waited.get(k, 0) >= v:
                            continue
                        eng_obj.wait_ge(sems[k], v)
                        waited[k] = v
                    ins = o["fn"](eng_obj)
                    if o["tok"] is not None:
                        k, v = o["tok"]
                        ins.then_inc(sems[k], 16 if k[0] == "chan" else 1)
                if eng_name == "sp":
                    need = {}
                    for d in final_wait_ops:
                        k, v = ops[d]["tok"]
                        if need.get(k, 0) < v:
                            need[k] = v
                    for k, v in need.items():
                        eng_obj.wait_ge(sems[k], v)

            blk.sync(lambda e: run_engine("sp", e))
            blk.tensor(lambda e: run_engine("pe", e))
            blk.scalar(lambda e: run_engine("act", e))
            blk.vector(lambda e: run_engine("dve", e))
            blk.gpsimd(lambda e: run_engine("pool", e))


def host_consts(T, BLK):
    c64 = {}
    i = np.arange(64)
    c64["tri"] = (i[:, None] <= i[None, :]).astype(np.float32)
    c64["id"] = np.eye(64, dtype=np.float32)
    c64["ones"] = np.ones((64, 128), np.float32)
    c64["bigm"] = np.where(i[None, :] < i[:, None], 0.0, BIGM).astype(np.float32)
    lg = np.log1p(-np.exp2(-5.0 - np.arange(4, dtype=np.float32))).astype(np.float32)
    idx = i.astype(np.float32)
    dec = np.exp(lg[:, None, None] * np.abs(idx[:, None] - idx[None, :])).astype(np.float32)
    c64["rdec"] = np.transpose(dec, (1, 0, 2)).reshape(64, 256)
    c64["wd"] = np.exp(lg[None, :] * (63.0 - idx[:, None])).astype(np.float32)
    names64 = ["tri", "id", "ones", "bigm", "rdec", "wd"]
    off64 = {}
    o = 0
    for n in names64:
        off64[n] = o
        o += c64[n].shape[1]
    a64 = np.concatenate([c64[n] for n in names64], axis=1).astype(np.float32)
    c128 = {}
    c128["id"] = np.eye(128, dtype=np.float32)
    c128["ones"] = np.ones((128, 128), np.float32)
    rot = np.zeros((128, 128), np.float32)
    for m in range(64):
        rot[m + 64, m] = -1.0
    for m in range(64, 128):
        rot[m - 64, m] = 1.0
    c128["rot"] = rot
    rd = np.exp(lg[:, None] * (idx[None, :] + 1.0)).astype(np.float32)
    rdt = np.tile(rd, (1, BLK // 64))
    c128["rd"] = np.broadcast_to(rdt.reshape(1, 4 * BLK), (128, 4 * BLK)).astype(np.float32)
    cd = np.exp(lg * 64.0).astype(np.float32)
    names128 = ["id", "ones", "rot", "rd"]
    off128 = {}
    o = 0
    for n in names128:
        off128[n] = o
        o += c128[n].shape[1]
    a128 = np.concatenate([c128[n] for n in names128], axis=1).astype(np.float32)
    pos = np.arange(T, dtype=np.float32)
    inv_freq = (np.float32(10000.0) ** (-np.arange(0, 128, 2, dtype=np.float32) / np.float32(128))).astype(np.float32)
    ang = (pos[:, None] * inv_freq[None, :]).astype(np.float32)
    cos = np.cos(ang).astype(np.float32).T
    sin = np.sin(ang).astype(np.float32).T
    cs = np.concatenate([np.concatenate([cos, cos], 0), np.concatenate([sin, sin], 0)], axis=1).astype(np.float32)
    return a64, off64, a128, off128, cs, [float(x) for x in cd]


def build(NSEQ, T, DEPTH, BLK):
    import os
    SSTOP = float(os.environ.get("SSTOP", "100"))
    WSTOP = float(os.environ.get("WSTOP", "100"))
    NT = T // 128
    NBLK = T // BLK
    CPB = BLK // 64
    TPB = BLK // 128
    a64, off64, a128, off128, cs_np, cdec = host_consts(T, BLK)
    nc = bass.Bass("TRN2", target_bir_lowering=False)
    dr = {}

    def din(name, shape):
        dr[name] = nc.dram_tensor(name, list(shape), F32, kind="ExternalInput")
        return dr[name]

    din("x", [NSEQ, T, D])
    L = DEPTH
    specs = dict(w_in=[L, D, P_IN], conv_a=[L, 4, 1536], a_log_a=[L, 4], dt_bias_a=[L, 4], norm_a=[L, 128],
                 norm_b=[L, 512], conv_c=[L, 4, 1536], conv_bias_c=[L, 1536], dt_bias_c=[L, 16], a_log_c=[L, 16],
                 d_skip_c=[L, 16], norm_c=[L, 1024], b_gate=[L, 3, D], w_branch_a=[L, 512, D],
                 w_branch_b=[L, 512, D], w_branch_c=[L, 1024, D], w_out=[L, D, D], ln1_g=[L, D], ln1_b=[L, D],
                 w_router_group=[L, D, 4], b_router_group=[L, 4], w_router_expert=[L, D, 32],
                 b_router_expert=[L, 32], w_gate_e=[L, NE, D, DE], w_up_e=[L, NE, D, DE], w_down_e=[L, NE, DE, D],
                 ln2_g=[L, D], ln2_b=[L, D])
    for k, v in specs.items():
        din(k, v)
    din("c64", list(a64.shape))
    din("c128", list(a128.shape))
    din("cs", list(cs_np.shape))
    yout = nc.dram_tensor("y", [NSEQ, T, D], F32, kind="ExternalOutput")
    xres = [nc.dram_tensor("xres%d" % i, [T, D], F32, kind="Internal") for i in range(2)]
    ytd = nc.dram_tensor("ytd", [2, 128, 16, T], BF16, kind="Internal")

    P = Prog(nc)
    st = contextlib.ExitStack()

    class Tl:
        def __init__(self, h, shape, key, base=0, pstride=None):
            self.h = h
            self.shape = shape
            self.key = key
            self.base = base
            self.row = int(np.prod(shape[1:])) if pstride is None else pstride

        def v(self, off, dims, Pn=128, p0=0):
            return bass.AP(self.h, self.base + off + p0 * self.row, [[self.row, Pn]] + [list(d) for d in dims])

    def sb(name, shape, dt=F32):
        h = st.enter_context(nc.sbuf_tensor(name, list(shape), dt))
        return Tl(h, list(shape), name)

    def dv(t, off, dims):
        return bass.AP(t, off, [list(d) for d in dims])

    banks = [Tl(st.enter_context(nc.psum_tensor("pb%d" % i, [128, 512], F32)), [128, 512], "pb%d" % i)
             for i in range(8)]
    bank_i = [0]

    xT = sb("xT", [128, KC, T], BF16)
    WBSZ = 6208
    WB = [sb("WB%d" % i, [128, WBSZ], BF16) for i in range(2)]
    C64 = sb("C64", [64, a64.shape[1]])
    C128 = sb("C128", [128, 384])
    cwa = sb("cwa", [128, L, 12, 4])
    cwc = sb("cwc", [128, L, 12, 4])
    cbc = sb("cbc", [128, L, 12])
    bgt = sb("bgt", [128, L, 3, 8])
    pb_a = sb("pb_a", [128, L, 8])
    nrm_a = sb("nrm_a", [64, L, 128])
    pc = sb("pc", [64, L, 48])
    wr = sb("wr", [128, L, KC, 36])
    brt = sb("brt", [128, L, 36])
    comb = sb("comb", [128, NT, 32])
    nexa = sb("nexa", [128, L, 4])
    nac = sb("nac", [64, L, 16])
    epst = sb("epst", [128, 4])
    ARENA_W = 77 * 256
    ARh = st.enter_context(nc.sbuf_tensor("AR", [128, ARENA_W], F32))
    ARb = ARh.bitcast(BF16)

    class Phase:
        def __init__(self, name):
            self.name = name
            self.off = 0
            self.t = {}
            self.keys = []

        def a(self, nm, free, dt=F32):
            n = int(np.prod(free))
            sz = n * (4 if dt == F32 else 2)
            off = self.off
            self.off += (sz + 3) // 4 * 4
            assert self.off <= ARENA_W * 4, (self.name, nm, self.off)
            if dt == F32:
                tl = Tl(ARh, [128] + list(free), self.name + "_" + nm, base=off // 4, pstride=ARENA_W)
            else:
                tl = Tl(ARb, [128] + list(free), self.name + "_" + nm, base=off // 2, pstride=ARENA_W * 2)
            self.t[nm] = tl
            self.keys.append(tl.key)
            return tl

    PH = {}
    for nm in "GRSMWXEN":
        PH[nm] = Phase(nm)
    G_, R_, S_, M_, W_, X_, E_, N_ = [PH[k] for k in "GRSMWXEN"]
    for nm in ["fmq", "fmk", "fmv", "fmz", "tA", "tB"]:
        G_.a(nm, [BLK]); R_.a(nm, [BLK])
    for nm in ["raw0", "raw1"]:
        G_.a(nm, [BLK + 3]); S_.a(nm, [BLK + 3])
    G_.a("car", [12]); S_.a("car", [24])
    G_.a("ytb", [BLK], BF16); R_.a("ytb", [BLK], BF16); S_.a("ytb", [4 * BLK], BF16)
    G_.a("S_a", [128])
    for nm in ["g_t", "beta", "gc", "egl", "bex", "decS", "st1", "st2"]:
        G_.a(nm, [CPB])
    G_.a("gbr", [CPB * 64])
    for nm in ["rhs_u", "rhs_w", "kend", "u_t", "o_t", "sq_t"]:
        G_.a(nm, [CPB * 128])
    for nm in ["t1", "Pm", "Qm", "Pm2", "Qm2", "Rm", "wT"]:
        G_.a(nm, [CPB * 64])
    G_.a("delta", [128])
    R_.a("cst", [2 * BLK]); R_.a("rdt", [BLK]); R_.a("SM", [CPB * 64])
    for nm in ["v_tm", "kw", "o_t", "sq_t"]:
        R_.a(nm, [CPB * 128])
    R_.a("St", [(CPB + 1) * 128]); R_.a("st1", [CPB]); R_.a("st2", [CPB]); R_.a("nrmb", [128])
    S_.a("fx", [4 * BLK])
    for nm in ["fmq", "fmk", "tA"]:
        S_.a(nm, [BLK])
    S_.a("Sc", [512])
    for nm in ["dt_t", "dta", "lc", "wr_t", "elc", "decb"]:
        S_.a(nm, [CPB * 8])
    for nm in ["db", "e1", "x_tm", "xw", "sz_tm", "yi", "yg", "nrmc", "dsk"]:
        S_.a(nm, [512])
    S_.a("cbT", [64]); S_.a("B_tm", [128]); S_.a("st2", [4])
    mrgT = M_.a("mrg", [8 * T], BF16)
    W_.t["mrg"] = mrgT; W_.off = M_.off
    mkeys = ["mrg%d" % i for i in range(NBLK)]
    M_.keys += mkeys; W_.keys += mkeys + [mrgT.key]
    M_.a("ytl", [16 * BLK], BF16); M_.a("tA", [BLK]); M_.a("tB", [BLK])
    for ph in (W_, X_):
        ph.a("xl0", [1024]); ph.a("xl1", [1024])
    yacc = E_.a("yacc", [NT * 1024])
    N_.t["yacc"] = yacc; N_.off = E_.off
    ykeys = ["yacc%d" % i for i in range(NT)]
    E_.keys += ykeys; N_.keys += ykeys + [yacc.key]
    for ph in (W_, N_):
        for nm in ["xn", "lng", "lnb"]:
            ph.a(nm, [1024])
        ph.a("bst", [12]); ph.a("mv", [4])
    W_.a("xTf", [1024]); W_.a("rt", [160])
    N_.t["xTf"] = None
    E_.a("hs0", [BLK]); E_.a("hs1", [BLK]); E_.a("Hh0", [2 * BLK], BF16); E_.a("Hh1", [2 * BLK], BF16)
    cur_ph = [None]

    def enter(ph):
        old = cur_ph[0]
        if old is ph:
            return
        cur_ph[0] = ph
        if old is None:
            return
        P.op("dve", lambda e: e.memset(epst.v(3, [[1, 1]]), 0.0), writes=list(old.keys) + list(ph.keys) + ["epst3"])

    def c64(name, w=None, p0=0, Pn=64, coff=0):
        w = w if w is not None else {"tri": 64, "id": 64, "ones": 128, "bigm": 64, "rdec": 256, "wd": 4}[name]
        return C64.v(off64[name] + coff, [[1, w]], Pn=Pn, p0=p0)

    def c128(name, w=128, coff=0):
        return C128.v(off128[name] + coff, [[1, w]])

    P.dma("sp", lambda e: e.dma_start(out=C64.h[:, :], in_=dr["c64"][:, :]), writes=["C64"], chan="i_c64")
    P.dma("sp", lambda e: e.dma_start(out=C128.h[:, :], in_=dr["c128"][:, 0:384]), writes=["C128"], chan="i_c128")

    def small(dst_ap, src_ap, key, chan=None):
        P.dma("sp", lambda e: e.dma_start(out=dst_ap, in_=src_ap, allow_slow_non_contiguous=True), writes=[key], chan="i_" + key)

    for l in range(L):
        for k in range(4):
            small(cwa.v(l * 48 + k, [[4, 12]]), dv(dr["conv_a"], l * 6144 + k * 1536, [[1, 128], [128, 12]]), "cwa")
            small(cwc.v(l * 48 + k, [[4, 12]]), dv(dr["conv_c"], l * 6144 + k * 1536, [[1, 128], [128, 12]]), "cwc")
        small(cbc.v(l * 12, [[1, 12]]), dv(dr["conv_bias_c"], l * 1536, [[1, 128], [128, 12]]), "cbc")
        for br in range(3):
            small(bgt.v(l * 24 + br * 8, [[1, 8]]), dv(dr["b_gate"], l * 3072 + br * 1024, [[1, 128], [128, 8]]), "bgt")
        small(pb_a.v(l * 8, [[1, 4]]), dv(dr["a_log_a"], l * 4, [[0, 128], [1, 4]]), "pb_a")
        small(pb_a.v(l * 8 + 4, [[1, 4]]), dv(dr["dt_bias_a"], l * 4, [[0, 128], [1, 4]]), "pb_a")
        small(nrm_a.v(l * 128, [[1, 128]], Pn=64), dv(dr["norm_a"], l * 128, [[0, 64], [1, 128]]), "nrm_a")
        small(pc.v(l * 48, [[1, 16]], Pn=64), dv(dr["dt_bias_c"], l * 16, [[0, 64], [1, 16]]), "pc")
        small(pc.v(l * 48 + 16, [[1, 16]], Pn=64), dv(dr["a_log_c"], l * 16, [[0, 64], [1, 16]]), "pc")
        small(pc.v(l * 48 + 32, [[1, 16]], Pn=64), dv(dr["d_skip_c"], l * 16, [[0, 64], [1, 16]]), "pc")
        small(wr.v(l * KC * 36, [[36, KC], [1, 4]]), dv(dr["w_router_group"], l * D * 4, [[4, 128], [512, KC], [1, 4]]), "wr")
        small(wr.v(l * KC * 36 + 4, [[36, KC], [1, 32]]), dv(dr["w_router_expert"], l * D * 32, [[32, 128], [4096, KC], [1, 32]]), "wr")
        small(brt.v(l * 36, [[1, 4]]), dv(dr["b_router_group"], l * 4, [[0, 128], [1, 4]]), "brt")
        small(brt.v(l * 36 + 4, [[1, 32]]), dv(dr["b_router_expert"], l * 32, [[0, 128], [1, 32]]), "brt")
    P.op("dve", lambda e: e.memset(epst.v(0, [[1, 1]]), EPS), writes=["epst"])
    P.op("dve", lambda e: e.memset(epst.v(1, [[1, 1]]), 1.0), reads=["epst"], writes=["epst"])
    P.op("dve", lambda e: e.memset(epst.v(2, [[1, 1]]), 0.0), reads=["epst"], writes=["epst"])
    eps_ap = epst.v(0, [[1, 1]])
    one_ap = epst.v(1, [[1, 1]])
    for l in range(L):
        P.op("act", lambda e, l=l: e.activation(out=nexa.v(l * 4, [[1, 4]]), in_=pb_a.v(l * 8, [[1, 4]]), func=AF.Exp),
             reads=["pb_a"], writes=["nexa"])
        P.op("dve", lambda e, l=l: e.tensor_scalar(out=nexa.v(l * 4, [[1, 4]]), in0=nexa.v(l * 4, [[1, 4]]), scalar1=-1.0,
                                                   scalar2=None, op0=ALU.mult), reads=["nexa"], writes=["nexa"])
        P.op("act", lambda e, l=l: e.activation(out=nac.v(l * 16, [[1, 16]], Pn=64), in_=pc.v(l * 48 + 16, [[1, 16]], Pn=64),
                                                func=AF.Exp), reads=["pc"], writes=["nac"])
        P.op("dve", lambda e, l=l: e.tensor_scalar(out=nac.v(l * 16, [[1, 16]], Pn=64), in0=nac.v(l * 16, [[1, 16]], Pn=64),
                                                   scalar1=-1.0, scalar2=None, op0=ALU.mult), reads=["nac"], writes=["nac"])

    pinned = set()

    def ps():
        while True:
            b = banks[bank_i[0] % 8]
            bank_i[0] += 1
            if b.key not in pinned:
                return b

    def ld_w(slot, off_el, ncols, src_t, src_off, row_stride, nk=KC, krows=128):
        P.dma("pool", lambda e: e.dma_start(out=WB[slot].v(off_el, [[ncols, nk], [1, ncols]]),
                                            in_=dv(src_t, src_off, [[row_stride, 128], [128 * row_stride, nk], [1, ncols]]),
                                            allow_slow_non_contiguous=True),
              writes=["WB%d" % slot], chan="w%d" % slot)

    def xt_keys(tok0, n):
        return ["xT%d" % i for i in range(tok0 // 128, (tok0 + n + 127) // 128)]

    def proj_fm(slot, woff, wcols, c0, tok0, n, pbank, pcol=0):
        for kc in range(KC):
            P.op("pe", lambda e, kc=kc: e.matmul(pbank.v(pcol, [[1, n]]),
                                                 lhsT=WB[slot].v(woff + kc * wcols + c0, [[1, 128]]),
                                                 rhs=xT.v(kc * T + tok0, [[1, n]]), start=(kc == 0), stop=(kc == KC - 1)),
                 reads=["WB%d" % slot] + xt_keys(tok0, n), writes=[pbank.key])

    def proj_tm(slot, woff, wcols, c0, ncol, tok0, pbank, pcol=0):
        for kc in range(KC):
            P.op("pe", lambda e, kc=kc: e.matmul(pbank.v(pcol, [[1, ncol]], Pn=64),
                                                 lhsT=xT.v(kc * T + tok0, [[1, 64]]),
                                                 rhs=WB[slot].v(woff + kc * wcols + c0, [[1, ncol]]),
                                                 start=(kc == 0), stop=(kc == KC - 1)),
                 reads=["WB%d" % slot] + xt_keys(tok0, 64), writes=[pbank.key])

    def l2norm_fm(ph, t, scale):
        tB = ph.t["tB"]
        P.op("act", lambda e: e.activation(out=tB.v(0, [[1, BLK]]), in_=t.v(0, [[1, BLK]]), func=AF.Square), reads=[t.key], writes=[tB.key])
        pb = ps()
        P.op("pe", lambda e: e.matmul(pb.v(0, [[1, BLK]]), lhsT=c128("ones"), rhs=tB.v(0, [[1, BLK]]), start=True, stop=True),
             reads=[tB.key, "C128"], writes=[pb.key])
        P.op("act", lambda e: e.activation(out=tB.v(0, [[1, BLK]]), in_=pb.v(0, [[1, BLK]]), func=AF.Sqrt, bias=eps_ap, scale=1.0),
             reads=[pb.key, "epst"], writes=[tB.key])
        P.op("dve", lambda e: e.reciprocal(out=tB.v(0, [[1, BLK]]), in_=tB.v(0, [[1, BLK]])), reads=[tB.key], writes=[tB.key])
        P.op("dve", lambda e: e.scalar_tensor_tensor(out=t.v(0, [[1, BLK]]), in0=t.v(0, [[1, BLK]]), scalar=float(scale),
                                                     in1=tB.v(0, [[1, BLK]]), op0=ALU.mult, op1=ALU.mult),
             reads=[t.key, tB.key], writes=[t.key])

    def store_yT(ytb, fc0, nfc, tok0, s):
        P.dma("sp", lambda e: e.dma_start(out=dv(ytd, (s % 2) * 128 * 16 * T + fc0 * T + tok0, [[16 * T, 128], [T, nfc], [1, BLK]]),
                                          in_=ytb.v(0, [[BLK, nfc], [1, BLK]])),
              reads=[ytb.key], writes=["ytd%d_%d_%d" % (s % 2, fc0 + i, tok0 // BLK) for i in range(nfc)], chan="yt")

    def gdn_load(slot, l, h):
        base = l * D * P_IN
        for j, c0 in enumerate([O_AQKV + h * 128, O_AQKV + 512 + h * 128, O_AQKV + 1024 + h * 128, O_AZ + h * 128]):
            ld_w(slot, j * 1024, 128, dr["w_in"], base + c0, P_IN)
        ld_w(slot, 4096, 1, dr["w_in"], base + O_AA + h, P_IN)
        ld_w(slot, 4096 + 8, 1, dr["w_in"], base + O_AB + h, P_IN)

    def gdn_run(slot, l, h, s):
        wk = "WB%d" % slot
        enter(G_)
        ph = G_
        fmq, fmk, fmv, fmz, tA, tB, ytb, S_a, g_t, beta, gc, egl, bex, decS, st1, st2, gbr, rhs_u, rhs_w, kend, u_t, o_t, sq_t, t1, Pm, Qm, Pm2, Qm2, Rm, wT, delta = [G_.t[n] for n in ['fmq', 'fmk', 'fmv', 'fmz', 'tA', 'tB', 'ytb', 'S_a', 'g_t', 'beta', 'gc', 'egl', 'bex', 'decS', 'st1', 'st2', 'gbr', 'rhs_u', 'rhs_w', 'kend', 'u_t', 'o_t', 'sq_t', 't1', 'Pm', 'Qm', 'Pm2', 'Qm2', 'Rm', 'wT', 'delta']]
        for b in range(NBLK):
            tok0 = b * BLK
            for j, dst in enumerate([fmq, fmk, fmv]):
                pb = ps()
                proj_fm(slot, j * 1024, 128, 0, tok0, BLK, pb)
                cw = lambda k, j=j: cwa.v(l * 48 + (j * 4 + h) * 4 + k, [[1, 1]])
                conv_stream(ph, pb, j, b, cw, dst.v(0, [[1, BLK]]), dst.key)
            l2norm_fm(ph, fmq, 128.0 ** -0.5)
            l2norm_fm(ph, fmk, 1.0)
            pb = ps()
            proj_fm(slot, 3 * 1024, 128, 0, tok0, BLK, pb)
            P.op("act", lambda e, pb=pb: e.activation(out=fmz.v(0, [[1, BLK]]), in_=pb.v(0, [[1, BLK]]), func=AF.Silu),
                 reads=[pb.key], writes=[fmz.key])
            pb = ps()
            for c in range(CPB):
                proj_tm(slot, 4096, 1, 0, 1, tok0 + c * 64, pb, pcol=2 * c)
                proj_tm(slot, 4096 + 8, 1, 0, 1, tok0 + c * 64, pb, pcol=2 * c + 1)
            P.op("act", lambda e, pb=pb: e.activation(out=g_t.v(0, [[1, CPB]], Pn=64), in_=pb.v(0, [[2, CPB]], Pn=64), func=AF.Exp,
                                                      bias=pb_a.v(l * 8 + 4 + h, [[1, 1]], Pn=64)),
                 reads=[pb.key, "pb_a"], writes=[g_t.key])
            P.op("act", lambda e: e.activation(out=g_t.v(0, [[1, CPB]], Pn=64), in_=g_t.v(0, [[1, CPB]], Pn=64), func=AF.Ln,
                                               bias=one_ap[0:64, :]), reads=[g_t.key, "epst"], writes=[g_t.key])
            P.op("dve", lambda e: e.tensor_scalar(out=g_t.v(0, [[1, CPB]], Pn=64), in0=g_t.v(0, [[1, CPB]], Pn=64),
                                                  scalar1=nexa.v(l * 4 + h, [[1, 1]], Pn=64), scalar2=None, op0=ALU.mult),
                 reads=[g_t.key, "nexa"], writes=[g_t.key])
            P.op("act", lambda e, pb=pb: e.activation(out=beta.v(0, [[1, CPB]], Pn=64), in_=pb.v(1, [[2, CPB]], Pn=64),
                                                      func=AF.Sigmoid), reads=[pb.key], writes=[beta.key])
            pg = ps()
            P.op("pe", lambda e, pg=pg: e.matmul(pg.v(0, [[1, CPB]], Pn=64), lhsT=c64("tri"), rhs=g_t.v(0, [[1, CPB]], Pn=64),
                                                 start=True, stop=True), reads=[g_t.key, "C64"], writes=[pg.key])
            P.op("pe", lambda e, pg=pg: e.matmul(pg.v(64, [[1, CPB]]), lhsT=c64("ones"), rhs=g_t.v(0, [[1, CPB]], Pn=64),
                                                 start=True, stop=True), reads=[g_t.key, "C64"], writes=[pg.key])
            P.op("dve", lambda e, pg=pg: e.tensor_copy(out=gc.v(0, [[1, CPB]], Pn=64), in_=pg.v(0, [[1, CPB]], Pn=64)),
                 reads=[pg.key], writes=[gc.key])
            P.op("act", lambda e, pg=pg: e.activation(out=decS.v(0, [[1, CPB]]), in_=pg.v(64, [[1, CPB]]), func=AF.Exp),
                 reads=[pg.key], writes=[decS.key])
            P.op("dve", lambda e, pg=pg: e.tensor_tensor(out=egl.v(0, [[1, CPB]], Pn=64), in0=pg.v(64, [[1, CPB]], Pn=64),
                                                         in1=gc.v(0, [[1, CPB]], Pn=64), op=ALU.subtract),
                 reads=[pg.key, gc.key], writes=[egl.key])
            P.op("act", lambda e: e.activation(out=egl.v(0, [[1, CPB]], Pn=64), in_=egl.v(0, [[1, CPB]], Pn=64), func=AF.Exp),
                 reads=[egl.key], writes=[egl.key])
            P.op("act", lambda e: e.activation(out=bex.v(0, [[1, CPB]], Pn=64), in_=gc.v(0, [[1, CPB]], Pn=64), func=AF.Exp),
                 reads=[gc.key], writes=[bex.key])
            P.op("dve", lambda e: e.tensor_tensor(out=bex.v(0, [[1, CPB]], Pn=64), in0=bex.v(0, [[1, CPB]], Pn=64),
                                                  in1=beta.v(0, [[1, CPB]], Pn=64), op=ALU.mult), reads=[bex.key, beta.key], writes=[bex.key])
            P.op("dve", lambda e: e.tensor_copy(out=gbr.v(0, [[64, CPB], [1, 64]], Pn=64), in_=g_t.v(0, [[1, CPB], [0, 64]], Pn=64)),
                 reads=[g_t.key], writes=[gbr.key])
            pG = ps()
            pK = ps()
            for c in range(CPB):
                P.op("pe", lambda e, c=c, pG=pG: e.matmul(pG.v(c * 64, [[1, 64]], Pn=64), lhsT=gbr.v(c * 64, [[1, 64]], Pn=64),
                                                          rhs=c64("tri"), start=True, stop=True), reads=[gbr.key, "C64"], writes=[pG.key])
                P.op("pe", lambda e, c=c, pK=pK: e.matmul(pK.v(c * 64, [[1, 64]], Pn=64), lhsT=fmk.v(c * 64, [[1, 64]]),
                                                          rhs=fmk.v(c * 64, [[1, 64]]), start=True, stop=True), reads=[fmk.key], writes=[pK.key])
            P.op("dve", lambda e, pG=pG: e.tensor_tensor(out=t1.v(0, [[64, CPB], [1, 64]], Pn=64), in0=pG.v(0, [[64, CPB], [1, 64]], Pn=64),
                                                         in1=gc.v(0, [[1, CPB], [0, 64]], Pn=64), op=ALU.subtract),
                 reads=[pG.key, gc.key], writes=[t1.key])
            P.op("dve", lambda e: e.tensor_tensor(out=t1.v(0, [[64, CPB], [1, 64]], Pn=64), in0=t1.v(0, [[64, CPB], [1, 64]], Pn=64),
                                                  in1=C64.v(off64["bigm"], [[0, CPB], [1, 64]], Pn=64), op=ALU.max),
                 reads=[t1.key, "C64"], writes=[t1.key])
            P.op("act", lambda e: e.activation(out=t1.v(0, [[1, CPB * 64]], Pn=64), in_=t1.v(0, [[1, CPB * 64]], Pn=64), func=AF.Exp, scale=-1.0),
                 reads=[t1.key], writes=[t1.key])
            P.op("dve", lambda e, pK=pK: e.tensor_tensor(out=Qm.v(0, [[64, CPB], [1, 64]], Pn=64), in0=pK.v(0, [[64, CPB], [1, 64]], Pn=64),
                                                         in1=beta.v(0, [[1, CPB], [0, 64]], Pn=64), op=ALU.mult),
                 reads=[pK.key, beta.key], writes=[Qm.key])
            P.op("dve", lambda e: e.tensor_tensor(out=Qm.v(0, [[1, CPB * 64]], Pn=64), in0=Qm.v(0, [[1, CPB * 64]], Pn=64),
                                                  in1=t1.v(0, [[1, CPB * 64]], Pn=64), op=ALU.mult), reads=[Qm.key, t1.key], writes=[Qm.key])
            pT = [ps(), ps()]
            pV = [ps(), ps()]
            pB_ = ps()
            for c in range(CPB):
                P.op("pe", lambda e, c=c: e.transpose(pT[c // 4].v((c % 4) * 128, [[1, 128]], Pn=64), fmk.v(c * 64, [[1, 64]]), c128("id")),
                     reads=[fmk.key, "C128"], writes=[pT[c // 4].key])
                P.op("pe", lambda e, c=c: e.transpose(pV[c // 4].v((c % 4) * 128, [[1, 128]], Pn=64), fmv.v(c * 64, [[1, 64]]), c128("id")),
                     reads=[fmv.key, "C128"], writes=[pV[c // 4].key])
                P.op("pe", lambda e, c=c: e.transpose(pB_.v(c * 64, [[1, 64]], Pn=64), Qm.v(c * 64, [[1, 64]], Pn=64), c64("id")),
                     reads=[Qm.key, "C64"], writes=[pB_.key])
            for hb in range((CPB + 3) // 4):
                n = min(4, CPB - hb * 4)
                P.op("dve", lambda e, hb=hb, n=n: e.tensor_tensor(out=rhs_w.v(hb * 512, [[128, n], [1, 128]], Pn=64),
                                                                  in0=pT[hb].v(0, [[128, n], [1, 128]], Pn=64),
                                                                  in1=bex.v(hb * 4, [[1, n], [0, 128]], Pn=64), op=ALU.mult),
                     reads=[pT[hb].key, bex.key], writes=[rhs_w.key])
                P.op("dve", lambda e, hb=hb, n=n: e.tensor_tensor(out=kend.v(hb * 512, [[128, n], [1, 128]], Pn=64),
                                                                  in0=pT[hb].v(0, [[128, n], [1, 128]], Pn=64),
                                                                  in1=egl.v(hb * 4, [[1, n], [0, 128]], Pn=64), op=ALU.mult),
                     reads=[pT[hb].key, egl.key], writes=[kend.key])
                P.op("dve", lambda e, hb=hb, n=n: e.tensor_tensor(out=rhs_u.v(hb * 512, [[128, n], [1, 128]], Pn=64),
                                                                  in0=pV[hb].v(0, [[128, n], [1, 128]], Pn=64),
                                                                  in1=beta.v(hb * 4, [[1, n], [0, 128]], Pn=64), op=ALU.mult),
                     reads=[pV[hb].key, beta.key], writes=[rhs_u.key])
            P.op("act", lambda e: e.activation(out=Pm.v(0, [[1, CPB * 64]], Pn=64), in_=pB_.v(0, [[1, CPB * 64]], Pn=64), func=AF.Copy),
                 reads=[pB_.key], writes=[Pm.key])
            P.op("dve", lambda e: e.tensor_tensor(out=Rm.v(0, [[64, CPB], [1, 64]], Pn=64), in0=C64.v(off64["id"], [[0, CPB], [1, 64]], Pn=64),
                                                  in1=pB_.v(0, [[64, CPB], [1, 64]], Pn=64), op=ALU.subtract),
                 reads=[pB_.key, "C64"], writes=[Rm.key])
            Pc, Qc, Pn_, Qn_ = Pm, Qm, Pm2, Qm2
            for lev in range(5):
                last = lev == 4
                pq = ps()
                pp = ps() if not last else None
                for c in range(CPB):
                    P.op("pe", lambda e, c=c, pq=pq, Pc=Pc, Qc=Qc: e.matmul(pq.v(c * 64, [[1, 64]], Pn=64), lhsT=Pc.v(c * 64, [[1, 64]], Pn=64),
                                                                         rhs=Qc.v(c * 64, [[1, 64]], Pn=64), start=True, stop=True),
                         reads=[Pc.key, Qc.key], writes=[pq.key])
                    if not last:
                        P.op("pe", lambda e, c=c, pp=pp, Pc=Pc, Qc=Qc: e.matmul(pp.v(c * 64, [[1, 64]], Pn=64), lhsT=Qc.v(c * 64, [[1, 64]], Pn=64),
                                                                             rhs=Pc.v(c * 64, [[1, 64]], Pn=64), start=True, stop=True),
                             reads=[Pc.key, Qc.key], writes=[pp.key])
                P.op("act", lambda e, pq=pq, Qn_=Qn_: e.activation(out=Qn_.v(0, [[1, CPB * 64]], Pn=64), in_=pq.v(0, [[1, CPB * 64]], Pn=64), func=AF.Copy),
                     reads=[pq.key], writes=[Qn_.key])
                if not last:
                    P.op("dve", lambda e, pp=pp, Pn_=Pn_: e.tensor_copy(out=Pn_.v(0, [[1, CPB * 64]], Pn=64), in_=pp.v(0, [[1, CPB * 64]], Pn=64)),
                         reads=[pp.key], writes=[Pn_.key])
                pr = ps()
                for c in range(CPB):
                    P.op("pe", lambda e, c=c, pr=pr, Qn_=Qn_: e.matmul(pr.v(c * 64, [[1, 64]], Pn=64), lhsT=Qn_.v(c * 64, [[1, 64]], Pn=64),
                                                                      rhs=Rm.v(c * 64, [[1, 64]], Pn=64), start=True, stop=True),
                         reads=[Qn_.key, Rm.key], writes=[pr.key])
                P.op("dve", lambda e, pr=pr: e.tensor_tensor(out=Rm.v(0, [[1, CPB * 64]], Pn=64), in0=Rm.v(0, [[1, CPB * 64]], Pn=64),
                                                             in1=pr.v(0, [[1, CPB * 64]], Pn=64), op=ALU.add), reads=[pr.key, Rm.key], writes=[Rm.key])
                Pc, Qc, Pn_, Qn_ = Pn_, Qn_, Pc, Qc
            pU = [ps(), ps()]
            pW = ps()
            for c in range(CPB):
                P.op("pe", lambda e, c=c: e.matmul(pU[c // 4].v((c % 4) * 128, [[1, 128]], Pn=64), lhsT=Rm.v(c * 64, [[1, 64]], Pn=64),
                                                   rhs=rhs_u.v(c * 128, [[1, 128]], Pn=64), start=True, stop=True),
                     reads=[Rm.key, rhs_u.key], writes=[pU[c // 4].key])
                P.op("pe", lambda e, c=c: e.matmul(pW.v(c * 64, [[1, 64]]), lhsT=rhs_w.v(c * 128, [[1, 128]], Pn=64),
                                                   rhs=Rm.v(c * 64, [[1, 64]], Pn=64), start=True, stop=True),
                     reads=[Rm.key, rhs_w.key], writes=[pW.key])
            for hb in range((CPB + 3) // 4):
                n = min(4, CPB - hb * 4)
                P.op("act", lambda e, hb=hb, n=n: e.activation(out=u_t.v(hb * 512, [[1, n * 128]], Pn=64), in_=pU[hb].v(0, [[1, n * 128]], Pn=64),
                                                               func=AF.Copy), reads=[pU[hb].key], writes=[u_t.key])
            P.op("act", lambda e: e.activation(out=wT.v(0, [[1, CPB * 64]]), in_=pW.v(0, [[1, CPB * 64]]), func=AF.Copy),
                 reads=[pW.key], writes=[wT.key])
            if b == 0:
                P.op("dve", lambda e: e.memset(S_a.v(0, [[1, 128]]), 0.0), writes=[S_a.key])
            pO = [ps(), ps()]
            pinned.update([pO[0].key, pO[1].key])
            for c in range(CPB):
                p1 = ps()
                P.op("pe", lambda e, c=c, p1=p1: e.matmul(p1.v(0, [[1, 128]], Pn=64), lhsT=wT.v(c * 64, [[1, 64]]), rhs=S_a.v(0, [[1, 128]]),
                                                          start=True, stop=True), reads=[wT.key, S_a.key], writes=[p1.key])
                P.op("dve", lambda e, c=c, p1=p1: e.tensor_tensor(out=delta.v(0, [[1, 128]], Pn=64), in0=u_t.v(c * 128, [[1, 128]], Pn=64),
                                                                  in1=p1.v(0, [[1, 128]], Pn=64), op=ALU.subtract),
                     reads=[u_t.key, p1.key], writes=[delta.key])
                p2 = ps()
                P.op("pe", lambda e, c=c, p2=p2: e.matmul(p2.v(0, [[1, 128]]), lhsT=kend.v(c * 128, [[1, 128]], Pn=64),
                                                          rhs=delta.v(0, [[1, 128]], Pn=64), start=True, stop=True),
                     reads=[kend.key, delta.key], writes=[p2.key])
                P.op("dve", lambda e, c=c, p2=p2: e.scalar_tensor_tensor(out=S_a.v(0, [[1, 128]]), in0=S_a.v(0, [[1, 128]]),
                                                                         scalar=decS.v(c, [[1, 1]]), in1=p2.v(0, [[1, 128]]),
                                                                         op0=ALU.mult, op1=ALU.add), reads=[S_a.key, decS.key, p2.key], writes=[S_a.key])
                P.op("pe", lambda e, c=c: e.matmul(pO[c // 4].v((c % 4) * 128, [[1, 128]], Pn=64), lhsT=fmq.v(c * 64, [[1, 64]]),
                                                   rhs=S_a.v(0, [[1, 128]]), start=True, stop=True), reads=[fmq.key, S_a.key], writes=[pO[c // 4].key])
            pinned.difference_update([pO[0].key, pO[1].key])
            for hb in range((CPB + 3) // 4):
                n = min(4, CPB - hb * 4)
                P.op("act", lambda e, hb=hb, n=n: e.activation(out=o_t.v(hb * 512, [[1, n * 128]], Pn=64), in_=pO[hb].v(0, [[1, n * 128]], Pn=64),
                                                               func=AF.Copy), reads=[pO[hb].key], writes=[o_t.key])
            rms_tm(ph, o_t, CPB, 128, nrm_a.v(l * 128, [[0, CPB], [1, 128]], Pn=64), "nrm_a")
            pY = ps()
            for c in range(CPB):
                P.op("pe", lambda e, c=c, pY=pY: e.transpose(pY.v(c * 64, [[1, 64]]), o_t.v(c * 128, [[1, 128]], Pn=64), c64("id")),
                     reads=[o_t.key, "C64"], writes=[pY.key])
            P.op("dve", lambda e, pY=pY: e.tensor_tensor(out=ytb.v(0, [[1, BLK]]), in0=pY.v(0, [[1, BLK]]), in1=fmz.v(0, [[1, BLK]]), op=ALU.mult),
                 reads=[pY.key, fmz.key], writes=[ytb.key])
            store_yT(ytb, h, 1, tok0, s)

    conv_cnt = [0]

    def conv_stream(ph, pb, sid, b, cw, out_ap, out_key, bias_ap=None):
        r = ph.t["raw%d" % (conv_cnt[0] % 2)]
        conv_cnt[0] += 1
        car = ph.t["car"]
        tA = ph.t["tA"]
        if b == 0:
            P.op("dve", lambda e: e.memset(r.v(0, [[1, 3]]), 0.0), writes=[r.key])
        else:
            P.op("dve", lambda e: e.tensor_copy(out=r.v(0, [[1, 3]]), in_=car.v(sid * 3, [[1, 3]])), reads=[car.key], writes=[r.key])
        P.op("act", lambda e: e.activation(out=r.v(3, [[1, BLK]]), in_=pb.v(0, [[1, BLK]]), func=AF.Copy),
             reads=[pb.key, r.key], writes=[r.key])
        P.op("dve", lambda e: e.tensor_copy(out=car.v(sid * 3, [[1, 3]]), in_=r.v(BLK, [[1, 3]])), reads=[r.key, car.key], writes=[car.key])
        P.op("dve", lambda e: e.tensor_scalar(out=tA.v(0, [[1, BLK]]), in0=r.v(0, [[1, BLK]]), scalar1=cw(0), scalar2=None, op0=ALU.mult),
             reads=[r.key, "cwa", "cwc"], writes=[tA.key])
        for k in range(1, 4):
            P.op("dve", lambda e, k=k: e.scalar_tensor_tensor(out=tA.v(0, [[1, BLK]]), in0=r.v(k, [[1, BLK]]), scalar=cw(k),
                                                              in1=tA.v(0, [[1, BLK]]), op0=ALU.mult, op1=ALU.add),
                 reads=[r.key, tA.key, "cwa", "cwc"], writes=[tA.key])
        if bias_ap is None:
            P.op("act", lambda e: e.activation(out=out_ap, in_=tA.v(0, [[1, BLK]]), func=AF.Silu), reads=[tA.key], writes=[out_key])
        else:
            P.op("act", lambda e: e.activation(out=out_ap, in_=tA.v(0, [[1, BLK]]), func=AF.Silu, bias=bias_ap),
                 reads=[tA.key, "cbc"], writes=[out_key])

    def rms_tm(ph, t, nch, width, w_ap, wkey, center=False):
        st1, st2, sq_t = ph.t["st1"], ph.t["st2"], ph.t["sq_t"]
        full = t.v(0, [[width, nch], [1, width]], Pn=64)
        if center:
            P.op("dve", lambda e: e.tensor_reduce(out=st1.v(0, [[1, nch]], Pn=64), in_=full, axis=AX.X, op=ALU.add), reads=[t.key], writes=[st1.key])
            P.op("dve", lambda e: e.tensor_scalar(out=st1.v(0, [[1, nch]], Pn=64), in0=st1.v(0, [[1, nch]], Pn=64), scalar1=1.0 / width,
                                                  scalar2=None, op0=ALU.mult), reads=[st1.key], writes=[st1.key])
            P.op("dve", lambda e: e.tensor_tensor(out=full, in0=full, in1=st1.v(0, [[1, nch], [0, width]], Pn=64), op=ALU.subtract),
                 reads=[t.key, st1.key], writes=[t.key])
        P.op("act", lambda e: e.activation(out=sq_t.v(0, [[1, nch * width]], Pn=64),
                                           in_=t.v(0, [[1, nch * width]], Pn=64), func=AF.Square), reads=[t.key], writes=[sq_t.key])
        P.op("dve", lambda e: e.tensor_reduce(out=st2.v(0, [[1, nch]], Pn=64), in_=sq_t.v(0, [[width, nch], [1, width]], Pn=64), axis=AX.X, op=ALU.add),
             reads=[sq_t.key], writes=[st2.key])
        P.op("act", lambda e: e.activation(out=st2.v(0, [[1, nch]], Pn=64), in_=st2.v(0, [[1, nch]], Pn=64), func=AF.Sqrt,
                                           bias=eps_ap[0:64, :], scale=1.0 / width), reads=[st2.key, "epst"], writes=[st2.key])
        P.op("dve", lambda e: e.reciprocal(out=st2.v(0, [[1, nch]], Pn=64), in_=st2.v(0, [[1, nch]], Pn=64)), reads=[st2.key], writes=[st2.key])
        P.op("dve", lambda e: e.tensor_tensor(out=full, in0=full, in1=st2.v(0, [[1, nch], [0, width]], Pn=64), op=ALU.mult),
             reads=[t.key, st2.key], writes=[t.key])
        P.op("dve", lambda e: e.tensor_tensor(out=full, in0=full, in1=w_ap, op=ALU.mult), reads=[t.key, wkey], writes=[t.key])

    def ret_load(slot, l, h):
        base = l * D * P_IN
        for j, c0 in enumerate([O_BQ + h * 128, O_BK + h * 128, O_BV + h * 128, O_BG + h * 128]):
            ld_w(slot, j * 1024, 128, dr["w_in"], base + c0, P_IN)

    def ret_run(slot, l, h, s):
        enter(R_)
        ph = R_
        fmq, fmk, fmv, fmz, tA, tB, ytb, cst, rdt, SM, v_tm, kw, o_t, sq_t, St, st1, st2, nrmb = [R_.t[n] for n in ['fmq', 'fmk', 'fmv', 'fmz', 'tA', 'tB', 'ytb', 'cst', 'rdt', 'SM', 'v_tm', 'kw', 'o_t', 'sq_t', 'St', 'st1', 'st2', 'nrmb']]
        small(rdt.v(0, [[1, BLK]]), dv(dr["c128"], off128["rd"] + h * BLK, [[a128.shape[1], 128], [1, BLK]]), rdt.key, chan="rp")
        small(nrmb.v(0, [[1, 128]], Pn=64), dv(dr["norm_b"], l * 512 + h * 128, [[0, 64], [1, 128]]), nrmb.key, chan="rp")
        for b in range(NBLK):
            tok0 = b * BLK
            P.dma("sp", lambda e, tok0=tok0: e.dma_start(out=cst.v(0, [[BLK, 2], [1, BLK]]),
                                                         in_=dv(dr["cs"], tok0, [[2 * T, 128], [T, 2], [1, BLK]])), writes=[cst.key], chan="cs")
            for j, dst in enumerate([fmq, fmk]):
                pb = ps()
                proj_fm(slot, j * 1024, 128, 0, tok0, BLK, pb)
                P.op("act", lambda e, pb=pb: e.activation(out=tA.v(0, [[1, BLK]]), in_=pb.v(0, [[1, BLK]]), func=AF.Copy), reads=[pb.key], writes=[tA.key])
                pr = ps()
                P.op("pe", lambda e, pr=pr: e.matmul(pr.v(0, [[1, BLK]]), lhsT=c128("rot"), rhs=tA.v(0, [[1, BLK]]), start=True, stop=True),
                     reads=[tA.key, "C128"], writes=[pr.key])
                sc = 1.0 if j == 0 else 128.0 ** -0.5
                P.op("dve", lambda e, pr=pr, sc=sc: e.scalar_tensor_tensor(out=tB.v(0, [[1, BLK]]), in0=pr.v(0, [[1, BLK]]), scalar=sc,
                                                                           in1=cst.v(BLK, [[1, BLK]]), op0=ALU.mult, op1=ALU.mult),
                     reads=[pr.key, cst.key], writes=[tB.key])
                P.op("dve", lambda e, sc=sc: e.scalar_tensor_tensor(out=tA.v(0, [[1, BLK]]), in0=tA.v(0, [[1, BLK]]), scalar=sc,
                                                                    in1=cst.v(0, [[1, BLK]]), op0=ALU.mult, op1=ALU.mult),
                     reads=[tA.key, cst.key], writes=[tA.key])
                P.op("dve", lambda e, dst=dst: e.tensor_tensor(out=dst.v(0, [[1, BLK]]), in0=tA.v(0, [[1, BLK]]), in1=tB.v(0, [[1, BLK]]), op=ALU.add),
                     reads=[tA.key, tB.key], writes=[dst.key])
            pb = ps()
            proj_fm(slot, 3 * 1024, 128, 0, tok0, BLK, pb)
            P.op("act", lambda e, pb=pb: e.activation(out=fmz.v(0, [[1, BLK]]), in_=pb.v(0, [[1, BLK]]), func=AF.Silu), reads=[pb.key], writes=[fmz.key])
            P.op("dve", lambda e: e.tensor_tensor(out=fmv.v(0, [[1, BLK]]), in0=fmq.v(0, [[1, BLK]]), in1=rdt.v(0, [[1, BLK]]), op=ALU.mult),
                 reads=[fmq.key, rdt.key], writes=[fmv.key])
            pV = [ps(), ps()]
            pT = [ps(), ps()]
            pS = ps()
            for c in range(CPB):
                proj_tm(slot, 2 * 1024, 128, 0, 128, tok0 + c * 64, pV[c // 4], pcol=(c % 4) * 128)
                P.op("pe", lambda e, c=c: e.transpose(pT[c // 4].v((c % 4) * 128, [[1, 128]], Pn=64), fmk.v(c * 64, [[1, 64]]), c128("id")),
                     reads=[fmk.key, "C128"], writes=[pT[c // 4].key])
                P.op("pe", lambda e, c=c: e.matmul(pS.v(c * 64, [[1, 64]], Pn=64), lhsT=fmk.v(c * 64, [[1, 64]]), rhs=fmq.v(c * 64, [[1, 64]]),
                                                   start=True, stop=True), reads=[fmk.key, fmq.key], writes=[pS.key])
            for hb in range((CPB + 3) // 4):
                n = min(4, CPB - hb * 4)
                P.op("act", lambda e, hb=hb, n=n: e.activation(out=v_tm.v(hb * 512, [[1, n * 128]], Pn=64), in_=pV[hb].v(0, [[1, n * 128]], Pn=64),
                                                               func=AF.Copy), reads=[pV[hb].key], writes=[v_tm.key])
                P.op("dve", lambda e, hb=hb, n=n: e.tensor_scalar(out=kw.v(hb * 512, [[1, n * 128]], Pn=64), in0=pT[hb].v(0, [[1, n * 128]], Pn=64),
                                                                  scalar1=c64("wd", 1, coff=h), scalar2=None, op0=ALU.mult),
                     reads=[pT[hb].key, "C64"], writes=[kw.key])
            P.op("dve", lambda e: e.tensor_tensor(out=SM.v(0, [[64, CPB], [1, 64]], Pn=64), in0=pS.v(0, [[64, CPB], [1, 64]], Pn=64),
                                                  in1=C64.v(off64["rdec"] + h * 64, [[0, CPB], [1, 64]], Pn=64), op=ALU.mult),
                 reads=[pS.key, "C64"], writes=[SM.key])
            if b == 0:
                P.op("dve", lambda e: e.memset(St.v(0, [[1, 128]]), 0.0), reads=[St.key], writes=[St.key])
            else:
                P.op("dve", lambda e: e.tensor_copy(out=St.v(0, [[1, 128]]), in_=St.v(CPB * 128, [[1, 128]])), reads=[St.key], writes=[St.key])
            for c in range(CPB):
                pu = ps()
                P.op("pe", lambda e, c=c, pu=pu: e.matmul(pu.v(0, [[1, 128]]), lhsT=kw.v(c * 128, [[1, 128]], Pn=64),
                                                          rhs=v_tm.v(c * 128, [[1, 128]], Pn=64), start=True, stop=True),
                     reads=[kw.key, v_tm.key], writes=[pu.key])
                P.op("dve", lambda e, c=c, pu=pu: e.scalar_tensor_tensor(out=St.v((c + 1) * 128, [[1, 128]]), in0=St.v(c * 128, [[1, 128]]),
                                                                         scalar=cdec[h], in1=pu.v(0, [[1, 128]]), op0=ALU.mult, op1=ALU.add),
                     reads=[St.key, pu.key], writes=[St.key])
            pO = [ps(), ps()]
            for c in range(CPB):
                P.op("pe", lambda e, c=c: e.matmul(pO[c // 4].v((c % 4) * 128, [[1, 128]], Pn=64), lhsT=SM.v(c * 64, [[1, 64]], Pn=64),
                                                   rhs=v_tm.v(c * 128, [[1, 128]], Pn=64), start=True, stop=False),
                     reads=[SM.key, v_tm.key], writes=[pO[c // 4].key])
                P.op("pe", lambda e, c=c: e.matmul(pO[c // 4].v((c % 4) * 128, [[1, 128]], Pn=64), lhsT=fmv.v(c * 64, [[1, 64]]),
                                                   rhs=St.v(c * 128, [[1, 128]]), start=False, stop=True),
                     reads=[fmv.key, St.key], writes=[pO[c // 4].key])
            for hb in range((CPB + 3) // 4):
                n = min(4, CPB - hb * 4)
                P.op("act", lambda e, hb=hb, n=n: e.activation(out=o_t.v(hb * 512, [[1, n * 128]], Pn=64), in_=pO[hb].v(0, [[1, n * 128]], Pn=64),
                                                               func=AF.Copy), reads=[pO[hb].key], writes=[o_t.key])
            rms_tm(ph, o_t, CPB, 128, nrmb.v(0, [[0, CPB], [1, 128]], Pn=64), nrmb.key, center=True)
            pY = ps()
            for c in range(CPB):
                P.op("pe", lambda e, c=c, pY=pY: e.transpose(pY.v(c * 64, [[1, 64]]), o_t.v(c * 128, [[1, 128]], Pn=64), c64("id")),
                     reads=[o_t.key, "C64"], writes=[pY.key])
            P.op("dve", lambda e, pY=pY: e.tensor_tensor(out=ytb.v(0, [[1, BLK]]), in0=pY.v(0, [[1, BLK]]), in1=fmz.v(0, [[1, BLK]]), op=ALU.mult),
                 reads=[pY.key, fmz.key], writes=[ytb.key])
            store_yT(ytb, 4 + h, 1, tok0, s)

    def ssd_load1(slot, l, g):
        base = l * D * P_IN
        ld_w(slot, 0, 512, dr["w_in"], base + O_CXBC + g * 512, P_IN)
        ld_w(slot, 4096, 128, dr["w_in"], base + O_CXBC + 1024 + g * 128, P_IN)
        ld_w(slot, 5120, 128, dr["w_in"], base + O_CXBC + 1280 + g * 128, P_IN)
        ld_w(slot, 6144, 8, dr["w_in"], base + O_CDT + g * 8, P_IN)

    def ssd_load2(slot, l, g):
        ld_w(slot, 0, 512, dr["w_in"], l * D * P_IN + O_CZ + g * 512, P_IN)

    def ssd_run(slot, l, g, s):
        enter(S_)
        ph = S_
        zslot = slot
        slot = 1 - slot
        fx, fmq, fmk, tA, Sc, dt_t, dta, lc, wr_t, elc, decb, db, e1, x_tm, xw, sz_tm, yi, yg, nrmc, dsk, cbT, B_tm, st2, ytb = [S_.t[n] for n in ['fx', 'fmq', 'fmk', 'tA', 'Sc', 'dt_t', 'dta', 'lc', 'wr_t', 'elc', 'decb', 'db', 'e1', 'x_tm', 'xw', 'sz_tm', 'yi', 'yg', 'nrmc', 'dsk', 'cbT', 'B_tm', 'st2', 'ytb']]
        small(nrmc.v(0, [[1, 512]], Pn=64), dv(dr["norm_c"], l * 1024 + g * 512, [[0, 64], [1, 512]]), nrmc.key, chan="rp")
        P.op("dve", lambda e: e.tensor_tensor(out=dsk.v(0, [[64, 8], [1, 64]], Pn=64), in0=C64.v(off64["id"], [[0, 8], [1, 64]], Pn=64),
                                              in1=pc.v(l * 48 + 32 + g * 8, [[1, 8], [0, 64]], Pn=64), op=ALU.mult), reads=["C64", "pc"], writes=[dsk.key])
        for b in range(NBLK):
            tok0 = b * BLK
            for j in range(4):
                pb = ps()
                proj_fm(slot, 0, 512, j * 128, tok0, BLK, pb)
                ch = g * 4 + j
                conv_stream(ph, pb, j, b, lambda k, ch=ch: cwc.v(l * 48 + ch * 4 + k, [[1, 1]]), fx.v(j * BLK, [[1, BLK]]), fx.key,
                            bias_ap=cbc.v(l * 12 + ch, [[1, 1]]))
            if SSTOP <= 0.1:
                continue
            for j, (woff, ch, dst) in enumerate([(4096, 8 + g, fmk), (5120, 10 + g, fmq)]):
                pb = ps()
                proj_fm(slot, woff, 128, 0, tok0, BLK, pb)
                conv_stream(ph, pb, 4 + j, b, lambda k, ch=ch: cwc.v(l * 48 + ch * 4 + k, [[1, 1]]), dst.v(0, [[1, BLK]]), dst.key,
                            bias_ap=cbc.v(l * 12 + ch, [[1, 1]]))
            if SSTOP <= 0.2:
                continue
            pd = ps()
            for c in range(CPB):
                proj_tm(slot, 6144, 8, 0, 8, tok0 + c * 64, pd, pcol=c * 8)
            P.op("dve", lambda e, pd=pd: e.tensor_tensor(out=dt_t.v(0, [[8, CPB], [1, 8]], Pn=64), in0=pd.v(0, [[8, CPB], [1, 8]], Pn=64),
                                                         in1=pc.v(l * 48 + g * 8, [[0, CPB], [1, 8]], Pn=64), op=ALU.add),
                 reads=[pd.key, "pc"], writes=[dt_t.key])
            P.op("act", lambda e: e.activation(out=dt_t.v(0, [[1, CPB * 8]], Pn=64), in_=dt_t.v(0, [[1, CPB * 8]], Pn=64), func=AF.Exp),
                 reads=[dt_t.key], writes=[dt_t.key])
            P.op("act", lambda e: e.activation(out=dt_t.v(0, [[1, CPB * 8]], Pn=64), in_=dt_t.v(0, [[1, CPB * 8]], Pn=64), func=AF.Ln,
                                               bias=one_ap[0:64, :]), reads=[dt_t.key, "epst"], writes=[dt_t.key])
            P.op("dve", lambda e: e.tensor_tensor(out=dta.v(0, [[8, CPB], [1, 8]], Pn=64), in0=dt_t.v(0, [[8, CPB], [1, 8]], Pn=64),
                                                  in1=nac.v(l * 16 + g * 8, [[0, CPB], [1, 8]], Pn=64), op=ALU.mult),
                 reads=[dt_t.key, "nac"], writes=[dta.key])
            if SSTOP <= 0.3:
                continue
            pl = ps()
            P.op("pe", lambda e, pl=pl: e.matmul(pl.v(0, [[1, CPB * 8]], Pn=64), lhsT=c64("tri"), rhs=dta.v(0, [[1, CPB * 8]], Pn=64),
                                                 start=True, stop=True), reads=[dta.key, "C64"], writes=[pl.key])
            P.op("pe", lambda e, pl=pl: e.matmul(pl.v(128, [[1, CPB * 8]]), lhsT=c64("ones"), rhs=dta.v(0, [[1, CPB * 8]], Pn=64),
                                                 start=True, stop=True), reads=[dta.key, "C64"], writes=[pl.key])
            P.op("dve", lambda e, pl=pl: e.tensor_copy(out=lc.v(0, [[1, CPB * 8]], Pn=64), in_=pl.v(0, [[1, CPB * 8]], Pn=64)),
                 reads=[pl.key], writes=[lc.key])
            if SSTOP <= 0.45:
                continue
            P.op("dve", lambda e, pl=pl: e.tensor_copy(out=decb.v(0, [[1, CPB * 8]]), in_=pl.v(128, [[1, CPB * 8]])),
                 reads=[pl.key], writes=[decb.key])
            P.op("act", lambda e: e.activation(out=decb.v(0, [[1, CPB * 8]]), in_=decb.v(0, [[1, CPB * 8]]), func=AF.Exp),
                 reads=[decb.key], writes=[decb.key])
            if SSTOP <= 0.5:
                continue
            P.op("dve", lambda e, pl=pl: e.tensor_tensor(out=wr_t.v(0, [[1, CPB * 8]], Pn=64), in0=pl.v(128, [[1, CPB * 8]], Pn=64),
                                                         in1=lc.v(0, [[1, CPB * 8]], Pn=64), op=ALU.subtract), reads=[pl.key, lc.key], writes=[wr_t.key])
            P.op("act", lambda e: e.activation(out=wr_t.v(0, [[1, CPB * 8]], Pn=64), in_=wr_t.v(0, [[1, CPB * 8]], Pn=64), func=AF.Exp),
                 reads=[wr_t.key], writes=[wr_t.key])
            P.op("dve", lambda e: e.tensor_tensor(out=wr_t.v(0, [[1, CPB * 8]], Pn=64), in0=wr_t.v(0, [[1, CPB * 8]], Pn=64),
                                                  in1=dt_t.v(0, [[1, CPB * 8]], Pn=64), op=ALU.mult), reads=[wr_t.key, dt_t.key], writes=[wr_t.key])
            P.op("act", lambda e: e.activation(out=elc.v(0, [[1, CPB * 8]], Pn=64), in_=lc.v(0, [[1, CPB * 8]], Pn=64), func=AF.Exp),
                 reads=[lc.key], writes=[elc.key])
            if SSTOP <= 1:
                continue
            if b == 0:
                P.op("dve", lambda e: e.memset(Sc.v(0, [[1, 512]]), 0.0), reads=[Sc.key], writes=[Sc.key])
            for c in range(CPB):
                ct = tok0 + c * 64
                P.op("dve", lambda e, c=c: e.tensor_copy(out=db.v(0, [[64, 8], [1, 64]], Pn=64), in_=dta.v(c * 8, [[1, 8], [0, 64]], Pn=64)),
                     reads=[dta.key], writes=[db.key])
                pL = ps()
                for hh in range(8):
                    P.op("pe", lambda e, hh=hh, pL=pL: e.matmul(pL.v(hh * 64, [[1, 64]], Pn=64), lhsT=db.v(hh * 64, [[1, 64]], Pn=64),
                                                                rhs=c64("tri"), start=True, stop=True), reads=[db.key, "C64"], writes=[pL.key])
                P.op("dve", lambda e, c=c, pL=pL: e.tensor_tensor(out=e1.v(0, [[64, 8], [1, 64]], Pn=64), in0=pL.v(0, [[64, 8], [1, 64]], Pn=64),
                                                                  in1=lc.v(c * 8, [[1, 8], [0, 64]], Pn=64), op=ALU.subtract),
                     reads=[pL.key, lc.key], writes=[e1.key])
                P.op("dve", lambda e: e.scalar_tensor_tensor(out=e1.v(0, [[1, 512]], Pn=64), in0=e1.v(0, [[1, 512]], Pn=64), scalar=-1.0,
                                                             in1=e1.v(0, [[1, 512]], Pn=64), op0=ALU.mult, op1=ALU.max),
                     reads=[e1.key], writes=[e1.key])
                P.op("act", lambda e: e.activation(out=e1.v(0, [[1, 512]], Pn=64), in_=e1.v(0, [[1, 512]], Pn=64), func=AF.Exp, scale=-1.0),
                     reads=[e1.key], writes=[e1.key])
                if SSTOP <= 2:
                    continue
                pc_ = ps()
                P.op("pe", lambda e, c=c, pc_=pc_: e.matmul(pc_.v(0, [[1, 64]], Pn=64), lhsT=fmk.v(c * 64, [[1, 64]]), rhs=fmq.v(c * 64, [[1, 64]]),
                                                            start=True, stop=True), reads=[fmk.key, fmq.key], writes=[pc_.key])
                P.op("act", lambda e, pc_=pc_: e.activation(out=cbT.v(0, [[1, 64]], Pn=64), in_=pc_.v(0, [[1, 64]], Pn=64), func=AF.Copy),
                     reads=[pc_.key], writes=[cbT.key])
                P.op("dve", lambda e: e.tensor_tensor(out=e1.v(0, [[64, 8], [1, 64]], Pn=64), in0=e1.v(0, [[64, 8], [1, 64]], Pn=64),
                                                      in1=cbT.v(0, [[0, 8], [1, 64]], Pn=64), op=ALU.mult), reads=[e1.key, cbT.key], writes=[e1.key])
                P.op("dve", lambda e, c=c: e.tensor_tensor(out=e1.v(0, [[64, 8], [1, 64]], Pn=64), in0=e1.v(0, [[64, 8], [1, 64]], Pn=64),
                                                           in1=dt_t.v(c * 8, [[1, 8], [0, 64]], Pn=64), op=ALU.mult), reads=[e1.key, dt_t.key], writes=[e1.key])
                P.op("dve", lambda e: e.tensor_tensor(out=e1.v(0, [[1, 512]], Pn=64), in0=e1.v(0, [[1, 512]], Pn=64),
                                                      in1=dsk.v(0, [[1, 512]], Pn=64), op=ALU.add), reads=[e1.key, dsk.key], writes=[e1.key])
                if SSTOP <= 3:
                    continue
                pX = ps()
                for j in range(4):
                    P.op("pe", lambda e, c=c, j=j, pX=pX: e.transpose(pX.v(j * 128, [[1, 128]], Pn=64), fx.v(j * BLK + c * 64, [[1, 64]]), c128("id")),
                         reads=[fx.key, "C128"], writes=[pX.key])
                P.op("act", lambda e, pX=pX: e.activation(out=x_tm.v(0, [[1, 512]], Pn=64), in_=pX.v(0, [[1, 512]], Pn=64), func=AF.Copy),
                     reads=[pX.key], writes=[x_tm.key])
                P.op("dve", lambda e, c=c, pX=pX: e.tensor_tensor(out=xw.v(0, [[64, 8], [1, 64]], Pn=64), in0=pX.v(0, [[64, 8], [1, 64]], Pn=64),
                                                                  in1=wr_t.v(c * 8, [[1, 8], [0, 64]], Pn=64), op=ALU.mult),
                     reads=[pX.key, wr_t.key], writes=[xw.key])
                pBt = ps()
                P.op("pe", lambda e, c=c, pBt=pBt: e.transpose(pBt.v(0, [[1, 128]], Pn=64), fmk.v(c * 64, [[1, 64]]), c128("id")),
                     reads=[fmk.key, "C128"], writes=[pBt.key])
                P.op("act", lambda e, pBt=pBt: e.activation(out=B_tm.v(0, [[1, 128]], Pn=64), in_=pBt.v(0, [[1, 128]], Pn=64), func=AF.Copy),
                     reads=[pBt.key], writes=[B_tm.key])
                pI = ps()
                for hh in range(8):
                    P.op("pe", lambda e, hh=hh, pI=pI: e.matmul(pI.v(hh * 64, [[1, 64]], Pn=64), lhsT=e1.v(hh * 64, [[1, 64]], Pn=64),
                                                                rhs=x_tm.v(hh * 64, [[1, 64]], Pn=64), start=True, stop=True),
                         reads=[e1.key, x_tm.key], writes=[pI.key])
                pN = ps()
                for qq in range(4):
                    P.op("pe", lambda e, c=c, pN=pN, qq=qq: e.matmul(pN.v(qq * 128, [[1, 128]], Pn=64), lhsT=fmq.v(c * 64, [[1, 64]]),
                                                                     rhs=Sc.v(qq * 128, [[1, 128]]), start=True, stop=True),
                         reads=[fmq.key, Sc.key], writes=[pN.key])
                P.op("act", lambda e, pI=pI: e.activation(out=yi.v(0, [[1, 512]], Pn=64), in_=pI.v(0, [[1, 512]], Pn=64), func=AF.Copy),
                     reads=[pI.key], writes=[yi.key])
                P.op("dve", lambda e, c=c, pN=pN: e.tensor_tensor(out=yg.v(0, [[64, 8], [1, 64]], Pn=64), in0=pN.v(0, [[64, 8], [1, 64]], Pn=64),
                                                                  in1=elc.v(c * 8, [[1, 8], [0, 64]], Pn=64), op=ALU.mult),
                     reads=[pN.key, elc.key], writes=[yg.key])
                P.op("dve", lambda e: e.tensor_tensor(out=yg.v(0, [[1, 512]], Pn=64), in0=yg.v(0, [[1, 512]], Pn=64), in1=yi.v(0, [[1, 512]], Pn=64),
                                                      op=ALU.add), reads=[yg.key, yi.key], writes=[yg.key])
                if SSTOP <= 4:
                    continue
                pS_ = ps()
                for qq in range(4):
                    P.op("pe", lambda e, pS_=pS_, qq=qq: e.matmul(pS_.v(qq * 128, [[1, 128]]), lhsT=B_tm.v(0, [[1, 128]], Pn=64),
                                                                  rhs=xw.v(qq * 128, [[1, 128]], Pn=64), start=True, stop=True),
                         reads=[B_tm.key, xw.key], writes=[pS_.key])
                P.op("dve", lambda e, c=c: e.tensor_tensor(out=Sc.v(0, [[64, 8], [1, 64]]), in0=Sc.v(0, [[64, 8], [1, 64]]),
                                                           in1=decb.v(c * 8, [[1, 8], [0, 64]]), op=ALU.mult), reads=[Sc.key, decb.key], writes=[Sc.key])
                P.op("dve", lambda e, pS_=pS_: e.tensor_tensor(out=Sc.v(0, [[1, 512]]), in0=Sc.v(0, [[1, 512]]), in1=pS_.v(0, [[1, 512]]), op=ALU.add),
                     reads=[Sc.key, pS_.key], writes=[Sc.key])
                if SSTOP <= 5:
                    continue
                pZ = ps()
                proj_tm(zslot, 0, 512, 0, 512, ct, pZ)
                P.op("act", lambda e, pZ=pZ: e.activation(out=sz_tm.v(0, [[1, 512]], Pn=64), in_=pZ.v(0, [[1, 512]], Pn=64), func=AF.Silu),
                     reads=[pZ.key], writes=[sz_tm.key])
                P.op("dve", lambda e: e.tensor_tensor(out=yg.v(0, [[1, 512]], Pn=64), in0=yg.v(0, [[1, 512]], Pn=64), in1=sz_tm.v(0, [[1, 512]], Pn=64),
                                                      op=ALU.mult), reads=[yg.key, sz_tm.key], writes=[yg.key])
                P.op("act", lambda e: e.activation(out=yi.v(0, [[1, 512]], Pn=64), in_=yg.v(0, [[1, 512]], Pn=64), func=AF.Square),
                     reads=[yg.key], writes=[yi.key])
                P.op("dve", lambda e: e.tensor_reduce(out=st2.v(0, [[1, 1]], Pn=64), in_=yi.v(0, [[1, 512]], Pn=64), axis=AX.X, op=ALU.add),
                     reads=[yi.key], writes=[st2.key])
                P.op("act", lambda e: e.activation(out=st2.v(0, [[1, 1]], Pn=64), in_=st2.v(0, [[1, 1]], Pn=64), func=AF.Sqrt,
                                                   bias=eps_ap[0:64, :], scale=1.0 / 512), reads=[st2.key, "epst"], writes=[st2.key])
                P.op("dve", lambda e: e.reciprocal(out=st2.v(0, [[1, 1]], Pn=64), in_=st2.v(0, [[1, 1]], Pn=64)), reads=[st2.key], writes=[st2.key])
                P.op("dve", lambda e: e.scalar_tensor_tensor(out=yg.v(0, [[1, 512]], Pn=64), in0=yg.v(0, [[1, 512]], Pn=64), scalar=st2.v(0, [[1, 1]], Pn=64),
                                                             in1=nrmc.v(0, [[1, 512]], Pn=64), op0=ALU.mult, op1=ALU.mult),
                     reads=[yg.key, st2.key, nrmc.key], writes=[yg.key])
                pY = ps()
                for j in range(4):
                    P.op("pe", lambda e, j=j, pY=pY: e.transpose(pY.v(j * 64, [[1, 64]]), yg.v(j * 128, [[1, 128]], Pn=64), c64("id")),
                         reads=[yg.key, "C64"], writes=[pY.key])
                P.op("act", lambda e, c=c, pY=pY: e.activation(out=ytb.v(c * 64, [[BLK, 4], [1, 64]]), in_=pY.v(0, [[64, 4], [1, 64]]), func=AF.Copy),
                     reads=[pY.key], writes=[ytb.key])
            store_yT(ytb, 8 + g * 4, 4, tok0, s)

    def mrg_load(slot, l, dc):
        base = l * D * P_IN
        for br in range(3):
            ld_w(slot, br * 1024, 128, dr["w_in"], base + O_GATE + br * 1024 + dc * 128, P_IN)
        ld_w(slot, 3072, 128, dr["w_branch_a"], l * 512 * D + dc * 128, D, nk=4)
        ld_w(slot, 3072 + 512, 128, dr["w_branch_b"], l * 512 * D + dc * 128, D, nk=4)
        ld_w(slot, 3072 + 1024, 128, dr["w_branch_c"], l * 1024 * D + dc * 128, D, nk=8)

    def mrg_run(slot, l, dc, s):
        enter(M_)
        ytl, tA, tB = [M_.t[n] for n in ['ytl', 'tA', 'tB']]
        for b in range(NBLK):
            tok0 = b * BLK
            P.dma("sp", lambda e, tok0=tok0: e.dma_start(out=ytl.v(0, [[BLK, 16], [1, BLK]]),
                                                         in_=dv(ytd, (s % 2) * 128 * 16 * T + tok0, [[16 * T, 128], [T, 16], [1, BLK]])),
                  reads=["ytd%d_%d_%d" % (s % 2, i, b) for i in range(16)], writes=[ytl.key], chan=ytl.key)
            first = True
            for br, (f0, nf, woff) in enumerate([(0, 4, 3072), (4, 4, 3072 + 512), (8, 8, 3072 + 1024)]):
                pg = ps()
                proj_fm(slot, br * 1024, 128, 0, tok0, BLK, pg)
                P.op("act", lambda e, pg=pg, br=br: e.activation(out=tA.v(0, [[1, BLK]]), in_=pg.v(0, [[1, BLK]]), func=AF.Sigmoid,
                                                                 bias=bgt.v(l * 24 + br * 8 + dc, [[1, 1]])), reads=[pg.key, "bgt"], writes=[tA.key])
                pb = ps()
                for k in range(nf):
                    P.op("pe", lambda e, k=k, pb=pb, woff=woff, f0=f0, nf=nf: e.matmul(pb.v(0, [[1, BLK]]), lhsT=WB[slot].v(woff + k * 128, [[1, 128]]),
                                                                                       rhs=ytl.v((f0 + k) * BLK, [[1, BLK]]), start=(k == 0), stop=(k == nf - 1)),
                         reads=["WB%d" % slot, ytl.key], writes=[pb.key])
                if first:
                    P.op("dve", lambda e, pb=pb: e.tensor_tensor(out=tB.v(0, [[1, BLK]]), in0=pb.v(0, [[1, BLK]]), in1=tA.v(0, [[1, BLK]]), op=ALU.mult),
                         reads=[pb.key, tA.key], writes=[tB.key])
                    first = False
                else:
                    P.op("dve", lambda e, pb=pb: e.tensor_tensor(out=tA.v(0, [[1, BLK]]), in0=pb.v(0, [[1, BLK]]), in1=tA.v(0, [[1, BLK]]), op=ALU.mult),
                         reads=[pb.key, tA.key], writes=[tA.key])
                    if br == 1:
                        P.op("dve", lambda e: e.tensor_tensor(out=tB.v(0, [[1, BLK]]), in0=tB.v(0, [[1, BLK]]), in1=tA.v(0, [[1, BLK]]), op=ALU.add),
                             reads=[tA.key, tB.key], writes=[tB.key])
                    else:
                        P.op("dve", lambda e, tok0=tok0: e.tensor_tensor(out=mrgT.v(dc * T + tok0, [[1, BLK]]), in0=tB.v(0, [[1, BLK]]),
                                                                         in1=tA.v(0, [[1, BLK]]), op=ALU.add),
                             reads=[tA.key, tB.key], writes=["mrg%d" % b])


    def ln_tile(ph, src_ap, src_keys, l, which, tile, s, final, do_router):
        xn, lng, lnb, xTf, bst, mv = [ph.t[n] for n in ['xn', 'lng', 'lnb', 'xTf', 'bst', 'mv']]
        if WSTOP <= 1:
            return
        for hh in range(2):
            P.op("dve", lambda e, hh=hh: e.bn_stats(out=bst.v(hh * 6, [[1, 6]]), in_=src_ap[:, hh * 512:(hh + 1) * 512]), reads=src_keys, writes=[bst.key])
        P.op("dve", lambda e: e.bn_aggr(out=mv.v(0, [[1, 2]]), in_=bst.v(0, [[1, 12]])), reads=[bst.key], writes=[mv.key])
        P.op("act", lambda e: e.activation(out=mv.v(2, [[1, 1]]), in_=mv.v(1, [[1, 1]]), func=AF.Sqrt, bias=eps_ap, scale=1.0), reads=[mv.key, "epst"], writes=[mv.key])
        P.op("dve", lambda e: e.reciprocal(out=mv.v(2, [[1, 1]]), in_=mv.v(2, [[1, 1]])), reads=[mv.key], writes=[mv.key])
        P.op("dve", lambda e: e.tensor_scalar(out=xn.v(0, [[1, 1024]]), in0=src_ap, scalar1=mv.v(0, [[1, 1]]), scalar2=mv.v(2, [[1, 1]]),
                                              op0=ALU.subtract, op1=ALU.mult), reads=list(src_keys) + [mv.key], writes=[xn.key])
        P.op("dve", lambda e: e.tensor_tensor(out=xn.v(0, [[1, 1024]]), in0=xn.v(0, [[1, 1024]]), in1=lng.v(0, [[1, 1024]]), op=ALU.mult),
             reads=[xn.key, lng.key], writes=[xn.key])
        P.op("dve", lambda e: e.tensor_tensor(out=xn.v(0, [[1, 1024]]), in0=xn.v(0, [[1, 1024]]), in1=lnb.v(0, [[1, 1024]]), op=ALU.add),
             reads=[xn.key, lnb.key], writes=[xn.key])
        if WSTOP <= 2:
            return
        if final:
            o = P.dma("sp", lambda e: e.dma_start(out=dv(yout, (s * T + tile * 128) * D, [[D, 128], [1, D]]), in_=xn.v(0, [[1, 1024]])),
                      reads=[xn.key], writes=["yout%d_%d" % (s, tile)], chan="out")
            outs.append(o)
        else:
            P.dma("sp", lambda e: e.dma_start(out=dv(xres[which], tile * 128 * D, [[D, 128], [1, D]]), in_=xn.v(0, [[1, 1024]])),
                  reads=[xn.key], writes=["xres%d" % which], chan="xr%d" % which)
            if WSTOP <= 2.5:
                return
            for half in range(2):
                pt = ps()
                for k in range(4):
                    kc = half * 4 + k
                    P.op("pe", lambda e, k=k, kc=kc, pt=pt: e.transpose(pt.v(k * 128, [[1, 128]]), xn.v(kc * 128, [[1, 128]]), c128("id")),
                         reads=[xn.key, "C128"], writes=[pt.key])
                P.op("act", lambda e, half=half, pt=pt: e.activation(out=xT.v(half * 4 * T + tile * 128, [[T, 4], [1, 128]]), in_=pt.v(0, [[128, 4], [1, 128]]),
                                                                     func=AF.Copy), reads=[pt.key], writes=["xT%d" % tile])
                if do_router and WSTOP > 2.7:
                    P.op("dve", lambda e, half=half, pt=pt: e.tensor_copy(out=xTf.v(half * 512, [[1, 512]]), in_=pt.v(0, [[1, 512]])),
                         reads=[pt.key], writes=[xTf.key])
            if do_router and WSTOP > 3:
                router(ph, l, tile)

    outs = []

    def router(ph, l, tile):
        xTf, rt = ph.t['xTf'], ph.t['rt']
        pr = ps()
        for kc in range(KC):
            P.op("pe", lambda e, kc=kc, pr=pr: e.matmul(pr.v(0, [[1, 36]]), lhsT=xTf.v(kc * 128, [[1, 128]]), rhs=wr.v(l * KC * 36 + kc * 36, [[1, 36]]),
                                                        start=(kc == 0), stop=(kc == KC - 1)), reads=[xTf.key, "wr"], writes=[pr.key])
        R = lambda o, n: rt.v(o, [[1, n]])
        P.op("dve", lambda e: e.tensor_tensor(out=R(0, 36), in0=pr.v(0, [[1, 36]]), in1=brt.v(l * 36, [[1, 36]]), op=ALU.add), reads=[pr.key, "brt"], writes=[rt.key])
        k = [rt.key]
        P.op("dve", lambda e: e.tensor_reduce(out=R(36, 1), in_=R(0, 4), axis=AX.X, op=ALU.max), reads=k, writes=k)
        P.op("dve", lambda e: e.tensor_scalar(out=R(37, 4), in0=R(0, 4), scalar1=R(36, 1), scalar2=None, op0=ALU.is_ge), reads=k, writes=k)
        P.op("dve", lambda e: e.tensor_scalar(out=R(41, 4), in0=R(0, 4), scalar1=R(36, 1), scalar2=None, op0=ALU.subtract), reads=k, writes=k)
        P.op("act", lambda e: e.activation(out=R(41, 4), in_=R(41, 4), func=AF.Exp), reads=k, writes=k)
        P.op("dve", lambda e: e.tensor_reduce(out=R(45, 1), in_=R(41, 4), axis=AX.X, op=ALU.add), reads=k, writes=k)
        P.op("dve", lambda e: e.reciprocal(out=R(46, 1), in_=R(45, 1)), reads=k, writes=k)
        P.op("dve", lambda e: e.tensor_scalar(out=R(41, 4), in0=R(37, 4), scalar1=-1.0, scalar2=BIGM, op0=ALU.add, op1=ALU.mult), reads=k, writes=k)
        P.op("dve", lambda e: e.tensor_tensor(out=rt.v(48, [[8, 4], [1, 8]]), in0=rt.v(4, [[8, 4], [1, 8]]), in1=rt.v(41, [[1, 4], [0, 8]]), op=ALU.add),
             reads=k, writes=k)
        P.op("dve", lambda e: e.tensor_reduce(out=R(80, 1), in_=R(48, 32), axis=AX.X, op=ALU.max), reads=k, writes=k)
        P.op("dve", lambda e: e.tensor_scalar(out=R(82, 32), in0=R(48, 32), scalar1=R(80, 1), scalar2=None, op0=ALU.is_ge), reads=k, writes=k)
        P.op("dve", lambda e: e.scalar_tensor_tensor(out=R(48, 32), in0=R(82, 32), scalar=-BIGM, in1=R(48, 32), op0=ALU.mult, op1=ALU.add), reads=k, writes=k)
        P.op("dve", lambda e: e.tensor_reduce(out=R(81, 1), in_=R(48, 32), axis=AX.X, op=ALU.max), reads=k, writes=k)
        P.op("dve", lambda e: e.tensor_scalar(out=R(114, 32), in0=R(48, 32), scalar1=R(81, 1), scalar2=None, op0=ALU.is_ge), reads=k, writes=k)
        P.op("dve", lambda e: e.tensor_tensor(out=R(146, 1), in0=R(81, 1), in1=R(80, 1), op=ALU.subtract), reads=k, writes=k)
        P.op("act", lambda e: e.activation(out=R(146, 1), in_=R(146, 1), func=AF.Exp), reads=k, writes=k)
        P.op("dve", lambda e: e.tensor_scalar(out=R(146, 1), in0=R(146, 1), scalar1=1.0, scalar2=None, op0=ALU.add), reads=k, writes=k)
        P.op("dve", lambda e: e.reciprocal(out=R(146, 1), in_=R(146, 1)), reads=k, writes=k)
        P.op("dve", lambda e: e.tensor_tensor(out=R(146, 1), in0=R(146, 1), in1=R(46, 1), op=ALU.mult), reads=k, writes=k)
        P.op("dve", lambda e: e.tensor_tensor(out=R(147, 1), in0=R(46, 1), in1=R(146, 1), op=ALU.subtract), reads=k, writes=k)
        P.op("dve", lambda e: e.tensor_scalar(out=R(82, 32), in0=R(82, 32), scalar1=R(146, 1), scalar2=None, op0=ALU.mult), reads=k, writes=k)
        P.op("dve", lambda e: e.scalar_tensor_tensor(out=comb.v(tile * 32, [[1, 32]]), in0=R(114, 32), scalar=R(147, 1), in1=R(82, 32),
                                                     op0=ALU.mult, op1=ALU.add), reads=k, writes=["comb%d" % tile])

    def load_ln(ph, l, which):
        lng, lnb = ph.t['lng'], ph.t['lnb']
        g, b_ = ("ln1_g", "ln1_b") if which == 1 else ("ln2_g", "ln2_b")
        P.dma("sp", lambda e: e.dma_start(out=lng.v(0, [[1, 1024]]), in_=dv(dr[g], l * D, [[0, 128], [1, D]])), writes=[lng.key], chan="lng")
        P.dma("sp", lambda e: e.dma_start(out=lnb.v(0, [[1, 1024]]), in_=dv(dr[b_], l * D, [[0, 128], [1, D]])), writes=[lnb.key], chan="lnb")

    def wout_load1(slot, l):
        ld_w(slot, 0, 512, dr["w_out"], l * D * D, D)

    def wout_load2(slot, l):
        ld_w(slot, 0, 512, dr["w_out"], l * D * D + 512, D)

    def wout_run(slot, l, s, src_dram_ap_fn, src_keys_fn):
        enter(W_)
        xl = [W_.t["xl0"], W_.t["xl1"]]
        slots = [1 - slot, slot]
        load_ln(W_, l, 1)
        for tile in range(NT):
            xo = xl[tile % 2]
            P.dma("sp", lambda e, tile=tile, xo=xo: e.dma_start(out=xo.v(0, [[1, 1024]]), in_=src_dram_ap_fn(tile)), reads=src_keys_fn(tile),
                  writes=[xo.key], chan="xl%d" % (tile % 2))
            for half in range(2):
                pm = ps()
                for kc in range(KC):
                    P.op("pe", lambda e, kc=kc, pm=pm, half=half, tile=tile: e.matmul(pm.v(0, [[1, 512]]), lhsT=mrgT.v(kc * T + tile * 128, [[1, 128]]),
                                                                                      rhs=WB[slots[half]].v(kc * 512, [[1, 512]]),
                                                                                      start=(kc == 0), stop=(kc == KC - 1)),
                         reads=["WB%d" % slots[half], "mrg%d" % (tile * 128 // BLK)], writes=[pm.key])
                P.op("dve", lambda e, pm=pm, half=half, xo=xo: e.scalar_tensor_tensor(out=xo.v(half * 512, [[1, 512]]), in0=xo.v(half * 512, [[1, 512]]),
                                                                                      scalar=ALPHA, in1=pm.v(0, [[1, 512]]), op0=ALU.mult, op1=ALU.add),
                     reads=[xo.key, pm.key], writes=[xo.key])
            ln_tile(W_, xo.v(0, [[1, 1024]]), [xo.key], l, 0, tile, s, False, True)

    def moe_load(slot, l, e_, hf):
        ld_w(slot, 0, 256, dr["w_gate_e"], ((l * NE + e_) * D) * DE + hf * 256, DE)
        ld_w(slot, 2048, 256, dr["w_up_e"], ((l * NE + e_) * D) * DE + hf * 256, DE)
        ld_w(slot, 4096, 1024, dr["w_down_e"], ((l * NE + e_) * DE + hf * 256) * D, D, nk=2)

    def moe_init(l, s):
        enter(E_)
        for tile in range(NT):
            P.dma("sp", lambda e, tile=tile: e.dma_start(out=yacc.v(tile * 1024, [[1, 1024]]), in_=dv(xres[0], tile * 128 * D, [[D, 128], [1, D]])),
                  reads=["xres0"], writes=["yacc%d" % tile], chan="ya%d" % tile)
            P.op("pool", lambda e, tile=tile: e.tensor_scalar(out=yacc.v(tile * 1024, [[1, 1024]]), in0=yacc.v(tile * 1024, [[1, 1024]]), scalar1=ALPHA,
                                                               scalar2=None, op0=ALU.mult), reads=["yacc%d" % tile], writes=["yacc%d" % tile])

    def moe_run(slot, l, e_, hf, s):
        wk = "WB%d" % slot
        Hh = [E_.t["Hh0"], E_.t["Hh1"]]
        hs = [E_.t["hs0"], E_.t["hs1"]]
        for b in range(NBLK):
            tok0 = b * BLK
            H = Hh[b % 2]
            for k2 in range(2):
                pg = ps()
                pu = ps()
                for kc in range(KC):
                    P.op("pe", lambda e, kc=kc, pg=pg, k2=k2, tok0=tok0: e.matmul(pg.v(0, [[1, BLK]]), lhsT=WB[slot].v(kc * 256 + k2 * 128, [[1, 128]]),
                                                                                  rhs=xT.v(kc * T + tok0, [[1, BLK]]), start=(kc == 0), stop=(kc == KC - 1)),
                         reads=[wk] + xt_keys(tok0, BLK), writes=[pg.key])
                for kc in range(KC):
                    P.op("pe", lambda e, kc=kc, pu=pu, k2=k2, tok0=tok0: e.matmul(pu.v(0, [[1, BLK]]), lhsT=WB[slot].v(2048 + kc * 256 + k2 * 128, [[1, 128]]),
                                                                                  rhs=xT.v(kc * T + tok0, [[1, BLK]]), start=(kc == 0), stop=(kc == KC - 1)),
                         reads=[wk] + xt_keys(tok0, BLK), writes=[pu.key])
                hsx = hs[k2]
                P.op("act", lambda e, pg=pg, hsx=hsx: e.activation(out=hsx.v(0, [[1, BLK]]), in_=pg.v(0, [[1, BLK]]), func=AF.Silu), reads=[pg.key], writes=[hsx.key])
                P.op("dve", lambda e, pu=pu, hsx=hsx, H=H, k2=k2: e.tensor_tensor(out=H.v(k2 * BLK, [[1, BLK]]), in0=hsx.v(0, [[1, BLK]]), in1=pu.v(0, [[1, BLK]]),
                                                                                  op=ALU.mult), reads=[pu.key, hsx.key], writes=[H.key])
            for t in range(TPB):
                tile = b * TPB + t
                for half in range(2):
                    py = ps()
                    for k2 in range(2):
                        P.op("pe", lambda e, k2=k2, py=py, t=t, half=half, H=H: e.matmul(py.v(0, [[1, 512]]), lhsT=H.v(k2 * BLK + t * 128, [[1, 128]]),
                                                                                         rhs=WB[slot].v(4096 + k2 * 1024 + half * 512, [[1, 512]]),
                                                                                         start=(k2 == 0), stop=(k2 == 1)), reads=[wk, H.key], writes=[py.key])
                    P.op("dve", lambda e, py=py, tile=tile, half=half: e.scalar_tensor_tensor(
                        out=yacc.v(tile * 1024 + half * 512, [[1, 512]]), in0=py.v(0, [[1, 512]]), scalar=comb.v(tile * 32 + e_, [[1, 1]]),
                        in1=yacc.v(tile * 1024 + half * 512, [[1, 512]]), op0=ALU.mult, op1=ALU.add),
                        reads=[py.key, "comb%d" % tile, "yacc%d" % tile], writes=["yacc%d" % tile])

    def ln2_run(l, s, final):
        enter(N_)
        load_ln(N_, l, 2)
        for tile in range(NT):
            ln_tile(N_, yacc.v(tile * 1024, [[1, 1024]]), ["yacc%d" % tile], l, 1, tile, s, final, False)

    def x0_run(s):
        enter(X_)
        xl = [X_.t["xl0"], X_.t["xl1"]]
        for tile in range(NT):
            xo = xl[tile % 2]
            P.dma("sp", lambda e, tile=tile, xo=xo: e.dma_start(out=xo.v(0, [[1, 1024]]), in_=dv(dr["x"], (s * T + tile * 128) * D, [[D, 128], [1, D]])),
                  writes=[xo.key], chan="xl%d" % (tile % 2))
            for half in range(2):
                pt = ps()
                for k in range(4):
                    kc = half * 4 + k
                    P.op("pe", lambda e, k=k, kc=kc, pt=pt, xo=xo: e.transpose(pt.v(k * 128, [[1, 128]]), xo.v(kc * 128, [[1, 128]]), c128("id")),
                         reads=[xo.key, "C128"], writes=[pt.key])
                P.op("act", lambda e, half=half, pt=pt, tile=tile: e.activation(out=xT.v(half * 4 * T + tile * 128, [[T, 4], [1, 128]]),
                                                                                in_=pt.v(0, [[128, 4], [1, 128]]), func=AF.Copy),
                     reads=[pt.key], writes=["xT%d" % tile])

    units = []
    for s in range(NSEQ):
        units.append((None, lambda slot, s=s: x0_run(s)))
        for l in range(L):
            for h in range(4):
                units.append((lambda slot, l=l, h=h: gdn_load(slot, l, h), lambda slot, l=l, h=h, s=s: gdn_run(slot, l, h, s)))
            for h in range(4):
                units.append((lambda slot, l=l, h=h: ret_load(slot, l, h), lambda slot, l=l, h=h, s=s: ret_run(slot, l, h, s)))
            for g in range(2):
                units.append((lambda slot, l=l, g=g: ssd_load1(slot, l, g), lambda slot: None))
                units.append((lambda slot, l=l, g=g: ssd_load2(slot, l, g), lambda slot, l=l, g=g, s=s: ssd_run(slot, l, g, s), True))
            for dc in range(8):
                units.append((lambda slot, l=l, dc=dc: mrg_load(slot, l, dc), lambda slot, l=l, dc=dc, s=s: mrg_run(slot, l, dc, s)))
            if l == 0:
                srcf = lambda tile, s=s: dv(dr["x"], (s * T + tile * 128) * D, [[D, 128], [1, D]])
                srck = lambda tile: []
            else:
                srcf = lambda tile: dv(xres[1], tile * 128 * D, [[D, 128], [1, D]])
                srck = lambda tile: ["xres1"]
            units.append((lambda slot, l=l: wout_load1(slot, l), lambda slot: None))
            units.append((lambda slot, l=l: wout_load2(slot, l), lambda slot, l=l, s=s, srcf=srcf, srck=srck: wout_run(slot, l, s, srcf, srck), True))
            units.append((None, lambda slot, l=l, s=s: moe_init(l, s)))
            for e_ in range(NE):
                for hf in range(2):
                    units.append((lambda slot, l=l, e_=e_, hf=hf: moe_load(slot, l, e_, hf),
                                  lambda slot, l=l, e_=e_, hf=hf, s=s: moe_run(slot, l, e_, hf, s)))
            units.append((None, lambda slot, l=l, s=s: ln2_run(l, s, l == L - 1)))

    wl = [u for u in units if u[0] is not None]
    slot_of = {}
    k = 0
    for i, u in enumerate(units):
        if u[0] is not None:
            slot_of[i] = k % 2
            k += 1
    loaded = set()
    idxs = [i for i, u in enumerate(units) if u[0] is not None]

    def ensure_loaded(i):
        if i not in loaded:
            units[i][0](slot_of[i])
            loaded.add(i)

    kstop = int(os.environ.get("KSTOP", "100000"))
    for i, u in enumerate(units):
        if i >= kstop:
            break
        if u[0] is not None:
            ensure_loaded(i)
            nxt = [j for j in idxs if j > i]
            both = len(u) > 2
            if nxt and not both:
                ensure_loaded(nxt[0])
            u[1](slot_of[i])
            if nxt and both:
                ensure_loaded(nxt[0])
        else:
            u[1](None)

    P.emit(final_wait_ops=outs)
    st.close()
    return nc, (a64, a128, cs_np)


_CACHE = {}


def kernel(**inputs):
    NCORES = 8
    x = np.ascontiguousarray(inputs["x"], dtype=np.float32)
    Bt, T, _ = x.shape
    NSEQ = Bt // NCORES
    DEPTH = inputs["w_in"].shape[0]
    key = (NSEQ, T, DEPTH)
    if key not in _CACHE:
        _CACHE[key] = build(NSEQ, T, DEPTH, 512)
    nc, (a64, a128, cs_np) = _CACHE[key]
    shared = {k: np.ascontiguousarray(v, dtype=np.float32) for k, v in inputs.items() if k != "x"}
    shared["c64"] = a64
    shared["c128"] = a128
    shared["cs"] = cs_np
    in_maps = []
    for c in range(NCORES):
        m = dict(shared)
        m["x"] = np.ascontiguousarray(x[c * NSEQ:(c + 1) * NSEQ])
        in_maps.append(m)
    res = run_bass_kernel_spmd(nc, in_maps, core_ids=list(range(NCORES)))
    return np.concatenate([r["y"] for r in res.results], axis=0).astype(np.float32)
```

```python
import contextlib
import numpy as np
import concourse.bass as bass
import concourse.mybir as mybir
from concourse.bass_utils import run_bass_kernel_spmd

F32 = mybir.dt.float32
BF16 = mybir.dt.bfloat16
AF = mybir.ActivationFunctionType
ALU = mybir.AluOpType
AX = mybir.AxisListType

D = 1024
KC = 8
P_IN = 9752
O_AQKV, O_AZ, O_AA, O_AB = 0, 1536, 2048, 2052
O_BQ, O_BK, O_BV, O_BG = 2056, 2568, 3080, 3592
O_CZ, O_CXBC, O_CDT, O_GATE = 4104, 5128, 6664, 6680
EPS = 1e-6
ALPHA = 4.0 ** 0.25
NE = 32
DE = 512
BIGM = 30000.0
ENGS = ("pe", "act", "dve", "pool", "sp")


class _Rec:
    def __getattr__(self, name):
        def f(*a, **k):
            self.__dict__["call"] = (name, a, k)
            return self
        return f


class Prog:
    def __init__(self, nc):
        self.nc = nc
        self.ops = []
        self.last_w = {}
        self.readers = {}

    def _add(self, eng, fn, reads, writes, chan=None):
        rec = _Rec()
        fn(rec)
        call = rec.call
        fn = lambda e, call=call: getattr(e, call[0])(*call[1], **call[2])
        idx = len(self.ops)
        deps = set()
        for r in reads:
            w = self.last_w.get(r)
            if w is not None:
                deps.add(w)
            if isinstance(r, str) and r.startswith("pb"):
                for rd in self.readers.get(r, ()):
                    if self.ops[rd]["eng"] != eng:
                        deps.add(rd)
        for r in writes:
            w = self.last_w.get(r)
            if w is not None and not (chan is not None and self.ops[w]["chan"] == chan and self.ops[w]["eng"] == eng):
                deps.add(w)
            for rd in self.readers.get(r, ()):
                deps.add(rd)
        for r in reads:
            self.readers.setdefault(r, []).append(idx)
        for r in writes:
            self.last_w[r] = idx
            self.readers[r] = []
        deps.discard(idx)
        self.ops.append(dict(eng=eng, fn=fn, deps=deps, chan=chan, has_dep=False))
        return idx

    def op(self, eng, fn, reads=(), writes=()):
        return self._add(eng, fn, tuple(reads), tuple(writes))

    def dma(self, eng, fn, reads=(), writes=(), chan="d0"):
        return self._add(eng, fn, tuple(reads), tuple(writes), chan=chan)

    def emit(self, final_wait_ops=()):
        nc = self.nc
        ops = self.ops
        for i, o in enumerate(ops):
            nd = set()
            for d in o["deps"]:
                p = ops[d]
                if p["chan"] is None and o["chan"] is None and p["eng"] == "pe" and o["eng"] == "pe":
                    continue
                nd.add(d)
            o["deps"] = nd
            for d in nd:
                ops[d]["has_dep"] = True
        for d in final_wait_ops:
            ops[d]["has_dep"] = True
        eng_cnt = {e: 0 for e in ENGS}
        chan_cnt = {}
        chans = []
        for o in ops:
            if o["chan"] is not None:
                c = o["chan"]
                if c not in chan_cnt:
                    chan_cnt[c] = 0
                    chans.append(c)
                chan_cnt[c] += 16
                o["tok"] = (("chan", c), chan_cnt[c])
            elif o["has_dep"]:
                eng_cnt[o["eng"]] += 1
                o["tok"] = (("eng", o["eng"]), eng_cnt[o["eng"]])
            else:
                o["tok"] = None
        sem_keys = [("eng", e) for e in ENGS] + [("chan", c) for c in chans]
        with contextlib.ExitStack() as st:
            sems = {}
            for k in sem_keys:
                sems[k] = st.enter_context(nc.semaphore("s_%s_%s" % k))
            blk = st.enter_context(nc.Block())

            def run_engine(eng_name, eng_obj):
                waited = {}
                for i, o in enumerate(ops):
                    if o["eng"] != eng_name:
                        continue
                    need = {}
                    for d in o["deps"]:
                        k, v = ops[d]["tok"]
                        if need.get(k, 0) < v:
                            need[k] = v
                    for k, v in need.items():
                        if waited.get(k, 0) >= v:
                            continue
                        eng_obj.wait_ge(sems[k], v)
                        waited[k] = v
                    ins = o["fn"](eng_obj)
                    if o["tok"] is not None:
                        k, v = o["tok"]
                        ins.then_inc(sems[k], 16 if k[0] == "chan" else 1)
                if eng_name == "sp":
                    need = {}
                    for d in final_wait_ops:
                        k, v = ops[d]["tok"]
                        if need.get(k, 0) < v:
                            need[k] = v
                    for k, v in need.items():
                        eng_obj.wait_ge(sems[k], v)

            blk.sync(lambda e: run_engine("sp", e))
            blk.tensor(lambda e: run_engine("pe", e))
            blk.scalar(lambda e: run_engine("act", e))
            blk.vector(lambda e: run_engine("dve", e))
            blk.gpsimd(lambda e: run_engine("pool", e))


def host_consts(T, BLK):
    c64 = {}
    i = np.arange(64)
    c64["tri"] = (i[:, None] <= i[None, :]).astype(np.float32)
    c64["id"] = np.eye(64, dtype=np.float32)
    c64["ones"] = np.ones((64, 128), np.float32)
    c64["bigm"] = np.where(i[None, :] < i[:, None], 0.0, BIGM).astype(np.float32)
    lg = np.log1p(-np.exp2(-5.0 - np.arange(4, dtype=np.float32))).astype(np.float32)
    idx = i.astype(np.float32)
    dec = np.exp(lg[:, None, None] * np.abs(idx[:, None] - idx[None, :])).astype(np.float32)
    c64["rdec"] = np.transpose(dec, (1, 0, 2)).reshape(64, 256)
    c64["wd"] = np.exp(lg[None, :] * (63.0 - idx[:, None])).astype(np.float32)
    names64 = ["tri", "id", "ones", "bigm", "rdec", "wd"]
    off64 = {}
    o = 0
    for n in names64:
        off64[n] = o
        o += c64[n].shape[1]
    a64 = np.concatenate([c64[n] for n in names64], axis=1).astype(np.float32)
    c128 = {}
    c128["id"] = np.eye(128, dtype=np.float32)
    c128["ones"] = np.ones((128, 128), np.float32)
    rot = np.zeros((128, 128), np.float32)
    for m in range(64):
        rot[m + 64, m] = -1.0
    for m in range(64, 128):
        rot[m - 64, m] = 1.0
    c128["rot"] = rot
    rd = np.exp(lg[:, None] * (idx[None, :] + 1.0)).astype(np.float32)
    rdt = np.tile(rd, (1, BLK // 64))
    c128["rd"] = np.broadcast_to(rdt.reshape(1, 4 * BLK), (128, 4 * BLK)).astype(np.float32)
    cd = np.exp(lg * 64.0).astype(np.float32)
    names128 = ["id", "ones", "rot", "rd"]
    off128 = {}
    o = 0
    for n in names128:
        off128[n] = o
        o += c128[n].shape[1]
    a128 = np.concatenate([c128[n] for n in names128], axis=1).astype(np.float32)
    pos = np.arange(T, dtype=np.float32)
    inv_freq = (np.float32(10000.0) ** (-np.arange(0, 128, 2, dtype=np.float32) / np.float32(128))).astype(np.float32)
    ang = (pos[:, None] * inv_freq[None, :]).astype(np.float32)
    cos = np.cos(ang).astype(np.float32).T
    sin = np.sin(ang).astype(np.float32).T
    cs = np.concatenate([np.concatenate([cos, cos], 0), np.concatenate([sin, sin], 0)], axis=1).astype(np.float32)
    return a64, off64, a128, off128, cs, [float(x) for x in cd]


def build(NSEQ, T, DEPTH, BLK):
    import os
    SSTOP = float(os.environ.get("SSTOP", "100"))
    WSTOP = float(os.environ.get("WSTOP", "100"))
    NT = T // 128
    NBLK = T // BLK
    CPB = BLK // 64
    TPB = BLK // 128
    a64, off64, a128, off128, cs_np, cdec = host_consts(T, BLK)
    nc = bass.Bass("TRN2", target_bir_lowering=False)
    dr = {}

    def din(name, shape):
        dr[name] = nc.dram_tensor(name, list(shape), F32, kind="ExternalInput")
        return dr[name]

    din("x", [NSEQ, T, D])
    L = DEPTH
    specs = dict(w_in=[L, D, P_IN], conv_a=[L, 4, 1536], a_log_a=[L, 4], dt_bias_a=[L, 4], norm_a=[L, 128],
                 norm_b=[L, 512], conv_c=[L, 4, 1536], conv_bias_c=[L, 1536], dt_bias_c=[L, 16], a_log_c=[L, 16],
                 d_skip_c=[L, 16], norm_c=[L, 1024], b_gate=[L, 3, D], w_branch_a=[L, 512, D],
                 w_branch_b=[L, 512, D], w_branch_c=[L, 1024, D], w_out=[L, D, D], ln1_g=[L, D], ln1_b=[L, D],
                 w_router_group=[L, D, 4], b_router_group=[L, 4], w_router_expert=[L, D, 32],
                 b_router_expert=[L, 32], w_gate_e=[L, NE, D, DE], w_up_e=[L, NE, D, DE], w_down_e=[L, NE, DE, D],
                 ln2_g=[L, D], ln2_b=[L, D])
    for k, v in specs.items():
        din(k, v)
    din("c64", list(a64.shape))
    din("c128", list(a128.shape))
    din("cs", list(cs_np.shape))
    yout = nc.dram_tensor("y", [NSEQ, T, D], F32, kind="ExternalOutput")
    xres = [nc.dram_tensor("xres%d" % i, [T, D], F32, kind="Internal") for i in range(2)]
    ytd = nc.dram_tensor("ytd", [2, 128, 16, T], BF16, kind="Internal")

    P = Prog(nc)
    st = contextlib.ExitStack()

    class Tl:
        def __init__(self, h, shape, key, base=0, pstride=None):
            self.h = h
            self.shape = shape
            self.key = key
            self.base = base
            self.row = int(np.prod(shape[1:])) if pstride is None else pstride

        def v(self, off, dims, Pn=128, p0=0):
            return bass.AP(self.h, self.base + off + p0 * self.row, [[self.row, Pn]] + [list(d) for d in dims])

    def sb(name, shape, dt=F32):
        h = st.enter_context(nc.sbuf_tensor(name, list(shape), dt))
        return Tl(h, list(shape), name)

    def dv(t, off, dims):
        return bass.AP(t, off, [list(d) for d in dims])

    banks = [Tl(st.enter_context(nc.psum_tensor("pb%d" % i, [128, 512], F32)), [128, 512], "pb%d" % i)
             for i in range(8)]
    bank_i = [0]

    xT = sb("xT", [128, KC, T], BF16)
    WBSZ = 6208
    WB = [sb("WB%d" % i, [128, WBSZ], BF16) for i in range(2)]
    C64 = sb("C64", [64, a64.shape[1]])
    C128 = sb("C128", [128, 384])
    cwa = sb("cwa", [128, L, 12, 4])
    cwc = sb("cwc", [128, L, 12, 4])
    cbc = sb("cbc", [128, L, 12])
    bgt = sb("bgt", [128, L, 3, 8])
    pb_a = sb("pb_a", [128, L, 8])
    nrm_a = sb("nrm_a", [64, L, 128])
    pc = sb("pc", [64, L, 48])
    wr = sb("wr", [128, L, KC, 36])
    brt = sb("brt", [128, L, 36])
    comb = sb("comb", [128, NT, 32])
    nexa = sb("nexa", [128, L, 4])
    nac = sb("nac", [64, L, 16])
    epst = sb("epst", [128, 4])
    ARENA_W = 88 * 256
    ARh = st.enter_context(nc.sbuf_tensor("AR", [128, ARENA_W], F32))
    ARb = ARh.bitcast(BF16)

    class Phase:
        def __init__(self, name):
            self.name = name
            self.off = 0
            self.t = {}
            self.keys = []

        def a(self, nm, free, dt=F32):
            n = int(np.prod(free))
            sz = n * (4 if dt == F32 else 2)
            off = self.off
            self.off += (sz + 3) // 4 * 4
            assert self.off <= ARENA_W * 4, (self.name, nm, self.off)
            if dt == F32:
                tl = Tl(ARh, [128] + list(free), self.name + "_" + nm, base=off // 4, pstride=ARENA_W)
            else:
                tl = Tl(ARb, [128] + list(free), self.name + "_" + nm, base=off // 2, pstride=ARENA_W * 2)
            self.t[nm] = tl
            self.keys.append(tl.key)
            return tl

    PH = {}
    for nm in "GRSMWXEN":
        PH[nm] = Phase(nm)
    G_, R_, S_, M_, W_, X_, E_, N_ = [PH[k] for k in "GRSMWXEN"]
    for nm in ["fmq", "fmk", "fmv", "fmz", "tA", "tB"]:
        R_.a(nm, [BLK])
    for nm in ["tA", "tB"]:
        G_.a(nm, [BLK])
    for par in range(2):
        for nm in ["fmq", "fmk", "fmv", "fmz"]:
            G_.a(nm + str(par), [BLK])
        for nm in ["rhs_u", "rhs_w", "kend", "u_t"]:
            G_.a(nm + str(par), [CPB * 128])
        G_.a("wT" + str(par), [CPB * 64]); G_.a("decS" + str(par), [CPB])
    for nm in ["raw0", "raw1"]:
        G_.a(nm, [BLK + 3]); S_.a(nm, [BLK + 3])
    G_.a("car", [12]); S_.a("car", [24])
    G_.a("ytb", [BLK], BF16); R_.a("ytb", [BLK], BF16); S_.a("ytb", [4 * BLK], BF16)
    G_.a("S_a", [128])
    for nm in ["g_t", "beta", "gc", "egl", "bex", "st1", "st2"]:
        G_.a(nm, [CPB])
    G_.a("gbr", [CPB * 64])
    for nm in ["o_t", "sq_t"]:
        G_.a(nm, [CPB * 128])
    for nm in ["t1", "Pm", "Qm", "Pm2", "Qm2", "Rm"]:
        G_.a(nm, [CPB * 64])
    G_.a("delta", [128])
    R_.a("cst", [2 * BLK]); R_.a("rdt", [BLK]); R_.a("SM", [CPB * 64])
    for nm in ["v_tm", "kw", "o_t", "sq_t"]:
        R_.a(nm, [CPB * 128])
    R_.a("St", [(CPB + 1) * 128]); R_.a("st1", [CPB]); R_.a("st2", [CPB]); R_.a("nrmb", [128])
    S_.a("fx", [4 * BLK])
    for nm in ["fmq", "fmk", "tA"]:
        S_.a(nm, [BLK])
    S_.a("Sc", [512])
    for nm in ["dt_t", "dta", "lc", "wr_t", "elc", "decb"]:
        S_.a(nm, [CPB * 8])
    for nm in ["nrmc", "dsk"]:
        S_.a(nm, [512])
    S_.a("szb", [CPB * 512])
    for par in range(2):
        for nm in ["db", "e1", "x_tm", "xw", "yi", "yg"]:
            S_.a(nm + str(par), [512])
        S_.a("cbT" + str(par), [64]); S_.a("B_tm" + str(par), [128]); S_.a("st2" + str(par), [4])
    mrgT = M_.a("mrg", [8 * T], BF16)
    W_.t["mrg"] = mrgT; W_.off = M_.off
    mkeys = ["mrg%d" % i for i in range(NBLK)]
    M_.keys += mkeys; W_.keys += mkeys + [mrgT.key]
    M_.a("ytl", [16 * BLK], BF16); M_.a("tA", [BLK]); M_.a("tB", [BLK])
    for ph in (W_, X_):
        ph.a("xl0", [1024]); ph.a("xl1", [1024])
    yacc = E_.a("yacc", [NT * 1024])
    N_.t["yacc"] = yacc; N_.off = E_.off
    ykeys = ["yacc%d" % i for i in range(NT)]
    E_.keys += ykeys; N_.keys += ykeys + [yacc.key]
    for ph in (W_, N_):
        for nm in ["lng", "lnb", "xn0", "xn1"]:
            ph.a(nm, [1024])
        for par in range(2):
            ph.a("bst%d" % par, [12]); ph.a("mv%d" % par, [4])
    for par in range(2):
        W_.a("xTf%d" % par, [1024]); W_.a("rt%d" % par, [160])
    E_.a("hs0", [BLK]); E_.a("hs1", [BLK]); E_.a("Hh0", [2 * BLK], BF16); E_.a("Hh1", [2 * BLK], BF16)
    cur_ph = [None]

    def enter(ph):
        old = cur_ph[0]
        if old is ph:
            return
        cur_ph[0] = ph
        if old is None:
            return
        P.op("dve", lambda e: e.memset(epst.v(3, [[1, 1]]), 0.0), writes=list(old.keys) + list(ph.keys) + ["epst3"])

    def c64(name, w=None, p0=0, Pn=64, coff=0):
        w = w if w is not None else {"tri": 64, "id": 64, "ones": 128, "bigm": 64, "rdec": 256, "wd": 4}[name]
        return C64.v(off64[name] + coff, [[1, w]], Pn=Pn, p0=p0)

    def c128(name, w=128, coff=0):
        return C128.v(off128[name] + coff, [[1, w]])

    P.dma("sp", lambda e: e.dma_start(out=C64.h[:, :], in_=dr["c64"][:, :]), writes=["C64"], chan="i_c64")
    P.dma("sp", lambda e: e.dma_start(out=C128.h[:, :], in_=dr["c128"][:, 0:384]), writes=["C128"], chan="i_c128")

    def small(dst_ap, src_ap, key, chan=None):
        P.dma("sp", lambda e: e.dma_start(out=dst_ap, in_=src_ap, allow_slow_non_contiguous=True), writes=[key], chan="i_" + key)

    for l in range(L):
        for k in range(4):
            small(cwa.v(l * 48 + k, [[4, 12]]), dv(dr["conv_a"], l * 6144 + k * 1536, [[1, 128], [128, 12]]), "cwa")
            small(cwc.v(l * 48 + k, [[4, 12]]), dv(dr["conv_c"], l * 6144 + k * 1536, [[1, 128], [128, 12]]), "cwc")
        small(cbc.v(l * 12, [[1, 12]]), dv(dr["conv_bias_c"], l * 1536, [[1, 128], [128, 12]]), "cbc")
        for br in range(3):
            small(bgt.v(l * 24 + br * 8, [[1, 8]]), dv(dr["b_gate"], l * 3072 + br * 1024, [[1, 128], [128, 8]]), "bgt")
        small(pb_a.v(l * 8, [[1, 4]]), dv(dr["a_log_a"], l * 4, [[0, 128], [1, 4]]), "pb_a")
        small(pb_a.v(l * 8 + 4, [[1, 4]]), dv(dr["dt_bias_a"], l * 4, [[0, 128], [1, 4]]), "pb_a")
        small(nrm_a.v(l * 128, [[1, 128]], Pn=64), dv(dr["norm_a"], l * 128, [[0, 64], [1, 128]]), "nrm_a")
        small(pc.v(l * 48, [[1, 16]], Pn=64), dv(dr["dt_bias_c"], l * 16, [[0, 64], [1, 16]]), "pc")
        small(pc.v(l * 48 + 16, [[1, 16]], Pn=64), dv(dr["a_log_c"], l * 16, [[0, 64], [1, 16]]), "pc")
        small(pc.v(l * 48 + 32, [[1, 16]], Pn=64), dv(dr["d_skip_c"], l * 16, [[0, 64], [1, 16]]), "pc")
        small(wr.v(l * KC * 36, [[36, KC], [1, 4]]), dv(dr["w_router_group"], l * D * 4, [[4, 128], [512, KC], [1, 4]]), "wr")
        small(wr.v(l * KC * 36 + 4, [[36, KC], [1, 32]]), dv(dr["w_router_expert"], l * D * 32, [[32, 128], [4096, KC], [1, 32]]), "wr")
        small(brt.v(l * 36, [[1, 4]]), dv(dr["b_router_group"], l * 4, [[0, 128], [1, 4]]), "brt")
        small(brt.v(l * 36 + 4, [[1, 32]]), dv(dr["b_router_expert"], l * 32, [[0, 128], [1, 32]]), "brt")
    P.op("dve", lambda e: e.memset(epst.v(0, [[1, 1]]), EPS), writes=["epst"])
    P.op("dve", lambda e: e.memset(epst.v(1, [[1, 1]]), 1.0), reads=["epst"], writes=["epst"])
    P.op("dve", lambda e: e.memset(epst.v(2, [[1, 1]]), 0.0), reads=["epst"], writes=["epst"])
    eps_ap = epst.v(0, [[1, 1]])
    one_ap = epst.v(1, [[1, 1]])
    for l in range(L):
        P.op("act", lambda e, l=l: e.activation(out=nexa.v(l * 4, [[1, 4]]), in_=pb_a.v(l * 8, [[1, 4]]), func=AF.Exp),
             reads=["pb_a"], writes=["nexa"])
        P.op("dve", lambda e, l=l: e.tensor_scalar(out=nexa.v(l * 4, [[1, 4]]), in0=nexa.v(l * 4, [[1, 4]]), scalar1=-1.0,
                                                   scalar2=None, op0=ALU.mult), reads=["nexa"], writes=["nexa"])
        P.op("act", lambda e, l=l: e.activation(out=nac.v(l * 16, [[1, 16]], Pn=64), in_=pc.v(l * 48 + 16, [[1, 16]], Pn=64),
                                                func=AF.Exp), reads=["pc"], writes=["nac"])
        P.op("dve", lambda e, l=l: e.tensor_scalar(out=nac.v(l * 16, [[1, 16]], Pn=64), in0=nac.v(l * 16, [[1, 16]], Pn=64),
                                                   scalar1=-1.0, scalar2=None, op0=ALU.mult), reads=["nac"], writes=["nac"])

    pinned = set()

    def ps():
        while True:
            b = banks[bank_i[0] % 8]
            bank_i[0] += 1
            if b.key not in pinned:
                return b

    def ld_w(slot, off_el, ncols, src_t, src_off, row_stride, nk=KC, krows=128):
        P.dma("pool", lambda e: e.dma_start(out=WB[slot].v(off_el, [[ncols, nk], [1, ncols]]),
                                            in_=dv(src_t, src_off, [[row_stride, 128], [128 * row_stride, nk], [1, ncols]]),
                                            allow_slow_non_contiguous=True),
              writes=["WB%d" % slot], chan="w%d" % slot)

    def xt_keys(tok0, n):
        return ["xT%d" % i for i in range(tok0 // 128, (tok0 + n + 127) // 128)]

    def proj_fm(slot, woff, wcols, c0, tok0, n, pbank, pcol=0):
        for kc in range(KC):
            P.op("pe", lambda e, kc=kc: e.matmul(pbank.v(pcol, [[1, n]]),
                                                 lhsT=WB[slot].v(woff + kc * wcols + c0, [[1, 128]]),
                                                 rhs=xT.v(kc * T + tok0, [[1, n]]), start=(kc == 0), stop=(kc == KC - 1)),
                 reads=["WB%d" % slot] + xt_keys(tok0, n), writes=[pbank.key])

    def proj_tm(slot, woff, wcols, c0, ncol, tok0, pbank, pcol=0):
        for kc in range(KC):
            P.op("pe", lambda e, kc=kc: e.matmul(pbank.v(pcol, [[1, ncol]], Pn=64),
                                                 lhsT=xT.v(kc * T + tok0, [[1, 64]]),
                                                 rhs=WB[slot].v(woff + kc * wcols + c0, [[1, ncol]]),
                                                 start=(kc == 0), stop=(kc == KC - 1)),
                 reads=["WB%d" % slot] + xt_keys(tok0, 64), writes=[pbank.key])

    def l2norm_fm(ph, t, scale):
        tB = ph.t["tB"]
        P.op("act", lambda e: e.activation(out=tB.v(0, [[1, BLK]]), in_=t.v(0, [[1, BLK]]), func=AF.Square), reads=[t.key], writes=[tB.key])
        pb = ps()
        P.op("pe", lambda e: e.matmul(pb.v(0, [[1, BLK]]), lhsT=c128("ones"), rhs=tB.v(0, [[1, BLK]]), start=True, stop=True),
             reads=[tB.key, "C128"], writes=[pb.key])
        P.op("act", lambda e: e.activation(out=tB.v(0, [[1, BLK]]), in_=pb.v(0, [[1, BLK]]), func=AF.Ln, bias=eps_ap, scale=1.0),
             reads=[pb.key, "epst"], writes=[tB.key])
        P.op("act", lambda e: e.activation(out=tB.v(0, [[1, BLK]]), in_=tB.v(0, [[1, BLK]]), func=AF.Exp, scale=-0.5), reads=[tB.key], writes=[tB.key])
        P.op("dve", lambda e: e.scalar_tensor_tensor(out=t.v(0, [[1, BLK]]), in0=t.v(0, [[1, BLK]]), scalar=float(scale),
                                                     in1=tB.v(0, [[1, BLK]]), op0=ALU.mult, op1=ALU.mult),
             reads=[t.key, tB.key], writes=[t.key])

    def store_yT(ytb, fc0, nfc, tok0, s):
        P.dma("sp", lambda e: e.dma_start(out=dv(ytd, (s % 2) * 128 * 16 * T + fc0 * T + tok0, [[16 * T, 128], [T, nfc], [1, BLK]]),
                                          in_=ytb.v(0, [[BLK, nfc], [1, BLK]])),
              reads=[ytb.key], writes=["ytd%d_%d_%d" % (s % 2, fc0 + i, tok0 // BLK) for i in range(nfc)], chan="yt")

    def gdn_load(slot, l, h):
        base = l * D * P_IN
        for j, c0 in enumerate([O_AQKV + h * 128, O_AQKV + 512 + h * 128, O_AQKV + 1024 + h * 128, O_AZ + h * 128]):
            ld_w(slot, j * 1024, 128, dr["w_in"], base + c0, P_IN)
        ld_w(slot, 4096, 1, dr["w_in"], base + O_AA + h, P_IN)
        ld_w(slot, 4096 + 8, 1, dr["w_in"], base + O_AB + h, P_IN)

    def gdn_run(slot, l, h, s):
        wk = "WB%d" % slot
        enter(G_)
        ph = G_
        tA, tB, ytb, S_a, g_t, beta, gc, egl, bex, st1, st2, gbr, o_t, sq_t, t1, Pm, Qm, Pm2, Qm2, Rm, delta = [G_.t[n] for n in ['tA', 'tB', 'ytb', 'S_a', 'g_t', 'beta', 'gc', 'egl', 'bex', 'st1', 'st2', 'gbr', 'o_t', 'sq_t', 't1', 'Pm', 'Qm', 'Pm2', 'Qm2', 'Rm', 'delta']]
        for b in range(NBLK):
            tok0 = b * BLK
            fmq, fmk, fmv, fmz, rhs_u, rhs_w, kend, u_t, wT, decS = [G_.t[n + str(b % 2)] for n in ['fmq', 'fmk', 'fmv', 'fmz', 'rhs_u', 'rhs_w', 'kend', 'u_t', 'wT', 'decS']]
            for j, dst in enumerate([fmq, fmk, fmv]):
                pb = ps()
                proj_fm(slot, j * 1024, 128, 0, tok0, BLK, pb)
                cw = lambda k, j=j: cwa.v(l * 48 + (j * 4 + h) * 4 + k, [[1, 1]])
                conv_stream(ph, pb, j, b, cw, dst.v(0, [[1, BLK]]), dst.key)
            l2norm_fm(ph, fmq, 128.0 ** -0.5)
            l2norm_fm(ph, fmk, 1.0)
            pb = ps()
            proj_fm(slot, 3 * 1024, 128, 0, tok0, BLK, pb)
            P.op("act", lambda e, pb=pb: e.activation(out=fmz.v(0, [[1, BLK]]), in_=pb.v(0, [[1, BLK]]), func=AF.Silu),
                 reads=[pb.key], writes=[fmz.key])
            pb = ps()
            for c in range(CPB):
                proj_tm(slot, 4096, 1, 0, 1, tok0 + c * 64, pb, pcol=2 * c)
                proj_tm(slot, 4096 + 8, 1, 0, 1, tok0 + c * 64, pb, pcol=2 * c + 1)
            P.op("act", lambda e, pb=pb: e.activation(out=g_t.v(0, [[1, CPB]], Pn=64), in_=pb.v(0, [[2, CPB]], Pn=64), func=AF.Exp,
                                                      bias=pb_a.v(l * 8 + 4 + h, [[1, 1]], Pn=64)),
                 reads=[pb.key, "pb_a"], writes=[g_t.key])
            P.op("act", lambda e: e.activation(out=g_t.v(0, [[1, CPB]], Pn=64), in_=g_t.v(0, [[1, CPB]], Pn=64), func=AF.Ln,
                                               bias=one_ap[0:64, :]), reads=[g_t.key, "epst"], writes=[g_t.key])
            P.op("dve", lambda e: e.tensor_scalar(out=g_t.v(0, [[1, CPB]], Pn=64), in0=g_t.v(0, [[1, CPB]], Pn=64),
                                                  scalar1=nexa.v(l * 4 + h, [[1, 1]], Pn=64), scalar2=None, op0=ALU.mult),
                 reads=[g_t.key, "nexa"], writes=[g_t.key])
            P.op("act", lambda e, pb=pb: e.activation(out=beta.v(0, [[1, CPB]], Pn=64), in_=pb.v(1, [[2, CPB]], Pn=64),
                                                      func=AF.Exp, scale=-1.0), reads=[pb.key], writes=[beta.key])
            P.op("dve", lambda e: e.tensor_scalar(out=beta.v(0, [[1, CPB]], Pn=64), in0=beta.v(0, [[1, CPB]], Pn=64), scalar1=1.0, scalar2=None,
                                                  op0=ALU.add), reads=[beta.key], writes=[beta.key])
            P.op("dve", lambda e: e.reciprocal(out=beta.v(0, [[1, CPB]], Pn=64), in_=beta.v(0, [[1, CPB]], Pn=64)), reads=[beta.key], writes=[beta.key])
            pg = ps()
            P.op("pe", lambda e, pg=pg: e.matmul(pg.v(0, [[1, CPB]], Pn=64), lhsT=c64("tri"), rhs=g_t.v(0, [[1, CPB]], Pn=64),
                                                 start=True, stop=True), reads=[g_t.key, "C64"], writes=[pg.key])
            P.op("pe", lambda e, pg=pg: e.matmul(pg.v(64, [[1, CPB]]), lhsT=c64("ones"), rhs=g_t.v(0, [[1, CPB]], Pn=64),
                                                 start=True, stop=True), reads=[g_t.key, "C64"], writes=[pg.key])
            P.op("dve", lambda e, pg=pg: e.tensor_copy(out=gc.v(0, [[1, CPB]], Pn=64), in_=pg.v(0, [[1, CPB]], Pn=64)),
                 reads=[pg.key], writes=[gc.key])
            P.op("act", lambda e, pg=pg: e.activation(out=decS.v(0, [[1, CPB]]), in_=pg.v(64, [[1, CPB]]), func=AF.Exp),
                 reads=[pg.key], writes=[decS.key])
            P.op("dve", lambda e, pg=pg: e.tensor_tensor(out=egl.v(0, [[1, CPB]], Pn=64), in0=pg.v(64, [[1, CPB]], Pn=64),
                                                         in1=gc.v(0, [[1, CPB]], Pn=64), op=ALU.subtract),
                 reads=[pg.key, gc.key], writes=[egl.key])
            P.op("act", lambda e: e.activation(out=egl.v(0, [[1, CPB]], Pn=64), in_=egl.v(0, [[1, CPB]], Pn=64), func=AF.Exp),
                 reads=[egl.key], writes=[egl.key])
            P.op("act", lambda e: e.activation(out=bex.v(0, [[1, CPB]], Pn=64), in_=gc.v(0, [[1, CPB]], Pn=64), func=AF.Exp),
                 reads=[gc.key], writes=[bex.key])
            P.op("dve", lambda e: e.tensor_tensor(out=bex.v(0, [[1, CPB]], Pn=64), in0=bex.v(0, [[1, CPB]], Pn=64),
                                                  in1=beta.v(0, [[1, CPB]], Pn=64), op=ALU.mult), reads=[bex.key, beta.key], writes=[bex.key])
            P.op("dve", lambda e: e.tensor_copy(out=gbr.v(0, [[64, CPB], [1, 64]], Pn=64), in_=g_t.v(0, [[1, CPB], [0, 64]], Pn=64)),
                 reads=[g_t.key], writes=[gbr.key])
            pG = ps()
            pK = ps()
            for c in range(CPB):
                P.op("pe", lambda e, c=c, pG=pG: e.matmul(pG.v(c * 64, [[1, 64]], Pn=64), lhsT=gbr.v(c * 64, [[1, 64]], Pn=64),
                                                          rhs=c64("tri"), start=True, stop=True), reads=[gbr.key, "C64"], writes=[pG.key])
                P.op("pe", lambda e, c=c, pK=pK: e.matmul(pK.v(c * 64, [[1, 64]], Pn=64), lhsT=fmk.v(c * 64, [[1, 64]]),
                                                          rhs=fmk.v(c * 64, [[1, 64]]), start=True, stop=True), reads=[fmk.key], writes=[pK.key])
            P.op("dve", lambda e, pG=pG: e.tensor_tensor(out=t1.v(0, [[64, CPB], [1, 64]], Pn=64), in0=pG.v(0, [[64, CPB], [1, 64]], Pn=64),
                                                         in1=gc.v(0, [[1, CPB], [0, 64]], Pn=64), op=ALU.subtract),
                 reads=[pG.key, gc.key], writes=[t1.key])
            P.op("dve", lambda e: e.tensor_tensor(out=t1.v(0, [[64, CPB], [1, 64]], Pn=64), in0=t1.v(0, [[64, CPB], [1, 64]], Pn=64),
                                                  in1=C64.v(off64["bigm"], [[0, CPB], [1, 64]], Pn=64), op=ALU.max),
                 reads=[t1.key, "C64"], writes=[t1.key])
            P.op("act", lambda e: e.activation(out=t1.v(0, [[1, CPB * 64]], Pn=64), in_=t1.v(0, [[1, CPB * 64]], Pn=64), func=AF.Exp, scale=-1.0),
                 reads=[t1.key], writes=[t1.key])
            P.op("dve", lambda e, pK=pK: e.tensor_tensor(out=Qm.v(0, [[64, CPB], [1, 64]], Pn=64), in0=pK.v(0, [[64, CPB], [1, 64]], Pn=64),
                                                         in1=beta.v(0, [[1, CPB], [0, 64]], Pn=64), op=ALU.mult),
                 reads=[pK.key, beta.key], writes=[Qm.key])
            P.op("dve", lambda e: e.tensor_tensor(out=Qm.v(0, [[1, CPB * 64]], Pn=64), in0=Qm.v(0, [[1, CPB * 64]], Pn=64),
                                                  in1=t1.v(0, [[1, CPB * 64]], Pn=64), op=ALU.mult), reads=[Qm.key, t1.key], writes=[Qm.key])
            pT = [ps(), ps()]
            pV = [ps(), ps()]
            pB_ = ps()
            for c in range(CPB):
                P.op("pe", lambda e, c=c: e.transpose(pT[c // 4].v((c % 4) * 128, [[1, 128]], Pn=64), fmk.v(c * 64, [[1, 64]]), c128("id")),
                     reads=[fmk.key, "C128"], writes=[pT[c // 4].key])
                P.op("pe", lambda e, c=c: e.transpose(pV[c // 4].v((c % 4) * 128, [[1, 128]], Pn=64), fmv.v(c * 64, [[1, 64]]), c128("id")),
                     reads=[fmv.key, "C128"], writes=[pV[c // 4].key])
                P.op("pe", lambda e, c=c: e.transpose(pB_.v(c * 64, [[1, 64]], Pn=64), Qm.v(c * 64, [[1, 64]], Pn=64), c64("id")),
                     reads=[Qm.key, "C64"], writes=[pB_.key])
            for hb in range((CPB + 3) // 4):
                n = min(4, CPB - hb * 4)
                P.op("dve", lambda e, hb=hb, n=n: e.tensor_tensor(out=rhs_w.v(hb * 512, [[128, n], [1, 128]], Pn=64),
                                                                  in0=pT[hb].v(0, [[128, n], [1, 128]], Pn=64),
                                                                  in1=bex.v(hb * 4, [[1, n], [0, 128]], Pn=64), op=ALU.mult),
                     reads=[pT[hb].key, bex.key], writes=[rhs_w.key])
                P.op("dve", lambda e, hb=hb, n=n: e.tensor_tensor(out=kend.v(hb * 512, [[128, n], [1, 128]], Pn=64),
                                                                  in0=pT[hb].v(0, [[128, n], [1, 128]], Pn=64),
                                                                  in1=egl.v(hb * 4, [[1, n], [0, 128]], Pn=64), op=ALU.mult),
                     reads=[pT[hb].key, egl.key], writes=[kend.key])
                P.op("dve", lambda e, hb=hb, n=n: e.tensor_tensor(out=rhs_u.v(hb * 512, [[128, n], [1, 128]], Pn=64),
                                                                  in0=pV[hb].v(0, [[128, n], [1, 128]], Pn=64),
                                                                  in1=beta.v(hb * 4, [[1, n], [0, 128]], Pn=64), op=ALU.mult),
                     reads=[pV[hb].key, beta.key], writes=[rhs_u.key])
            P.op("act", lambda e: e.activation(out=Pm.v(0, [[1, CPB * 64]], Pn=64), in_=pB_.v(0, [[1, CPB * 64]], Pn=64), func=AF.Copy),
                 reads=[pB_.key], writes=[Pm.key])
            P.op("dve", lambda e: e.tensor_tensor(out=Rm.v(0, [[64, CPB], [1, 64]], Pn=64), in0=C64.v(off64["id"], [[0, CPB], [1, 64]], Pn=64),
                                                  in1=pB_.v(0, [[64, CPB], [1, 64]], Pn=64), op=ALU.subtract),
                 reads=[pB_.key, "C64"], writes=[Rm.key])
            Pc, Qc, Pn_, Qn_ = Pm, Qm, Pm2, Qm2
            for lev in range(5):
                last = lev == 4
                pq = ps()
                pp = ps() if not last else None
                for c in range(CPB):
                    P.op("pe", lambda e, c=c, pq=pq, Pc=Pc, Qc=Qc: e.matmul(pq.v(c * 64, [[1, 64]], Pn=64), lhsT=Pc.v(c * 64, [[1, 64]], Pn=64),
                                                                         rhs=Qc.v(c * 64, [[1, 64]], Pn=64), start=True, stop=True),
                         reads=[Pc.key, Qc.key], writes=[pq.key])
                    if not last:
                        P.op("pe", lambda e, c=c, pp=pp, Pc=Pc, Qc=Qc: e.matmul(pp.v(c * 64, [[1, 64]], Pn=64), lhsT=Qc.v(c * 64, [[1, 64]], Pn=64),
                                                                             rhs=Pc.v(c * 64, [[1, 64]], Pn=64), start=True, stop=True),
                             reads=[Pc.key, Qc.key], writes=[pp.key])
                P.op("act", lambda e, pq=pq, Qn_=Qn_: e.activation(out=Qn_.v(0, [[1, CPB * 64]], Pn=64), in_=pq.v(0, [[1, CPB * 64]], Pn=64), func=AF.Copy),
                     reads=[pq.key], writes=[Qn_.key])
                if not last:
                    P.op("dve", lambda e, pp=pp, Pn_=Pn_: e.tensor_copy(out=Pn_.v(0, [[1, CPB * 64]], Pn=64), in_=pp.v(0, [[1, CPB * 64]], Pn=64)),
                         reads=[pp.key], writes=[Pn_.key])
                pr = ps()
                for c in range(CPB):
                    P.op("pe", lambda e, c=c, pr=pr, Qn_=Qn_: e.matmul(pr.v(c * 64, [[1, 64]], Pn=64), lhsT=Qn_.v(c * 64, [[1, 64]], Pn=64),
                                                                      rhs=Rm.v(c * 64, [[1, 64]], Pn=64), start=True, stop=True),
                         reads=[Qn_.key, Rm.key], writes=[pr.key])
                P.op("dve", lambda e, pr=pr: e.tensor_tensor(out=Rm.v(0, [[1, CPB * 64]], Pn=64), in0=Rm.v(0, [[1, CPB * 64]], Pn=64),
                                                             in1=pr.v(0, [[1, CPB * 64]], Pn=64), op=ALU.add), reads=[pr.key, Rm.key], writes=[Rm.key])
                Pc, Qc, Pn_, Qn_ = Pn_, Qn_, Pc, Qc
            pU = [ps(), ps()]
            pW = ps()
            for c in range(CPB):
                P.op("pe", lambda e, c=c: e.matmul(pU[c // 4].v((c % 4) * 128, [[1, 128]], Pn=64), lhsT=Rm.v(c * 64, [[1, 64]], Pn=64),
                                                   rhs=rhs_u.v(c * 128, [[1, 128]], Pn=64), start=True, stop=True),
                     reads=[Rm.key, rhs_u.key], writes=[pU[c // 4].key])
                P.op("pe", lambda e, c=c: e.matmul(pW.v(c * 64, [[1, 64]]), lhsT=rhs_w.v(c * 128, [[1, 128]], Pn=64),
                                                   rhs=Rm.v(c * 64, [[1, 64]], Pn=64), start=True, stop=True),
                     reads=[Rm.key, rhs_w.key], writes=[pW.key])
            for hb in range((CPB + 3) // 4):
                n = min(4, CPB - hb * 4)
                P.op("act", lambda e, hb=hb, n=n: e.activation(out=u_t.v(hb * 512, [[1, n * 128]], Pn=64), in_=pU[hb].v(0, [[1, n * 128]], Pn=64),
                                                               func=AF.Copy), reads=[pU[hb].key], writes=[u_t.key])
            P.op("act", lambda e: e.activation(out=wT.v(0, [[1, CPB * 64]]), in_=pW.v(0, [[1, CPB * 64]]), func=AF.Copy),
                 reads=[pW.key], writes=[wT.key])
            if b == 0:
                P.op("dve", lambda e: e.memset(S_a.v(0, [[1, 128]]), 0.0), writes=[S_a.key])
            pO = [ps(), ps()]
            pinned.update([pO[0].key, pO[1].key])
            for c in range(CPB):
                p1 = ps()
                P.op("pe", lambda e, c=c, p1=p1: e.matmul(p1.v(0, [[1, 128]], Pn=64), lhsT=wT.v(c * 64, [[1, 64]]), rhs=S_a.v(0, [[1, 128]]),
                                                          start=True, stop=True), reads=[wT.key, S_a.key], writes=[p1.key])
                P.op("dve", lambda e, c=c, p1=p1: e.tensor_tensor(out=delta.v(0, [[1, 128]], Pn=64), in0=u_t.v(c * 128, [[1, 128]], Pn=64),
                                                                  in1=p1.v(0, [[1, 128]], Pn=64), op=ALU.subtract),
                     reads=[u_t.key, p1.key], writes=[delta.key])
                p2 = ps()
                P.op("pe", lambda e, c=c, p2=p2: e.matmul(p2.v(0, [[1, 128]]), lhsT=kend.v(c * 128, [[1, 128]], Pn=64),
                                                          rhs=delta.v(0, [[1, 128]], Pn=64), start=True, stop=True),
                     reads=[kend.key, delta.key], writes=[p2.key])
                P.op("dve", lambda e, c=c, p2=p2: e.scalar_tensor_tensor(out=S_a.v(0, [[1, 128]]), in0=S_a.v(0, [[1, 128]]),
                                                                         scalar=decS.v(c, [[1, 1]]), in1=p2.v(0, [[1, 128]]),
                                                                         op0=ALU.mult, op1=ALU.add), reads=[S_a.key, decS.key, p2.key], writes=[S_a.key])
                P.op("pe", lambda e, c=c: e.matmul(pO[c // 4].v((c % 4) * 128, [[1, 128]], Pn=64), lhsT=fmq.v(c * 64, [[1, 64]]),
                                                   rhs=S_a.v(0, [[1, 128]]), start=True, stop=True), reads=[fmq.key, S_a.key], writes=[pO[c // 4].key])
            pinned.difference_update([pO[0].key, pO[1].key])
            for hb in range((CPB + 3) // 4):
                n = min(4, CPB - hb * 4)
                P.op("act", lambda e, hb=hb, n=n: e.activation(out=o_t.v(hb * 512, [[1, n * 128]], Pn=64), in_=pO[hb].v(0, [[1, n * 128]], Pn=64),
                                                               func=AF.Copy), reads=[pO[hb].key], writes=[o_t.key])
            rms_tm(ph, o_t, CPB, 128, nrm_a.v(l * 128, [[0, CPB], [1, 128]], Pn=64), "nrm_a")
            pY = ps()
            for c in range(CPB):
                P.op("pe", lambda e, c=c, pY=pY: e.transpose(pY.v(c * 64, [[1, 64]]), o_t.v(c * 128, [[1, 128]], Pn=64), c64("id")),
                     reads=[o_t.key, "C64"], writes=[pY.key])
            P.op("dve", lambda e, pY=pY: e.tensor_tensor(out=ytb.v(0, [[1, BLK]]), in0=pY.v(0, [[1, BLK]]), in1=fmz.v(0, [[1, BLK]]), op=ALU.mult),
                 reads=[pY.key, fmz.key], writes=[ytb.key])
            store_yT(ytb, h, 1, tok0, s)

    conv_cnt = [0]

    def conv_stream(ph, pb, sid, b, cw, out_ap, out_key, bias_ap=None):
        r = ph.t["raw%d" % (conv_cnt[0] % 2)]
        conv_cnt[0] += 1
        car = ph.t["car"]
        tA = ph.t["tA"]
        if b == 0:
            P.op("dve", lambda e: e.memset(r.v(0, [[1, 3]]), 0.0), writes=[r.key])
        else:
            P.op("dve", lambda e: e.tensor_copy(out=r.v(0, [[1, 3]]), in_=car.v(sid * 3, [[1, 3]])), reads=[car.key], writes=[r.key])
        P.op("act", lambda e: e.activation(out=r.v(3, [[1, BLK]]), in_=pb.v(0, [[1, BLK]]), func=AF.Copy),
             reads=[pb.key, r.key], writes=[r.key])
        P.op("dve", lambda e: e.tensor_copy(out=car.v(sid * 3, [[1, 3]]), in_=r.v(BLK, [[1, 3]])), reads=[r.key, car.key], writes=[car.key])
        P.op("dve", lambda e: e.tensor_scalar(out=tA.v(0, [[1, BLK]]), in0=r.v(0, [[1, BLK]]), scalar1=cw(0), scalar2=None, op0=ALU.mult),
             reads=[r.key, "cwa", "cwc"], writes=[tA.key])
        for k in range(1, 4):
            P.op("dve", lambda e, k=k: e.scalar_tensor_tensor(out=tA.v(0, [[1, BLK]]), in0=r.v(k, [[1, BLK]]), scalar=cw(k),
                                                              in1=tA.v(0, [[1, BLK]]), op0=ALU.mult, op1=ALU.add),
                 reads=[r.key, tA.key, "cwa", "cwc"], writes=[tA.key])
        if bias_ap is None:
            P.op("act", lambda e: e.activation(out=out_ap, in_=tA.v(0, [[1, BLK]]), func=AF.Silu), reads=[tA.key], writes=[out_key])
        else:
            P.op("act", lambda e: e.activation(out=out_ap, in_=tA.v(0, [[1, BLK]]), func=AF.Silu, bias=bias_ap),
                 reads=[tA.key, "cbc"], writes=[out_key])

    def rms_tm(ph, t, nch, width, w_ap, wkey, center=False):
        st1, st2, sq_t = ph.t["st1"], ph.t["st2"], ph.t["sq_t"]
        full = t.v(0, [[width, nch], [1, width]], Pn=64)
        if center:
            P.op("dve", lambda e: e.tensor_reduce(out=st1.v(0, [[1, nch]], Pn=64), in_=full, axis=AX.X, op=ALU.add), reads=[t.key], writes=[st1.key])
            P.op("dve", lambda e: e.tensor_scalar(out=st1.v(0, [[1, nch]], Pn=64), in0=st1.v(0, [[1, nch]], Pn=64), scalar1=1.0 / width,
                                                  scalar2=None, op0=ALU.mult), reads=[st1.key], writes=[st1.key])
            P.op("dve", lambda e: e.tensor_tensor(out=full, in0=full, in1=st1.v(0, [[1, nch], [0, width]], Pn=64), op=ALU.subtract),
                 reads=[t.key, st1.key], writes=[t.key])
        P.op("act", lambda e: e.activation(out=sq_t.v(0, [[1, nch * width]], Pn=64),
                                           in_=t.v(0, [[1, nch * width]], Pn=64), func=AF.Square), reads=[t.key], writes=[sq_t.key])
        P.op("dve", lambda e: e.tensor_reduce(out=st2.v(0, [[1, nch]], Pn=64), in_=sq_t.v(0, [[width, nch], [1, width]], Pn=64), axis=AX.X, op=ALU.add),
             reads=[sq_t.key], writes=[st2.key])
        P.op("act", lambda e: e.activation(out=st2.v(0, [[1, nch]], Pn=64), in_=st2.v(0, [[1, nch]], Pn=64), func=AF.Ln,
                                           bias=eps_ap[0:64, :], scale=1.0 / width), reads=[st2.key, "epst"], writes=[st2.key])
        P.op("act", lambda e: e.activation(out=st2.v(0, [[1, nch]], Pn=64), in_=st2.v(0, [[1, nch]], Pn=64), func=AF.Exp, scale=-0.5),
             reads=[st2.key], writes=[st2.key])
        P.op("dve", lambda e: e.tensor_tensor(out=full, in0=full, in1=st2.v(0, [[1, nch], [0, width]], Pn=64), op=ALU.mult),
             reads=[t.key, st2.key], writes=[t.key])
        P.op("dve", lambda e: e.tensor_tensor(out=full, in0=full, in1=w_ap, op=ALU.mult), reads=[t.key, wkey], writes=[t.key])

    def ret_load(slot, l, h):
        base = l * D * P_IN
        for j, c0 in enumerate([O_BQ + h * 128, O_BK + h * 128, O_BV + h * 128, O_BG + h * 128]):
            ld_w(slot, j * 1024, 128, dr["w_in"], base + c0, P_IN)

    def ret_run(slot, l, h, s):
        enter(R_)
        ph = R_
        fmq, fmk, fmv, fmz, tA, tB, ytb, cst, rdt, SM, v_tm, kw, o_t, sq_t, St, st1, st2, nrmb = [R_.t[n] for n in ['fmq', 'fmk', 'fmv', 'fmz', 'tA', 'tB', 'ytb', 'cst', 'rdt', 'SM', 'v_tm', 'kw', 'o_t', 'sq_t', 'St', 'st1', 'st2', 'nrmb']]
        small(rdt.v(0, [[1, BLK]]), dv(dr["c128"], off128["rd"] + h * BLK, [[a128.shape[1], 128], [1, BLK]]), rdt.key, chan="rp")
        small(nrmb.v(0, [[1, 128]], Pn=64), dv(dr["norm_b"], l * 512 + h * 128, [[0, 64], [1, 128]]), nrmb.key, chan="rp")
        for b in range(NBLK):
            tok0 = b * BLK
            P.dma("sp", lambda e, tok0=tok0: e.dma_start(out=cst.v(0, [[BLK, 2], [1, BLK]]),
                                                         in_=dv(dr["cs"], tok0, [[2 * T, 128], [T, 2], [1, BLK]])), writes=[cst.key], chan="cs")
            for j, dst in enumerate([fmq, fmk]):
                pb = ps()
                proj_fm(slot, j * 1024, 128, 0, tok0, BLK, pb)
                P.op("act", lambda e, pb=pb: e.activation(out=tA.v(0, [[1, BLK]]), in_=pb.v(0, [[1, BLK]]), func=AF.Copy), reads=[pb.key], writes=[tA.key])
                pr = ps()
                P.op("pe", lambda e, pr=pr: e.matmul(pr.v(0, [[1, BLK]]), lhsT=c128("rot"), rhs=tA.v(0, [[1, BLK]]), start=True, stop=True),
                     reads=[tA.key, "C128"], writes=[pr.key])
                sc = 1.0 if j == 0 else 128.0 ** -0.5
                P.op("dve", lambda e, pr=pr, sc=sc: e.scalar_tensor_tensor(out=tB.v(0, [[1, BLK]]), in0=pr.v(0, [[1, BLK]]), scalar=sc,
                                                                           in1=cst.v(BLK, [[1, BLK]]), op0=ALU.mult, op1=ALU.mult),
                     reads=[pr.key, cst.key], writes=[tB.key])
                P.op("dve", lambda e, sc=sc: e.scalar_tensor_tensor(out=tA.v(0, [[1, BLK]]), in0=tA.v(0, [[1, BLK]]), scalar=sc,
                                                                    in1=cst.v(0, [[1, BLK]]), op0=ALU.mult, op1=ALU.mult),
                     reads=[tA.key, cst.key], writes=[tA.key])
                P.op("dve", lambda e, dst=dst: e.tensor_tensor(out=dst.v(0, [[1, BLK]]), in0=tA.v(0, [[1, BLK]]), in1=tB.v(0, [[1, BLK]]), op=ALU.add),
                     reads=[tA.key, tB.key], writes=[dst.key])
            pb = ps()
            proj_fm(slot, 3 * 1024, 128, 0, tok0, BLK, pb)
            P.op("act", lambda e, pb=pb: e.activation(out=fmz.v(0, [[1, BLK]]), in_=pb.v(0, [[1, BLK]]), func=AF.Silu), reads=[pb.key], writes=[fmz.key])
            P.op("dve", lambda e: e.tensor_tensor(out=fmv.v(0, [[1, BLK]]), in0=fmq.v(0, [[1, BLK]]), in1=rdt.v(0, [[1, BLK]]), op=ALU.mult),
                 reads=[fmq.key, rdt.key], writes=[fmv.key])
            pV = [ps(), ps()]
            pT = [ps(), ps()]
            pS = ps()
            for c in range(CPB):
                proj_tm(slot, 2 * 1024, 128, 0, 128, tok0 + c * 64, pV[c // 4], pcol=(c % 4) * 128)
                P.op("pe", lambda e, c=c: e.transpose(pT[c // 4].v((c % 4) * 128, [[1, 128]], Pn=64), fmk.v(c * 64, [[1, 64]]), c128("id")),
                     reads=[fmk.key, "C128"], writes=[pT[c // 4].key])
                P.op("pe", lambda e, c=c: e.matmul(pS.v(c * 64, [[1, 64]], Pn=64), lhsT=fmk.v(c * 64, [[1, 64]]), rhs=fmq.v(c * 64, [[1, 64]]),
                                                   start=True, stop=True), reads=[fmk.key, fmq.key], writes=[pS.key])
            for hb in range((CPB + 3) // 4):
                n = min(4, CPB - hb * 4)
                P.op("act", lambda e, hb=hb, n=n: e.activation(out=v_tm.v(hb * 512, [[1, n * 128]], Pn=64), in_=pV[hb].v(0, [[1, n * 128]], Pn=64),
                                                               func=AF.Copy), reads=[pV[hb].key], writes=[v_tm.key])
                P.op("dve", lambda e, hb=hb, n=n: e.tensor_scalar(out=kw.v(hb * 512, [[1, n * 128]], Pn=64), in0=pT[hb].v(0, [[1, n * 128]], Pn=64),
                                                                  scalar1=c64("wd", 1, coff=h), scalar2=None, op0=ALU.mult),
                     reads=[pT[hb].key, "C64"], writes=[kw.key])
            P.op("dve", lambda e: e.tensor_tensor(out=SM.v(0, [[64, CPB], [1, 64]], Pn=64), in0=pS.v(0, [[64, CPB], [1, 64]], Pn=64),
                                                  in1=C64.v(off64["rdec"] + h * 64, [[0, CPB], [1, 64]], Pn=64), op=ALU.mult),
                 reads=[pS.key, "C64"], writes=[SM.key])
            if b == 0:
                P.op("dve", lambda e: e.memset(St.v(0, [[1, 128]]), 0.0), reads=[St.key], writes=[St.key])
            else:
                P.op("dve", lambda e: e.tensor_copy(out=St.v(0, [[1, 128]]), in_=St.v(CPB * 128, [[1, 128]])), reads=[St.key], writes=[St.key])
            for c in range(CPB):
                pu = ps()
                P.op("pe", lambda e, c=c, pu=pu: e.matmul(pu.v(0, [[1, 128]]), lhsT=kw.v(c * 128, [[1, 128]], Pn=64),
                                                          rhs=v_tm.v(c * 128, [[1, 128]], Pn=64), start=True, stop=True),
                     reads=[kw.key, v_tm.key], writes=[pu.key])
                P.op("dve", lambda e, c=c, pu=pu: e.scalar_tensor_tensor(out=St.v((c + 1) * 128, [[1, 128]]), in0=St.v(c * 128, [[1, 128]]),
                                                                         scalar=cdec[h], in1=pu.v(0, [[1, 128]]), op0=ALU.mult, op1=ALU.add),
                     reads=[St.key, pu.key], writes=[St.key])
            pO = [ps(), ps()]
            for c in range(CPB):
                P.op("pe", lambda e, c=c: e.matmul(pO[c // 4].v((c % 4) * 128, [[1, 128]], Pn=64), lhsT=SM.v(c * 64, [[1, 64]], Pn=64),
                                                   rhs=v_tm.v(c * 128, [[1, 128]], Pn=64), start=True, stop=False),
                     reads=[SM.key, v_tm.key], writes=[pO[c // 4].key])
                P.op("pe", lambda e, c=c: e.matmul(pO[c // 4].v((c % 4) * 128, [[1, 128]], Pn=64), lhsT=fmv.v(c * 64, [[1, 64]]),
                                                   rhs=St.v(c * 128, [[1, 128]]), start=False, stop=True),
                     reads=[fmv.key, St.key], writes=[pO[c // 4].key])
            for hb in range((CPB + 3) // 4):
                n = min(4, CPB - hb * 4)
                P.op("act", lambda e, hb=hb, n=n: e.activation(out=o_t.v(hb * 512, [[1, n * 128]], Pn=64), in_=pO[hb].v(0, [[1, n * 128]], Pn=64),
                                                               func=AF.Copy), reads=[pO[hb].key], writes=[o_t.key])
            rms_tm(ph, o_t, CPB, 128, nrmb.v(0, [[0, CPB], [1, 128]], Pn=64), nrmb.key, center=True)
            pY = ps()
            for c in range(CPB):
                P.op("pe", lambda e, c=c, pY=pY: e.transpose(pY.v(c * 64, [[1, 64]]), o_t.v(c * 128, [[1, 128]], Pn=64), c64("id")),
                     reads=[o_t.key, "C64"], writes=[pY.key])
            P.op("dve", lambda e, pY=pY: e.tensor_tensor(out=ytb.v(0, [[1, BLK]]), in0=pY.v(0, [[1, BLK]]), in1=fmz.v(0, [[1, BLK]]), op=ALU.mult),
                 reads=[pY.key, fmz.key], writes=[ytb.key])
            store_yT(ytb, 4 + h, 1, tok0, s)

    def ssd_load1(slot, l, g):
        base = l * D * P_IN
        ld_w(slot, 0, 512, dr["w_in"], base + O_CXBC + g * 512, P_IN)
        ld_w(slot, 4096, 128, dr["w_in"], base + O_CXBC + 1024 + g * 128, P_IN)
        ld_w(slot, 5120, 128, dr["w_in"], base + O_CXBC + 1280 + g * 128, P_IN)
        ld_w(slot, 6144, 8, dr["w_in"], base + O_CDT + g * 8, P_IN)

    def ssd_load2(slot, l, g):
        ld_w(slot, 0, 512, dr["w_in"], l * D * P_IN + O_CZ + g * 512, P_IN)

    def ssd_run(slot, l, g, s):
        enter(S_)
        ph = S_
        zslot = slot
        slot = 1 - slot
        fx, fmq, fmk, tA, Sc, dt_t, dta, lc, wr_t, elc, decb, nrmc, dsk, ytb, szb = [S_.t[n] for n in ['fx', 'fmq', 'fmk', 'tA', 'Sc', 'dt_t', 'dta', 'lc', 'wr_t', 'elc', 'decb', 'nrmc', 'dsk', 'ytb', 'szb']]
        small(nrmc.v(0, [[1, 512]], Pn=64), dv(dr["norm_c"], l * 1024 + g * 512, [[0, 64], [1, 512]]), nrmc.key, chan="rp")
        P.op("dve", lambda e: e.tensor_tensor(out=dsk.v(0, [[64, 8], [1, 64]], Pn=64), in0=C64.v(off64["id"], [[0, 8], [1, 64]], Pn=64),
                                              in1=pc.v(l * 48 + 32 + g * 8, [[1, 8], [0, 64]], Pn=64), op=ALU.mult), reads=["C64", "pc"], writes=[dsk.key])
        for b in range(NBLK):
            tok0 = b * BLK
            for j in range(4):
                pb = ps()
                proj_fm(slot, 0, 512, j * 128, tok0, BLK, pb)
                ch = g * 4 + j
                conv_stream(ph, pb, j, b, lambda k, ch=ch: cwc.v(l * 48 + ch * 4 + k, [[1, 1]]), fx.v(j * BLK, [[1, BLK]]), fx.key,
                            bias_ap=cbc.v(l * 12 + ch, [[1, 1]]))
            if SSTOP <= 0.1:
                continue
            for j, (woff, ch, dst) in enumerate([(4096, 8 + g, fmk), (5120, 10 + g, fmq)]):
                pb = ps()
                proj_fm(slot, woff, 128, 0, tok0, BLK, pb)
                conv_stream(ph, pb, 4 + j, b, lambda k, ch=ch: cwc.v(l * 48 + ch * 4 + k, [[1, 1]]), dst.v(0, [[1, BLK]]), dst.key,
                            bias_ap=cbc.v(l * 12 + ch, [[1, 1]]))
            if SSTOP <= 0.2:
                continue
            for c in range(CPB):
                pZ = ps()
                proj_tm(zslot, 0, 512, 0, 512, tok0 + c * 64, pZ)
                P.op("act", lambda e, pZ=pZ, c=c: e.activation(out=szb.v(c * 512, [[1, 512]], Pn=64), in_=pZ.v(0, [[1, 512]], Pn=64), func=AF.Silu),
                     reads=[pZ.key], writes=[szb.key])
            pd = ps()
            for c in range(CPB):
                proj_tm(slot, 6144, 8, 0, 8, tok0 + c * 64, pd, pcol=c * 8)
            P.op("dve", lambda e, pd=pd: e.tensor_tensor(out=dt_t.v(0, [[8, CPB], [1, 8]], Pn=64), in0=pd.v(0, [[8, CPB], [1, 8]], Pn=64),
                                                         in1=pc.v(l * 48 + g * 8, [[0, CPB], [1, 8]], Pn=64), op=ALU.add),
                 reads=[pd.key, "pc"], writes=[dt_t.key])
            P.op("act", lambda e: e.activation(out=dt_t.v(0, [[1, CPB * 8]], Pn=64), in_=dt_t.v(0, [[1, CPB * 8]], Pn=64), func=AF.Exp),
                 reads=[dt_t.key], writes=[dt_t.key])
            P.op("act", lambda e: e.activation(out=dt_t.v(0, [[1, CPB * 8]], Pn=64), in_=dt_t.v(0, [[1, CPB * 8]], Pn=64), func=AF.Ln,
                                               bias=one_ap[0:64, :]), reads=[dt_t.key, "epst"], writes=[dt_t.key])
            P.op("dve", lambda e: e.tensor_tensor(out=dta.v(0, [[8, CPB], [1, 8]], Pn=64), in0=dt_t.v(0, [[8, CPB], [1, 8]], Pn=64),
                                                  in1=nac.v(l * 16 + g * 8, [[0, CPB], [1, 8]], Pn=64), op=ALU.mult),
                 reads=[dt_t.key, "nac"], writes=[dta.key])
            if SSTOP <= 0.3:
                continue
            pl = ps()
            P.op("pe", lambda e, pl=pl: e.matmul(pl.v(0, [[1, CPB * 8]], Pn=64), lhsT=c64("tri"), rhs=dta.v(0, [[1, CPB * 8]], Pn=64),
                                                 start=True, stop=True), reads=[dta.key, "C64"], writes=[pl.key])
            P.op("pe", lambda e, pl=pl: e.matmul(pl.v(128, [[1, CPB * 8]]), lhsT=c64("ones"), rhs=dta.v(0, [[1, CPB * 8]], Pn=64),
                                                 start=True, stop=True), reads=[dta.key, "C64"], writes=[pl.key])
            P.op("dve", lambda e, pl=pl: e.tensor_copy(out=lc.v(0, [[1, CPB * 8]], Pn=64), in_=pl.v(0, [[1, CPB * 8]], Pn=64)),
                 reads=[pl.key], writes=[lc.key])
            if SSTOP <= 0.45:
                continue
            P.op("dve", lambda e, pl=pl: e.tensor_copy(out=decb.v(0, [[1, CPB * 8]]), in_=pl.v(128, [[1, CPB * 8]])),
                 reads=[pl.key], writes=[decb.key])
            P.op("act", lambda e: e.activation(out=decb.v(0, [[1, CPB * 8]]), in_=decb.v(0, [[1, CPB * 8]]), func=AF.Exp),
                 reads=[decb.key], writes=[decb.key])
            if SSTOP <= 0.5:
                continue
            P.op("dve", lambda e, pl=pl: e.tensor_tensor(out=wr_t.v(0, [[1, CPB * 8]], Pn=64), in0=pl.v(128, [[1, CPB * 8]], Pn=64),
                                                         in1=lc.v(0, [[1, CPB * 8]], Pn=64), op=ALU.subtract), reads=[pl.key, lc.key], writes=[wr_t.key])
            P.op("act", lambda e: e.activation(out=wr_t.v(0, [[1, CPB * 8]], Pn=64), in_=wr_t.v(0, [[1, CPB * 8]], Pn=64), func=AF.Exp),
                 reads=[wr_t.key], writes=[wr_t.key])
            P.op("dve", lambda e: e.tensor_tensor(out=wr_t.v(0, [[1, CPB * 8]], Pn=64), in0=wr_t.v(0, [[1, CPB * 8]], Pn=64),
                                                  in1=dt_t.v(0, [[1, CPB * 8]], Pn=64), op=ALU.mult), reads=[wr_t.key, dt_t.key], writes=[wr_t.key])
            P.op("act", lambda e: e.activation(out=elc.v(0, [[1, CPB * 8]], Pn=64), in_=lc.v(0, [[1, CPB * 8]], Pn=64), func=AF.Exp),
                 reads=[lc.key], writes=[elc.key])
            if SSTOP <= 1:
                continue
            if b == 0:
                P.op("dve", lambda e: e.memset(Sc.v(0, [[1, 512]]), 0.0), reads=[Sc.key], writes=[Sc.key])
            for c in range(CPB):
                ct = tok0 + c * 64
                db, e1, x_tm, xw, yi, yg, cbT, B_tm, st2 = [S_.t[n + str(c % 2)] for n in ['db', 'e1', 'x_tm', 'xw', 'yi', 'yg', 'cbT', 'B_tm', 'st2']]
                P.op("dve", lambda e, c=c: e.tensor_copy(out=db.v(0, [[64, 8], [1, 64]], Pn=64), in_=dta.v(c * 8, [[1, 8], [0, 64]], Pn=64)),
                     reads=[dta.key], writes=[db.key])
                pL = ps()
                for hh in range(8):
                    P.op("pe", lambda e, hh=hh, pL=pL: e.matmul(pL.v(hh * 64, [[1, 64]], Pn=64), lhsT=db.v(hh * 64, [[1, 64]], Pn=64),
                                                                rhs=c64("tri"), start=True, stop=True), reads=[db.key, "C64"], writes=[pL.key])
                P.op("dve", lambda e, c=c, pL=pL: e.tensor_tensor(out=e1.v(0, [[64, 8], [1, 64]], Pn=64), in0=pL.v(0, [[64, 8], [1, 64]], Pn=64),
                                                                  in1=lc.v(c * 8, [[1, 8], [0, 64]], Pn=64), op=ALU.subtract),
                     reads=[pL.key, lc.key], writes=[e1.key])
                P.op("dve", lambda e: e.scalar_tensor_tensor(out=e1.v(0, [[1, 512]], Pn=64), in0=e1.v(0, [[1, 512]], Pn=64), scalar=-1.0,
                                                             in1=e1.v(0, [[1, 512]], Pn=64), op0=ALU.mult, op1=ALU.max),
                     reads=[e1.key], writes=[e1.key])
                P.op("act", lambda e: e.activation(out=e1.v(0, [[1, 512]], Pn=64), in_=e1.v(0, [[1, 512]], Pn=64), func=AF.Exp, scale=-1.0),
                     reads=[e1.key], writes=[e1.key])
                if SSTOP <= 2:
                    continue
                pc_ = ps()
                P.op("pe", lambda e, c=c, pc_=pc_: e.matmul(pc_.v(0, [[1, 64]], Pn=64), lhsT=fmk.v(c * 64, [[1, 64]]), rhs=fmq.v(c * 64, [[1, 64]]),
                                                            start=True, stop=True), reads=[fmk.key, fmq.key], writes=[pc_.key])
                P.op("act", lambda e, pc_=pc_: e.activation(out=cbT.v(0, [[1, 64]], Pn=64), in_=pc_.v(0, [[1, 64]], Pn=64), func=AF.Copy),
                     reads=[pc_.key], writes=[cbT.key])
                P.op("dve", lambda e: e.tensor_tensor(out=e1.v(0, [[64, 8], [1, 64]], Pn=64), in0=e1.v(0, [[64, 8], [1, 64]], Pn=64),
                                                      in1=cbT.v(0, [[0, 8], [1, 64]], Pn=64), op=ALU.mult), reads=[e1.key, cbT.key], writes=[e1.key])
                P.op("dve", lambda e, c=c: e.tensor_tensor(out=e1.v(0, [[64, 8], [1, 64]], Pn=64), in0=e1.v(0, [[64, 8], [1, 64]], Pn=64),
                                                           in1=dt_t.v(c * 8, [[1, 8], [0, 64]], Pn=64), op=ALU.mult), reads=[e1.key, dt_t.key], writes=[e1.key])
                P.op("dve", lambda e: e.tensor_tensor(out=e1.v(0, [[1, 512]], Pn=64), in0=e1.v(0, [[1, 512]], Pn=64),
                                                      in1=dsk.v(0, [[1, 512]], Pn=64), op=ALU.add), reads=[e1.key, dsk.key], writes=[e1.key])
                if SSTOP <= 3:
                    continue
                pX = ps()
                for j in range(4):
                    P.op("pe", lambda e, c=c, j=j, pX=pX: e.transpose(pX.v(j * 128, [[1, 128]], Pn=64), fx.v(j * BLK + c * 64, [[1, 64]]), c128("id")),
                         reads=[fx.key, "C128"], writes=[pX.key])
                P.op("act", lambda e, pX=pX: e.activation(out=x_tm.v(0, [[1, 512]], Pn=64), in_=pX.v(0, [[1, 512]], Pn=64), func=AF.Copy),
                     reads=[pX.key], writes=[x_tm.key])
                P.op("dve", lambda e, c=c, pX=pX: e.tensor_tensor(out=xw.v(0, [[64, 8], [1, 64]], Pn=64), in0=pX.v(0, [[64, 8], [1, 64]], Pn=64),
                                                                  in1=wr_t.v(c * 8, [[1, 8], [0, 64]], Pn=64), op=ALU.mult),
                     reads=[pX.key, wr_t.key], writes=[xw.key])
                pBt = ps()
                P.op("pe", lambda e, c=c, pBt=pBt: e.transpose(pBt.v(0, [[1, 128]], Pn=64), fmk.v(c * 64, [[1, 64]]), c128("id")),
                     reads=[fmk.key, "C128"], writes=[pBt.key])
                P.op("act", lambda e, pBt=pBt: e.activation(out=B_tm.v(0, [[1, 128]], Pn=64), in_=pBt.v(0, [[1, 128]], Pn=64), func=AF.Copy),
                     reads=[pBt.key], writes=[B_tm.key])
                pI = ps()
                for hh in range(8):
                    P.op("pe", lambda e, hh=hh, pI=pI: e.matmul(pI.v(hh * 64, [[1, 64]], Pn=64), lhsT=e1.v(hh * 64, [[1, 64]], Pn=64),
                                                                rhs=x_tm.v(hh * 64, [[1, 64]], Pn=64), start=True, stop=True),
                         reads=[e1.key, x_tm.key], writes=[pI.key])
                pN = ps()
                for qq in range(4):
                    P.op("pe", lambda e, c=c, pN=pN, qq=qq: e.matmul(pN.v(qq * 128, [[1, 128]], Pn=64), lhsT=fmq.v(c * 64, [[1, 64]]),
                                                                     rhs=Sc.v(qq * 128, [[1, 128]]), start=True, stop=True),
                         reads=[fmq.key, Sc.key], writes=[pN.key])
                P.op("act", lambda e, pI=pI: e.activation(out=yi.v(0, [[1, 512]], Pn=64), in_=pI.v(0, [[1, 512]], Pn=64), func=AF.Copy),
                     reads=[pI.key], writes=[yi.key])
                P.op("dve", lambda e, c=c, pN=pN: e.tensor_tensor(out=yg.v(0, [[64, 8], [1, 64]], Pn=64), in0=pN.v(0, [[64, 8], [1, 64]], Pn=64),
                                                                  in1=elc.v(c * 8, [[1, 8], [0, 64]], Pn=64), op=ALU.mult),
                     reads=[pN.key, elc.key], writes=[yg.key])
                P.op("dve", lambda e: e.tensor_tensor(out=yg.v(0, [[1, 512]], Pn=64), in0=yg.v(0, [[1, 512]], Pn=64), in1=yi.v(0, [[1, 512]], Pn=64),
                                                      op=ALU.add), reads=[yg.key, yi.key], writes=[yg.key])
                if SSTOP <= 4:
                    continue
                pS_ = ps()
                for qq in range(4):
                    P.op("pe", lambda e, pS_=pS_, qq=qq: e.matmul(pS_.v(qq * 128, [[1, 128]]), lhsT=B_tm.v(0, [[1, 128]], Pn=64),
                                                                  rhs=xw.v(qq * 128, [[1, 128]], Pn=64), start=True, stop=True),
                         reads=[B_tm.key, xw.key], writes=[pS_.key])
                P.op("dve", lambda e, c=c: e.tensor_tensor(out=Sc.v(0, [[64, 8], [1, 64]]), in0=Sc.v(0, [[64, 8], [1, 64]]),
                                                           in1=decb.v(c * 8, [[1, 8], [0, 64]]), op=ALU.mult), reads=[Sc.key, decb.key], writes=[Sc.key])
                P.op("dve", lambda e, pS_=pS_: e.tensor_tensor(out=Sc.v(0, [[1, 512]]), in0=Sc.v(0, [[1, 512]]), in1=pS_.v(0, [[1, 512]]), op=ALU.add),
                     reads=[Sc.key, pS_.key], writes=[Sc.key])
                if SSTOP <= 5:
                    continue
                P.op("dve", lambda e: e.tensor_tensor(out=yg.v(0, [[1, 512]], Pn=64), in0=yg.v(0, [[1, 512]], Pn=64), in1=szb.v(c * 512, [[1, 512]], Pn=64),
                                                      op=ALU.mult), reads=[yg.key, szb.key], writes=[yg.key])
                P.op("act", lambda e: e.activation(out=yi.v(0, [[1, 512]], Pn=64), in_=yg.v(0, [[1, 512]], Pn=64), func=AF.Square),
                     reads=[yg.key], writes=[yi.key])
                P.op("dve", lambda e: e.tensor_reduce(out=st2.v(0, [[1, 1]], Pn=64), in_=yi.v(0, [[1, 512]], Pn=64), axis=AX.X, op=ALU.add),
                     reads=[yi.key], writes=[st2.key])
                P.op("act", lambda e: e.activation(out=st2.v(0, [[1, 1]], Pn=64), in_=st2.v(0, [[1, 1]], Pn=64), func=AF.Ln,
                                                   bias=eps_ap[0:64, :], scale=1.0 / 512), reads=[st2.key, "epst"], writes=[st2.key])
                P.op("act", lambda e: e.activation(out=st2.v(0, [[1, 1]], Pn=64), in_=st2.v(0, [[1, 1]], Pn=64), func=AF.Exp, scale=-0.5),
                     reads=[st2.key], writes=[st2.key])
                P.op("dve", lambda e: e.scalar_tensor_tensor(out=yg.v(0, [[1, 512]], Pn=64), in0=yg.v(0, [[1, 512]], Pn=64), scalar=st2.v(0, [[1, 1]], Pn=64),
                                                             in1=nrmc.v(0, [[1, 512]], Pn=64), op0=ALU.mult, op1=ALU.mult),
                     reads=[yg.key, st2.key, nrmc.key], writes=[yg.key])
                pY = ps()
                for j in range(4):
                    P.op("pe", lambda e, j=j, pY=pY: e.transpose(pY.v(j * 64, [[1, 64]]), yg.v(j * 128, [[1, 128]], Pn=64), c64("id")),
                         reads=[yg.key, "C64"], writes=[pY.key])
                P.op("act", lambda e, c=c, pY=pY: e.activation(out=ytb.v(c * 64, [[BLK, 4], [1, 64]]), in_=pY.v(0, [[64, 4], [1, 64]]), func=AF.Copy),
                     reads=[pY.key], writes=[ytb.key])
            store_yT(ytb, 8 + g * 4, 4, tok0, s)

    def mrg_load(slot, l, dc):
        base = l * D * P_IN
        for br in range(3):
            ld_w(slot, br * 1024, 128, dr["w_in"], base + O_GATE + br * 1024 + dc * 128, P_IN)
        ld_w(slot, 3072, 128, dr["w_branch_a"], l * 512 * D + dc * 128, D, nk=4)
        ld_w(slot, 3072 + 512, 128, dr["w_branch_b"], l * 512 * D + dc * 128, D, nk=4)
        ld_w(slot, 3072 + 1024, 128, dr["w_branch_c"], l * 1024 * D + dc * 128, D, nk=8)

    def mrg_run(slot, l, dc, s):
        enter(M_)
        ytl, tA, tB = [M_.t[n] for n in ['ytl', 'tA', 'tB']]
        for b in range(NBLK):
            tok0 = b * BLK
            P.dma("sp", lambda e, tok0=tok0: e.dma_start(out=ytl.v(0, [[BLK, 16], [1, BLK]]),
                                                         in_=dv(ytd, (s % 2) * 128 * 16 * T + tok0, [[16 * T, 128], [T, 16], [1, BLK]])),
                  reads=["ytd%d_%d_%d" % (s % 2, i, b) for i in range(16)], writes=[ytl.key], chan=ytl.key)
            first = True
            for br, (f0, nf, woff) in enumerate([(0, 4, 3072), (4, 4, 3072 + 512), (8, 8, 3072 + 1024)]):
                pg = ps()
                proj_fm(slot, br * 1024, 128, 0, tok0, BLK, pg)
                P.op("act", lambda e, pg=pg, br=br: e.activation(out=tA.v(0, [[1, BLK]]), in_=pg.v(0, [[1, BLK]]), func=AF.Sigmoid,
                                                                 bias=bgt.v(l * 24 + br * 8 + dc, [[1, 1]])), reads=[pg.key, "bgt"], writes=[tA.key])
                pb = ps()
                for k in range(nf):
                    P.op("pe", lambda e, k=k, pb=pb, woff=woff, f0=f0, nf=nf: e.matmul(pb.v(0, [[1, BLK]]), lhsT=WB[slot].v(woff + k * 128, [[1, 128]]),
                                                                                       rhs=ytl.v((f0 + k) * BLK, [[1, BLK]]), start=(k == 0), stop=(k == nf - 1)),
                         reads=["WB%d" % slot, ytl.key], writes=[pb.key])
                if first:
                    P.op("dve", lambda e, pb=pb: e.tensor_tensor(out=tB.v(0, [[1, BLK]]), in0=pb.v(0, [[1, BLK]]), in1=tA.v(0, [[1, BLK]]), op=ALU.mult),
                         reads=[pb.key, tA.key], writes=[tB.key])
                    first = False
                else:
                    P.op("dve", lambda e, pb=pb: e.tensor_tensor(out=tA.v(0, [[1, BLK]]), in0=pb.v(0, [[1, BLK]]), in1=tA.v(0, [[1, BLK]]), op=ALU.mult),
                         reads=[pb.key, tA.key], writes=[tA.key])
                    if br == 1:
                        P.op("dve", lambda e: e.tensor_tensor(out=tB.v(0, [[1, BLK]]), in0=tB.v(0, [[1, BLK]]), in1=tA.v(0, [[1, BLK]]), op=ALU.add),
                             reads=[tA.key, tB.key], writes=[tB.key])
                    else:
                        P.op("dve", lambda e, tok0=tok0: e.tensor_tensor(out=mrgT.v(dc * T + tok0, [[1, BLK]]), in0=tB.v(0, [[1, BLK]]),
                                                                         in1=tA.v(0, [[1, BLK]]), op=ALU.add),
                             reads=[tA.key, tB.key], writes=["mrg%d" % b])


    def ln_tile(ph, src_ap, src_keys, l, which, tile, s, final, do_router):
        par = str(tile % 2)
        lng, lnb = ph.t['lng'], ph.t['lnb']
        xn, bst, mv = ph.t['xn' + par], ph.t['bst' + par], ph.t['mv' + par]
        xTf = ph.t.get('xTf' + par)
        if WSTOP <= 1:
            return
        for hh in range(2):
            P.op("dve", lambda e, hh=hh: e.bn_stats(out=bst.v(hh * 6, [[1, 6]]), in_=src_ap[:, hh * 512:(hh + 1) * 512]), reads=src_keys, writes=[bst.key])
        P.op("dve", lambda e: e.bn_aggr(out=mv.v(0, [[1, 2]]), in_=bst.v(0, [[1, 12]])), reads=[bst.key], writes=[mv.key])
        P.op("act", lambda e: e.activation(out=mv.v(2, [[1, 1]]), in_=mv.v(1, [[1, 1]]), func=AF.Ln, bias=eps_ap, scale=1.0), reads=[mv.key, "epst"], writes=[mv.key])
        P.op("act", lambda e: e.activation(out=mv.v(2, [[1, 1]]), in_=mv.v(2, [[1, 1]]), func=AF.Exp, scale=-0.5), reads=[mv.key], writes=[mv.key])
        P.op("dve", lambda e: e.tensor_scalar(out=xn.v(0, [[1, 1024]]), in0=src_ap, scalar1=mv.v(0, [[1, 1]]), scalar2=mv.v(2, [[1, 1]]),
                                              op0=ALU.subtract, op1=ALU.mult), reads=list(src_keys) + [mv.key], writes=[xn.key])
        P.op("dve", lambda e: e.tensor_tensor(out=xn.v(0, [[1, 1024]]), in0=xn.v(0, [[1, 1024]]), in1=lng.v(0, [[1, 1024]]), op=ALU.mult),
             reads=[xn.key, lng.key], writes=[xn.key])
        P.op("dve", lambda e: e.tensor_tensor(out=xn.v(0, [[1, 1024]]), in0=xn.v(0, [[1, 1024]]), in1=lnb.v(0, [[1, 1024]]), op=ALU.add),
             reads=[xn.key, lnb.key], writes=[xn.key])
        if WSTOP <= 2:
            return
        if final:
            o = P.dma("sp", lambda e: e.dma_start(out=dv(yout, (s * T + tile * 128) * D, [[D, 128], [1, D]]), in_=xn.v(0, [[1, 1024]])),
                      reads=[xn.key], writes=["yout%d_%d" % (s, tile)], chan="out")
            outs.append(o)
        else:
            P.dma("sp", lambda e: e.dma_start(out=dv(xres[which], tile * 128 * D, [[D, 128], [1, D]]), in_=xn.v(0, [[1, 1024]])),
                  reads=[xn.key], writes=["xres%d" % which], chan="xr%d" % which)
            if WSTOP <= 2.5:
                return
            for half in range(2):
                pt = ps()
                for k in range(4):
                    kc = half * 4 + k
                    P.op("pe", lambda e, k=k, kc=kc, pt=pt: e.transpose(pt.v(k * 128, [[1, 128]]), xn.v(kc * 128, [[1, 128]]), c128("id")),
                         reads=[xn.key, "C128"], writes=[pt.key])
                P.op("act", lambda e, half=half, pt=pt: e.activation(out=xT.v(half * 4 * T + tile * 128, [[T, 4], [1, 128]]), in_=pt.v(0, [[128, 4], [1, 128]]),
                                                                     func=AF.Copy), reads=[pt.key], writes=["xT%d" % tile])
                if do_router and WSTOP > 2.7:
                    P.op("dve", lambda e, half=half, pt=pt: e.tensor_copy(out=xTf.v(half * 512, [[1, 512]]), in_=pt.v(0, [[1, 512]])),
                         reads=[pt.key], writes=[xTf.key])
            if do_router and WSTOP > 3:
                router(ph, l, tile)

    outs = []

    def router(ph, l, tile):
        xTf, rt = ph.t['xTf' + str(tile % 2)], ph.t['rt' + str(tile % 2)]
        pr = ps()
        for kc in range(KC):
            P.op("pe", lambda e, kc=kc, pr=pr: e.matmul(pr.v(0, [[1, 36]]), lhsT=xTf.v(kc * 128, [[1, 128]]), rhs=wr.v(l * KC * 36 + kc * 36, [[1, 36]]),
                                                        start=(kc == 0), stop=(kc == KC - 1)), reads=[xTf.key, "wr"], writes=[pr.key])
        R = lambda o, n: rt.v(o, [[1, n]])
        P.op("dve", lambda e: e.tensor_tensor(out=R(0, 36), in0=pr.v(0, [[1, 36]]), in1=brt.v(l * 36, [[1, 36]]), op=ALU.add), reads=[pr.key, "brt"], writes=[rt.key])
        k = [rt.key]
        P.op("dve", lambda e: e.tensor_reduce(out=R(36, 1), in_=R(0, 4), axis=AX.X, op=ALU.max), reads=k, writes=k)
        P.op("dve", lambda e: e.tensor_scalar(out=R(37, 4), in0=R(0, 4), scalar1=R(36, 1), scalar2=None, op0=ALU.is_ge), reads=k, writes=k)
        P.op("dve", lambda e: e.tensor_scalar(out=R(41, 4), in0=R(0, 4), scalar1=R(36, 1), scalar2=None, op0=ALU.subtract), reads=k, writes=k)
        P.op("act", lambda e: e.activation(out=R(41, 4), in_=R(41, 4), func=AF.Exp), reads=k, writes=k)
        P.op("dve", lambda e: e.tensor_reduce(out=R(45, 1), in_=R(41, 4), axis=AX.X, op=ALU.add), reads=k, writes=k)
        P.op("dve", lambda e: e.reciprocal(out=R(46, 1), in_=R(45, 1)), reads=k, writes=k)
        P.op("dve", lambda e: e.tensor_scalar(out=R(41, 4), in0=R(37, 4), scalar1=-1.0, scalar2=BIGM, op0=ALU.add, op1=ALU.mult), reads=k, writes=k)
        P.op("dve", lambda e: e.tensor_tensor(out=rt.v(48, [[8, 4], [1, 8]]), in0=rt.v(4, [[8, 4], [1, 8]]), in1=rt.v(41, [[1, 4], [0, 8]]), op=ALU.add),
             reads=k, writes=k)
        P.op("dve", lambda e: e.tensor_reduce(out=R(80, 1), in_=R(48, 32), axis=AX.X, op=ALU.max), reads=k, writes=k)
        P.op("dve", lambda e: e.tensor_scalar(out=R(82, 32), in0=R(48, 32), scalar1=R(80, 1), scalar2=None, op0=ALU.is_ge), reads=k, writes=k)
        P.op("dve", lambda e: e.scalar_tensor_tensor(out=R(48, 32), in0=R(82, 32), scalar=-BIGM, in1=R(48, 32), op0=ALU.mult, op1=ALU.add), reads=k, writes=k)
        P.op("dve", lambda e: e.tensor_reduce(out=R(81, 1), in_=R(48, 32), axis=AX.X, op=ALU.max), reads=k, writes=k)
        P.op("dve", lambda e: e.tensor_scalar(out=R(114, 32), in0=R(48, 32), scalar1=R(81, 1), scalar2=None, op0=ALU.is_ge), reads=k, writes=k)
        P.op("dve", lambda e: e.tensor_tensor(out=R(146, 1), in0=R(81, 1), in1=R(80, 1), op=ALU.subtract), reads=k, writes=k)
        P.op("act", lambda e: e.activation(out=R(146, 1), in_=R(146, 1), func=AF.Exp), reads=k, writes=k)
        P.op("dve", lambda e: e.tensor_scalar(out=R(146, 1), in0=R(146, 1), scalar1=1.0, scalar2=None, op0=ALU.add), reads=k, writes=k)
        P.op("dve", lambda e: e.reciprocal(out=R(146, 1), in_=R(146, 1)), reads=k, writes=k)
        P.op("dve", lambda e: e.tensor_tensor(out=R(146, 1), in0=R(146, 1), in1=R(46, 1), op=ALU.mult), reads=k, writes=k)
        P.op("dve", lambda e: e.tensor_tensor(out=R(147, 1), in0=R(46, 1), in1=R(146, 1), op=ALU.subtract), reads=k, writes=k)
        P.op("dve", lambda e: e.tensor_scalar(out=R(82, 32), in0=R(82, 32), scalar1=R(146, 1), scalar2=None, op0=ALU.mult), reads=k, writes=k)
        P.op("dve", lambda e: e.scalar_tensor_tensor(out=comb.v(tile * 32, [[1, 32]]), in0=R(114, 32), scalar=R(147, 1), in1=R(82, 32),
                                                     op0=ALU.mult, op1=ALU.add), reads=k, writes=["comb%d" % tile])

    def load_ln(ph, l, which):
        lng, lnb = ph.t['lng'], ph.t['lnb']
        g, b_ = ("ln1_g", "ln1_b") if which == 1 else ("ln2_g", "ln2_b")
        P.dma("sp", lambda e: e.dma_start(out=lng.v(0, [[1, 1024]]), in_=dv(dr[g], l * D, [[0, 128], [1, D]])), writes=[lng.key], chan="lng")
        P.dma("sp", lambda e: e.dma_start(out=lnb.v(0, [[1, 1024]]), in_=dv(dr[b_], l * D, [[0, 128], [1, D]])), writes=[lnb.key], chan="lnb")

    def wout_load1(slot, l):
        ld_w(slot, 0, 512, dr["w_out"], l * D * D, D)

    def wout_load2(slot, l):
        ld_w(slot, 0, 512, dr["w_out"], l * D * D + 512, D)

    def wout_run(slot, l, s, src_dram_ap_fn, src_keys_fn):
        enter(W_)
        xl = [W_.t["xl0"], W_.t["xl1"]]
        slots = [1 - slot, slot]
        load_ln(W_, l, 1)
        for tile in range(NT):
            xo = xl[tile % 2]
            P.dma("sp", lambda e, tile=tile, xo=xo: e.dma_start(out=xo.v(0, [[1, 1024]]), in_=src_dram_ap_fn(tile)), reads=src_keys_fn(tile),
                  writes=[xo.key], chan="xl%d" % (tile % 2))
            for half in range(2):
                pm = ps()
                for kc in range(KC):
                    P.op("pe", lambda e, kc=kc, pm=pm, half=half, tile=tile: e.matmul(pm.v(0, [[1, 512]]), lhsT=mrgT.v(kc * T + tile * 128, [[1, 128]]),
                                                                                      rhs=WB[slots[half]].v(kc * 512, [[1, 512]]),
                                                                                      start=(kc == 0), stop=(kc == KC - 1)),
                         reads=["WB%d" % slots[half], "mrg%d" % (tile * 128 // BLK)], writes=[pm.key])
                P.op("dve", lambda e, pm=pm, half=half, xo=xo: e.scalar_tensor_tensor(out=xo.v(half * 512, [[1, 512]]), in0=xo.v(half * 512, [[1, 512]]),
                                                                                      scalar=ALPHA, in1=pm.v(0, [[1, 512]]), op0=ALU.mult, op1=ALU.add),
                     reads=[xo.key, pm.key], writes=[xo.key])
            ln_tile(W_, xo.v(0, [[1, 1024]]), [xo.key], l, 0, tile, s, False, True)

    def moe_load(slot, l, e_, hf):
        ld_w(slot, 0, 256, dr["w_gate_e"], ((l * NE + e_) * D) * DE + hf * 256, DE)
        ld_w(slot, 2048, 256, dr["w_up_e"], ((l * NE + e_) * D) * DE + hf * 256, DE)
        ld_w(slot, 4096, 1024, dr["w_down_e"], ((l * NE + e_) * DE + hf * 256) * D, D, nk=2)

    def moe_init(l, s):
        enter(E_)
        for tile in range(NT):
            P.dma("sp", lambda e, tile=tile: e.dma_start(out=yacc.v(tile * 1024, [[1, 1024]]), in_=dv(xres[0], tile * 128 * D, [[D, 128], [1, D]])),
                  reads=["xres0"], writes=["yacc%d" % tile], chan="ya%d" % tile)
            P.op("pool", lambda e, tile=tile: e.tensor_scalar(out=yacc.v(tile * 1024, [[1, 1024]]), in0=yacc.v(tile * 1024, [[1, 1024]]), scalar1=ALPHA,
                                                               scalar2=None, op0=ALU.mult), reads=["yacc%d" % tile], writes=["yacc%d" % tile])

    def moe_run(slot, l, e_, hf, s):
        wk = "WB%d" % slot
        Hh = [E_.t["Hh0"], E_.t["Hh1"]]
        hs = [E_.t["hs0"], E_.t["hs1"]]
        for b in range(NBLK):
            tok0 = b * BLK
            H = Hh[b % 2]
            for k2 in range(2):
                pg = ps()
                pu = ps()
                for kc in range(KC):
                    P.op("pe", lambda e, kc=kc, pg=pg, k2=k2, tok0=tok0: e.matmul(pg.v(0, [[1, BLK]]), lhsT=WB[slot].v(kc * 256 + k2 * 128, [[1, 128]]),
                                                                                  rhs=xT.v(kc * T + tok0, [[1, BLK]]), start=(kc == 0), stop=(kc == KC - 1)),
                         reads=[wk] + xt_keys(tok0, BLK), writes=[pg.key])
                for kc in range(KC):
                    P.op("pe", lambda e, kc=kc, pu=pu, k2=k2, tok0=tok0: e.matmul(pu.v(0, [[1, BLK]]), lhsT=WB[slot].v(2048 + kc * 256 + k2 * 128, [[1, 128]]),
                                                                                  rhs=xT.v(kc * T + tok0, [[1, BLK]]), start=(kc == 0), stop=(kc == KC - 1)),
                         reads=[wk] + xt_keys(tok0, BLK), writes=[pu.key])
                hsx = hs[k2]
                P.op("act", lambda e, pg=pg, hsx=hsx: e.activation(out=hsx.v(0, [[1, BLK]]), in_=pg.v(0, [[1, BLK]]), func=AF.Silu), reads=[pg.key], writes=[hsx.key])
                P.op("dve", lambda e, pu=pu, hsx=hsx, H=H, k2=k2: e.tensor_tensor(out=H.v(k2 * BLK, [[1, BLK]]), in0=hsx.v(0, [[1, BLK]]), in1=pu.v(0, [[1, BLK]]),
                                                                                  op=ALU.mult), reads=[pu.key, hsx.key], writes=[H.key])
            for t in range(TPB):
                tile = b * TPB + t
                for half in range(2):
                    py = ps()
                    for k2 in range(2):
                        P.op("pe", lambda e, k2=k2, py=py, t=t, half=half, H=H: e.matmul(py.v(0, [[1, 512]]), lhsT=H.v(k2 * BLK + t * 128, [[1, 128]]),
                                                                                         rhs=WB[slot].v(4096 + k2 * 1024 + half * 512, [[1, 512]]),
                                                                                         start=(k2 == 0), stop=(k2 == 1)), reads=[wk, H.key], writes=[py.key])
                    P.op("dve", lambda e, py=py, tile=tile, half=half: e.scalar_tensor_tensor(
                        out=yacc.v(tile * 1024 + half * 512, [[1, 512]]), in0=py.v(0, [[1, 512]]), scalar=comb.v(tile * 32 + e_, [[1, 1]]),
                        in1=yacc.v(tile * 1024 + half * 512, [[1, 512]]), op0=ALU.mult, op1=ALU.add),
                        reads=[py.key, "comb%d" % tile, "yacc%d" % tile], writes=["yacc%d" % tile])

    def ln2_run(l, s, final):
        enter(N_)
        load_ln(N_, l, 2)
        for tile in range(NT):
            ln_tile(N_, yacc.v(tile * 1024, [[1, 1024]]), ["yacc%d" % tile], l, 1, tile, s, final, False)

    def x0_run(s):
        enter(X_)
        xl = [X_.t["xl0"], X_.t["xl1"]]
        for tile in range(NT):
            xo = xl[tile % 2]
            P.dma("sp", lambda e, tile=tile, xo=xo: e.dma_start(out=xo.v(0, [[1, 1024]]), in_=dv(dr["x"], (s * T + tile * 128) * D, [[D, 128], [1, D]])),
                  writes=[xo.key], chan="xl%d" % (tile % 2))
            for half in range(2):
                pt = ps()
                for k in range(4):
                    kc = half * 4 + k
                    P.op("pe", lambda e, k=k, kc=kc, pt=pt, xo=xo: e.transpose(pt.v(k * 128, [[1, 128]]), xo.v(kc * 128, [[1, 128]]), c128("id")),
                         reads=[xo.key, "C128"], writes=[pt.key])
                P.op("act", lambda e, half=half, pt=pt, tile=tile: e.activation(out=xT.v(half * 4 * T + tile * 128, [[T, 4], [1, 128]]),
                                                                                in_=pt.v(0, [[128, 4], [1, 128]]), func=AF.Copy),
                     reads=[pt.key], writes=["xT%d" % tile])

    units = []
    for s in range(NSEQ):
        units.append((None, lambda slot, s=s: x0_run(s)))
        for l in range(L):
            for h in range(4):
                units.append((lambda slot, l=l, h=h: gdn_load(slot, l, h), lambda slot, l=l, h=h, s=s: gdn_run(slot, l, h, s)))
            for h in range(4):
                units.append((lambda slot, l=l, h=h: ret_load(slot, l, h), lambda slot, l=l, h=h, s=s: ret_run(slot, l, h, s)))
            for g in range(2):
                units.append((lambda slot, l=l, g=g: ssd_load1(slot, l, g), lambda slot: None))
                units.append((lambda slot, l=l, g=g: ssd_load2(slot, l, g), lambda slot, l=l, g=g, s=s: ssd_run(slot, l, g, s), True))
            for dc in range(8):
                units.append((lambda slot, l=l, dc=dc: mrg_load(slot, l, dc), lambda slot, l=l, dc=dc, s=s: mrg_run(slot, l, dc, s)))
            if l == 0:
                srcf = lambda tile, s=s: dv(dr["x"], (s * T + tile * 128) * D, [[D, 128], [1, D]])
                srck = lambda tile: []
            else:
                srcf = lambda tile: dv(xres[1], tile * 128 * D, [[D, 128], [1, D]])
                srck = lambda tile: ["xres1"]
            units.append((lambda slot, l=l: wout_load1(slot, l), lambda slot: None))
            units.append((lambda slot, l=l: wout_load2(slot, l), lambda slot, l=l, s=s, srcf=srcf, srck=srck: wout_run(slot, l, s, srcf, srck), True))
            units.append((None, lambda slot, l=l, s=s: moe_init(l, s)))
            for e_ in range(NE):
                for hf in range(2):
                    units.append((lambda slot, l=l, e_=e_, hf=hf: moe_load(slot, l, e_, hf),
                                  lambda slot, l=l, e_=e_, hf=hf, s=s: moe_run(slot, l, e_, hf, s)))
            units.append((None, lambda slot, l=l, s=s: ln2_run(l, s, l == L - 1)))

    wl = [u for u in units if u[0] is not None]
    slot_of = {}
    k = 0
    for i, u in enumerate(units):
        if u[0] is not None:
            slot_of[i] = k % 2
            k += 1
    loaded = set()
    idxs = [i for i, u in enumerate(units) if u[0] is not None]

    def ensure_loaded(i):
        if i not in loaded:
            units[i][0](slot_of[i])
            loaded.add(i)

    kstop = int(os.environ.get("KSTOP", "100000"))
    for i, u in enumerate(units):
        if i >= kstop:
            break
        if u[0] is not None:
            ensure_loaded(i)
            nxt = [j for j in idxs if j > i]
            both = len(u) > 2
            if nxt and not both:
                ensure_loaded(nxt[0])
            u[1](slot_of[i])
            if nxt and both:
                ensure_loaded(nxt[0])
        else:
            u[1](None)

    P.emit(final_wait_ops=outs)
    st.close()
    return nc, (a64, a128, cs_np)


_CACHE = {}


def kernel(**inputs):
    NCORES = 8
    x = np.ascontiguousarray(inputs["x"], dtype=np.float32)
    Bt, T, _ = x.shape
    NSEQ = Bt // NCORES
    DEPTH = inputs["w_in"].shape[0]
    key = (NSEQ, T, DEPTH)
    if key not in _CACHE:
        _CACHE[key] = build(NSEQ, T, DEPTH, 512)
    nc, (a64, a128, cs_np) = _CACHE[key]
    shared = {k: np.ascontiguousarray(v, dtype=np.float32) for k, v in inputs.items() if k != "x"}
    shared["c64"] = a64
    shared["c128"] = a128
    shared["cs"] = cs_np
    in_maps = []
    for c in range(NCORES):
        m = dict(shared)
        m["x"] = np.ascontiguousarray(x[c * NSEQ:(c + 1) * NSEQ])
        in_maps.append(m)
    res = run_bass_kernel_spmd(nc, in_maps, core_ids=list(range(NCORES)))
    return np.concatenate([r["y"] for r in res.results], axis=0).astype(np.float32)
```

```python
import contextlib
import numpy as np
import concourse.bass as bass
import concourse.mybir as mybir
from concourse.bass_utils import run_bass_kernel_spmd

F32 = mybir.dt.float32
BF16 = mybir.dt.bfloat16
AF = mybir.ActivationFunctionType
ALU = mybir.AluOpType
AX = mybir.AxisListType

D = 1024
KC = 8
P_IN = 9752
O_AQKV, O_AZ, O_AA, O_AB = 0, 1536, 2048, 2052
O_BQ, O_BK, O_BV, O_BG = 2056, 2568, 3080, 3592
O_CZ, O_CXBC, O_CDT, O_GATE = 4104, 5128, 6664, 6680
EPS = 1e-6
ALPHA = 4.0 ** 0.25
NE = 32
DE = 512
BIGM = 30000.0
ENGS = ("pe", "act", "dve", "pool", "sp")


class _Rec:
    def __getattr__(self, name):
        def f(*a, **k):
            self.__dict__["call"] = (name, a, k)
            return self
        return f


class Prog:
    def __init__(self, nc):
        self.nc = nc
        self.ops = []
        self.last_w = {}
        self.readers = {}
        self.cap = None

    def _add(self, eng, fn, reads, writes, chan=None):
        rec = _Rec()
        fn(rec)
        call = rec.call
        if self.cap is not None:
            self.cap.append((eng, call, reads, writes, chan))
            return -1
        return self._commit(eng, call, reads, writes, chan)

    def capture(self, f):
        old = self.cap
        self.cap = []
        f()
        lst = self.cap
        self.cap = old
        return lst

    def merge(self, A, B):
        ia = ib = 0
        na, nb = len(A), len(B)
        while ia < na or ib < nb:
            if ib >= nb or (ia < na and ia * nb <= ib * na):
                self._commit(*A[ia]); ia += 1
            else:
                self._commit(*B[ib]); ib += 1

    def _commit(self, eng, call, reads, writes, chan=None):
        if self.cap is not None:
            self.cap.append((eng, call, reads, writes, chan))
            return -1
        fn = lambda e, call=call: getattr(e, call[0])(*call[1], **call[2])
        idx = len(self.ops)
        deps = set()
        for r in reads:
            w = self.last_w.get(r)
            if w is not None:
                deps.add(w)
            if isinstance(r, str) and r.startswith("pb"):
                for rd in self.readers.get(r, ()):
                    if self.ops[rd]["eng"] != eng:
                        deps.add(rd)
        for r in writes:
            w = self.last_w.get(r)
            if w is not None and not (chan is not None and self.ops[w]["chan"] == chan and self.ops[w]["eng"] == eng):
                deps.add(w)
            for rd in self.readers.get(r, ()):
                deps.add(rd)
        for r in reads:
            self.readers.setdefault(r, []).append(idx)
        for r in writes:
            self.last_w[r] = idx
            self.readers[r] = []
        deps.discard(idx)
        self.ops.append(dict(eng=eng, fn=fn, deps=deps, chan=chan, has_dep=False))
        return idx

    def op(self, eng, fn, reads=(), writes=()):
        return self._add(eng, fn, tuple(reads), tuple(writes))

    def dma(self, eng, fn, reads=(), writes=(), chan="d0"):
        return self._add(eng, fn, tuple(reads), tuple(writes), chan=chan)

    def emit(self, final_wait_ops=()):
        nc = self.nc
        ops = self.ops
        for i, o in enumerate(ops):
            nd = set()
            for d in o["deps"]:
                p = ops[d]
                if p["chan"] is None and o["chan"] is None and p["eng"] == "pe" and o["eng"] == "pe":
                    continue
                nd.add(d)
            o["deps"] = nd
            for d in nd:
                ops[d]["has_dep"] = True
        for d in final_wait_ops:
            ops[d]["has_dep"] = True
        eng_cnt = {e: 0 for e in ENGS}
        chan_cnt = {}
        chans = []
        for o in ops:
            if o["chan"] is not None:
                c = o["chan"]
                if c not in chan_cnt:
                    chan_cnt[c] = 0
                    chans.append(c)
                chan_cnt[c] += 16
                o["tok"] = (("chan", c), chan_cnt[c])
            elif o["has_dep"]:
                eng_cnt[o["eng"]] += 1
                o["tok"] = (("eng", o["eng"]), eng_cnt[o["eng"]])
            else:
                o["tok"] = None
        sem_keys = [("eng", e) for e in ENGS] + [("chan", c) for c in chans]
        with contextlib.ExitStack() as st:
            sems = {}
            for k in sem_keys:
                sems[k] = st.enter_context(nc.semaphore("s_%s_%s" % k))
            blk = st.enter_context(nc.Block())

            def run_engine(eng_name, eng_obj):
                waited = {}
                for i, o in enumerate(ops):
                    if o["eng"] != eng_name:
                        continue
                    need = {}
                    for d in o["deps"]:
                        k, v = ops[d]["tok"]
                        if need.get(k, 0) < v:
                            need[k] = v
                    for k, v in need.items():
                        if waited.get(k, 0) >= v:
                            continue
                        eng_obj.wait_ge(sems[k], v)
                        waited[k] = v
                    ins = o["fn"](eng_obj)
                    if o["tok"] is not None:
                        k, v = o["tok"]
                        ins.then_inc(sems[k], 16 if k[0] == "chan" else 1)
                if eng_name == "sp":
                    need = {}
                    for d in final_wait_ops:
                        k, v = ops[d]["tok"]
                        if need.get(k, 0) < v:
                            need[k] = v
                    for k, v in need.items():
                        eng_obj.wait_ge(sems[k], v)

            blk.sync(lambda e: run_engine("sp", e))
            blk.tensor(lambda e: run_engine("pe", e))
            blk.scalar(lambda e: run_engine("act", e))
            blk.vector(lambda e: run_engine("dve", e))
            blk.gpsimd(lambda e: run_engine("pool", e))


def host_consts(T, BLK):
    c64 = {}
    i = np.arange(64)
    c64["tri"] = (i[:, None] <= i[None, :]).astype(np.float32)
    c64["id"] = np.eye(64, dtype=np.float32)
    c64["ones"] = np.ones((64, 128), np.float32)
    c64["bigm"] = np.where(i[None, :] < i[:, None], 0.0, BIGM).astype(np.float32)
    lg = np.log1p(-np.exp2(-5.0 - np.arange(4, dtype=np.float32))).astype(np.float32)
    idx = i.astype(np.float32)
    dec = np.exp(lg[:, None, None] * np.abs(idx[:, None] - idx[None, :])).astype(np.float32)
    c64["rdec"] = np.transpose(dec, (1, 0, 2)).reshape(64, 256)
    c64["wd"] = np.exp(lg[None, :] * (63.0 - idx[:, None])).astype(np.float32)
    names64 = ["tri", "id", "ones", "bigm", "rdec", "wd"]
    off64 = {}
    o = 0
    for n in names64:
        off64[n] = o
        o += c64[n].shape[1]
    a64 = np.concatenate([c64[n] for n in names64], axis=1).astype(np.float32)
    c128 = {}
    c128["id"] = np.eye(128, dtype=np.float32)
    c128["ones"] = np.ones((128, 128), np.float32)
    rot = np.zeros((128, 128), np.float32)
    for m in range(64):
        rot[m + 64, m] = -1.0
    for m in range(64, 128):
        rot[m - 64, m] = 1.0
    c128["rot"] = rot
    rd = np.exp(lg[:, None] * (idx[None, :] + 1.0)).astype(np.float32)
    rdt = np.tile(rd, (1, BLK // 64))
    c128["rd"] = np.broadcast_to(rdt.reshape(1, 4 * BLK), (128, 4 * BLK)).astype(np.float32)
    cd = np.exp(lg * 64.0).astype(np.float32)
    names128 = ["id", "ones", "rot", "rd"]
    off128 = {}
    o = 0
    for n in names128:
        off128[n] = o
        o += c128[n].shape[1]
    a128 = np.concatenate([c128[n] for n in names128], axis=1).astype(np.float32)
    pos = np.arange(T, dtype=np.float32)
    inv_freq = (np.float32(10000.0) ** (-np.arange(0, 128, 2, dtype=np.float32) / np.float32(128))).astype(np.float32)
    ang = (pos[:, None] * inv_freq[None, :]).astype(np.float32)
    cos = np.cos(ang).astype(np.float32).T
    sin = np.sin(ang).astype(np.float32).T
    cs = np.concatenate([np.concatenate([cos, cos], 0), np.concatenate([sin, sin], 0)], axis=1).astype(np.float32)
    return a64, off64, a128, off128, cs, [float(x) for x in cd]


def build(NSEQ, T, DEPTH, BLK):
    import os
    SSTOP = float(os.environ.get("SSTOP", "100"))
    WSTOP = float(os.environ.get("WSTOP", "100"))
    NT = T // 128
    NBLK = T // BLK
    CPB = BLK // 64
    TPB = BLK // 128
    a64, off64, a128, off128, cs_np, cdec = host_consts(T, BLK)
    nc = bass.Bass("TRN2", target_bir_lowering=False)
    dr = {}

    def din(name, shape):
        dr[name] = nc.dram_tensor(name, list(shape), F32, kind="ExternalInput")
        return dr[name]

    din("x", [NSEQ, T, D])
    L = DEPTH
    specs = dict(w_in=[L, D, P_IN], conv_a=[L, 4, 1536], a_log_a=[L, 4], dt_bias_a=[L, 4], norm_a=[L, 128],
                 norm_b=[L, 512], conv_c=[L, 4, 1536], conv_bias_c=[L, 1536], dt_bias_c=[L, 16], a_log_c=[L, 16],
                 d_skip_c=[L, 16], norm_c=[L, 1024], b_gate=[L, 3, D], w_branch_a=[L, 512, D],
                 w_branch_b=[L, 512, D], w_branch_c=[L, 1024, D], w_out=[L, D, D], ln1_g=[L, D], ln1_b=[L, D],
                 w_router_group=[L, D, 4], b_router_group=[L, 4], w_router_expert=[L, D, 32],
                 b_router_expert=[L, 32], w_gate_e=[L, NE, D, DE], w_up_e=[L, NE, D, DE], w_down_e=[L, NE, DE, D],
                 ln2_g=[L, D], ln2_b=[L, D])
    for k, v in specs.items():
        din(k, v)
    din("c64", list(a64.shape))
    din("c128", list(a128.shape))
    din("cs", list(cs_np.shape))
    yout = nc.dram_tensor("y", [NSEQ, T, D], F32, kind="ExternalOutput")
    xres = [nc.dram_tensor("xres%d" % i, [T, D], F32, kind="Internal") for i in range(2)]
    ytd = nc.dram_tensor("ytd", [2, 128, 16, T], BF16, kind="Internal")

    P = Prog(nc)
    st = contextlib.ExitStack()

    class Tl:
        def __init__(self, h, shape, key, base=0, pstride=None):
            self.h = h
            self.shape = shape
            self.key = key
            self.base = base
            self.row = int(np.prod(shape[1:])) if pstride is None else pstride

        def v(self, off, dims, Pn=128, p0=0):
            return bass.AP(self.h, self.base + off + p0 * self.row, [[self.row, Pn]] + [list(d) for d in dims])

    def sb(name, shape, dt=F32):
        h = st.enter_context(nc.sbuf_tensor(name, list(shape), dt))
        return Tl(h, list(shape), name)

    def dv(t, off, dims):
        return bass.AP(t, off, [list(d) for d in dims])

    banks = [Tl(st.enter_context(nc.psum_tensor("pb%d" % i, [128, 512], F32)), [128, 512], "pb%d" % i)
             for i in range(8)]
    bank_i = [0]

    xT = sb("xT", [128, KC, T], BF16)
    WBSZ = 6208
    WB = [sb("WB%d" % i, [128, WBSZ], BF16) for i in range(2)]
    C64 = sb("C64", [64, a64.shape[1]])
    C128 = sb("C128", [128, 384])
    cwa = sb("cwa", [128, L, 12, 4])
    cwc = sb("cwc", [128, L, 12, 4])
    cbc = sb("cbc", [128, L, 12])
    bgt = sb("bgt", [128, L, 3, 8])
    pb_a = sb("pb_a", [128, L, 8])
    nrm_a = sb("nrm_a", [64, L, 128])
    pc = sb("pc", [64, L, 48])
    wr = sb("wr", [128, L, KC, 36])
    brt = sb("brt", [128, L, 36])
    comb = sb("comb", [128, NT, 32])
    nexa = sb("nexa", [128, L, 4])
    nac = sb("nac", [64, L, 16])
    epst = sb("epst", [128, 4])
    ARENA_W = 88 * 256
    ARh = st.enter_context(nc.sbuf_tensor("AR", [128, ARENA_W], F32))
    ARb = ARh.bitcast(BF16)

    class Phase:
        def __init__(self, name):
            self.name = name
            self.off = 0
            self.t = {}
            self.keys = []

        def a(self, nm, free, dt=F32):
            n = int(np.prod(free))
            sz = n * (4 if dt == F32 else 2)
            off = self.off
            self.off += (sz + 3) // 4 * 4
            assert self.off <= ARENA_W * 4, (self.name, nm, self.off)
            if dt == F32:
                tl = Tl(ARh, [128] + list(free), self.name + "_" + nm, base=off // 4, pstride=ARENA_W)
            else:
                tl = Tl(ARb, [128] + list(free), self.name + "_" + nm, base=off // 2, pstride=ARENA_W * 2)
            self.t[nm] = tl
            self.keys.append(tl.key)
            return tl

    PH = {}
    for nm in "GRSMWXEN":
        PH[nm] = Phase(nm)
    G_, R_, S_, M_, W_, X_, E_, N_ = [PH[k] for k in "GRSMWXEN"]
    for nm in ["fmq", "fmk", "fmv", "fmz", "tA", "tB"]:
        R_.a(nm, [BLK])
    for nm in ["tA", "tB"]:
        G_.a(nm, [BLK])
    for par in range(2):
        for nm in ["fmq", "fmk", "fmv", "fmz"]:
            G_.a(nm + str(par), [BLK])
        for nm in ["rhs_u", "rhs_w", "kend", "u_t"]:
            G_.a(nm + str(par), [CPB * 128])
        G_.a("wT" + str(par), [CPB * 64]); G_.a("decS" + str(par), [CPB])
    for nm in ["raw0", "raw1"]:
        G_.a(nm, [BLK + 3]); S_.a(nm, [BLK + 3])
    G_.a("car", [12]); S_.a("car", [24])
    G_.a("ytb", [BLK], BF16); R_.a("ytb", [BLK], BF16); S_.a("ytb", [4 * BLK], BF16)
    G_.a("S_a", [128])
    for nm in ["g_t", "beta", "gc", "egl", "bex", "st1", "st2"]:
        G_.a(nm, [CPB])
    G_.a("gbr", [CPB * 64])
    for nm in ["o_t", "sq_t"]:
        G_.a(nm, [CPB * 128])
    for nm in ["t1", "Pm", "Qm", "Pm2", "Qm2", "Rm"]:
        G_.a(nm, [CPB * 64])
    G_.a("delta", [128])
    R_.a("cst", [2 * BLK]); R_.a("rdt", [BLK]); R_.a("SM", [CPB * 64])
    for nm in ["v_tm", "kw", "o_t", "sq_t"]:
        R_.a(nm, [CPB * 128])
    R_.a("St", [(CPB + 1) * 128]); R_.a("st1", [CPB]); R_.a("st2", [CPB]); R_.a("nrmb", [128])
    S_.a("fx", [4 * BLK])
    for nm in ["fmq", "fmk", "tA"]:
        S_.a(nm, [BLK])
    S_.a("Sc", [512])
    for nm in ["dt_t", "dta", "lc", "wr_t", "elc", "decb"]:
        S_.a(nm, [CPB * 8])
    for nm in ["nrmc", "dsk"]:
        S_.a(nm, [512])
    S_.a("szb", [CPB * 512])
    for par in range(2):
        for nm in ["db", "e1", "x_tm", "xw", "yi", "yg"]:
            S_.a(nm + str(par), [512])
        S_.a("cbT" + str(par), [64]); S_.a("B_tm" + str(par), [128]); S_.a("st2" + str(par), [4])
    mrgT = M_.a("mrg", [8 * T], BF16)
    W_.t["mrg"] = mrgT; W_.off = M_.off
    mkeys = ["mrg%d" % i for i in range(NBLK)]
    M_.keys += mkeys; W_.keys += mkeys + [mrgT.key]
    M_.a("ytl", [16 * BLK], BF16); M_.a("tA", [BLK]); M_.a("tB", [BLK])
    for ph in (W_, X_):
        ph.a("xl0", [1024]); ph.a("xl1", [1024])
    yacc = E_.a("yacc", [NT * 1024])
    N_.t["yacc"] = yacc; N_.off = E_.off
    ykeys = ["yacc%d" % i for i in range(NT)]
    E_.keys += ykeys; N_.keys += ykeys + [yacc.key]
    for ph in (W_, N_):
        for nm in ["lng", "lnb", "xn0", "xn1"]:
            ph.a(nm, [1024])
        for par in range(2):
            ph.a("bst%d" % par, [12]); ph.a("mv%d" % par, [4])
    for par in range(2):
        W_.a("xTf%d" % par, [1024]); W_.a("rt%d" % par, [160])
    E_.a("hs0", [BLK]); E_.a("hs1", [BLK]); E_.a("Hh0", [2 * BLK], BF16); E_.a("Hh1", [2 * BLK], BF16)
    cur_ph = [None]

    def enter(ph):
        old = cur_ph[0]
        if old is ph:
            return
        cur_ph[0] = ph
        if old is None:
            return
        P.op("dve", lambda e: e.memset(epst.v(3, [[1, 1]]), 0.0), writes=list(old.keys) + list(ph.keys) + ["epst3"])

    def c64(name, w=None, p0=0, Pn=64, coff=0):
        w = w if w is not None else {"tri": 64, "id": 64, "ones": 128, "bigm": 64, "rdec": 256, "wd": 4}[name]
        return C64.v(off64[name] + coff, [[1, w]], Pn=Pn, p0=p0)

    def c128(name, w=128, coff=0):
        return C128.v(off128[name] + coff, [[1, w]])

    P.dma("sp", lambda e: e.dma_start(out=C64.h[:, :], in_=dr["c64"][:, :]), writes=["C64"], chan="i_c64")
    P.dma("sp", lambda e: e.dma_start(out=C128.h[:, :], in_=dr["c128"][:, 0:384]), writes=["C128"], chan="i_c128")

    def small(dst_ap, src_ap, key, chan=None):
        P.dma("sp", lambda e: e.dma_start(out=dst_ap, in_=src_ap, allow_slow_non_contiguous=True), writes=[key], chan="i_" + key)

    for l in range(L):
        for k in range(4):
            small(cwa.v(l * 48 + k, [[4, 12]]), dv(dr["conv_a"], l * 6144 + k * 1536, [[1, 128], [128, 12]]), "cwa")
            small(cwc.v(l * 48 + k, [[4, 12]]), dv(dr["conv_c"], l * 6144 + k * 1536, [[1, 128], [128, 12]]), "cwc")
        small(cbc.v(l * 12, [[1, 12]]), dv(dr["conv_bias_c"], l * 1536, [[1, 128], [128, 12]]), "cbc")
        for br in range(3):
            small(bgt.v(l * 24 + br * 8, [[1, 8]]), dv(dr["b_gate"], l * 3072 + br * 1024, [[1, 128], [128, 8]]), "bgt")
        small(pb_a.v(l * 8, [[1, 4]]), dv(dr["a_log_a"], l * 4, [[0, 128], [1, 4]]), "pb_a")
        small(pb_a.v(l * 8 + 4, [[1, 4]]), dv(dr["dt_bias_a"], l * 4, [[0, 128], [1, 4]]), "pb_a")
        small(nrm_a.v(l * 128, [[1, 128]], Pn=64), dv(dr["norm_a"], l * 128, [[0, 64], [1, 128]]), "nrm_a")
        small(pc.v(l * 48, [[1, 16]], Pn=64), dv(dr["dt_bias_c"], l * 16, [[0, 64], [1, 16]]), "pc")
        small(pc.v(l * 48 + 16, [[1, 16]], Pn=64), dv(dr["a_log_c"], l * 16, [[0, 64], [1, 16]]), "pc")
        small(pc.v(l * 48 + 32, [[1, 16]], Pn=64), dv(dr["d_skip_c"], l * 16, [[0, 64], [1, 16]]), "pc")
        small(wr.v(l * KC * 36, [[36, KC], [1, 4]]), dv(dr["w_router_group"], l * D * 4, [[4, 128], [512, KC], [1, 4]]), "wr")
        small(wr.v(l * KC * 36 + 4, [[36, KC], [1, 32]]), dv(dr["w_router_expert"], l * D * 32, [[32, 128], [4096, KC], [1, 32]]), "wr")
        small(brt.v(l * 36, [[1, 4]]), dv(dr["b_router_group"], l * 4, [[0, 128], [1, 4]]), "brt")
        small(brt.v(l * 36 + 4, [[1, 32]]), dv(dr["b_router_expert"], l * 32, [[0, 128], [1, 32]]), "brt")
    P.op("dve", lambda e: e.memset(epst.v(0, [[1, 1]]), EPS), writes=["epst"])
    P.op("dve", lambda e: e.memset(epst.v(1, [[1, 1]]), 1.0), reads=["epst"], writes=["epst"])
    P.op("dve", lambda e: e.memset(epst.v(2, [[1, 1]]), 0.0), reads=["epst"], writes=["epst"])
    eps_ap = epst.v(0, [[1, 1]])
    one_ap = epst.v(1, [[1, 1]])
    for l in range(L):
        P.op("act", lambda e, l=l: e.activation(out=nexa.v(l * 4, [[1, 4]]), in_=pb_a.v(l * 8, [[1, 4]]), func=AF.Exp),
             reads=["pb_a"], writes=["nexa"])
        P.op("dve", lambda e, l=l: e.tensor_scalar(out=nexa.v(l * 4, [[1, 4]]), in0=nexa.v(l * 4, [[1, 4]]), scalar1=-1.0,
                                                   scalar2=None, op0=ALU.mult), reads=["nexa"], writes=["nexa"])
        P.op("act", lambda e, l=l: e.activation(out=nac.v(l * 16, [[1, 16]], Pn=64), in_=pc.v(l * 48 + 16, [[1, 16]], Pn=64),
                                                func=AF.Exp), reads=["pc"], writes=["nac"])
        P.op("dve", lambda e, l=l: e.tensor_scalar(out=nac.v(l * 16, [[1, 16]], Pn=64), in0=nac.v(l * 16, [[1, 16]], Pn=64),
                                                   scalar1=-1.0, scalar2=None, op0=ALU.mult), reads=["nac"], writes=["nac"])

    pinned = set()
    bpool = [list(range(8))]

    def ps():
        while True:
            pool = bpool[0]
            b = banks[pool[bank_i[0] % len(pool)]]
            bank_i[0] += 1
            if b.key not in pinned:
                return b

    def with_banks(lst, f):
        old = bpool[0]
        bpool[0] = lst
        f()
        bpool[0] = old

    def ld_w(slot, off_el, ncols, src_t, src_off, row_stride, nk=KC, krows=128):
        P.dma("pool", lambda e: e.dma_start(out=WB[slot].v(off_el, [[ncols, nk], [1, ncols]]),
                                            in_=dv(src_t, src_off, [[row_stride, 128], [128 * row_stride, nk], [1, ncols]]),
                                            allow_slow_non_contiguous=True),
              writes=["WB%d" % slot], chan="w%d" % slot)

    def xt_keys(tok0, n):
        return ["xT%d" % i for i in range(tok0 // 128, (tok0 + n + 127) // 128)]

    def proj_fm(slot, woff, wcols, c0, tok0, n, pbank, pcol=0):
        for kc in range(KC):
            P.op("pe", lambda e, kc=kc: e.matmul(pbank.v(pcol, [[1, n]]),
                                                 lhsT=WB[slot].v(woff + kc * wcols + c0, [[1, 128]]),
                                                 rhs=xT.v(kc * T + tok0, [[1, n]]), start=(kc == 0), stop=(kc == KC - 1)),
                 reads=["WB%d" % slot] + xt_keys(tok0, n), writes=[pbank.key])

    def proj_tm(slot, woff, wcols, c0, ncol, tok0, pbank, pcol=0):
        for kc in range(KC):
            P.op("pe", lambda e, kc=kc: e.matmul(pbank.v(pcol, [[1, ncol]], Pn=64),
                                                 lhsT=xT.v(kc * T + tok0, [[1, 64]]),
                                                 rhs=WB[slot].v(woff + kc * wcols + c0, [[1, ncol]]),
                                                 start=(kc == 0), stop=(kc == KC - 1)),
                 reads=["WB%d" % slot] + xt_keys(tok0, 64), writes=[pbank.key])

    def l2norm_fm(ph, t, scale):
        tB = ph.t["tB"]
        P.op("act", lambda e: e.activation(out=tB.v(0, [[1, BLK]]), in_=t.v(0, [[1, BLK]]), func=AF.Square), reads=[t.key], writes=[tB.key])
        pb = ps()
        P.op("pe", lambda e: e.matmul(pb.v(0, [[1, BLK]]), lhsT=c128("ones"), rhs=tB.v(0, [[1, BLK]]), start=True, stop=True),
             reads=[tB.key, "C128"], writes=[pb.key])
        P.op("act", lambda e: e.activation(out=tB.v(0, [[1, BLK]]), in_=pb.v(0, [[1, BLK]]), func=AF.Ln, bias=eps_ap, scale=1.0),
             reads=[pb.key, "epst"], writes=[tB.key])
        P.op("act", lambda e: e.activation(out=tB.v(0, [[1, BLK]]), in_=tB.v(0, [[1, BLK]]), func=AF.Exp, scale=-0.5), reads=[tB.key], writes=[tB.key])
        P.op("dve", lambda e: e.scalar_tensor_tensor(out=t.v(0, [[1, BLK]]), in0=t.v(0, [[1, BLK]]), scalar=float(scale),
                                                     in1=tB.v(0, [[1, BLK]]), op0=ALU.mult, op1=ALU.mult),
             reads=[t.key, tB.key], writes=[t.key])

    def store_yT(ytb, fc0, nfc, tok0, s):
        P.dma("sp", lambda e: e.dma_start(out=dv(ytd, (s % 2) * 128 * 16 * T + fc0 * T + tok0, [[16 * T, 128], [T, nfc], [1, BLK]]),
                                          in_=ytb.v(0, [[BLK, nfc], [1, BLK]])),
              reads=[ytb.key], writes=["ytd%d_%d_%d" % (s % 2, fc0 + i, tok0 // BLK) for i in range(nfc)], chan="yt")

    def gdn_load(slot, l, h):
        base = l * D * P_IN
        for j, c0 in enumerate([O_AQKV + h * 128, O_AQKV + 512 + h * 128, O_AQKV + 1024 + h * 128, O_AZ + h * 128]):
            ld_w(slot, j * 1024, 128, dr["w_in"], base + c0, P_IN)
        ld_w(slot, 4096, 1, dr["w_in"], base + O_AA + h, P_IN)
        ld_w(slot, 4096 + 8, 1, dr["w_in"], base + O_AB + h, P_IN)

    def gdn_run(slot, l, h, s):
        wk = "WB%d" % slot
        enter(G_)
        ph = G_
        tA, tB, ytb, S_a, g_t, beta, gc, egl, bex, st1, st2, gbr, o_t, sq_t, t1, Pm, Qm, Pm2, Qm2, Rm, delta = [G_.t[n] for n in ['tA', 'tB', 'ytb', 'S_a', 'g_t', 'beta', 'gc', 'egl', 'bex', 'st1', 'st2', 'gbr', 'o_t', 'sq_t', 't1', 'Pm', 'Qm', 'Pm2', 'Qm2', 'Rm', 'delta']]
        def pre(b):
            tok0 = b * BLK
            fmq, fmk, fmv, fmz, rhs_u, rhs_w, kend, u_t, wT, decS = [G_.t[n + str(b % 2)] for n in ['fmq', 'fmk', 'fmv', 'fmz', 'rhs_u', 'rhs_w', 'kend', 'u_t', 'wT', 'decS']]
            for j, dst in enumerate([fmq, fmk, fmv]):
                pb = ps()
                proj_fm(slot, j * 1024, 128, 0, tok0, BLK, pb)
                cw = lambda k, j=j: cwa.v(l * 48 + (j * 4 + h) * 4 + k, [[1, 1]])
                conv_stream(ph, pb, j, b, cw, dst.v(0, [[1, BLK]]), dst.key)
            l2norm_fm(ph, fmq, 128.0 ** -0.5)
            l2norm_fm(ph, fmk, 1.0)
            pb = ps()
            proj_fm(slot, 3 * 1024, 128, 0, tok0, BLK, pb)
            P.op("act", lambda e, pb=pb: e.activation(out=fmz.v(0, [[1, BLK]]), in_=pb.v(0, [[1, BLK]]), func=AF.Silu),
                 reads=[pb.key], writes=[fmz.key])
            pb = ps()
            for c in range(CPB):
                proj_tm(slot, 4096, 1, 0, 1, tok0 + c * 64, pb, pcol=2 * c)
                proj_tm(slot, 4096 + 8, 1, 0, 1, tok0 + c * 64, pb, pcol=2 * c + 1)
            P.op("act", lambda e, pb=pb: e.activation(out=g_t.v(0, [[1, CPB]], Pn=64), in_=pb.v(0, [[2, CPB]], Pn=64), func=AF.Exp,
                                                      bias=pb_a.v(l * 8 + 4 + h, [[1, 1]], Pn=64)),
                 reads=[pb.key, "pb_a"], writes=[g_t.key])
            P.op("act", lambda e: e.activation(out=g_t.v(0, [[1, CPB]], Pn=64), in_=g_t.v(0, [[1, CPB]], Pn=64), func=AF.Ln,
                                               bias=one_ap[0:64, :]), reads=[g_t.key, "epst"], writes=[g_t.key])
            P.op("dve", lambda e: e.tensor_scalar(out=g_t.v(0, [[1, CPB]], Pn=64), in0=g_t.v(0, [[1, CPB]], Pn=64),
                                                  scalar1=nexa.v(l * 4 + h, [[1, 1]], Pn=64), scalar2=None, op0=ALU.mult),
                 reads=[g_t.key, "nexa"], writes=[g_t.key])
            P.op("act", lambda e, pb=pb: e.activation(out=beta.v(0, [[1, CPB]], Pn=64), in_=pb.v(1, [[2, CPB]], Pn=64),
                                                      func=AF.Exp, scale=-1.0), reads=[pb.key], writes=[beta.key])
            P.op("dve", lambda e: e.tensor_scalar(out=beta.v(0, [[1, CPB]], Pn=64), in0=beta.v(0, [[1, CPB]], Pn=64), scalar1=1.0, scalar2=None,
                                                  op0=ALU.add), reads=[beta.key], writes=[beta.key])
            P.op("dve", lambda e: e.reciprocal(out=beta.v(0, [[1, CPB]], Pn=64), in_=beta.v(0, [[1, CPB]], Pn=64)), reads=[beta.key], writes=[beta.key])
            pg = ps()
            P.op("pe", lambda e, pg=pg: e.matmul(pg.v(0, [[1, CPB]], Pn=64), lhsT=c64("tri"), rhs=g_t.v(0, [[1, CPB]], Pn=64),
                                                 start=True, stop=True), reads=[g_t.key, "C64"], writes=[pg.key])
            P.op("pe", lambda e, pg=pg: e.matmul(pg.v(64, [[1, CPB]]), lhsT=c64("ones"), rhs=g_t.v(0, [[1, CPB]], Pn=64),
                                                 start=True, stop=True), reads=[g_t.key, "C64"], writes=[pg.key])
            P.op("dve", lambda e, pg=pg: e.tensor_copy(out=gc.v(0, [[1, CPB]], Pn=64), in_=pg.v(0, [[1, CPB]], Pn=64)),
                 reads=[pg.key], writes=[gc.key])
            P.op("act", lambda e, pg=pg: e.activation(out=decS.v(0, [[1, CPB]]), in_=pg.v(64, [[1, CPB]]), func=AF.Exp),
                 reads=[pg.key], writes=[decS.key])
            P.op("dve", lambda e, pg=pg: e.tensor_tensor(out=egl.v(0, [[1, CPB]], Pn=64), in0=pg.v(64, [[1, CPB]], Pn=64),
                                                         in1=gc.v(0, [[1, CPB]], Pn=64), op=ALU.subtract),
                 reads=[pg.key, gc.key], writes=[egl.key])
            P.op("act", lambda e: e.activation(out=egl.v(0, [[1, CPB]], Pn=64), in_=egl.v(0, [[1, CPB]], Pn=64), func=AF.Exp),
                 reads=[egl.key], writes=[egl.key])
            P.op("act", lambda e: e.activation(out=bex.v(0, [[1, CPB]], Pn=64), in_=gc.v(0, [[1, CPB]], Pn=64), func=AF.Exp),
                 reads=[gc.key], writes=[bex.key])
            P.op("dve", lambda e: e.tensor_tensor(out=bex.v(0, [[1, CPB]], Pn=64), in0=bex.v(0, [[1, CPB]], Pn=64),
                                                  in1=beta.v(0, [[1, CPB]], Pn=64), op=ALU.mult), reads=[bex.key, beta.key], writes=[bex.key])
            P.op("dve", lambda e: e.tensor_copy(out=gbr.v(0, [[64, CPB], [1, 64]], Pn=64), in_=g_t.v(0, [[1, CPB], [0, 64]], Pn=64)),
                 reads=[g_t.key], writes=[gbr.key])
            pG = ps()
            pK = ps()
            for c in range(CPB):
                P.op("pe", lambda e, c=c, pG=pG: e.matmul(pG.v(c * 64, [[1, 64]], Pn=64), lhsT=gbr.v(c * 64, [[1, 64]], Pn=64),
                                                          rhs=c64("tri"), start=True, stop=True), reads=[gbr.key, "C64"], writes=[pG.key])
                P.op("pe", lambda e, c=c, pK=pK: e.matmul(pK.v(c * 64, [[1, 64]], Pn=64), lhsT=fmk.v(c * 64, [[1, 64]]),
                                                          rhs=fmk.v(c * 64, [[1, 64]]), start=True, stop=True), reads=[fmk.key], writes=[pK.key])
            P.op("dve", lambda e, pG=pG: e.tensor_tensor(out=t1.v(0, [[64, CPB], [1, 64]], Pn=64), in0=pG.v(0, [[64, CPB], [1, 64]], Pn=64),
                                                         in1=gc.v(0, [[1, CPB], [0, 64]], Pn=64), op=ALU.subtract),
                 reads=[pG.key, gc.key], writes=[t1.key])
            P.op("dve", lambda e: e.tensor_tensor(out=t1.v(0, [[64, CPB], [1, 64]], Pn=64), in0=t1.v(0, [[64, CPB], [1, 64]], Pn=64),
                                                  in1=C64.v(off64["bigm"], [[0, CPB], [1, 64]], Pn=64), op=ALU.max),
                 reads=[t1.key, "C64"], writes=[t1.key])
            P.op("act", lambda e: e.activation(out=t1.v(0, [[1, CPB * 64]], Pn=64), in_=t1.v(0, [[1, CPB * 64]], Pn=64), func=AF.Exp, scale=-1.0),
                 reads=[t1.key], writes=[t1.key])
            P.op("dve", lambda e, pK=pK: e.tensor_tensor(out=Qm.v(0, [[64, CPB], [1, 64]], Pn=64), in0=pK.v(0, [[64, CPB], [1, 64]], Pn=64),
                                                         in1=beta.v(0, [[1, CPB], [0, 64]], Pn=64), op=ALU.mult),
                 reads=[pK.key, beta.key], writes=[Qm.key])
            P.op("dve", lambda e: e.tensor_tensor(out=Qm.v(0, [[1, CPB * 64]], Pn=64), in0=Qm.v(0, [[1, CPB * 64]], Pn=64),
                                                  in1=t1.v(0, [[1, CPB * 64]], Pn=64), op=ALU.mult), reads=[Qm.key, t1.key], writes=[Qm.key])
            pT = [ps(), ps()]
            pV = [ps(), ps()]
            pB_ = ps()
            for c in range(CPB):
                P.op("pe", lambda e, c=c: e.transpose(pT[c // 4].v((c % 4) * 128, [[1, 128]], Pn=64), fmk.v(c * 64, [[1, 64]]), c128("id")),
                     reads=[fmk.key, "C128"], writes=[pT[c // 4].key])
                P.op("pe", lambda e, c=c: e.transpose(pV[c // 4].v((c % 4) * 128, [[1, 128]], Pn=64), fmv.v(c * 64, [[1, 64]]), c128("id")),
                     reads=[fmv.key, "C128"], writes=[pV[c // 4].key])
                P.op("pe", lambda e, c=c: e.transpose(pB_.v(c * 64, [[1, 64]], Pn=64), Qm.v(c * 64, [[1, 64]], Pn=64), c64("id")),
                     reads=[Qm.key, "C64"], writes=[pB_.key])
            for hb in range((CPB + 3) // 4):
                n = min(4, CPB - hb * 4)
                P.op("dve", lambda e, hb=hb, n=n: e.tensor_tensor(out=rhs_w.v(hb * 512, [[128, n], [1, 128]], Pn=64),
                                                                  in0=pT[hb].v(0, [[128, n], [1, 128]], Pn=64),
                                                                  in1=bex.v(hb * 4, [[1, n], [0, 128]], Pn=64), op=ALU.mult),
                     reads=[pT[hb].key, bex.key], writes=[rhs_w.key])
                P.op("dve", lambda e, hb=hb, n=n: e.tensor_tensor(out=kend.v(hb * 512, [[128, n], [1, 128]], Pn=64),
                                                                  in0=pT[hb].v(0, [[128, n], [1, 128]], Pn=64),
                                                                  in1=egl.v(hb * 4, [[1, n], [0, 128]], Pn=64), op=ALU.mult),
                     reads=[pT[hb].key, egl.key], writes=[kend.key])
                P.op("dve", lambda e, hb=hb, n=n: e.tensor_tensor(out=rhs_u.v(hb * 512, [[128, n], [1, 128]], Pn=64),
                                                                  in0=pV[hb].v(0, [[128, n], [1, 128]], Pn=64),
                                                                  in1=beta.v(hb * 4, [[1, n], [0, 128]], Pn=64), op=ALU.mult),
                     reads=[pV[hb].key, beta.key], writes=[rhs_u.key])
            P.op("act", lambda e: e.activation(out=Pm.v(0, [[1, CPB * 64]], Pn=64), in_=pB_.v(0, [[1, CPB * 64]], Pn=64), func=AF.Copy),
                 reads=[pB_.key], writes=[Pm.key])
            P.op("dve", lambda e: e.tensor_tensor(out=Rm.v(0, [[64, CPB], [1, 64]], Pn=64), in0=C64.v(off64["id"], [[0, CPB], [1, 64]], Pn=64),
                                                  in1=pB_.v(0, [[64, CPB], [1, 64]], Pn=64), op=ALU.subtract),
                 reads=[pB_.key, "C64"], writes=[Rm.key])
            Pc, Qc, Pn_, Qn_ = Pm, Qm, Pm2, Qm2
            for lev in range(5):
                last = lev == 4
                pq = ps()
                pp = ps() if not last else None
                for c in range(CPB):
                    P.op("pe", lambda e, c=c, pq=pq, Pc=Pc, Qc=Qc: e.matmul(pq.v(c * 64, [[1, 64]], Pn=64), lhsT=Pc.v(c * 64, [[1, 64]], Pn=64),
                                                                         rhs=Qc.v(c * 64, [[1, 64]], Pn=64), start=True, stop=True),
                         reads=[Pc.key, Qc.key], writes=[pq.key])
                    if not last:
                        P.op("pe", lambda e, c=c, pp=pp, Pc=Pc, Qc=Qc: e.matmul(pp.v(c * 64, [[1, 64]], Pn=64), lhsT=Qc.v(c * 64, [[1, 64]], Pn=64),
                                                                             rhs=Pc.v(c * 64, [[1, 64]], Pn=64), start=True, stop=True),
                             reads=[Pc.key, Qc.key], writes=[pp.key])
                P.op("act", lambda e, pq=pq, Qn_=Qn_: e.activation(out=Qn_.v(0, [[1, CPB * 64]], Pn=64), in_=pq.v(0, [[1, CPB * 64]], Pn=64), func=AF.Copy),
                     reads=[pq.key], writes=[Qn_.key])
                if not last:
                    P.op("dve", lambda e, pp=pp, Pn_=Pn_: e.tensor_copy(out=Pn_.v(0, [[1, CPB * 64]], Pn=64), in_=pp.v(0, [[1, CPB * 64]], Pn=64)),
                         reads=[pp.key], writes=[Pn_.key])
                pr = ps()
                for c in range(CPB):
                    P.op("pe", lambda e, c=c, pr=pr, Qn_=Qn_: e.matmul(pr.v(c * 64, [[1, 64]], Pn=64), lhsT=Qn_.v(c * 64, [[1, 64]], Pn=64),
                                                                      rhs=Rm.v(c * 64, [[1, 64]], Pn=64), start=True, stop=True),
                         reads=[Qn_.key, Rm.key], writes=[pr.key])
                P.op("dve", lambda e, pr=pr: e.tensor_tensor(out=Rm.v(0, [[1, CPB * 64]], Pn=64), in0=Rm.v(0, [[1, CPB * 64]], Pn=64),
                                                             in1=pr.v(0, [[1, CPB * 64]], Pn=64), op=ALU.add), reads=[pr.key, Rm.key], writes=[Rm.key])
                Pc, Qc, Pn_, Qn_ = Pn_, Qn_, Pc, Qc
            pU = [ps(), ps()]
            pW = ps()
            for c in range(CPB):
                P.op("pe", lambda e, c=c: e.matmul(pU[c // 4].v((c % 4) * 128, [[1, 128]], Pn=64), lhsT=Rm.v(c * 64, [[1, 64]], Pn=64),
                                                   rhs=rhs_u.v(c * 128, [[1, 128]], Pn=64), start=True, stop=True),
                     reads=[Rm.key, rhs_u.key], writes=[pU[c // 4].key])
                P.op("pe", lambda e, c=c: e.matmul(pW.v(c * 64, [[1, 64]]), lhsT=rhs_w.v(c * 128, [[1, 128]], Pn=64),
                                                   rhs=Rm.v(c * 64, [[1, 64]], Pn=64), start=True, stop=True),
                     reads=[Rm.key, rhs_w.key], writes=[pW.key])
            for hb in range((CPB + 3) // 4):
                n = min(4, CPB - hb * 4)
                P.op("act", lambda e, hb=hb, n=n: e.activation(out=u_t.v(hb * 512, [[1, n * 128]], Pn=64), in_=pU[hb].v(0, [[1, n * 128]], Pn=64),
                                                               func=AF.Copy), reads=[pU[hb].key], writes=[u_t.key])
            P.op("act", lambda e: e.activation(out=wT.v(0, [[1, CPB * 64]]), in_=pW.v(0, [[1, CPB * 64]]), func=AF.Copy),
                 reads=[pW.key], writes=[wT.key])

        def scanpost(b):
            tok0 = b * BLK
            fmq, fmk, fmv, fmz, rhs_u, rhs_w, kend, u_t, wT, decS = [G_.t[n + str(b % 2)] for n in ['fmq', 'fmk', 'fmv', 'fmz', 'rhs_u', 'rhs_w', 'kend', 'u_t', 'wT', 'decS']]
            if b == 0:
                P.op("dve", lambda e: e.memset(S_a.v(0, [[1, 128]]), 0.0), writes=[S_a.key])
            pO = [ps(), ps()]
            pinned.update([pO[0].key, pO[1].key])
            for c in range(CPB):
                p1 = ps()
                P.op("pe", lambda e, c=c, p1=p1: e.matmul(p1.v(0, [[1, 128]], Pn=64), lhsT=wT.v(c * 64, [[1, 64]]), rhs=S_a.v(0, [[1, 128]]),
                                                          start=True, stop=True), reads=[wT.key, S_a.key], writes=[p1.key])
                P.op("dve", lambda e, c=c, p1=p1: e.tensor_tensor(out=delta.v(0, [[1, 128]], Pn=64), in0=u_t.v(c * 128, [[1, 128]], Pn=64),
                                                                  in1=p1.v(0, [[1, 128]], Pn=64), op=ALU.subtract),
                     reads=[u_t.key, p1.key], writes=[delta.key])
                p2 = ps()
                P.op("pe", lambda e, c=c, p2=p2: e.matmul(p2.v(0, [[1, 128]]), lhsT=kend.v(c * 128, [[1, 128]], Pn=64),
                                                          rhs=delta.v(0, [[1, 128]], Pn=64), start=True, stop=True),
                     reads=[kend.key, delta.key], writes=[p2.key])
                P.op("dve", lambda e, c=c, p2=p2: e.scalar_tensor_tensor(out=S_a.v(0, [[1, 128]]), in0=S_a.v(0, [[1, 128]]),
                                                                         scalar=decS.v(c, [[1, 1]]), in1=p2.v(0, [[1, 128]]),
                                                                         op0=ALU.mult, op1=ALU.add), reads=[S_a.key, decS.key, p2.key], writes=[S_a.key])
                P.op("pe", lambda e, c=c: e.matmul(pO[c // 4].v((c % 4) * 128, [[1, 128]], Pn=64), lhsT=fmq.v(c * 64, [[1, 64]]),
                                                   rhs=S_a.v(0, [[1, 128]]), start=True, stop=True), reads=[fmq.key, S_a.key], writes=[pO[c // 4].key])
            pinned.difference_update([pO[0].key, pO[1].key])
            for hb in range((CPB + 3) // 4):
                n = min(4, CPB - hb * 4)
                P.op("act", lambda e, hb=hb, n=n: e.activation(out=o_t.v(hb * 512, [[1, n * 128]], Pn=64), in_=pO[hb].v(0, [[1, n * 128]], Pn=64),
                                                               func=AF.Copy), reads=[pO[hb].key], writes=[o_t.key])
            rms_tm(ph, o_t, CPB, 128, nrm_a.v(l * 128, [[0, CPB], [1, 128]], Pn=64), "nrm_a")
            pY = ps()
            for c in range(CPB):
                P.op("pe", lambda e, c=c, pY=pY: e.transpose(pY.v(c * 64, [[1, 64]]), o_t.v(c * 128, [[1, 128]], Pn=64), c64("id")),
                     reads=[o_t.key, "C64"], writes=[pY.key])
            P.op("dve", lambda e, pY=pY: e.tensor_tensor(out=ytb.v(0, [[1, BLK]]), in0=pY.v(0, [[1, BLK]]), in1=fmz.v(0, [[1, BLK]]), op=ALU.mult),
                 reads=[pY.key, fmz.key], writes=[ytb.key])
            store_yT(ytb, h, 1, tok0, s)


        pre(0)
        for b in range(NBLK):
            A = P.capture(lambda: with_banks([0, 1, 2], lambda: scanpost(b)))
            B = P.capture(lambda: with_banks([3, 4, 5, 6, 7], lambda: pre(b + 1))) if b + 1 < NBLK else []
            P.merge(A, B)

    conv_cnt = [0]

    def conv_stream(ph, pb, sid, b, cw, out_ap, out_key, bias_ap=None):
        r = ph.t["raw%d" % (conv_cnt[0] % 2)]
        conv_cnt[0] += 1
        car = ph.t["car"]
        tA = ph.t["tA"]
        if b == 0:
            P.op("dve", lambda e: e.memset(r.v(0, [[1, 3]]), 0.0), writes=[r.key])
        else:
            P.op("dve", lambda e: e.tensor_copy(out=r.v(0, [[1, 3]]), in_=car.v(sid * 3, [[1, 3]])), reads=[car.key], writes=[r.key])
        P.op("act", lambda e: e.activation(out=r.v(3, [[1, BLK]]), in_=pb.v(0, [[1, BLK]]), func=AF.Copy),
             reads=[pb.key, r.key], writes=[r.key])
        P.op("dve", lambda e: e.tensor_copy(out=car.v(sid * 3, [[1, 3]]), in_=r.v(BLK, [[1, 3]])), reads=[r.key, car.key], writes=[car.key])
        P.op("dve", lambda e: e.tensor_scalar(out=tA.v(0, [[1, BLK]]), in0=r.v(0, [[1, BLK]]), scalar1=cw(0), scalar2=None, op0=ALU.mult),
             reads=[r.key, "cwa", "cwc"], writes=[tA.key])
        for k in range(1, 4):
            P.op("dve", lambda e, k=k: e.scalar_tensor_tensor(out=tA.v(0, [[1, BLK]]), in0=r.v(k, [[1, BLK]]), scalar=cw(k),
                                                              in1=tA.v(0, [[1, BLK]]), op0=ALU.mult, op1=ALU.add),
                 reads=[r.key, tA.key, "cwa", "cwc"], writes=[tA.key])
        if bias_ap is None:
            P.op("act", lambda e: e.activation(out=out_ap, in_=tA.v(0, [[1, BLK]]), func=AF.Silu), reads=[tA.key], writes=[out_key])
        else:
            P.op("act", lambda e: e.activation(out=out_ap, in_=tA.v(0, [[1, BLK]]), func=AF.Silu, bias=bias_ap),
                 reads=[tA.key, "cbc"], writes=[out_key])

    def rms_tm(ph, t, nch, width, w_ap, wkey, center=False):
        st1, st2, sq_t = ph.t["st1"], ph.t["st2"], ph.t["sq_t"]
        full = t.v(0, [[width, nch], [1, width]], Pn=64)
        if center:
            P.op("dve", lambda e: e.tensor_reduce(out=st1.v(0, [[1, nch]], Pn=64), in_=full, axis=AX.X, op=ALU.add), reads=[t.key], writes=[st1.key])
            P.op("dve", lambda e: e.tensor_scalar(out=st1.v(0, [[1, nch]], Pn=64), in0=st1.v(0, [[1, nch]], Pn=64), scalar1=1.0 / width,
                                                  scalar2=None, op0=ALU.mult), reads=[st1.key], writes=[st1.key])
            P.op("dve", lambda e: e.tensor_tensor(out=full, in0=full, in1=st1.v(0, [[1, nch], [0, width]], Pn=64), op=ALU.subtract),
                 reads=[t.key, st1.key], writes=[t.key])
        P.op("act", lambda e: e.activation(out=sq_t.v(0, [[1, nch * width]], Pn=64),
                                           in_=t.v(0, [[1, nch * width]], Pn=64), func=AF.Square), reads=[t.key], writes=[sq_t.key])
        P.op("dve", lambda e: e.tensor_reduce(out=st2.v(0, [[1, nch]], Pn=64), in_=sq_t.v(0, [[width, nch], [1, width]], Pn=64), axis=AX.X, op=ALU.add),
             reads=[sq_t.key], writes=[st2.key])
        P.op("act", lambda e: e.activation(out=st2.v(0, [[1, nch]], Pn=64), in_=st2.v(0, [[1, nch]], Pn=64), func=AF.Ln,
                                           bias=eps_ap[0:64, :], scale=1.0 / width), reads=[st2.key, "epst"], writes=[st2.key])
        P.op("act", lambda e: e.activation(out=st2.v(0, [[1, nch]], Pn=64), in_=st2.v(0, [[1, nch]], Pn=64), func=AF.Exp, scale=-0.5),
             reads=[st2.key], writes=[st2.key])
        P.op("dve", lambda e: e.tensor_tensor(out=full, in0=full, in1=st2.v(0, [[1, nch], [0, width]], Pn=64), op=ALU.mult),
             reads=[t.key, st2.key], writes=[t.key])
        P.op("dve", lambda e: e.tensor_tensor(out=full, in0=full, in1=w_ap, op=ALU.mult), reads=[t.key, wkey], writes=[t.key])

    def ret_load(slot, l, h):
        base = l * D * P_IN
        for j, c0 in enumerate([O_BQ + h * 128, O_BK + h * 128, O_BV + h * 128, O_BG + h * 128]):
            ld_w(slot, j * 1024, 128, dr["w_in"], base + c0, P_IN)

    def ret_run(slot, l, h, s):
        enter(R_)
        ph = R_
        fmq, fmk, fmv, fmz, tA, tB, ytb, cst, rdt, SM, v_tm, kw, o_t, sq_t, St, st1, st2, nrmb = [R_.t[n] for n in ['fmq', 'fmk', 'fmv', 'fmz', 'tA', 'tB', 'ytb', 'cst', 'rdt', 'SM', 'v_tm', 'kw', 'o_t', 'sq_t', 'St', 'st1', 'st2', 'nrmb']]
        small(rdt.v(0, [[1, BLK]]), dv(dr["c128"], off128["rd"] + h * BLK, [[a128.shape[1], 128], [1, BLK]]), rdt.key, chan="rp")
        small(nrmb.v(0, [[1, 128]], Pn=64), dv(dr["norm_b"], l * 512 + h * 128, [[0, 64], [1, 128]]), nrmb.key, chan="rp")
        for b in range(NBLK):
            tok0 = b * BLK
            P.dma("sp", lambda e, tok0=tok0: e.dma_start(out=cst.v(0, [[BLK, 2], [1, BLK]]),
                                                         in_=dv(dr["cs"], tok0, [[2 * T, 128], [T, 2], [1, BLK]])), writes=[cst.key], chan="cs")
            for j, dst in enumerate([fmq, fmk]):
                pb = ps()
                proj_fm(slot, j * 1024, 128, 0, tok0, BLK, pb)
                P.op("act", lambda e, pb=pb: e.activation(out=tA.v(0, [[1, BLK]]), in_=pb.v(0, [[1, BLK]]), func=AF.Copy), reads=[pb.key], writes=[tA.key])
                pr = ps()
                P.op("pe", lambda e, pr=pr: e.matmul(pr.v(0, [[1, BLK]]), lhsT=c128("rot"), rhs=tA.v(0, [[1, BLK]]), start=True, stop=True),
                     reads=[tA.key, "C128"], writes=[pr.key])
                sc = 1.0 if j == 0 else 128.0 ** -0.5
                P.op("dve", lambda e, pr=pr, sc=sc: e.scalar_tensor_tensor(out=tB.v(0, [[1, BLK]]), in0=pr.v(0, [[1, BLK]]), scalar=sc,
                                                                           in1=cst.v(BLK, [[1, BLK]]), op0=ALU.mult, op1=ALU.mult),
                     reads=[pr.key, cst.key], writes=[tB.key])
                P.op("dve", lambda e, sc=sc: e.scalar_tensor_tensor(out=tA.v(0, [[1, BLK]]), in0=tA.v(0, [[1, BLK]]), scalar=sc,
                                                                    in1=cst.v(0, [[1, BLK]]), op0=ALU.mult, op1=ALU.mult),
                     reads=[tA.key, cst.key], writes=[tA.key])
                P.op("dve", lambda e, dst=dst: e.tensor_tensor(out=dst.v(0, [[1, BLK]]), in0=tA.v(0, [[1, BLK]]), in1=tB.v(0, [[1, BLK]]), op=ALU.add),
                     reads=[tA.key, tB.key], writes=[dst.key])
            pb = ps()
            proj_fm(slot, 3 * 1024, 128, 0, tok0, BLK, pb)
            P.op("act", lambda e, pb=pb: e.activation(out=fmz.v(0, [[1, BLK]]), in_=pb.v(0, [[1, BLK]]), func=AF.Silu), reads=[pb.key], writes=[fmz.key])
            P.op("dve", lambda e: e.tensor_tensor(out=fmv.v(0, [[1, BLK]]), in0=fmq.v(0, [[1, BLK]]), in1=rdt.v(0, [[1, BLK]]), op=ALU.mult),
                 reads=[fmq.key, rdt.key], writes=[fmv.key])
            pV = [ps(), ps()]
            pT = [ps(), ps()]
            pS = ps()
            for c in range(CPB):
                proj_tm(slot, 2 * 1024, 128, 0, 128, tok0 + c * 64, pV[c // 4], pcol=(c % 4) * 128)
                P.op("pe", lambda e, c=c: e.transpose(pT[c // 4].v((c % 4) * 128, [[1, 128]], Pn=64), fmk.v(c * 64, [[1, 64]]), c128("id")),
                     reads=[fmk.key, "C128"], writes=[pT[c // 4].key])
                P.op("pe", lambda e, c=c: e.matmul(pS.v(c * 64, [[1, 64]], Pn=64), lhsT=fmk.v(c * 64, [[1, 64]]), rhs=fmq.v(c * 64, [[1, 64]]),
                                                   start=True, stop=True), reads=[fmk.key, fmq.key], writes=[pS.key])
            for hb in range((CPB + 3) // 4):
                n = min(4, CPB - hb * 4)
                P.op("act", lambda e, hb=hb, n=n: e.activation(out=v_tm.v(hb * 512, [[1, n * 128]], Pn=64), in_=pV[hb].v(0, [[1, n * 128]], Pn=64),
                                                               func=AF.Copy), reads=[pV[hb].key], writes=[v_tm.key])
                P.op("dve", lambda e, hb=hb, n=n: e.tensor_scalar(out=kw.v(hb * 512, [[1, n * 128]], Pn=64), in0=pT[hb].v(0, [[1, n * 128]], Pn=64),
                                                                  scalar1=c64("wd", 1, coff=h), scalar2=None, op0=ALU.mult),
                     reads=[pT[hb].key, "C64"], writes=[kw.key])
            P.op("dve", lambda e: e.tensor_tensor(out=SM.v(0, [[64, CPB], [1, 64]], Pn=64), in0=pS.v(0, [[64, CPB], [1, 64]], Pn=64),
                                                  in1=C64.v(off64["rdec"] + h * 64, [[0, CPB], [1, 64]], Pn=64), op=ALU.mult),
                 reads=[pS.key, "C64"], writes=[SM.key])
            if b == 0:
                P.op("dve", lambda e: e.memset(St.v(0, [[1, 128]]), 0.0), reads=[St.key], writes=[St.key])
            else:
                P.op("dve", lambda e: e.tensor_copy(out=St.v(0, [[1, 128]]), in_=St.v(CPB * 128, [[1, 128]])), reads=[St.key], writes=[St.key])
            for c in range(CPB):
                pu = ps()
                P.op("pe", lambda e, c=c, pu=pu: e.matmul(pu.v(0, [[1, 128]]), lhsT=kw.v(c * 128, [[1, 128]], Pn=64),
                                                          rhs=v_tm.v(c * 128, [[1, 128]], Pn=64), start=True, stop=True),
                     reads=[kw.key, v_tm.key], writes=[pu.key])
                P.op("dve", lambda e, c=c, pu=pu: e.scalar_tensor_tensor(out=St.v((c + 1) * 128, [[1, 128]]), in0=St.v(c * 128, [[1, 128]]),
                                                                         scalar=cdec[h], in1=pu.v(0, [[1, 128]]), op0=ALU.mult, op1=ALU.add),
                     reads=[St.key, pu.key], writes=[St.key])
            pO = [ps(), ps()]
            for c in range(CPB):
                P.op("pe", lambda e, c=c: e.matmul(pO[c // 4].v((c % 4) * 128, [[1, 128]], Pn=64), lhsT=SM.v(c * 64, [[1, 64]], Pn=64),
                                                   rhs=v_tm.v(c * 128, [[1, 128]], Pn=64), start=True, stop=False),
                     reads=[SM.key, v_tm.key], writes=[pO[c // 4].key])
                P.op("pe", lambda e, c=c: e.matmul(pO[c // 4].v((c % 4) * 128, [[1, 128]], Pn=64), lhsT=fmv.v(c * 64, [[1, 64]]),
                                                   rhs=St.v(c * 128, [[1, 128]]), start=False, stop=True),
                     reads=[fmv.key, St.key], writes=[pO[c // 4].key])
            for hb in range((CPB + 3) // 4):
                n = min(4, CPB - hb * 4)
                P.op("act", lambda e, hb=hb, n=n: e.activation(out=o_t.v(hb * 512, [[1, n * 128]], Pn=64), in_=pO[hb].v(0, [[1, n * 128]], Pn=64),
                                                               func=AF.Copy), reads=[pO[hb].key], writes=[o_t.key])
            rms_tm(ph, o_t, CPB, 128, nrmb.v(0, [[0, CPB], [1, 128]], Pn=64), nrmb.key, center=True)
            pY = ps()
            for c in range(CPB):
                P.op("pe", lambda e, c=c, pY=pY: e.transpose(pY.v(c * 64, [[1, 64]]), o_t.v(c * 128, [[1, 128]], Pn=64), c64("id")),
                     reads=[o_t.key, "C64"], writes=[pY.key])
            P.op("dve", lambda e, pY=pY: e.tensor_tensor(out=ytb.v(0, [[1, BLK]]), in0=pY.v(0, [[1, BLK]]), in1=fmz.v(0, [[1, BLK]]), op=ALU.mult),
                 reads=[pY.key, fmz.key], writes=[ytb.key])
            store_yT(ytb, 4 + h, 1, tok0, s)

    def ssd_load1(slot, l, g):
        base = l * D * P_IN
        ld_w(slot, 0, 512, dr["w_in"], base + O_CXBC + g * 512, P_IN)
        ld_w(slot, 4096, 128, dr["w_in"], base + O_CXBC + 1024 + g * 128, P_IN)
        ld_w(slot, 5120, 128, dr["w_in"], base + O_CXBC + 1280 + g * 128, P_IN)
        ld_w(slot, 6144, 8, dr["w_in"], base + O_CDT + g * 8, P_IN)

    def ssd_load2(slot, l, g):
        ld_w(slot, 0, 512, dr["w_in"], l * D * P_IN + O_CZ + g * 512, P_IN)

    def ssd_run(slot, l, g, s):
        enter(S_)
        ph = S_
        zslot = slot
        slot = 1 - slot
        fx, fmq, fmk, tA, Sc, dt_t, dta, lc, wr_t, elc, decb, nrmc, dsk, ytb, szb = [S_.t[n] for n in ['fx', 'fmq', 'fmk', 'tA', 'Sc', 'dt_t', 'dta', 'lc', 'wr_t', 'elc', 'decb', 'nrmc', 'dsk', 'ytb', 'szb']]
        small(nrmc.v(0, [[1, 512]], Pn=64), dv(dr["norm_c"], l * 1024 + g * 512, [[0, 64], [1, 512]]), nrmc.key, chan="rp")
        P.op("dve", lambda e: e.tensor_tensor(out=dsk.v(0, [[64, 8], [1, 64]], Pn=64), in0=C64.v(off64["id"], [[0, 8], [1, 64]], Pn=64),
                                              in1=pc.v(l * 48 + 32 + g * 8, [[1, 8], [0, 64]], Pn=64), op=ALU.mult), reads=["C64", "pc"], writes=[dsk.key])
        for b in range(NBLK):
            tok0 = b * BLK
            for j in range(4):
                pb = ps()
                proj_fm(slot, 0, 512, j * 128, tok0, BLK, pb)
                ch = g * 4 + j
                conv_stream(ph, pb, j, b, lambda k, ch=ch: cwc.v(l * 48 + ch * 4 + k, [[1, 1]]), fx.v(j * BLK, [[1, BLK]]), fx.key,
                            bias_ap=cbc.v(l * 12 + ch, [[1, 1]]))
            if SSTOP <= 0.1:
                continue
            for j, (woff, ch, dst) in enumerate([(4096, 8 + g, fmk), (5120, 10 + g, fmq)]):
                pb = ps()
                proj_fm(slot, woff, 128, 0, tok0, BLK, pb)
                conv_stream(ph, pb, 4 + j, b, lambda k, ch=ch: cwc.v(l * 48 + ch * 4 + k, [[1, 1]]), dst.v(0, [[1, BLK]]), dst.key,
                            bias_ap=cbc.v(l * 12 + ch, [[1, 1]]))
            if SSTOP <= 0.2:
                continue
            for c in range(CPB):
                pZ = ps()
                proj_tm(zslot, 0, 512, 0, 512, tok0 + c * 64, pZ)
                P.op("act", lambda e, pZ=pZ, c=c: e.activation(out=szb.v(c * 512, [[1, 512]], Pn=64), in_=pZ.v(0, [[1, 512]], Pn=64), func=AF.Silu),
                     reads=[pZ.key], writes=[szb.key])
            pd = ps()
            for c in range(CPB):
                proj_tm(slot, 6144, 8, 0, 8, tok0 + c * 64, pd, pcol=c * 8)
            P.op("dve", lambda e, pd=pd: e.tensor_tensor(out=dt_t.v(0, [[8, CPB], [1, 8]], Pn=64), in0=pd.v(0, [[8, CPB], [1, 8]], Pn=64),
                                                         in1=pc.v(l * 48 + g * 8, [[0, CPB], [1, 8]], Pn=64), op=ALU.add),
                 reads=[pd.key, "pc"], writes=[dt_t.key])
            P.op("act", lambda e: e.activation(out=dt_t.v(0, [[1, CPB * 8]], Pn=64), in_=dt_t.v(0, [[1, CPB * 8]], Pn=64), func=AF.Exp),
                 reads=[dt_t.key], writes=[dt_t.key])
            P.op("act", lambda e: e.activation(out=dt_t.v(0, [[1, CPB * 8]], Pn=64), in_=dt_t.v(0, [[1, CPB * 8]], Pn=64), func=AF.Ln,
                                               bias=one_ap[0:64, :]), reads=[dt_t.key, "epst"], writes=[dt_t.key])
            P.op("dve", lambda e: e.tensor_tensor(out=dta.v(0, [[8, CPB], [1, 8]], Pn=64), in0=dt_t.v(0, [[8, CPB], [1, 8]], Pn=64),
                                                  in1=nac.v(l * 16 + g * 8, [[0, CPB], [1, 8]], Pn=64), op=ALU.mult),
                 reads=[dt_t.key, "nac"], writes=[dta.key])
            if SSTOP <= 0.3:
                continue
            pl = ps()
            P.op("pe", lambda e, pl=pl: e.matmul(pl.v(0, [[1, CPB * 8]], Pn=64), lhsT=c64("tri"), rhs=dta.v(0, [[1, CPB * 8]], Pn=64),
                                                 start=True, stop=True), reads=[dta.key, "C64"], writes=[pl.key])
            P.op("pe", lambda e, pl=pl: e.matmul(pl.v(128, [[1, CPB * 8]]), lhsT=c64("ones"), rhs=dta.v(0, [[1, CPB * 8]], Pn=64),
                                                 start=True, stop=True), reads=[dta.key, "C64"], writes=[pl.key])
            P.op("dve", lambda e, pl=pl: e.tensor_copy(out=lc.v(0, [[1, CPB * 8]], Pn=64), in_=pl.v(0, [[1, CPB * 8]], Pn=64)),
                 reads=[pl.key], writes=[lc.key])
            if SSTOP <= 0.45:
                continue
            P.op("dve", lambda e, pl=pl: e.tensor_copy(out=decb.v(0, [[1, CPB * 8]]), in_=pl.v(128, [[1, CPB * 8]])),
                 reads=[pl.key], writes=[decb.key])
            P.op("act", lambda e: e.activation(out=decb.v(0, [[1, CPB * 8]]), in_=decb.v(0, [[1, CPB * 8]]), func=AF.Exp),
                 reads=[decb.key], writes=[decb.key])
            if SSTOP <= 0.5:
                continue
            P.op("dve", lambda e, pl=pl: e.tensor_tensor(out=wr_t.v(0, [[1, CPB * 8]], Pn=64), in0=pl.v(128, [[1, CPB * 8]], Pn=64),
                                                         in1=lc.v(0, [[1, CPB * 8]], Pn=64), op=ALU.subtract), reads=[pl.key, lc.key], writes=[wr_t.key])
            P.op("act", lambda e: e.activation(out=wr_t.v(0, [[1, CPB * 8]], Pn=64), in_=wr_t.v(0, [[1, CPB * 8]], Pn=64), func=AF.Exp),
                 reads=[wr_t.key], writes=[wr_t.key])
            P.op("dve", lambda e: e.tensor_tensor(out=wr_t.v(0, [[1, CPB * 8]], Pn=64), in0=wr_t.v(0, [[1, CPB * 8]], Pn=64),
                                                  in1=dt_t.v(0, [[1, CPB * 8]], Pn=64), op=ALU.mult), reads=[wr_t.key, dt_t.key], writes=[wr_t.key])
            P.op("act", lambda e: e.activation(out=elc.v(0, [[1, CPB * 8]], Pn=64), in_=lc.v(0, [[1, CPB * 8]], Pn=64), func=AF.Exp),
                 reads=[lc.key], writes=[elc.key])
            if SSTOP <= 1:
                continue
            if b == 0:
                P.op("dve", lambda e: e.memset(Sc.v(0, [[1, 512]]), 0.0), reads=[Sc.key], writes=[Sc.key])
            def stage1(c):
                    ct = tok0 + c * 64
                    db, e1, x_tm, xw, yi, yg, cbT, B_tm, st2 = [S_.t[n + str(c % 2)] for n in ['db', 'e1', 'x_tm', 'xw', 'yi', 'yg', 'cbT', 'B_tm', 'st2']]
                    P.op("dve", lambda e, c=c: e.tensor_copy(out=db.v(0, [[64, 8], [1, 64]], Pn=64), in_=dta.v(c * 8, [[1, 8], [0, 64]], Pn=64)),
                         reads=[dta.key], writes=[db.key])
                    pL = ps()
                    for hh in range(8):
                        P.op("pe", lambda e, hh=hh, pL=pL: e.matmul(pL.v(hh * 64, [[1, 64]], Pn=64), lhsT=db.v(hh * 64, [[1, 64]], Pn=64),
                                                                    rhs=c64("tri"), start=True, stop=True), reads=[db.key, "C64"], writes=[pL.key])
                    P.op("dve", lambda e, c=c, pL=pL: e.tensor_tensor(out=e1.v(0, [[64, 8], [1, 64]], Pn=64), in0=pL.v(0, [[64, 8], [1, 64]], Pn=64),
                                                                      in1=lc.v(c * 8, [[1, 8], [0, 64]], Pn=64), op=ALU.subtract),
                         reads=[pL.key, lc.key], writes=[e1.key])
                    P.op("dve", lambda e: e.scalar_tensor_tensor(out=e1.v(0, [[1, 512]], Pn=64), in0=e1.v(0, [[1, 512]], Pn=64), scalar=-1.0,
                                                                 in1=e1.v(0, [[1, 512]], Pn=64), op0=ALU.mult, op1=ALU.max),
                         reads=[e1.key], writes=[e1.key])
                    P.op("act", lambda e: e.activation(out=e1.v(0, [[1, 512]], Pn=64), in_=e1.v(0, [[1, 512]], Pn=64), func=AF.Exp, scale=-1.0),
                         reads=[e1.key], writes=[e1.key])
                    if SSTOP <= 2:
                        return
                    pc_ = ps()
                    P.op("pe", lambda e, c=c, pc_=pc_: e.matmul(pc_.v(0, [[1, 64]], Pn=64), lhsT=fmk.v(c * 64, [[1, 64]]), rhs=fmq.v(c * 64, [[1, 64]]),
                                                                start=True, stop=True), reads=[fmk.key, fmq.key], writes=[pc_.key])
                    P.op("act", lambda e, pc_=pc_: e.activation(out=cbT.v(0, [[1, 64]], Pn=64), in_=pc_.v(0, [[1, 64]], Pn=64), func=AF.Copy),
                         reads=[pc_.key], writes=[cbT.key])
                    P.op("dve", lambda e: e.tensor_tensor(out=e1.v(0, [[64, 8], [1, 64]], Pn=64), in0=e1.v(0, [[64, 8], [1, 64]], Pn=64),
                                                          in1=cbT.v(0, [[0, 8], [1, 64]], Pn=64), op=ALU.mult), reads=[e1.key, cbT.key], writes=[e1.key])
                    P.op("dve", lambda e, c=c: e.tensor_tensor(out=e1.v(0, [[64, 8], [1, 64]], Pn=64), in0=e1.v(0, [[64, 8], [1, 64]], Pn=64),
                                                               in1=dt_t.v(c * 8, [[1, 8], [0, 64]], Pn=64), op=ALU.mult), reads=[e1.key, dt_t.key], writes=[e1.key])
                    P.op("dve", lambda e: e.tensor_tensor(out=e1.v(0, [[1, 512]], Pn=64), in0=e1.v(0, [[1, 512]], Pn=64),
                                                          in1=dsk.v(0, [[1, 512]], Pn=64), op=ALU.add), reads=[e1.key, dsk.key], writes=[e1.key])
                    if SSTOP <= 3:
                        return
                    pX = ps()
                    for j in range(4):
                        P.op("pe", lambda e, c=c, j=j, pX=pX: e.transpose(pX.v(j * 128, [[1, 128]], Pn=64), fx.v(j * BLK + c * 64, [[1, 64]]), c128("id")),
                             reads=[fx.key, "C128"], writes=[pX.key])
                    P.op("act", lambda e, pX=pX: e.activation(out=x_tm.v(0, [[1, 512]], Pn=64), in_=pX.v(0, [[1, 512]], Pn=64), func=AF.Copy),
                         reads=[pX.key], writes=[x_tm.key])
                    P.op("dve", lambda e, c=c, pX=pX: e.tensor_tensor(out=xw.v(0, [[64, 8], [1, 64]], Pn=64), in0=pX.v(0, [[64, 8], [1, 64]], Pn=64),
                                                                      in1=wr_t.v(c * 8, [[1, 8], [0, 64]], Pn=64), op=ALU.mult),
                         reads=[pX.key, wr_t.key], writes=[xw.key])
                    pBt = ps()
                    P.op("pe", lambda e, c=c, pBt=pBt: e.transpose(pBt.v(0, [[1, 128]], Pn=64), fmk.v(c * 64, [[1, 64]]), c128("id")),
                         reads=[fmk.key, "C128"], writes=[pBt.key])
                    P.op("act", lambda e, pBt=pBt: e.activation(out=B_tm.v(0, [[1, 128]], Pn=64), in_=pBt.v(0, [[1, 128]], Pn=64), func=AF.Copy),
                         reads=[pBt.key], writes=[B_tm.key])
                    pI = ps()
                    for hh in range(8):
                        P.op("pe", lambda e, hh=hh, pI=pI: e.matmul(pI.v(hh * 64, [[1, 64]], Pn=64), lhsT=e1.v(hh * 64, [[1, 64]], Pn=64),
                                                                    rhs=x_tm.v(hh * 64, [[1, 64]], Pn=64), start=True, stop=True),
                             reads=[e1.key, x_tm.key], writes=[pI.key])
                    pN = ps()
                    for qq in range(4):
                        P.op("pe", lambda e, c=c, pN=pN, qq=qq: e.matmul(pN.v(qq * 128, [[1, 128]], Pn=64), lhsT=fmq.v(c * 64, [[1, 64]]),
                                                                         rhs=Sc.v(qq * 128, [[1, 128]]), start=True, stop=True),
                             reads=[fmq.key, Sc.key], writes=[pN.key])
                    P.op("act", lambda e, pI=pI: e.activation(out=yi.v(0, [[1, 512]], Pn=64), in_=pI.v(0, [[1, 512]], Pn=64), func=AF.Copy),
                         reads=[pI.key], writes=[yi.key])
                    P.op("dve", lambda e, c=c, pN=pN: e.tensor_tensor(out=yg.v(0, [[64, 8], [1, 64]], Pn=64), in0=pN.v(0, [[64, 8], [1, 64]], Pn=64),
                                                                      in1=elc.v(c * 8, [[1, 8], [0, 64]], Pn=64), op=ALU.mult),
                         reads=[pN.key, elc.key], writes=[yg.key])
                    P.op("dve", lambda e: e.tensor_tensor(out=yg.v(0, [[1, 512]], Pn=64), in0=yg.v(0, [[1, 512]], Pn=64), in1=yi.v(0, [[1, 512]], Pn=64),
                                                          op=ALU.add), reads=[yg.key, yi.key], writes=[yg.key])
                    if SSTOP <= 4:
                        return
                    pS_ = ps()
                    for qq in range(4):
                        P.op("pe", lambda e, pS_=pS_, qq=qq: e.matmul(pS_.v(qq * 128, [[1, 128]]), lhsT=B_tm.v(0, [[1, 128]], Pn=64),
                                                                      rhs=xw.v(qq * 128, [[1, 128]], Pn=64), start=True, stop=True),
                             reads=[B_tm.key, xw.key], writes=[pS_.key])
                    P.op("dve", lambda e, c=c: e.tensor_tensor(out=Sc.v(0, [[64, 8], [1, 64]]), in0=Sc.v(0, [[64, 8], [1, 64]]),
                                                               in1=decb.v(c * 8, [[1, 8], [0, 64]]), op=ALU.mult), reads=[Sc.key, decb.key], writes=[Sc.key])
                    P.op("dve", lambda e, pS_=pS_: e.tensor_tensor(out=Sc.v(0, [[1, 512]]), in0=Sc.v(0, [[1, 512]]), in1=pS_.v(0, [[1, 512]]), op=ALU.add),
                         reads=[Sc.key, pS_.key], writes=[Sc.key])

            def stage2(c):
                    ct = tok0 + c * 64
                    db, e1, x_tm, xw, yi, yg, cbT, B_tm, st2 = [S_.t[n + str(c % 2)] for n in ['db', 'e1', 'x_tm', 'xw', 'yi', 'yg', 'cbT', 'B_tm', 'st2']]
                    P.op("dve", lambda e: e.tensor_tensor(out=yg.v(0, [[1, 512]], Pn=64), in0=yg.v(0, [[1, 512]], Pn=64), in1=szb.v(c * 512, [[1, 512]], Pn=64),
                                                          op=ALU.mult), reads=[yg.key, szb.key], writes=[yg.key])
                    P.op("act", lambda e: e.activation(out=yi.v(0, [[1, 512]], Pn=64), in_=yg.v(0, [[1, 512]], Pn=64), func=AF.Square),
                         reads=[yg.key], writes=[yi.key])
                    P.op("dve", lambda e: e.tensor_reduce(out=st2.v(0, [[1, 1]], Pn=64), in_=yi.v(0, [[1, 512]], Pn=64), axis=AX.X, op=ALU.add),
                         reads=[yi.key], writes=[st2.key])
                    P.op("act", lambda e: e.activation(out=st2.v(0, [[1, 1]], Pn=64), in_=st2.v(0, [[1, 1]], Pn=64), func=AF.Ln,
                                                       bias=eps_ap[0:64, :], scale=1.0 / 512), reads=[st2.key, "epst"], writes=[st2.key])
                    P.op("act", lambda e: e.activation(out=st2.v(0, [[1, 1]], Pn=64), in_=st2.v(0, [[1, 1]], Pn=64), func=AF.Exp, scale=-0.5),
                         reads=[st2.key], writes=[st2.key])
                    P.op("dve", lambda e: e.scalar_tensor_tensor(out=yg.v(0, [[1, 512]], Pn=64), in0=yg.v(0, [[1, 512]], Pn=64), scalar=st2.v(0, [[1, 1]], Pn=64),
                                                                 in1=nrmc.v(0, [[1, 512]], Pn=64), op0=ALU.mult, op1=ALU.mult),
                         reads=[yg.key, st2.key, nrmc.key], writes=[yg.key])
                    pY = ps()
                    for j in range(4):
                        P.op("pe", lambda e, j=j, pY=pY: e.transpose(pY.v(j * 64, [[1, 64]]), yg.v(j * 128, [[1, 128]], Pn=64), c64("id")),
                             reads=[yg.key, "C64"], writes=[pY.key])
                    P.op("act", lambda e, c=c, pY=pY: e.activation(out=ytb.v(c * 64, [[BLK, 4], [1, 64]]), in_=pY.v(0, [[64, 4], [1, 64]]), func=AF.Copy),
                         reads=[pY.key], writes=[ytb.key])

            stage1(0)
            for c in range(CPB):
                A = P.capture(lambda: with_banks([0, 1, 2], lambda: stage2(c)))
                B = P.capture(lambda: with_banks([3, 4, 5, 6, 7], lambda: stage1(c + 1))) if c + 1 < CPB else []
                P.merge(A, B)
            store_yT(ytb, 8 + g * 4, 4, tok0, s)

    def mrg_load(slot, l, dc):
        base = l * D * P_IN
        for br in range(3):
            ld_w(slot, br * 1024, 128, dr["w_in"], base + O_GATE + br * 1024 + dc * 128, P_IN)
        ld_w(slot, 3072, 128, dr["w_branch_a"], l * 512 * D + dc * 128, D, nk=4)
        ld_w(slot, 3072 + 512, 128, dr["w_branch_b"], l * 512 * D + dc * 128, D, nk=4)
        ld_w(slot, 3072 + 1024, 128, dr["w_branch_c"], l * 1024 * D + dc * 128, D, nk=8)

    def mrg_run(slot, l, dc, s):
        enter(M_)
        ytl, tA, tB = [M_.t[n] for n in ['ytl', 'tA', 'tB']]
        for b in range(NBLK):
            tok0 = b * BLK
            P.dma("sp", lambda e, tok0=tok0: e.dma_start(out=ytl.v(0, [[BLK, 16], [1, BLK]]),
                                                         in_=dv(ytd, (s % 2) * 128 * 16 * T + tok0, [[16 * T, 128], [T, 16], [1, BLK]])),
                  reads=["ytd%d_%d_%d" % (s % 2, i, b) for i in range(16)], writes=[ytl.key], chan=ytl.key)
            first = True
            for br, (f0, nf, woff) in enumerate([(0, 4, 3072), (4, 4, 3072 + 512), (8, 8, 3072 + 1024)]):
                pg = ps()
                proj_fm(slot, br * 1024, 128, 0, tok0, BLK, pg)
                P.op("act", lambda e, pg=pg, br=br: e.activation(out=tA.v(0, [[1, BLK]]), in_=pg.v(0, [[1, BLK]]), func=AF.Sigmoid,
                                                                 bias=bgt.v(l * 24 + br * 8 + dc, [[1, 1]])), reads=[pg.key, "bgt"], writes=[tA.key])
                pb = ps()
                for k in range(nf):
                    P.op("pe", lambda e, k=k, pb=pb, woff=woff, f0=f0, nf=nf: e.matmul(pb.v(0, [[1, BLK]]), lhsT=WB[slot].v(woff + k * 128, [[1, 128]]),
                                                                                       rhs=ytl.v((f0 + k) * BLK, [[1, BLK]]), start=(k == 0), stop=(k == nf - 1)),
                         reads=["WB%d" % slot, ytl.key], writes=[pb.key])
                if first:
                    P.op("dve", lambda e, pb=pb: e.tensor_tensor(out=tB.v(0, [[1, BLK]]), in0=pb.v(0, [[1, BLK]]), in1=tA.v(0, [[1, BLK]]), op=ALU.mult),
                         reads=[pb.key, tA.key], writes=[tB.key])
                    first = False
                else:
                    P.op("dve", lambda e, pb=pb: e.tensor_tensor(out=tA.v(0, [[1, BLK]]), in0=pb.v(0, [[1, BLK]]), in1=tA.v(0, [[1, BLK]]), op=ALU.mult),
                         reads=[pb.key, tA.key], writes=[tA.key])
                    if br == 1:
                        P.op("dve", lambda e: e.tensor_tensor(out=tB.v(0, [[1, BLK]]), in0=tB.v(0, [[1, BLK]]), in1=tA.v(0, [[1, BLK]]), op=ALU.add),
                             reads=[tA.key, tB.key], writes=[tB.key])
                    else:
                        P.op("dve", lambda e, tok0=tok0: e.tensor_tensor(out=mrgT.v(dc * T + tok0, [[1, BLK]]), in0=tB.v(0, [[1, BLK]]),
                                                                         in1=tA.v(0, [[1, BLK]]), op=ALU.add),
                             reads=[tA.key, tB.key], writes=["mrg%d" % b])


    def ln_tile(ph, src_ap, src_keys, l, which, tile, s, final, do_router):
        par = str(tile % 2)
        lng, lnb = ph.t['lng'], ph.t['lnb']
        xn, bst, mv = ph.t['xn' + par], ph.t['bst' + par], ph.t['mv' + par]
        xTf = ph.t.get('xTf' + par)
        if WSTOP <= 1:
            return
        for hh in range(2):
            P.op("dve", lambda e, hh=hh: e.bn_stats(out=bst.v(hh * 6, [[1, 6]]), in_=src_ap[:, hh * 512:(hh + 1) * 512]), reads=src_keys, writes=[bst.key])
        P.op("dve", lambda e: e.bn_aggr(out=mv.v(0, [[1, 2]]), in_=bst.v(0, [[1, 12]])), reads=[bst.key], writes=[mv.key])
        P.op("act", lambda e: e.activation(out=mv.v(2, [[1, 1]]), in_=mv.v(1, [[1, 1]]), func=AF.Ln, bias=eps_ap, scale=1.0), reads=[mv.key, "epst"], writes=[mv.key])
        P.op("act", lambda e: e.activation(out=mv.v(2, [[1, 1]]), in_=mv.v(2, [[1, 1]]), func=AF.Exp, scale=-0.5), reads=[mv.key], writes=[mv.key])
        P.op("dve", lambda e: e.tensor_scalar(out=xn.v(0, [[1, 1024]]), in0=src_ap, scalar1=mv.v(0, [[1, 1]]), scalar2=mv.v(2, [[1, 1]]),
                                              op0=ALU.subtract, op1=ALU.mult), reads=list(src_keys) + [mv.key], writes=[xn.key])
        P.op("dve", lambda e: e.tensor_tensor(out=xn.v(0, [[1, 1024]]), in0=xn.v(0, [[1, 1024]]), in1=lng.v(0, [[1, 1024]]), op=ALU.mult),
             reads=[xn.key, lng.key], writes=[xn.key])
        P.op("dve", lambda e: e.tensor_tensor(out=xn.v(0, [[1, 1024]]), in0=xn.v(0, [[1, 1024]]), in1=lnb.v(0, [[1, 1024]]), op=ALU.add),
             reads=[xn.key, lnb.key], writes=[xn.key])
        if WSTOP <= 2:
            return
        if final:
            o = P.dma("sp", lambda e: e.dma_start(out=dv(yout, (s * T + tile * 128) * D, [[D, 128], [1, D]]), in_=xn.v(0, [[1, 1024]])),
                      reads=[xn.key], writes=["yout%d_%d" % (s, tile)], chan="out" + par)
            outs.append(o)
        else:
            P.dma("sp", lambda e: e.dma_start(out=dv(xres[which], tile * 128 * D, [[D, 128], [1, D]]), in_=xn.v(0, [[1, 1024]])),
                  reads=[xn.key], writes=["xres%d_p%s" % (which, par)], chan="xr%d_%s" % (which, par))
            if WSTOP <= 2.5:
                return
            for half in range(2):
                pt = ps()
                for k in range(4):
                    kc = half * 4 + k
                    P.op("pe", lambda e, k=k, kc=kc, pt=pt: e.transpose(pt.v(k * 128, [[1, 128]]), xn.v(kc * 128, [[1, 128]]), c128("id")),
                         reads=[xn.key, "C128"], writes=[pt.key])
                P.op("act", lambda e, half=half, pt=pt: e.activation(out=xT.v(half * 4 * T + tile * 128, [[T, 4], [1, 128]]), in_=pt.v(0, [[128, 4], [1, 128]]),
                                                                     func=AF.Copy), reads=[pt.key], writes=["xT%d" % tile])
                if do_router and WSTOP > 2.7:
                    P.op("dve", lambda e, half=half, pt=pt: e.tensor_copy(out=xTf.v(half * 512, [[1, 512]]), in_=pt.v(0, [[1, 512]])),
                         reads=[pt.key], writes=[xTf.key])
            if do_router and WSTOP > 3:
                router(ph, l, tile)

    outs = []

    def router(ph, l, tile):
        xTf, rt = ph.t['xTf' + str(tile % 2)], ph.t['rt' + str(tile % 2)]
        pr = ps()
        for kc in range(KC):
            P.op("pe", lambda e, kc=kc, pr=pr: e.matmul(pr.v(0, [[1, 36]]), lhsT=xTf.v(kc * 128, [[1, 128]]), rhs=wr.v(l * KC * 36 + kc * 36, [[1, 36]]),
                                                        start=(kc == 0), stop=(kc == KC - 1)), reads=[xTf.key, "wr"], writes=[pr.key])
        R = lambda o, n: rt.v(o, [[1, n]])
        P.op("dve", lambda e: e.tensor_tensor(out=R(0, 36), in0=pr.v(0, [[1, 36]]), in1=brt.v(l * 36, [[1, 36]]), op=ALU.add), reads=[pr.key, "brt"], writes=[rt.key])
        k = [rt.key]
        P.op("dve", lambda e: e.tensor_reduce(out=R(36, 1), in_=R(0, 4), axis=AX.X, op=ALU.max), reads=k, writes=k)
        P.op("dve", lambda e: e.tensor_scalar(out=R(37, 4), in0=R(0, 4), scalar1=R(36, 1), scalar2=None, op0=ALU.is_ge), reads=k, writes=k)
        P.op("dve", lambda e: e.tensor_scalar(out=R(41, 4), in0=R(0, 4), scalar1=R(36, 1), scalar2=None, op0=ALU.subtract), reads=k, writes=k)
        P.op("act", lambda e: e.activation(out=R(41, 4), in_=R(41, 4), func=AF.Exp), reads=k, writes=k)
        P.op("dve", lambda e: e.tensor_reduce(out=R(45, 1), in_=R(41, 4), axis=AX.X, op=ALU.add), reads=k, writes=k)
        P.op("dve", lambda e: e.reciprocal(out=R(46, 1), in_=R(45, 1)), reads=k, writes=k)
        P.op("dve", lambda e: e.tensor_scalar(out=R(41, 4), in0=R(37, 4), scalar1=-1.0, scalar2=BIGM, op0=ALU.add, op1=ALU.mult), reads=k, writes=k)
        P.op("dve", lambda e: e.tensor_tensor(out=rt.v(48, [[8, 4], [1, 8]]), in0=rt.v(4, [[8, 4], [1, 8]]), in1=rt.v(41, [[1, 4], [0, 8]]), op=ALU.add),
             reads=k, writes=k)
        P.op("dve", lambda e: e.tensor_reduce(out=R(80, 1), in_=R(48, 32), axis=AX.X, op=ALU.max), reads=k, writes=k)
        P.op("dve", lambda e: e.tensor_scalar(out=R(82, 32), in0=R(48, 32), scalar1=R(80, 1), scalar2=None, op0=ALU.is_ge), reads=k, writes=k)
        P.op("dve", lambda e: e.scalar_tensor_tensor(out=R(48, 32), in0=R(82, 32), scalar=-BIGM, in1=R(48, 32), op0=ALU.mult, op1=ALU.add), reads=k, writes=k)
        P.op("dve", lambda e: e.tensor_reduce(out=R(81, 1), in_=R(48, 32), axis=AX.X, op=ALU.max), reads=k, writes=k)
        P.op("dve", lambda e: e.tensor_scalar(out=R(114, 32), in0=R(48, 32), scalar1=R(81, 1), scalar2=None, op0=ALU.is_ge), reads=k, writes=k)
        P.op("dve", lambda e: e.tensor_tensor(out=R(146, 1), in0=R(81, 1), in1=R(80, 1), op=ALU.subtract), reads=k, writes=k)
        P.op("act", lambda e: e.activation(out=R(146, 1), in_=R(146, 1), func=AF.Exp), reads=k, writes=k)
        P.op("dve", lambda e: e.tensor_scalar(out=R(146, 1), in0=R(146, 1), scalar1=1.0, scalar2=None, op0=ALU.add), reads=k, writes=k)
        P.op("dve", lambda e: e.reciprocal(out=R(146, 1), in_=R(146, 1)), reads=k, writes=k)
        P.op("dve", lambda e: e.tensor_tensor(out=R(146, 1), in0=R(146, 1), in1=R(46, 1), op=ALU.mult), reads=k, writes=k)
        P.op("dve", lambda e: e.tensor_tensor(out=R(147, 1), in0=R(46, 1), in1=R(146, 1), op=ALU.subtract), reads=k, writes=k)
        P.op("dve", lambda e: e.tensor_scalar(out=R(82, 32), in0=R(82, 32), scalar1=R(146, 1), scalar2=None, op0=ALU.mult), reads=k, writes=k)
        P.op("dve", lambda e: e.scalar_tensor_tensor(out=comb.v(tile * 32, [[1, 32]]), in0=R(114, 32), scalar=R(147, 1), in1=R(82, 32),
                                                     op0=ALU.mult, op1=ALU.add), reads=k, writes=["comb%d" % tile])

    def load_ln(ph, l, which):
        lng, lnb = ph.t['lng'], ph.t['lnb']
        g, b_ = ("ln1_g", "ln1_b") if which == 1 else ("ln2_g", "ln2_b")
        P.dma("sp", lambda e: e.dma_start(out=lng.v(0, [[1, 1024]]), in_=dv(dr[g], l * D, [[0, 128], [1, D]])), writes=[lng.key], chan="lng")
        P.dma("sp", lambda e: e.dma_start(out=lnb.v(0, [[1, 1024]]), in_=dv(dr[b_], l * D, [[0, 128], [1, D]])), writes=[lnb.key], chan="lnb")

    def wout_load1(slot, l):
        ld_w(slot, 0, 512, dr["w_out"], l * D * D, D)

    def wout_load2(slot, l):
        ld_w(slot, 0, 512, dr["w_out"], l * D * D + 512, D)

    def wout_run(slot, l, s, src_dram_ap_fn, src_keys_fn):
        enter(W_)
        xl = [W_.t["xl0"], W_.t["xl1"]]
        slots = [1 - slot, slot]
        load_ln(W_, l, 1)
        for tile in range(NT):
            xo = xl[tile % 2]
            P.dma("sp", lambda e, tile=tile, xo=xo: e.dma_start(out=xo.v(0, [[1, 1024]]), in_=src_dram_ap_fn(tile)), reads=src_keys_fn(tile),
                  writes=[xo.key], chan="xl%d" % (tile % 2))
            for half in range(2):
                pm = ps()
                for kc in range(KC):
                    P.op("pe", lambda e, kc=kc, pm=pm, half=half, tile=tile: e.matmul(pm.v(0, [[1, 512]]), lhsT=mrgT.v(kc * T + tile * 128, [[1, 128]]),
                                                                                      rhs=WB[slots[half]].v(kc * 512, [[1, 512]]),
                                                                                      start=(kc == 0), stop=(kc == KC - 1)),
                         reads=["WB%d" % slots[half], "mrg%d" % (tile * 128 // BLK)], writes=[pm.key])
                P.op("dve", lambda e, pm=pm, half=half, xo=xo: e.scalar_tensor_tensor(out=xo.v(half * 512, [[1, 512]]), in0=xo.v(half * 512, [[1, 512]]),
                                                                                      scalar=ALPHA, in1=pm.v(0, [[1, 512]]), op0=ALU.mult, op1=ALU.add),
                     reads=[xo.key, pm.key], writes=[xo.key])
            ln_tile(W_, xo.v(0, [[1, 1024]]), [xo.key], l, 0, tile, s, False, True)

    def moe_load(slot, l, e_, hf):
        ld_w(slot, 0, 256, dr["w_gate_e"], ((l * NE + e_) * D) * DE + hf * 256, DE)
        ld_w(slot, 2048, 256, dr["w_up_e"], ((l * NE + e_) * D) * DE + hf * 256, DE)
        ld_w(slot, 4096, 1024, dr["w_down_e"], ((l * NE + e_) * DE + hf * 256) * D, D, nk=2)

    def moe_init(l, s):
        enter(E_)
        for tile in range(NT):
            P.dma("sp", lambda e, tile=tile: e.dma_start(out=yacc.v(tile * 1024, [[1, 1024]]), in_=dv(xres[0], tile * 128 * D, [[D, 128], [1, D]])),
                  reads=["xres0_p0", "xres0_p1"], writes=["yacc%d" % tile], chan="ya%d" % tile)
            P.op("pool", lambda e, tile=tile: e.tensor_scalar(out=yacc.v(tile * 1024, [[1, 1024]]), in0=yacc.v(tile * 1024, [[1, 1024]]), scalar1=ALPHA,
                                                               scalar2=None, op0=ALU.mult), reads=["yacc%d" % tile], writes=["yacc%d" % tile])

    def moe_run(slot, l, e_, hf, s):
        wk = "WB%d" % slot
        Hh = [E_.t["Hh0"], E_.t["Hh1"]]
        hs = [E_.t["hs0"], E_.t["hs1"]]
        for b in range(NBLK):
            tok0 = b * BLK
            H = Hh[b % 2]
            for k2 in range(2):
                pg = ps()
                pu = ps()
                for kc in range(KC):
                    P.op("pe", lambda e, kc=kc, pg=pg, k2=k2, tok0=tok0: e.matmul(pg.v(0, [[1, BLK]]), lhsT=WB[slot].v(kc * 256 + k2 * 128, [[1, 128]]),
                                                                                  rhs=xT.v(kc * T + tok0, [[1, BLK]]), start=(kc == 0), stop=(kc == KC - 1)),
                         reads=[wk] + xt_keys(tok0, BLK), writes=[pg.key])
                for kc in range(KC):
                    P.op("pe", lambda e, kc=kc, pu=pu, k2=k2, tok0=tok0: e.matmul(pu.v(0, [[1, BLK]]), lhsT=WB[slot].v(2048 + kc * 256 + k2 * 128, [[1, 128]]),
                                                                                  rhs=xT.v(kc * T + tok0, [[1, BLK]]), start=(kc == 0), stop=(kc == KC - 1)),
                         reads=[wk] + xt_keys(tok0, BLK), writes=[pu.key])
                hsx = hs[k2]
                P.op("act", lambda e, pg=pg, hsx=hsx: e.activation(out=hsx.v(0, [[1, BLK]]), in_=pg.v(0, [[1, BLK]]), func=AF.Silu), reads=[pg.key], writes=[hsx.key])
                P.op("dve", lambda e, pu=pu, hsx=hsx, H=H, k2=k2: e.tensor_tensor(out=H.v(k2 * BLK, [[1, BLK]]), in0=hsx.v(0, [[1, BLK]]), in1=pu.v(0, [[1, BLK]]),
                                                                                  op=ALU.mult), reads=[pu.key, hsx.key], writes=[H.key])
            for t in range(TPB):
                tile = b * TPB + t
                for half in range(2):
                    py = ps()
                    for k2 in range(2):
                        P.op("pe", lambda e, k2=k2, py=py, t=t, half=half, H=H: e.matmul(py.v(0, [[1, 512]]), lhsT=H.v(k2 * BLK + t * 128, [[1, 128]]),
                                                                                         rhs=WB[slot].v(4096 + k2 * 1024 + half * 512, [[1, 512]]),
                                                                                         start=(k2 == 0), stop=(k2 == 1)), reads=[wk, H.key], writes=[py.key])
                    P.op("dve", lambda e, py=py, tile=tile, half=half: e.scalar_tensor_tensor(
                        out=yacc.v(tile * 1024 + half * 512, [[1, 512]]), in0=py.v(0, [[1, 512]]), scalar=comb.v(tile * 32 + e_, [[1, 1]]),
                        in1=yacc.v(tile * 1024 + half * 512, [[1, 512]]), op0=ALU.mult, op1=ALU.add),
                        reads=[py.key, "comb%d" % tile, "yacc%d" % tile], writes=["yacc%d" % tile])

    def ln2_run(l, s, final):
        enter(N_)
        load_ln(N_, l, 2)
        for tile in range(NT):
            ln_tile(N_, yacc.v(tile * 1024, [[1, 1024]]), ["yacc%d" % tile], l, 1, tile, s, final, False)

    def x0_run(s):
        enter(X_)
        xl = [X_.t["xl0"], X_.t["xl1"]]
        for tile in range(NT):
            xo = xl[tile % 2]
            P.dma("sp", lambda e, tile=tile, xo=xo: e.dma_start(out=xo.v(0, [[1, 1024]]), in_=dv(dr["x"], (s * T + tile * 128) * D, [[D, 128], [1, D]])),
                  writes=[xo.key], chan="xl%d" % (tile % 2))
            for half in range(2):
                pt = ps()
                for k in range(4):
                    kc = half * 4 + k
                    P.op("pe", lambda e, k=k, kc=kc, pt=pt, xo=xo: e.transpose(pt.v(k * 128, [[1, 128]]), xo.v(kc * 128, [[1, 128]]), c128("id")),
                         reads=[xo.key, "C128"], writes=[pt.key])
                P.op("act", lambda e, half=half, pt=pt, tile=tile: e.activation(out=xT.v(half * 4 * T + tile * 128, [[T, 4], [1, 128]]),
                                                                                in_=pt.v(0, [[128, 4], [1, 128]]), func=AF.Copy),
                     reads=[pt.key], writes=["xT%d" % tile])

    units = []
    for s in range(NSEQ):
        units.append((None, lambda slot, s=s: x0_run(s)))
        for l in range(L):
            for h in range(4):
                units.append((lambda slot, l=l, h=h: gdn_load(slot, l, h), lambda slot, l=l, h=h, s=s: gdn_run(slot, l, h, s)))
            for h in range(4):
                units.append((lambda slot, l=l, h=h: ret_load(slot, l, h), lambda slot, l=l, h=h, s=s: ret_run(slot, l, h, s)))
            for g in range(2):
                units.append((lambda slot, l=l, g=g: ssd_load1(slot, l, g), lambda slot: None))
                units.append((lambda slot, l=l, g=g: ssd_load2(slot, l, g), lambda slot, l=l, g=g, s=s: ssd_run(slot, l, g, s), True))
            for dc in range(8):
                units.append((lambda slot, l=l, dc=dc: mrg_load(slot, l, dc), lambda slot, l=l, dc=dc, s=s: mrg_run(slot, l, dc, s)))
            if l == 0:
                srcf = lambda tile, s=s: dv(dr["x"], (s * T + tile * 128) * D, [[D, 128], [1, D]])
                srck = lambda tile: []
            else:
                srcf = lambda tile: dv(xres[1], tile * 128 * D, [[D, 128], [1, D]])
                srck = lambda tile: ["xres1_p0", "xres1_p1"]
            units.append((lambda slot, l=l: wout_load1(slot, l), lambda slot: None))
            units.append((lambda slot, l=l: wout_load2(slot, l), lambda slot, l=l, s=s, srcf=srcf, srck=srck: wout_run(slot, l, s, srcf, srck), True))
            units.append((None, lambda slot, l=l, s=s: moe_init(l, s)))
            for e_ in range(NE):
                for hf in range(2):
                    units.append((lambda slot, l=l, e_=e_, hf=hf: moe_load(slot, l, e_, hf),
                                  lambda slot, l=l, e_=e_, hf=hf, s=s: moe_run(slot, l, e_, hf, s)))
            units.append((None, lambda slot, l=l, s=s: ln2_run(l, s, l == L - 1)))

    wl = [u for u in units if u[0] is not None]
    slot_of = {}
    k = 0
    for i, u in enumerate(units):
        if u[0] is not None:
            slot_of[i] = k % 2
            k += 1
    loaded = set()
    idxs = [i for i, u in enumerate(units) if u[0] is not None]

    def ensure_loaded(i):
        if i not in loaded:
            units[i][0](slot_of[i])
            loaded.add(i)

    kstop = int(os.environ.get("KSTOP", "100000"))
    for i, u in enumerate(units):
        if i >= kstop:
            break
        if u[0] is not None:
            ensure_loaded(i)
            nxt = [j for j in idxs if j > i]
            both = len(u) > 2
            if nxt and not both:
                ensure_loaded(nxt[0])
            u[1](slot_of[i])
            if nxt and both:
                ensure_loaded(nxt[0])
        else:
            u[1](None)

    P.emit(final_wait_ops=outs)
    st.close()
    return nc, (a64, a128, cs_np)


_CACHE = {}


def kernel(**inputs):
    NCORES = 8
    x = np.ascontiguousarray(inputs["x"], dtype=np.float32)
    Bt, T, _ = x.shape
    NSEQ = Bt // NCORES
    DEPTH = inputs["w_in"].shape[0]
    key = (NSEQ, T, DEPTH)
    if key not in _CACHE:
        _CACHE[key] = build(NSEQ, T, DEPTH, 512)
    nc, (a64, a128, cs_np) = _CACHE[key]
    shared = {k: np.ascontiguousarray(v, dtype=np.float32) for k, v in inputs.items() if k != "x"}
    shared["c64"] = a64
    shared["c128"] = a128
    shared["cs"] = cs_np
    in_maps = []
    for c in range(NCORES):
        m = dict(shared)
        m["x"] = np.ascontiguousarray(x[c * NSEQ:(c + 1) * NSEQ])
        in_maps.append(m)
    res = run_bass_kernel_spmd(nc, in_maps, core_ids=list(range(NCORES)))
    return np.concatenate([r["y"] for r in res.results], axis=0).astype(np.float32)
```

```python
import contextlib
import numpy as np
import concourse.bass as bass
import concourse.mybir as mybir
from concourse.bass_utils import run_bass_kernel_spmd

F32 = mybir.dt.float32
BF16 = mybir.dt.bfloat16
AF = mybir.ActivationFunctionType
ALU = mybir.AluOpType
AX = mybir.AxisListType

D = 1024
KC = 8
P_IN = 9752
O_AQKV, O_AZ, O_AA, O_AB = 0, 1536, 2048, 2052
O_BQ, O_BK, O_BV, O_BG = 2056, 2568, 3080, 3592
O_CZ, O_CXBC, O_CDT, O_GATE = 4104, 5128, 6664, 6680
EPS = 1e-6
ALPHA = 4.0 ** 0.25
NE = 32
DE = 512
BIGM = 30000.0
ENGS = ("pe", "act", "dve", "pool", "sp")


class _Rec:
    def __getattr__(self, name):
        def f(*a, **k):
            self.__dict__["call"] = (name, a, k)
            return self
        return f


class Prog:
    def __init__(self, nc):
        self.nc = nc
        self.ops = []
        self.last_w = {}
        self.readers = {}
        self.cap = None

    def _add(self, eng, fn, reads, writes, chan=None):
        rec = _Rec()
        fn(rec)
        call = rec.call
        if self.cap is not None:
            self.cap.append((eng, call, reads, writes, chan))
            return -1
        return self._commit(eng, call, reads, writes, chan)

    def capture(self, f):
        old = self.cap
        self.cap = []
        f()
        lst = self.cap
        self.cap = old
        return lst

    def merge(self, A, B):
        ia = ib = 0
        na, nb = len(A), len(B)
        while ia < na or ib < nb:
            if ib >= nb or (ia < na and ia * nb <= ib * na):
                self._commit(*A[ia]); ia += 1
            else:
                self._commit(*B[ib]); ib += 1

    def _commit(self, eng, call, reads, writes, chan=None):
        if self.cap is not None:
            self.cap.append((eng, call, reads, writes, chan))
            return -1
        fn = lambda e, call=call: getattr(e, call[0])(*call[1], **call[2])
        idx = len(self.ops)
        deps = set()
        for r in reads:
            w = self.last_w.get(r)
            if w is not None:
                deps.add(w)
            if isinstance(r, str) and r.startswith("pb"):
                for rd in self.readers.get(r, ()):
                    if self.ops[rd]["eng"] != eng:
                        deps.add(rd)
        for r in writes:
            w = self.last_w.get(r)
            if w is not None and not (chan is not None and self.ops[w]["chan"] == chan and self.ops[w]["eng"] == eng):
                deps.add(w)
            for rd in self.readers.get(r, ()):
                deps.add(rd)
        for r in reads:
            self.readers.setdefault(r, []).append(idx)
        for r in writes:
            self.last_w[r] = idx
            self.readers[r] = []
        deps.discard(idx)
        self.ops.append(dict(eng=eng, fn=fn, deps=deps, chan=chan, has_dep=False))
        return idx

    def op(self, eng, fn, reads=(), writes=()):
        return self._add(eng, fn, tuple(reads), tuple(writes))

    def dma(self, eng, fn, reads=(), writes=(), chan="d0"):
        return self._add(eng, fn, tuple(reads), tuple(writes), chan=chan)

    def emit(self, final_wait_ops=()):
        nc = self.nc
        ops = self.ops
        for i, o in enumerate(ops):
            nd = set()
            for d in o["deps"]:
                p = ops[d]
                if p["chan"] is None and o["chan"] is None and p["eng"] == "pe" and o["eng"] == "pe":
                    continue
                nd.add(d)
            o["deps"] = nd
            for d in nd:
                ops[d]["has_dep"] = True
        for d in final_wait_ops:
            ops[d]["has_dep"] = True
        eng_cnt = {e: 0 for e in ENGS}
        chan_cnt = {}
        chans = []
        for o in ops:
            if o["chan"] is not None:
                c = o["chan"]
                if c not in chan_cnt:
                    chan_cnt[c] = 0
                    chans.append(c)
                chan_cnt[c] += 16
                o["tok"] = (("chan", c), chan_cnt[c])
            elif o["has_dep"]:
                eng_cnt[o["eng"]] += 1
                o["tok"] = (("eng", o["eng"]), eng_cnt[o["eng"]])
            else:
                o["tok"] = None
        sem_keys = [("eng", e) for e in ENGS] + [("chan", c) for c in chans]
        with contextlib.ExitStack() as st:
            sems = {}
            for k in sem_keys:
                sems[k] = st.enter_context(nc.semaphore("s_%s_%s" % k))
            blk = st.enter_context(nc.Block())

            def run_engine(eng_name, eng_obj):
                waited = {}
                for i, o in enumerate(ops):
                    if o["eng"] != eng_name:
                        continue
                    need = {}
                    for d in o["deps"]:
                        k, v = ops[d]["tok"]
                        if need.get(k, 0) < v:
                            need[k] = v
                    for k, v in need.items():
                        if waited.get(k, 0) >= v:
                            continue
                        eng_obj.wait_ge(sems[k], v)
                        waited[k] = v
                    ins = o["fn"](eng_obj)
                    if o["tok"] is not None:
                        k, v = o["tok"]
                        ins.then_inc(sems[k], 16 if k[0] == "chan" else 1)
                if eng_name == "sp":
                    need = {}
                    for d in final_wait_ops:
                        k, v = ops[d]["tok"]
                        if need.get(k, 0) < v:
                            need[k] = v
                    for k, v in need.items():
                        eng_obj.wait_ge(sems[k], v)

            blk.sync(lambda e: run_engine("sp", e))
            blk.tensor(lambda e: run_engine("pe", e))
            blk.scalar(lambda e: run_engine("act", e))
            blk.vector(lambda e: run_engine("dve", e))
            blk.gpsimd(lambda e: run_engine("pool", e))


def host_consts(T, BLK):
    c64 = {}
    i = np.arange(64)
    c64["tri"] = (i[:, None] <= i[None, :]).astype(np.float32)
    c64["id"] = np.eye(64, dtype=np.float32)
    c64["ones"] = np.ones((64, 128), np.float32)
    c64["bigm"] = np.where(i[None, :] < i[:, None], 0.0, BIGM).astype(np.float32)
    lg = np.log1p(-np.exp2(-5.0 - np.arange(4, dtype=np.float32))).astype(np.float32)
    idx = i.astype(np.float32)
    dec = np.exp(lg[:, None, None] * np.abs(idx[:, None] - idx[None, :])).astype(np.float32)
    c64["rdec"] = np.transpose(dec, (1, 0, 2)).reshape(64, 256)
    c64["wd"] = np.exp(lg[None, :] * (63.0 - idx[:, None])).astype(np.float32)
    names64 = ["tri", "id", "ones", "bigm", "rdec", "wd"]
    off64 = {}
    o = 0
    for n in names64:
        off64[n] = o
        o += c64[n].shape[1]
    a64 = np.concatenate([c64[n] for n in names64], axis=1).astype(np.float32)
    c128 = {}
    c128["id"] = np.eye(128, dtype=np.float32)
    c128["ones"] = np.ones((128, 128), np.float32)
    rot = np.zeros((128, 128), np.float32)
    for m in range(64):
        rot[m + 64, m] = -1.0
    for m in range(64, 128):
        rot[m - 64, m] = 1.0
    c128["rot"] = rot
    rd = np.exp(lg[:, None] * (idx[None, :] + 1.0)).astype(np.float32)
    rdt = np.tile(rd, (1, BLK // 64))
    c128["rd"] = np.broadcast_to(rdt.reshape(1, 4 * BLK), (128, 4 * BLK)).astype(np.float32)
    cd = np.exp(lg * 64.0).astype(np.float32)
    names128 = ["id", "ones", "rot", "rd"]
    off128 = {}
    o = 0
    for n in names128:
        off128[n] = o
        o += c128[n].shape[1]
    a128 = np.concatenate([c128[n] for n in names128], axis=1).astype(np.float32)
    pos = np.arange(T, dtype=np.float32)
    inv_freq = (np.float32(10000.0) ** (-np.arange(0, 128, 2, dtype=np.float32) / np.float32(128))).astype(np.float32)
    ang = (pos[:, None] * inv_freq[None, :]).astype(np.float32)
    cos = np.cos(ang).astype(np.float32).T
    sin = np.sin(ang).astype(np.float32).T
    cs = np.concatenate([np.concatenate([cos, cos], 0), np.concatenate([sin, sin], 0)], axis=1).astype(np.float32)
    return a64, off64, a128, off128, cs, [float(x) for x in cd]


def build(NSEQ, T, DEPTH, BLK):
    import os
    SSTOP = float(os.environ.get("SSTOP", "100"))
    WSTOP = float(os.environ.get("WSTOP", "100"))
    NT = T // 128
    NBLK = T // BLK
    CPB = BLK // 64
    TPB = BLK // 128
    a64, off64, a128, off128, cs_np, cdec = host_consts(T, BLK)
    nc = bass.Bass("TRN2", target_bir_lowering=False)
    dr = {}

    def din(name, shape):
        dr[name] = nc.dram_tensor(name, list(shape), F32, kind="ExternalInput")
        return dr[name]

    din("x", [NSEQ, T, D])
    L = DEPTH
    specs = dict(w_in=[L, D, P_IN], conv_a=[L, 4, 1536], a_log_a=[L, 4], dt_bias_a=[L, 4], norm_a=[L, 128],
                 norm_b=[L, 512], conv_c=[L, 4, 1536], conv_bias_c=[L, 1536], dt_bias_c=[L, 16], a_log_c=[L, 16],
                 d_skip_c=[L, 16], norm_c=[L, 1024], b_gate=[L, 3, D], w_branch_a=[L, 512, D],
                 w_branch_b=[L, 512, D], w_branch_c=[L, 1024, D], w_out=[L, D, D], ln1_g=[L, D], ln1_b=[L, D],
                 w_router_group=[L, D, 4], b_router_group=[L, 4], w_router_expert=[L, D, 32],
                 b_router_expert=[L, 32], w_gate_e=[L, NE, D, DE], w_up_e=[L, NE, D, DE], w_down_e=[L, NE, DE, D],
                 ln2_g=[L, D], ln2_b=[L, D])
    for k, v in specs.items():
        din(k, v)
    din("c64", list(a64.shape))
    din("c128", list(a128.shape))
    din("cs", list(cs_np.shape))
    yout = nc.dram_tensor("y", [NSEQ, T, D], F32, kind="ExternalOutput")
    xres = [nc.dram_tensor("xres%d" % i, [T, D], F32, kind="Internal") for i in range(2)]
    ytd = nc.dram_tensor("ytd", [2, 128, 16, T], BF16, kind="Internal")

    P = Prog(nc)
    st = contextlib.ExitStack()

    class Tl:
        def __init__(self, h, shape, key, base=0, pstride=None):
            self.h = h
            self.shape = shape
            self.key = key
            self.base = base
            self.row = int(np.prod(shape[1:])) if pstride is None else pstride

        def v(self, off, dims, Pn=128, p0=0):
            return bass.AP(self.h, self.base + off + p0 * self.row, [[self.row, Pn]] + [list(d) for d in dims])

    def sb(name, shape, dt=F32):
        h = st.enter_context(nc.sbuf_tensor(name, list(shape), dt))
        return Tl(h, list(shape), name)

    def dv(t, off, dims):
        return bass.AP(t, off, [list(d) for d in dims])

    banks = [Tl(st.enter_context(nc.psum_tensor("pb%d" % i, [128, 512], F32)), [128, 512], "pb%d" % i)
             for i in range(8)]
    bank_i = [0]

    xT = sb("xT", [128, KC, T], BF16)
    WBSZ = 6208
    WB = [sb("WB%d" % i, [128, WBSZ], BF16) for i in range(2)]
    C64 = sb("C64", [64, a64.shape[1]])
    C128 = sb("C128", [128, 384])
    cwa = sb("cwa", [128, L, 12, 4])
    cwc = sb("cwc", [128, L, 12, 4])
    cbc = sb("cbc", [128, L, 12])
    bgt = sb("bgt", [128, L, 3, 8])
    pb_a = sb("pb_a", [128, L, 8])
    nrm_a = sb("nrm_a", [64, L, 128])
    pc = sb("pc", [64, L, 48])
    wr = sb("wr", [128, L, KC, 36])
    brt = sb("brt", [128, L, 36])
    comb = sb("comb", [128, NT, 32])
    nexa = sb("nexa", [128, L, 4])
    nac = sb("nac", [64, L, 16])
    epst = sb("epst", [128, 4])
    ARENA_W = 88 * 256
    ARh = st.enter_context(nc.sbuf_tensor("AR", [128, ARENA_W], F32))
    ARb = ARh.bitcast(BF16)

    class Phase:
        def __init__(self, name):
            self.name = name
            self.off = 0
            self.t = {}
            self.keys = []

        def a(self, nm, free, dt=F32):
            n = int(np.prod(free))
            sz = n * (4 if dt == F32 else 2)
            off = self.off
            self.off += (sz + 3) // 4 * 4
            assert self.off <= ARENA_W * 4, (self.name, nm, self.off)
            if dt == F32:
                tl = Tl(ARh, [128] + list(free), self.name + "_" + nm, base=off // 4, pstride=ARENA_W)
            else:
                tl = Tl(ARb, [128] + list(free), self.name + "_" + nm, base=off // 2, pstride=ARENA_W * 2)
            self.t[nm] = tl
            self.keys.append(tl.key)
            return tl

    PH = {}
    for nm in "GRSMWXEN":
        PH[nm] = Phase(nm)
    G_, R_, S_, M_, W_, X_, E_, N_ = [PH[k] for k in "GRSMWXEN"]
    for nm in ["fmq", "fmk", "fmv", "fmz", "tA", "tB"]:
        R_.a(nm, [BLK])
    for nm in ["tA", "tB"]:
        G_.a(nm, [BLK])
    for par in range(2):
        for nm in ["fmq", "fmk", "fmv", "fmz"]:
            G_.a(nm + str(par), [BLK])
        for nm in ["rhs_u", "rhs_w", "kend", "u_t"]:
            G_.a(nm + str(par), [CPB * 128])
        G_.a("wT" + str(par), [CPB * 64]); G_.a("decS" + str(par), [CPB])
    for nm in ["raw0", "raw1"]:
        G_.a(nm, [BLK + 3]); S_.a(nm, [BLK + 3])
    G_.a("car", [12]); S_.a("car", [24])
    G_.a("ytb", [BLK], BF16); R_.a("ytb", [BLK], BF16); S_.a("ytb", [4 * BLK], BF16)
    G_.a("S_a", [128])
    for nm in ["g_t", "beta", "gc", "egl", "bex", "st1", "st2"]:
        G_.a(nm, [CPB])
    G_.a("gbr", [CPB * 64])
    for nm in ["o_t", "sq_t"]:
        G_.a(nm, [CPB * 128])
    for nm in ["t1", "Pm", "Qm", "Pm2", "Qm2", "Rm"]:
        G_.a(nm, [CPB * 64])
    G_.a("delta", [128])
    R_.a("cst", [2 * BLK]); R_.a("rdt", [BLK]); R_.a("SM", [CPB * 64])
    for nm in ["v_tm", "kw", "o_t", "sq_t"]:
        R_.a(nm, [CPB * 128])
    R_.a("St", [(CPB + 1) * 128]); R_.a("st1", [CPB]); R_.a("st2", [CPB]); R_.a("nrmb", [128])
    S_.a("fx", [4 * BLK])
    for nm in ["fmq", "fmk", "tA"]:
        S_.a(nm, [BLK])
    S_.a("Sc", [512])
    for nm in ["dt_t", "dta", "lc", "wr_t", "elc", "decb"]:
        S_.a(nm, [CPB * 8])
    for nm in ["nrmc", "dsk"]:
        S_.a(nm, [512])
    S_.a("szb", [4 * BLK])
    for par in range(2):
        for nm in ["db", "e1", "x_tm", "xw", "yi", "yg"]:
            S_.a(nm + str(par), [512])
        S_.a("cbT" + str(par), [64]); S_.a("B_tm" + str(par), [128]); S_.a("st2" + str(par), [4])
    mrgT = M_.a("mrg", [8 * T], BF16)
    W_.t["mrg"] = mrgT; W_.off = M_.off
    mkeys = ["mrg%d" % i for i in range(NBLK)]
    M_.keys += mkeys; W_.keys += mkeys + [mrgT.key]
    M_.a("ytl0", [16 * BLK], BF16); M_.a("ytl1", [16 * BLK], BF16); M_.a("tA", [BLK]); M_.a("tB", [BLK])
    for ph in (W_, X_):
        ph.a("xl0", [1024]); ph.a("xl1", [1024])
    yacc = E_.a("yacc", [NT * 1024])
    N_.t["yacc"] = yacc; N_.off = E_.off
    ykeys = ["yacc%d" % i for i in range(NT)]
    E_.keys += ykeys; N_.keys += ykeys + [yacc.key]
    for ph in (W_, N_):
        for nm in ["lng", "lnb", "xn0", "xn1"]:
            ph.a(nm, [1024])
        for par in range(2):
            ph.a("bst%d" % par, [12]); ph.a("mv%d" % par, [4])
    for par in range(2):
        W_.a("xTf%d" % par, [1024]); W_.a("rt%d" % par, [160])
    E_.a("hs0", [BLK]); E_.a("hs1", [BLK]); E_.a("Hh0", [2 * BLK], BF16); E_.a("Hh1", [2 * BLK], BF16)
    cur_ph = [None]

    def enter(ph):
        old = cur_ph[0]
        if old is ph:
            return
        cur_ph[0] = ph
        if old is None:
            return
        P.op("dve", lambda e: e.memset(epst.v(3, [[1, 1]]), 0.0), writes=list(old.keys) + list(ph.keys) + ["epst3"])

    def c64(name, w=None, p0=0, Pn=64, coff=0):
        w = w if w is not None else {"tri": 64, "id": 64, "ones": 128, "bigm": 64, "rdec": 256, "wd": 4}[name]
        return C64.v(off64[name] + coff, [[1, w]], Pn=Pn, p0=p0)

    def c128(name, w=128, coff=0):
        return C128.v(off128[name] + coff, [[1, w]])

    P.dma("sp", lambda e: e.dma_start(out=C64.h[:, :], in_=dr["c64"][:, :]), writes=["C64"], chan="i_c64")
    P.dma("sp", lambda e: e.dma_start(out=C128.h[:, :], in_=dr["c128"][:, 0:384]), writes=["C128"], chan="i_c128")

    def small(dst_ap, src_ap, key, chan=None):
        P.dma("sp", lambda e: e.dma_start(out=dst_ap, in_=src_ap, allow_slow_non_contiguous=True), writes=[key], chan="i_" + key)

    for l in range(L):
        for k in range(4):
            small(cwa.v(l * 48 + k, [[4, 12]]), dv(dr["conv_a"], l * 6144 + k * 1536, [[1, 128], [128, 12]]), "cwa")
            small(cwc.v(l * 48 + k, [[4, 12]]), dv(dr["conv_c"], l * 6144 + k * 1536, [[1, 128], [128, 12]]), "cwc")
        small(cbc.v(l * 12, [[1, 12]]), dv(dr["conv_bias_c"], l * 1536, [[1, 128], [128, 12]]), "cbc")
        for br in range(3):
            small(bgt.v(l * 24 + br * 8, [[1, 8]]), dv(dr["b_gate"], l * 3072 + br * 1024, [[1, 128], [128, 8]]), "bgt")
        small(pb_a.v(l * 8, [[1, 4]]), dv(dr["a_log_a"], l * 4, [[0, 128], [1, 4]]), "pb_a")
        small(pb_a.v(l * 8 + 4, [[1, 4]]), dv(dr["dt_bias_a"], l * 4, [[0, 128], [1, 4]]), "pb_a")
        small(nrm_a.v(l * 128, [[1, 128]], Pn=64), dv(dr["norm_a"], l * 128, [[0, 64], [1, 128]]), "nrm_a")
        small(pc.v(l * 48, [[1, 16]], Pn=64), dv(dr["dt_bias_c"], l * 16, [[0, 64], [1, 16]]), "pc")
        small(pc.v(l * 48 + 16, [[1, 16]], Pn=64), dv(dr["a_log_c"], l * 16, [[0, 64], [1, 16]]), "pc")
        small(pc.v(l * 48 + 32, [[1, 16]], Pn=64), dv(dr["d_skip_c"], l * 16, [[0, 64], [1, 16]]), "pc")
        small(wr.v(l * KC * 36, [[36, KC], [1, 4]]), dv(dr["w_router_group"], l * D * 4, [[4, 128], [512, KC], [1, 4]]), "wr")
        small(wr.v(l * KC * 36 + 4, [[36, KC], [1, 32]]), dv(dr["w_router_expert"], l * D * 32, [[32, 128], [4096, KC], [1, 32]]), "wr")
        small(brt.v(l * 36, [[1, 4]]), dv(dr["b_router_group"], l * 4, [[0, 128], [1, 4]]), "brt")
        small(brt.v(l * 36 + 4, [[1, 32]]), dv(dr["b_router_expert"], l * 32, [[0, 128], [1, 32]]), "brt")
    P.op("dve", lambda e: e.memset(epst.v(0, [[1, 1]]), EPS), writes=["epst"])
    P.op("dve", lambda e: e.memset(epst.v(1, [[1, 1]]), 1.0), reads=["epst"], writes=["epst"])
    P.op("dve", lambda e: e.memset(epst.v(2, [[1, 1]]), 0.0), reads=["epst"], writes=["epst"])
    eps_ap = epst.v(0, [[1, 1]])
    one_ap = epst.v(1, [[1, 1]])
    for l in range(L):
        P.op("act", lambda e, l=l: e.activation(out=nexa.v(l * 4, [[1, 4]]), in_=pb_a.v(l * 8, [[1, 4]]), func=AF.Exp),
             reads=["pb_a"], writes=["nexa"])
        P.op("dve", lambda e, l=l: e.tensor_scalar(out=nexa.v(l * 4, [[1, 4]]), in0=nexa.v(l * 4, [[1, 4]]), scalar1=-1.0,
                                                   scalar2=None, op0=ALU.mult), reads=["nexa"], writes=["nexa"])
        P.op("act", lambda e, l=l: e.activation(out=nac.v(l * 16, [[1, 16]], Pn=64), in_=pc.v(l * 48 + 16, [[1, 16]], Pn=64),
                                                func=AF.Exp), reads=["pc"], writes=["nac"])
        P.op("dve", lambda e, l=l: e.tensor_scalar(out=nac.v(l * 16, [[1, 16]], Pn=64), in0=nac.v(l * 16, [[1, 16]], Pn=64),
                                                   scalar1=-1.0, scalar2=None, op0=ALU.mult), reads=["nac"], writes=["nac"])

    pinned = set()
    bpool = [list(range(8))]

    def ps():
        while True:
            pool = bpool[0]
            b = banks[pool[bank_i[0] % len(pool)]]
            bank_i[0] += 1
            if b.key not in pinned:
                return b

    def with_banks(lst, f):
        old = bpool[0]
        bpool[0] = lst
        f()
        bpool[0] = old

    def ld_w(slot, off_el, ncols, src_t, src_off, row_stride, nk=KC, krows=128):
        P.dma("pool", lambda e: e.dma_start(out=WB[slot].v(off_el, [[ncols, nk], [1, ncols]]),
                                            in_=dv(src_t, src_off, [[row_stride, 128], [128 * row_stride, nk], [1, ncols]]),
                                            allow_slow_non_contiguous=True),
              writes=["WB%d" % slot], chan="w%d" % slot)

    def xt_keys(tok0, n):
        return ["xT%d" % i for i in range(tok0 // 128, (tok0 + n + 127) // 128)]

    def proj_fm(slot, woff, wcols, c0, tok0, n, pbank, pcol=0):
        for kc in range(KC):
            P.op("pe", lambda e, kc=kc: e.matmul(pbank.v(pcol, [[1, n]]),
                                                 lhsT=WB[slot].v(woff + kc * wcols + c0, [[1, 128]]),
                                                 rhs=xT.v(kc * T + tok0, [[1, n]]), start=(kc == 0), stop=(kc == KC - 1)),
                 reads=["WB%d" % slot] + xt_keys(tok0, n), writes=[pbank.key])

    def proj_tm(slot, woff, wcols, c0, ncol, tok0, pbank, pcol=0):
        for kc in range(KC):
            P.op("pe", lambda e, kc=kc: e.matmul(pbank.v(pcol, [[1, ncol]], Pn=64),
                                                 lhsT=xT.v(kc * T + tok0, [[1, 64]]),
                                                 rhs=WB[slot].v(woff + kc * wcols + c0, [[1, ncol]]),
                                                 start=(kc == 0), stop=(kc == KC - 1)),
                 reads=["WB%d" % slot] + xt_keys(tok0, 64), writes=[pbank.key])

    def l2norm_fm(ph, t, scale):
        tB = ph.t["tB"]
        P.op("act", lambda e: e.activation(out=tB.v(0, [[1, BLK]]), in_=t.v(0, [[1, BLK]]), func=AF.Square), reads=[t.key], writes=[tB.key])
        pb = ps()
        P.op("pe", lambda e: e.matmul(pb.v(0, [[1, BLK]]), lhsT=c128("ones"), rhs=tB.v(0, [[1, BLK]]), start=True, stop=True),
             reads=[tB.key, "C128"], writes=[pb.key])
        P.op("act", lambda e: e.activation(out=tB.v(0, [[1, BLK]]), in_=pb.v(0, [[1, BLK]]), func=AF.Ln, bias=eps_ap, scale=1.0),
             reads=[pb.key, "epst"], writes=[tB.key])
        P.op("act", lambda e: e.activation(out=tB.v(0, [[1, BLK]]), in_=tB.v(0, [[1, BLK]]), func=AF.Exp, scale=-0.5), reads=[tB.key], writes=[tB.key])
        P.op("dve", lambda e: e.scalar_tensor_tensor(out=t.v(0, [[1, BLK]]), in0=t.v(0, [[1, BLK]]), scalar=float(scale),
                                                     in1=tB.v(0, [[1, BLK]]), op0=ALU.mult, op1=ALU.mult),
             reads=[t.key, tB.key], writes=[t.key])

    def store_yT(ytb, fc0, nfc, tok0, s):
        P.dma("sp", lambda e: e.dma_start(out=dv(ytd, (s % 2) * 128 * 16 * T + fc0 * T + tok0, [[16 * T, 128], [T, nfc], [1, BLK]]),
                                          in_=ytb.v(0, [[BLK, nfc], [1, BLK]])),
              reads=[ytb.key], writes=["ytd%d_%d_%d" % (s % 2, fc0 + i, tok0 // BLK) for i in range(nfc)], chan="yt")

    def gdn_load(slot, l, h):
        base = l * D * P_IN
        for j, c0 in enumerate([O_AQKV + h * 128, O_AQKV + 512 + h * 128, O_AQKV + 1024 + h * 128, O_AZ + h * 128]):
            ld_w(slot, j * 1024, 128, dr["w_in"], base + c0, P_IN)
        ld_w(slot, 4096, 1, dr["w_in"], base + O_AA + h, P_IN)
        ld_w(slot, 4096 + 8, 1, dr["w_in"], base + O_AB + h, P_IN)

    def gdn_run(slot, l, h, s):
        wk = "WB%d" % slot
        enter(G_)
        ph = G_
        tA, tB, ytb, S_a, g_t, beta, gc, egl, bex, st1, st2, gbr, o_t, sq_t, t1, Pm, Qm, Pm2, Qm2, Rm, delta = [G_.t[n] for n in ['tA', 'tB', 'ytb', 'S_a', 'g_t', 'beta', 'gc', 'egl', 'bex', 'st1', 'st2', 'gbr', 'o_t', 'sq_t', 't1', 'Pm', 'Qm', 'Pm2', 'Qm2', 'Rm', 'delta']]
        def pre(b):
            tok0 = b * BLK
            fmq, fmk, fmv, fmz, rhs_u, rhs_w, kend, u_t, wT, decS = [G_.t[n + str(b % 2)] for n in ['fmq', 'fmk', 'fmv', 'fmz', 'rhs_u', 'rhs_w', 'kend', 'u_t', 'wT', 'decS']]
            for j, dst in enumerate([fmq, fmk, fmv]):
                pb = ps()
                proj_fm(slot, j * 1024, 128, 0, tok0, BLK, pb)
                cw = lambda k, j=j: cwa.v(l * 48 + (j * 4 + h) * 4 + k, [[1, 1]])
                conv_stream(ph, pb, j, b, cw, dst.v(0, [[1, BLK]]), dst.key)
            l2norm_fm(ph, fmq, 128.0 ** -0.5)
            l2norm_fm(ph, fmk, 1.0)
            pb = ps()
            proj_fm(slot, 3 * 1024, 128, 0, tok0, BLK, pb)
            P.op("act", lambda e, pb=pb: e.activation(out=fmz.v(0, [[1, BLK]]), in_=pb.v(0, [[1, BLK]]), func=AF.Silu),
                 reads=[pb.key], writes=[fmz.key])
            pb = ps()
            for c in range(CPB):
                proj_tm(slot, 4096, 1, 0, 1, tok0 + c * 64, pb, pcol=2 * c)
                proj_tm(slot, 4096 + 8, 1, 0, 1, tok0 + c * 64, pb, pcol=2 * c + 1)
            P.op("act", lambda e, pb=pb: e.activation(out=g_t.v(0, [[1, CPB]], Pn=64), in_=pb.v(0, [[2, CPB]], Pn=64), func=AF.Exp,
                                                      bias=pb_a.v(l * 8 + 4 + h, [[1, 1]], Pn=64)),
                 reads=[pb.key, "pb_a"], writes=[g_t.key])
            P.op("act", lambda e: e.activation(out=g_t.v(0, [[1, CPB]], Pn=64), in_=g_t.v(0, [[1, CPB]], Pn=64), func=AF.Ln,
                                               bias=one_ap[0:64, :]), reads=[g_t.key, "epst"], writes=[g_t.key])
            P.op("dve", lambda e: e.tensor_scalar(out=g_t.v(0, [[1, CPB]], Pn=64), in0=g_t.v(0, [[1, CPB]], Pn=64),
                                                  scalar1=nexa.v(l * 4 + h, [[1, 1]], Pn=64), scalar2=None, op0=ALU.mult),
                 reads=[g_t.key, "nexa"], writes=[g_t.key])
            P.op("act", lambda e, pb=pb: e.activation(out=beta.v(0, [[1, CPB]], Pn=64), in_=pb.v(1, [[2, CPB]], Pn=64),
                                                      func=AF.Exp, scale=-1.0), reads=[pb.key], writes=[beta.key])
            P.op("dve", lambda e: e.tensor_scalar(out=beta.v(0, [[1, CPB]], Pn=64), in0=beta.v(0, [[1, CPB]], Pn=64), scalar1=1.0, scalar2=None,
                                                  op0=ALU.add), reads=[beta.key], writes=[beta.key])
            P.op("dve", lambda e: e.reciprocal(out=beta.v(0, [[1, CPB]], Pn=64), in_=beta.v(0, [[1, CPB]], Pn=64)), reads=[beta.key], writes=[beta.key])
            pg = ps()
            P.op("pe", lambda e, pg=pg: e.matmul(pg.v(0, [[1, CPB]], Pn=64), lhsT=c64("tri"), rhs=g_t.v(0, [[1, CPB]], Pn=64),
                                                 start=True, stop=True), reads=[g_t.key, "C64"], writes=[pg.key])
            P.op("pe", lambda e, pg=pg: e.matmul(pg.v(64, [[1, CPB]]), lhsT=c64("ones"), rhs=g_t.v(0, [[1, CPB]], Pn=64),
                                                 start=True, stop=True), reads=[g_t.key, "C64"], writes=[pg.key])
            P.op("dve", lambda e, pg=pg: e.tensor_copy(out=gc.v(0, [[1, CPB]], Pn=64), in_=pg.v(0, [[1, CPB]], Pn=64)),
                 reads=[pg.key], writes=[gc.key])
            P.op("act", lambda e, pg=pg: e.activation(out=decS.v(0, [[1, CPB]]), in_=pg.v(64, [[1, CPB]]), func=AF.Exp),
                 reads=[pg.key], writes=[decS.key])
            P.op("dve", lambda e, pg=pg: e.tensor_tensor(out=egl.v(0, [[1, CPB]], Pn=64), in0=pg.v(64, [[1, CPB]], Pn=64),
                                                         in1=gc.v(0, [[1, CPB]], Pn=64), op=ALU.subtract),
                 reads=[pg.key, gc.key], writes=[egl.key])
            P.op("act", lambda e: e.activation(out=egl.v(0, [[1, CPB]], Pn=64), in_=egl.v(0, [[1, CPB]], Pn=64), func=AF.Exp),
                 reads=[egl.key], writes=[egl.key])
            P.op("act", lambda e: e.activation(out=bex.v(0, [[1, CPB]], Pn=64), in_=gc.v(0, [[1, CPB]], Pn=64), func=AF.Exp),
                 reads=[gc.key], writes=[bex.key])
            P.op("dve", lambda e: e.tensor_tensor(out=bex.v(0, [[1, CPB]], Pn=64), in0=bex.v(0, [[1, CPB]], Pn=64),
                                                  in1=beta.v(0, [[1, CPB]], Pn=64), op=ALU.mult), reads=[bex.key, beta.key], writes=[bex.key])
            P.op("dve", lambda e: e.tensor_copy(out=gbr.v(0, [[64, CPB], [1, 64]], Pn=64), in_=g_t.v(0, [[1, CPB], [0, 64]], Pn=64)),
                 reads=[g_t.key], writes=[gbr.key])
            pG = ps()
            pK = ps()
            for c in range(CPB):
                P.op("pe", lambda e, c=c, pG=pG: e.matmul(pG.v(c * 64, [[1, 64]], Pn=64), lhsT=gbr.v(c * 64, [[1, 64]], Pn=64),
                                                          rhs=c64("tri"), start=True, stop=True), reads=[gbr.key, "C64"], writes=[pG.key])
                P.op("pe", lambda e, c=c, pK=pK: e.matmul(pK.v(c * 64, [[1, 64]], Pn=64), lhsT=fmk.v(c * 64, [[1, 64]]),
                                                          rhs=fmk.v(c * 64, [[1, 64]]), start=True, stop=True), reads=[fmk.key], writes=[pK.key])
            P.op("dve", lambda e, pG=pG: e.tensor_tensor(out=t1.v(0, [[64, CPB], [1, 64]], Pn=64), in0=pG.v(0, [[64, CPB], [1, 64]], Pn=64),
                                                         in1=gc.v(0, [[1, CPB], [0, 64]], Pn=64), op=ALU.subtract),
                 reads=[pG.key, gc.key], writes=[t1.key])
            P.op("dve", lambda e: e.tensor_tensor(out=t1.v(0, [[64, CPB], [1, 64]], Pn=64), in0=t1.v(0, [[64, CPB], [1, 64]], Pn=64),
                                                  in1=C64.v(off64["bigm"], [[0, CPB], [1, 64]], Pn=64), op=ALU.max),
                 reads=[t1.key, "C64"], writes=[t1.key])
            P.op("act", lambda e: e.activation(out=t1.v(0, [[1, CPB * 64]], Pn=64), in_=t1.v(0, [[1, CPB * 64]], Pn=64), func=AF.Exp, scale=-1.0),
                 reads=[t1.key], writes=[t1.key])
            P.op("dve", lambda e, pK=pK: e.tensor_tensor(out=Qm.v(0, [[64, CPB], [1, 64]], Pn=64), in0=pK.v(0, [[64, CPB], [1, 64]], Pn=64),
                                                         in1=beta.v(0, [[1, CPB], [0, 64]], Pn=64), op=ALU.mult),
                 reads=[pK.key, beta.key], writes=[Qm.key])
            P.op("dve", lambda e: e.tensor_tensor(out=Qm.v(0, [[1, CPB * 64]], Pn=64), in0=Qm.v(0, [[1, CPB * 64]], Pn=64),
                                                  in1=t1.v(0, [[1, CPB * 64]], Pn=64), op=ALU.mult), reads=[Qm.key, t1.key], writes=[Qm.key])
            pT = [ps(), ps()]
            pV = [ps(), ps()]
            pB_ = ps()
            for c in range(CPB):
                P.op("pe", lambda e, c=c: e.transpose(pT[c // 4].v((c % 4) * 128, [[1, 128]], Pn=64), fmk.v(c * 64, [[1, 64]]), c128("id")),
                     reads=[fmk.key, "C128"], writes=[pT[c // 4].key])
                P.op("pe", lambda e, c=c: e.transpose(pV[c // 4].v((c % 4) * 128, [[1, 128]], Pn=64), fmv.v(c * 64, [[1, 64]]), c128("id")),
                     reads=[fmv.key, "C128"], writes=[pV[c // 4].key])
                P.op("pe", lambda e, c=c: e.transpose(pB_.v(c * 64, [[1, 64]], Pn=64), Qm.v(c * 64, [[1, 64]], Pn=64), c64("id")),
                     reads=[Qm.key, "C64"], writes=[pB_.key])
            for hb in range((CPB + 3) // 4):
                n = min(4, CPB - hb * 4)
                P.op("dve", lambda e, hb=hb, n=n: e.tensor_tensor(out=rhs_w.v(hb * 512, [[128, n], [1, 128]], Pn=64),
                                                                  in0=pT[hb].v(0, [[128, n], [1, 128]], Pn=64),
                                                                  in1=bex.v(hb * 4, [[1, n], [0, 128]], Pn=64), op=ALU.mult),
                     reads=[pT[hb].key, bex.key], writes=[rhs_w.key])
                P.op("dve", lambda e, hb=hb, n=n: e.tensor_tensor(out=kend.v(hb * 512, [[128, n], [1, 128]], Pn=64),
                                                                  in0=pT[hb].v(0, [[128, n], [1, 128]], Pn=64),
                                                                  in1=egl.v(hb * 4, [[1, n], [0, 128]], Pn=64), op=ALU.mult),
                     reads=[pT[hb].key, egl.key], writes=[kend.key])
                P.op("dve", lambda e, hb=hb, n=n: e.tensor_tensor(out=rhs_u.v(hb * 512, [[128, n], [1, 128]], Pn=64),
                                                                  in0=pV[hb].v(0, [[128, n], [1, 128]], Pn=64),
                                                                  in1=beta.v(hb * 4, [[1, n], [0, 128]], Pn=64), op=ALU.mult),
                     reads=[pV[hb].key, beta.key], writes=[rhs_u.key])
            P.op("act", lambda e: e.activation(out=Pm.v(0, [[1, CPB * 64]], Pn=64), in_=pB_.v(0, [[1, CPB * 64]], Pn=64), func=AF.Copy),
                 reads=[pB_.key], writes=[Pm.key])
            P.op("dve", lambda e: e.tensor_tensor(out=Rm.v(0, [[64, CPB], [1, 64]], Pn=64), in0=C64.v(off64["id"], [[0, CPB], [1, 64]], Pn=64),
                                                  in1=pB_.v(0, [[64, CPB], [1, 64]], Pn=64), op=ALU.subtract),
                 reads=[pB_.key, "C64"], writes=[Rm.key])
            Pc, Qc, Pn_, Qn_ = Pm, Qm, Pm2, Qm2
            for lev in range(5):
                last = lev == 4
                pq = ps()
                pp = ps() if not last else None
                for c in range(CPB):
                    P.op("pe", lambda e, c=c, pq=pq, Pc=Pc, Qc=Qc: e.matmul(pq.v(c * 64, [[1, 64]], Pn=64), lhsT=Pc.v(c * 64, [[1, 64]], Pn=64),
                                                                         rhs=Qc.v(c * 64, [[1, 64]], Pn=64), start=True, stop=True),
                         reads=[Pc.key, Qc.key], writes=[pq.key])
                    if not last:
                        P.op("pe", lambda e, c=c, pp=pp, Pc=Pc, Qc=Qc: e.matmul(pp.v(c * 64, [[1, 64]], Pn=64), lhsT=Qc.v(c * 64, [[1, 64]], Pn=64),
                                                                             rhs=Pc.v(c * 64, [[1, 64]], Pn=64), start=True, stop=True),
                             reads=[Pc.key, Qc.key], writes=[pp.key])
                P.op("act", lambda e, pq=pq, Qn_=Qn_: e.activation(out=Qn_.v(0, [[1, CPB * 64]], Pn=64), in_=pq.v(0, [[1, CPB * 64]], Pn=64), func=AF.Copy),
                     reads=[pq.key], writes=[Qn_.key])
                if not last:
                    P.op("dve", lambda e, pp=pp, Pn_=Pn_: e.tensor_copy(out=Pn_.v(0, [[1, CPB * 64]], Pn=64), in_=pp.v(0, [[1, CPB * 64]], Pn=64)),
                         reads=[pp.key], writes=[Pn_.key])
                pr = ps()
                for c in range(CPB):
                    P.op("pe", lambda e, c=c, pr=pr, Qn_=Qn_: e.matmul(pr.v(c * 64, [[1, 64]], Pn=64), lhsT=Qn_.v(c * 64, [[1, 64]], Pn=64),
                                                                      rhs=Rm.v(c * 64, [[1, 64]], Pn=64), start=True, stop=True),
                         reads=[Qn_.key, Rm.key], writes=[pr.key])
                P.op("dve", lambda e, pr=pr: e.tensor_tensor(out=Rm.v(0, [[1, CPB * 64]], Pn=64), in0=Rm.v(0, [[1, CPB * 64]], Pn=64),
                                                             in1=pr.v(0, [[1, CPB * 64]], Pn=64), op=ALU.add), reads=[pr.key, Rm.key], writes=[Rm.key])
                Pc, Qc, Pn_, Qn_ = Pn_, Qn_, Pc, Qc
            pU = [ps(), ps()]
            pW = ps()
            for c in range(CPB):
                P.op("pe", lambda e, c=c: e.matmul(pU[c // 4].v((c % 4) * 128, [[1, 128]], Pn=64), lhsT=Rm.v(c * 64, [[1, 64]], Pn=64),
                                                   rhs=rhs_u.v(c * 128, [[1, 128]], Pn=64), start=True, stop=True),
                     reads=[Rm.key, rhs_u.key], writes=[pU[c // 4].key])
                P.op("pe", lambda e, c=c: e.matmul(pW.v(c * 64, [[1, 64]]), lhsT=rhs_w.v(c * 128, [[1, 128]], Pn=64),
                                                   rhs=Rm.v(c * 64, [[1, 64]], Pn=64), start=True, stop=True),
                     reads=[Rm.key, rhs_w.key], writes=[pW.key])
            for hb in range((CPB + 3) // 4):
                n = min(4, CPB - hb * 4)
                P.op("act", lambda e, hb=hb, n=n: e.activation(out=u_t.v(hb * 512, [[1, n * 128]], Pn=64), in_=pU[hb].v(0, [[1, n * 128]], Pn=64),
                                                               func=AF.Copy), reads=[pU[hb].key], writes=[u_t.key])
            P.op("act", lambda e: e.activation(out=wT.v(0, [[1, CPB * 64]]), in_=pW.v(0, [[1, CPB * 64]]), func=AF.Copy),
                 reads=[pW.key], writes=[wT.key])

        def scanpost(b):
            tok0 = b * BLK
            fmq, fmk, fmv, fmz, rhs_u, rhs_w, kend, u_t, wT, decS = [G_.t[n + str(b % 2)] for n in ['fmq', 'fmk', 'fmv', 'fmz', 'rhs_u', 'rhs_w', 'kend', 'u_t', 'wT', 'decS']]
            if b == 0:
                P.op("dve", lambda e: e.memset(S_a.v(0, [[1, 128]]), 0.0), writes=[S_a.key])
            pO = [ps(), ps()]
            pinned.update([pO[0].key, pO[1].key])
            for c in range(CPB):
                p1 = ps()
                P.op("pe", lambda e, c=c, p1=p1: e.matmul(p1.v(0, [[1, 128]], Pn=64), lhsT=wT.v(c * 64, [[1, 64]]), rhs=S_a.v(0, [[1, 128]]),
                                                          start=True, stop=True), reads=[wT.key, S_a.key], writes=[p1.key])
                P.op("dve", lambda e, c=c, p1=p1: e.tensor_tensor(out=delta.v(0, [[1, 128]], Pn=64), in0=u_t.v(c * 128, [[1, 128]], Pn=64),
                                                                  in1=p1.v(0, [[1, 128]], Pn=64), op=ALU.subtract),
                     reads=[u_t.key, p1.key], writes=[delta.key])
                p2 = ps()
                P.op("pe", lambda e, c=c, p2=p2: e.matmul(p2.v(0, [[1, 128]]), lhsT=kend.v(c * 128, [[1, 128]], Pn=64),
                                                          rhs=delta.v(0, [[1, 128]], Pn=64), start=True, stop=True),
                     reads=[kend.key, delta.key], writes=[p2.key])
                P.op("dve", lambda e, c=c, p2=p2: e.scalar_tensor_tensor(out=S_a.v(0, [[1, 128]]), in0=S_a.v(0, [[1, 128]]),
                                                                         scalar=decS.v(c, [[1, 1]]), in1=p2.v(0, [[1, 128]]),
                                                                         op0=ALU.mult, op1=ALU.add), reads=[S_a.key, decS.key, p2.key], writes=[S_a.key])
                P.op("pe", lambda e, c=c: e.matmul(pO[c // 4].v((c % 4) * 128, [[1, 128]], Pn=64), lhsT=fmq.v(c * 64, [[1, 64]]),
                                                   rhs=S_a.v(0, [[1, 128]]), start=True, stop=True), reads=[fmq.key, S_a.key], writes=[pO[c // 4].key])
            pinned.difference_update([pO[0].key, pO[1].key])
            for hb in range((CPB + 3) // 4):
                n = min(4, CPB - hb * 4)
                P.op("act", lambda e, hb=hb, n=n: e.activation(out=o_t.v(hb * 512, [[1, n * 128]], Pn=64), in_=pO[hb].v(0, [[1, n * 128]], Pn=64),
                                                               func=AF.Copy), reads=[pO[hb].key], writes=[o_t.key])
            rms_tm(ph, o_t, CPB, 128, nrm_a.v(l * 128, [[0, CPB], [1, 128]], Pn=64), "nrm_a")
            pY = ps()
            for c in range(CPB):
                P.op("pe", lambda e, c=c, pY=pY: e.transpose(pY.v(c * 64, [[1, 64]]), o_t.v(c * 128, [[1, 128]], Pn=64), c64("id")),
                     reads=[o_t.key, "C64"], writes=[pY.key])
            P.op("dve", lambda e, pY=pY: e.tensor_tensor(out=ytb.v(0, [[1, BLK]]), in0=pY.v(0, [[1, BLK]]), in1=fmz.v(0, [[1, BLK]]), op=ALU.mult),
                 reads=[pY.key, fmz.key], writes=[ytb.key])
            store_yT(ytb, h, 1, tok0, s)


        pre(0)
        for b in range(NBLK):
            A = P.capture(lambda: with_banks([0, 1, 2], lambda: scanpost(b)))
            B = P.capture(lambda: with_banks([3, 4, 5, 6, 7], lambda: pre(b + 1))) if b + 1 < NBLK else []
            P.merge(A, B)

    conv_cnt = [0]

    def conv_stream(ph, pb, sid, b, cw, out_ap, out_key, bias_ap=None):
        r = ph.t["raw%d" % (conv_cnt[0] % 2)]
        conv_cnt[0] += 1
        car = ph.t["car"]
        tA = ph.t["tA"]
        if b == 0:
            P.op("dve", lambda e: e.memset(r.v(0, [[1, 3]]), 0.0), writes=[r.key])
        else:
            P.op("dve", lambda e: e.tensor_copy(out=r.v(0, [[1, 3]]), in_=car.v(sid * 3, [[1, 3]])), reads=[car.key], writes=[r.key])
        P.op("act", lambda e: e.activation(out=r.v(3, [[1, BLK]]), in_=pb.v(0, [[1, BLK]]), func=AF.Copy),
             reads=[pb.key, r.key], writes=[r.key])
        P.op("dve", lambda e: e.tensor_copy(out=car.v(sid * 3, [[1, 3]]), in_=r.v(BLK, [[1, 3]])), reads=[r.key, car.key], writes=[car.key])
        P.op("dve", lambda e: e.tensor_scalar(out=tA.v(0, [[1, BLK]]), in0=r.v(0, [[1, BLK]]), scalar1=cw(0), scalar2=None, op0=ALU.mult),
             reads=[r.key, "cwa", "cwc"], writes=[tA.key])
        for k in range(1, 4):
            P.op("dve", lambda e, k=k: e.scalar_tensor_tensor(out=tA.v(0, [[1, BLK]]), in0=r.v(k, [[1, BLK]]), scalar=cw(k),
                                                              in1=tA.v(0, [[1, BLK]]), op0=ALU.mult, op1=ALU.add),
                 reads=[r.key, tA.key, "cwa", "cwc"], writes=[tA.key])
        if bias_ap is None:
            P.op("act", lambda e: e.activation(out=out_ap, in_=tA.v(0, [[1, BLK]]), func=AF.Silu), reads=[tA.key], writes=[out_key])
        else:
            P.op("act", lambda e: e.activation(out=out_ap, in_=tA.v(0, [[1, BLK]]), func=AF.Silu, bias=bias_ap),
                 reads=[tA.key, "cbc"], writes=[out_key])

    def rms_tm(ph, t, nch, width, w_ap, wkey, center=False):
        st1, st2, sq_t = ph.t["st1"], ph.t["st2"], ph.t["sq_t"]
        full = t.v(0, [[width, nch], [1, width]], Pn=64)
        if center:
            P.op("dve", lambda e: e.tensor_reduce(out=st1.v(0, [[1, nch]], Pn=64), in_=full, axis=AX.X, op=ALU.add), reads=[t.key], writes=[st1.key])
            P.op("dve", lambda e: e.tensor_scalar(out=st1.v(0, [[1, nch]], Pn=64), in0=st1.v(0, [[1, nch]], Pn=64), scalar1=1.0 / width,
                                                  scalar2=None, op0=ALU.mult), reads=[st1.key], writes=[st1.key])
            P.op("dve", lambda e: e.tensor_tensor(out=full, in0=full, in1=st1.v(0, [[1, nch], [0, width]], Pn=64), op=ALU.subtract),
                 reads=[t.key, st1.key], writes=[t.key])
        P.op("act", lambda e: e.activation(out=sq_t.v(0, [[1, nch * width]], Pn=64),
                                           in_=t.v(0, [[1, nch * width]], Pn=64), func=AF.Square), reads=[t.key], writes=[sq_t.key])
        P.op("dve", lambda e: e.tensor_reduce(out=st2.v(0, [[1, nch]], Pn=64), in_=sq_t.v(0, [[width, nch], [1, width]], Pn=64), axis=AX.X, op=ALU.add),
             reads=[sq_t.key], writes=[st2.key])
        P.op("act", lambda e: e.activation(out=st2.v(0, [[1, nch]], Pn=64), in_=st2.v(0, [[1, nch]], Pn=64), func=AF.Ln,
                                           bias=eps_ap[0:64, :], scale=1.0 / width), reads=[st2.key, "epst"], writes=[st2.key])
        P.op("act", lambda e: e.activation(out=st2.v(0, [[1, nch]], Pn=64), in_=st2.v(0, [[1, nch]], Pn=64), func=AF.Exp, scale=-0.5),
             reads=[st2.key], writes=[st2.key])
        P.op("dve", lambda e: e.tensor_tensor(out=full, in0=full, in1=st2.v(0, [[1, nch], [0, width]], Pn=64), op=ALU.mult),
             reads=[t.key, st2.key], writes=[t.key])
        P.op("dve", lambda e: e.tensor_tensor(out=full, in0=full, in1=w_ap, op=ALU.mult), reads=[t.key, wkey], writes=[t.key])

    def ret_load(slot, l, h):
        base = l * D * P_IN
        for j, c0 in enumerate([O_BQ + h * 128, O_BK + h * 128, O_BV + h * 128, O_BG + h * 128]):
            ld_w(slot, j * 1024, 128, dr["w_in"], base + c0, P_IN)

    def ret_run(slot, l, h, s):
        enter(R_)
        ph = R_
        fmq, fmk, fmv, fmz, tA, tB, ytb, cst, rdt, SM, v_tm, kw, o_t, sq_t, St, st1, st2, nrmb = [R_.t[n] for n in ['fmq', 'fmk', 'fmv', 'fmz', 'tA', 'tB', 'ytb', 'cst', 'rdt', 'SM', 'v_tm', 'kw', 'o_t', 'sq_t', 'St', 'st1', 'st2', 'nrmb']]
        small(rdt.v(0, [[1, BLK]]), dv(dr["c128"], off128["rd"] + h * BLK, [[a128.shape[1], 128], [1, BLK]]), rdt.key, chan="rp")
        small(nrmb.v(0, [[1, 128]], Pn=64), dv(dr["norm_b"], l * 512 + h * 128, [[0, 64], [1, 128]]), nrmb.key, chan="rp")
        for b in range(NBLK):
            tok0 = b * BLK
            P.dma("sp", lambda e, tok0=tok0: e.dma_start(out=cst.v(0, [[BLK, 2], [1, BLK]]),
                                                         in_=dv(dr["cs"], tok0, [[2 * T, 128], [T, 2], [1, BLK]])), writes=[cst.key], chan="cs")
            for j, dst in enumerate([fmq, fmk]):
                pb = ps()
                proj_fm(slot, j * 1024, 128, 0, tok0, BLK, pb)
                P.op("act", lambda e, pb=pb: e.activation(out=tA.v(0, [[1, BLK]]), in_=pb.v(0, [[1, BLK]]), func=AF.Copy), reads=[pb.key], writes=[tA.key])
                pr = ps()
                P.op("pe", lambda e, pr=pr: e.matmul(pr.v(0, [[1, BLK]]), lhsT=c128("rot"), rhs=tA.v(0, [[1, BLK]]), start=True, stop=True),
                     reads=[tA.key, "C128"], writes=[pr.key])
                sc = 1.0 if j == 0 else 128.0 ** -0.5
                P.op("dve", lambda e, pr=pr, sc=sc: e.scalar_tensor_tensor(out=tB.v(0, [[1, BLK]]), in0=pr.v(0, [[1, BLK]]), scalar=sc,
                                                                           in1=cst.v(BLK, [[1, BLK]]), op0=ALU.mult, op1=ALU.mult),
                     reads=[pr.key, cst.key], writes=[tB.key])
                P.op("dve", lambda e, sc=sc: e.scalar_tensor_tensor(out=tA.v(0, [[1, BLK]]), in0=tA.v(0, [[1, BLK]]), scalar=sc,
                                                                    in1=cst.v(0, [[1, BLK]]), op0=ALU.mult, op1=ALU.mult),
                     reads=[tA.key, cst.key], writes=[tA.key])
                P.op("dve", lambda e, dst=dst: e.tensor_tensor(out=dst.v(0, [[1, BLK]]), in0=tA.v(0, [[1, BLK]]), in1=tB.v(0, [[1, BLK]]), op=ALU.add),
                     reads=[tA.key, tB.key], writes=[dst.key])
            pb = ps()
            proj_fm(slot, 3 * 1024, 128, 0, tok0, BLK, pb)
            P.op("act", lambda e, pb=pb: e.activation(out=fmz.v(0, [[1, BLK]]), in_=pb.v(0, [[1, BLK]]), func=AF.Silu), reads=[pb.key], writes=[fmz.key])
            P.op("dve", lambda e: e.tensor_tensor(out=fmv.v(0, [[1, BLK]]), in0=fmq.v(0, [[1, BLK]]), in1=rdt.v(0, [[1, BLK]]), op=ALU.mult),
                 reads=[fmq.key, rdt.key], writes=[fmv.key])
            pV = [ps(), ps()]
            pT = [ps(), ps()]
            pS = ps()
            for c in range(CPB):
                proj_tm(slot, 2 * 1024, 128, 0, 128, tok0 + c * 64, pV[c // 4], pcol=(c % 4) * 128)
                P.op("pe", lambda e, c=c: e.transpose(pT[c // 4].v((c % 4) * 128, [[1, 128]], Pn=64), fmk.v(c * 64, [[1, 64]]), c128("id")),
                     reads=[fmk.key, "C128"], writes=[pT[c // 4].key])
                P.op("pe", lambda e, c=c: e.matmul(pS.v(c * 64, [[1, 64]], Pn=64), lhsT=fmk.v(c * 64, [[1, 64]]), rhs=fmq.v(c * 64, [[1, 64]]),
                                                   start=True, stop=True), reads=[fmk.key, fmq.key], writes=[pS.key])
            for hb in range((CPB + 3) // 4):
                n = min(4, CPB - hb * 4)
                P.op("act", lambda e, hb=hb, n=n: e.activation(out=v_tm.v(hb * 512, [[1, n * 128]], Pn=64), in_=pV[hb].v(0, [[1, n * 128]], Pn=64),
                                                               func=AF.Copy), reads=[pV[hb].key], writes=[v_tm.key])
                P.op("dve", lambda e, hb=hb, n=n: e.tensor_scalar(out=kw.v(hb * 512, [[1, n * 128]], Pn=64), in0=pT[hb].v(0, [[1, n * 128]], Pn=64),
                                                                  scalar1=c64("wd", 1, coff=h), scalar2=None, op0=ALU.mult),
                     reads=[pT[hb].key, "C64"], writes=[kw.key])
            P.op("dve", lambda e: e.tensor_tensor(out=SM.v(0, [[64, CPB], [1, 64]], Pn=64), in0=pS.v(0, [[64, CPB], [1, 64]], Pn=64),
                                                  in1=C64.v(off64["rdec"] + h * 64, [[0, CPB], [1, 64]], Pn=64), op=ALU.mult),
                 reads=[pS.key, "C64"], writes=[SM.key])
            if b == 0:
                P.op("dve", lambda e: e.memset(St.v(0, [[1, 128]]), 0.0), reads=[St.key], writes=[St.key])
            else:
                P.op("dve", lambda e: e.tensor_copy(out=St.v(0, [[1, 128]]), in_=St.v(CPB * 128, [[1, 128]])), reads=[St.key], writes=[St.key])
            for c in range(CPB):
                pu = ps()
                P.op("pe", lambda e, c=c, pu=pu: e.matmul(pu.v(0, [[1, 128]]), lhsT=kw.v(c * 128, [[1, 128]], Pn=64),
                                                          rhs=v_tm.v(c * 128, [[1, 128]], Pn=64), start=True, stop=True),
                     reads=[kw.key, v_tm.key], writes=[pu.key])
                P.op("dve", lambda e, c=c, pu=pu: e.scalar_tensor_tensor(out=St.v((c + 1) * 128, [[1, 128]]), in0=St.v(c * 128, [[1, 128]]),
                                                                         scalar=cdec[h], in1=pu.v(0, [[1, 128]]), op0=ALU.mult, op1=ALU.add),
                     reads=[St.key, pu.key], writes=[St.key])
            pO = [ps(), ps()]
            for c in range(CPB):
                P.op("pe", lambda e, c=c: e.matmul(pO[c // 4].v((c % 4) * 128, [[1, 128]], Pn=64), lhsT=SM.v(c * 64, [[1, 64]], Pn=64),
                                                   rhs=v_tm.v(c * 128, [[1, 128]], Pn=64), start=True, stop=False),
                     reads=[SM.key, v_tm.key], writes=[pO[c // 4].key])
                P.op("pe", lambda e, c=c: e.matmul(pO[c // 4].v((c % 4) * 128, [[1, 128]], Pn=64), lhsT=fmv.v(c * 64, [[1, 64]]),
                                                   rhs=St.v(c * 128, [[1, 128]]), start=False, stop=True),
                     reads=[fmv.key, St.key], writes=[pO[c // 4].key])
            for hb in range((CPB + 3) // 4):
                n = min(4, CPB - hb * 4)
                P.op("act", lambda e, hb=hb, n=n: e.activation(out=o_t.v(hb * 512, [[1, n * 128]], Pn=64), in_=pO[hb].v(0, [[1, n * 128]], Pn=64),
                                                               func=AF.Copy), reads=[pO[hb].key], writes=[o_t.key])
            rms_tm(ph, o_t, CPB, 128, nrmb.v(0, [[0, CPB], [1, 128]], Pn=64), nrmb.key, center=True)
            pY = ps()
            for c in range(CPB):
                P.op("pe", lambda e, c=c, pY=pY: e.transpose(pY.v(c * 64, [[1, 64]]), o_t.v(c * 128, [[1, 128]], Pn=64), c64("id")),
                     reads=[o_t.key, "C64"], writes=[pY.key])
            P.op("dve", lambda e, pY=pY: e.tensor_tensor(out=ytb.v(0, [[1, BLK]]), in0=pY.v(0, [[1, BLK]]), in1=fmz.v(0, [[1, BLK]]), op=ALU.mult),
                 reads=[pY.key, fmz.key], writes=[ytb.key])
            store_yT(ytb, 4 + h, 1, tok0, s)

    def ssd_load1(slot, l, g):
        base = l * D * P_IN
        ld_w(slot, 0, 512, dr["w_in"], base + O_CXBC + g * 512, P_IN)
        ld_w(slot, 4096, 128, dr["w_in"], base + O_CXBC + 1024 + g * 128, P_IN)
        ld_w(slot, 5120, 128, dr["w_in"], base + O_CXBC + 1280 + g * 128, P_IN)
        ld_w(slot, 6144, 8, dr["w_in"], base + O_CDT + g * 8, P_IN)

    def ssd_load2(slot, l, g):
        ld_w(slot, 0, 512, dr["w_in"], l * D * P_IN + O_CZ + g * 512, P_IN)

    def ssd_run(slot, l, g, s):
        enter(S_)
        ph = S_
        zslot = slot
        slot = 1 - slot
        fx, fmq, fmk, tA, Sc, dt_t, dta, lc, wr_t, elc, decb, nrmc, dsk, ytb, szb = [S_.t[n] for n in ['fx', 'fmq', 'fmk', 'tA', 'Sc', 'dt_t', 'dta', 'lc', 'wr_t', 'elc', 'decb', 'nrmc', 'dsk', 'ytb', 'szb']]
        small(nrmc.v(0, [[1, 512]], Pn=64), dv(dr["norm_c"], l * 1024 + g * 512, [[0, 64], [1, 512]]), nrmc.key, chan="rp")
        P.op("dve", lambda e: e.tensor_tensor(out=dsk.v(0, [[64, 8], [1, 64]], Pn=64), in0=C64.v(off64["id"], [[0, 8], [1, 64]], Pn=64),
                                              in1=pc.v(l * 48 + 32 + g * 8, [[1, 8], [0, 64]], Pn=64), op=ALU.mult), reads=["C64", "pc"], writes=[dsk.key])
        for b in range(NBLK):
            tok0 = b * BLK
            for j in range(4):
                pb = ps()
                proj_fm(slot, 0, 512, j * 128, tok0, BLK, pb)
                ch = g * 4 + j
                conv_stream(ph, pb, j, b, lambda k, ch=ch: cwc.v(l * 48 + ch * 4 + k, [[1, 1]]), fx.v(j * BLK, [[1, BLK]]), fx.key,
                            bias_ap=cbc.v(l * 12 + ch, [[1, 1]]))
            if SSTOP <= 0.1:
                continue
            for j, (woff, ch, dst) in enumerate([(4096, 8 + g, fmk), (5120, 10 + g, fmq)]):
                pb = ps()
                proj_fm(slot, woff, 128, 0, tok0, BLK, pb)
                conv_stream(ph, pb, 4 + j, b, lambda k, ch=ch: cwc.v(l * 48 + ch * 4 + k, [[1, 1]]), dst.v(0, [[1, BLK]]), dst.key,
                            bias_ap=cbc.v(l * 12 + ch, [[1, 1]]))
            if SSTOP <= 0.2:
                continue
            for j in range(4):
                pZ = ps()
                proj_fm(zslot, 0, 512, j * 128, tok0, BLK, pZ)
                P.op("act", lambda e, pZ=pZ, j=j: e.activation(out=szb.v(j * BLK, [[1, BLK]]), in_=pZ.v(0, [[1, BLK]]), func=AF.Silu),
                     reads=[pZ.key], writes=[szb.key])
            pd = ps()
            for c in range(CPB):
                proj_tm(slot, 6144, 8, 0, 8, tok0 + c * 64, pd, pcol=c * 8)
            P.op("dve", lambda e, pd=pd: e.tensor_tensor(out=dt_t.v(0, [[8, CPB], [1, 8]], Pn=64), in0=pd.v(0, [[8, CPB], [1, 8]], Pn=64),
                                                         in1=pc.v(l * 48 + g * 8, [[0, CPB], [1, 8]], Pn=64), op=ALU.add),
                 reads=[pd.key, "pc"], writes=[dt_t.key])
            P.op("act", lambda e: e.activation(out=dt_t.v(0, [[1, CPB * 8]], Pn=64), in_=dt_t.v(0, [[1, CPB * 8]], Pn=64), func=AF.Exp),
                 reads=[dt_t.key], writes=[dt_t.key])
            P.op("act", lambda e: e.activation(out=dt_t.v(0, [[1, CPB * 8]], Pn=64), in_=dt_t.v(0, [[1, CPB * 8]], Pn=64), func=AF.Ln,
                                               bias=one_ap[0:64, :]), reads=[dt_t.key, "epst"], writes=[dt_t.key])
            P.op("dve", lambda e: e.tensor_tensor(out=dta.v(0, [[8, CPB], [1, 8]], Pn=64), in0=dt_t.v(0, [[8, CPB], [1, 8]], Pn=64),
                                                  in1=nac.v(l * 16 + g * 8, [[0, CPB], [1, 8]], Pn=64), op=ALU.mult),
                 reads=[dt_t.key, "nac"], writes=[dta.key])
            if SSTOP <= 0.3:
                continue
            pl = ps()
            P.op("pe", lambda e, pl=pl: e.matmul(pl.v(0, [[1, CPB * 8]], Pn=64), lhsT=c64("tri"), rhs=dta.v(0, [[1, CPB * 8]], Pn=64),
                                                 start=True, stop=True), reads=[dta.key, "C64"], writes=[pl.key])
            P.op("pe", lambda e, pl=pl: e.matmul(pl.v(128, [[1, CPB * 8]]), lhsT=c64("ones"), rhs=dta.v(0, [[1, CPB * 8]], Pn=64),
                                                 start=True, stop=True), reads=[dta.key, "C64"], writes=[pl.key])
            P.op("dve", lambda e, pl=pl: e.tensor_copy(out=lc.v(0, [[1, CPB * 8]], Pn=64), in_=pl.v(0, [[1, CPB * 8]], Pn=64)),
                 reads=[pl.key], writes=[lc.key])
            if SSTOP <= 0.45:
                continue
            P.op("dve", lambda e, pl=pl: e.tensor_copy(out=decb.v(0, [[1, CPB * 8]]), in_=pl.v(128, [[1, CPB * 8]])),
                 reads=[pl.key], writes=[decb.key])
            P.op("act", lambda e: e.activation(out=decb.v(0, [[1, CPB * 8]]), in_=decb.v(0, [[1, CPB * 8]]), func=AF.Exp),
                 reads=[decb.key], writes=[decb.key])
            if SSTOP <= 0.5:
                continue
            P.op("dve", lambda e, pl=pl: e.tensor_tensor(out=wr_t.v(0, [[1, CPB * 8]], Pn=64), in0=pl.v(128, [[1, CPB * 8]], Pn=64),
                                                         in1=lc.v(0, [[1, CPB * 8]], Pn=64), op=ALU.subtract), reads=[pl.key, lc.key], writes=[wr_t.key])
            P.op("act", lambda e: e.activation(out=wr_t.v(0, [[1, CPB * 8]], Pn=64), in_=wr_t.v(0, [[1, CPB * 8]], Pn=64), func=AF.Exp),
                 reads=[wr_t.key], writes=[wr_t.key])
            P.op("dve", lambda e: e.tensor_tensor(out=wr_t.v(0, [[1, CPB * 8]], Pn=64), in0=wr_t.v(0, [[1, CPB * 8]], Pn=64),
                                                  in1=dt_t.v(0, [[1, CPB * 8]], Pn=64), op=ALU.mult), reads=[wr_t.key, dt_t.key], writes=[wr_t.key])
            P.op("act", lambda e: e.activation(out=elc.v(0, [[1, CPB * 8]], Pn=64), in_=lc.v(0, [[1, CPB * 8]], Pn=64), func=AF.Exp),
                 reads=[lc.key], writes=[elc.key])
            if SSTOP <= 1:
                continue
            if b == 0:
                P.op("dve", lambda e: e.memset(Sc.v(0, [[1, 512]]), 0.0), reads=[Sc.key], writes=[Sc.key])
            def stage1(c):
                    ct = tok0 + c * 64
                    db, e1, x_tm, xw, yi, yg, cbT, B_tm, st2 = [S_.t[n + str(c % 2)] for n in ['db', 'e1', 'x_tm', 'xw', 'yi', 'yg', 'cbT', 'B_tm', 'st2']]
                    P.op("dve", lambda e, c=c: e.tensor_copy(out=db.v(0, [[64, 8], [1, 64]], Pn=64), in_=dta.v(c * 8, [[1, 8], [0, 64]], Pn=64)),
                         reads=[dta.key], writes=[db.key])
                    pL = ps()
                    for hh in range(8):
                        P.op("pe", lambda e, hh=hh, pL=pL: e.matmul(pL.v(hh * 64, [[1, 64]], Pn=64), lhsT=db.v(hh * 64, [[1, 64]], Pn=64),
                                                                    rhs=c64("tri"), start=True, stop=True), reads=[db.key, "C64"], writes=[pL.key])
                    P.op("dve", lambda e, c=c, pL=pL: e.tensor_tensor(out=e1.v(0, [[64, 8], [1, 64]], Pn=64), in0=pL.v(0, [[64, 8], [1, 64]], Pn=64),
                                                                      in1=lc.v(c * 8, [[1, 8], [0, 64]], Pn=64), op=ALU.subtract),
                         reads=[pL.key, lc.key], writes=[e1.key])
                    P.op("dve", lambda e: e.scalar_tensor_tensor(out=e1.v(0, [[1, 512]], Pn=64), in0=e1.v(0, [[1, 512]], Pn=64), scalar=-1.0,
                                                                 in1=e1.v(0, [[1, 512]], Pn=64), op0=ALU.mult, op1=ALU.max),
                         reads=[e1.key], writes=[e1.key])
                    P.op("act", lambda e: e.activation(out=e1.v(0, [[1, 512]], Pn=64), in_=e1.v(0, [[1, 512]], Pn=64), func=AF.Exp, scale=-1.0),
                         reads=[e1.key], writes=[e1.key])
                    if SSTOP <= 2:
                        return
                    pc_ = ps()
                    P.op("pe", lambda e, c=c, pc_=pc_: e.matmul(pc_.v(0, [[1, 64]], Pn=64), lhsT=fmk.v(c * 64, [[1, 64]]), rhs=fmq.v(c * 64, [[1, 64]]),
                                                                start=True, stop=True), reads=[fmk.key, fmq.key], writes=[pc_.key])
                    P.op("act", lambda e, pc_=pc_: e.activation(out=cbT.v(0, [[1, 64]], Pn=64), in_=pc_.v(0, [[1, 64]], Pn=64), func=AF.Copy),
                         reads=[pc_.key], writes=[cbT.key])
                    P.op("dve", lambda e: e.tensor_tensor(out=e1.v(0, [[64, 8], [1, 64]], Pn=64), in0=e1.v(0, [[64, 8], [1, 64]], Pn=64),
                                                          in1=cbT.v(0, [[0, 8], [1, 64]], Pn=64), op=ALU.mult), reads=[e1.key, cbT.key], writes=[e1.key])
                    P.op("dve", lambda e, c=c: e.tensor_tensor(out=e1.v(0, [[64, 8], [1, 64]], Pn=64), in0=e1.v(0, [[64, 8], [1, 64]], Pn=64),
                                                               in1=dt_t.v(c * 8, [[1, 8], [0, 64]], Pn=64), op=ALU.mult), reads=[e1.key, dt_t.key], writes=[e1.key])
                    P.op("dve", lambda e: e.tensor_tensor(out=e1.v(0, [[1, 512]], Pn=64), in0=e1.v(0, [[1, 512]], Pn=64),
                                                          in1=dsk.v(0, [[1, 512]], Pn=64), op=ALU.add), reads=[e1.key, dsk.key], writes=[e1.key])
                    if SSTOP <= 3:
                        return
                    pX = ps()
                    for j in range(4):
                        P.op("pe", lambda e, c=c, j=j, pX=pX: e.transpose(pX.v(j * 128, [[1, 128]], Pn=64), fx.v(j * BLK + c * 64, [[1, 64]]), c128("id")),
                             reads=[fx.key, "C128"], writes=[pX.key])
                    P.op("act", lambda e, pX=pX: e.activation(out=x_tm.v(0, [[1, 512]], Pn=64), in_=pX.v(0, [[1, 512]], Pn=64), func=AF.Copy),
                         reads=[pX.key], writes=[x_tm.key])
                    P.op("dve", lambda e, c=c, pX=pX: e.tensor_tensor(out=xw.v(0, [[64, 8], [1, 64]], Pn=64), in0=pX.v(0, [[64, 8], [1, 64]], Pn=64),
                                                                      in1=wr_t.v(c * 8, [[1, 8], [0, 64]], Pn=64), op=ALU.mult),
                         reads=[pX.key, wr_t.key], writes=[xw.key])
                    pBt = ps()
                    P.op("pe", lambda e, c=c, pBt=pBt: e.transpose(pBt.v(0, [[1, 128]], Pn=64), fmk.v(c * 64, [[1, 64]]), c128("id")),
                         reads=[fmk.key, "C128"], writes=[pBt.key])
                    P.op("act", lambda e, pBt=pBt: e.activation(out=B_tm.v(0, [[1, 128]], Pn=64), in_=pBt.v(0, [[1, 128]], Pn=64), func=AF.Copy),
                         reads=[pBt.key], writes=[B_tm.key])
                    pI = ps()
                    for hh in range(8):
                        P.op("pe", lambda e, hh=hh, pI=pI: e.matmul(pI.v(hh * 64, [[1, 64]], Pn=64), lhsT=e1.v(hh * 64, [[1, 64]], Pn=64),
                                                                    rhs=x_tm.v(hh * 64, [[1, 64]], Pn=64), start=True, stop=True),
                             reads=[e1.key, x_tm.key], writes=[pI.key])
                    pN = ps()
                    for qq in range(4):
                        P.op("pe", lambda e, c=c, pN=pN, qq=qq: e.matmul(pN.v(qq * 128, [[1, 128]], Pn=64), lhsT=fmq.v(c * 64, [[1, 64]]),
                                                                         rhs=Sc.v(qq * 128, [[1, 128]]), start=True, stop=True),
                             reads=[fmq.key, Sc.key], writes=[pN.key])
                    P.op("act", lambda e, pI=pI: e.activation(out=yi.v(0, [[1, 512]], Pn=64), in_=pI.v(0, [[1, 512]], Pn=64), func=AF.Copy),
                         reads=[pI.key], writes=[yi.key])
                    P.op("dve", lambda e, c=c, pN=pN: e.tensor_tensor(out=yg.v(0, [[64, 8], [1, 64]], Pn=64), in0=pN.v(0, [[64, 8], [1, 64]], Pn=64),
                                                                      in1=elc.v(c * 8, [[1, 8], [0, 64]], Pn=64), op=ALU.mult),
                         reads=[pN.key, elc.key], writes=[yg.key])
                    P.op("dve", lambda e: e.tensor_tensor(out=yg.v(0, [[1, 512]], Pn=64), in0=yg.v(0, [[1, 512]], Pn=64), in1=yi.v(0, [[1, 512]], Pn=64),
                                                          op=ALU.add), reads=[yg.key, yi.key], writes=[yg.key])
                    if SSTOP <= 4:
                        return
                    pS_ = ps()
                    for qq in range(4):
                        P.op("pe", lambda e, pS_=pS_, qq=qq: e.matmul(pS_.v(qq * 128, [[1, 128]]), lhsT=B_tm.v(0, [[1, 128]], Pn=64),
                                                                      rhs=xw.v(qq * 128, [[1, 128]], Pn=64), start=True, stop=True),
                             reads=[B_tm.key, xw.key], writes=[pS_.key])
                    P.op("dve", lambda e, c=c: e.tensor_tensor(out=Sc.v(0, [[64, 8], [1, 64]]), in0=Sc.v(0, [[64, 8], [1, 64]]),
                                                               in1=decb.v(c * 8, [[1, 8], [0, 64]]), op=ALU.mult), reads=[Sc.key, decb.key], writes=[Sc.key])
                    P.op("dve", lambda e, pS_=pS_: e.tensor_tensor(out=Sc.v(0, [[1, 512]]), in0=Sc.v(0, [[1, 512]]), in1=pS_.v(0, [[1, 512]]), op=ALU.add),
                         reads=[Sc.key, pS_.key], writes=[Sc.key])

            def stage2(c):
                    ct = tok0 + c * 64
                    db, e1, x_tm, xw, yi, yg, cbT, B_tm, st2 = [S_.t[n + str(c % 2)] for n in ['db', 'e1', 'x_tm', 'xw', 'yi', 'yg', 'cbT', 'B_tm', 'st2']]
                    pZ = ps()
                    for j in range(4):
                        P.op("pe", lambda e, j=j, pZ=pZ: e.transpose(pZ.v(j * 128, [[1, 128]], Pn=64), szb.v(j * BLK + c * 64, [[1, 64]]), c128("id")),
                             reads=[szb.key, "C128"], writes=[pZ.key])
                    P.op("dve", lambda e, pZ=pZ: e.tensor_tensor(out=yg.v(0, [[1, 512]], Pn=64), in0=yg.v(0, [[1, 512]], Pn=64), in1=pZ.v(0, [[1, 512]], Pn=64),
                                                                 op=ALU.mult), reads=[yg.key, pZ.key], writes=[yg.key])
                    P.op("act", lambda e: e.activation(out=yi.v(0, [[1, 512]], Pn=64), in_=yg.v(0, [[1, 512]], Pn=64), func=AF.Square),
                         reads=[yg.key], writes=[yi.key])
                    P.op("dve", lambda e: e.tensor_reduce(out=st2.v(0, [[1, 1]], Pn=64), in_=yi.v(0, [[1, 512]], Pn=64), axis=AX.X, op=ALU.add),
                         reads=[yi.key], writes=[st2.key])
                    P.op("act", lambda e: e.activation(out=st2.v(0, [[1, 1]], Pn=64), in_=st2.v(0, [[1, 1]], Pn=64), func=AF.Ln,
                                                       bias=eps_ap[0:64, :], scale=1.0 / 512), reads=[st2.key, "epst"], writes=[st2.key])
                    P.op("act", lambda e: e.activation(out=st2.v(0, [[1, 1]], Pn=64), in_=st2.v(0, [[1, 1]], Pn=64), func=AF.Exp, scale=-0.5),
                         reads=[st2.key], writes=[st2.key])
                    P.op("dve", lambda e: e.scalar_tensor_tensor(out=yg.v(0, [[1, 512]], Pn=64), in0=yg.v(0, [[1, 512]], Pn=64), scalar=st2.v(0, [[1, 1]], Pn=64),
                                                                 in1=nrmc.v(0, [[1, 512]], Pn=64), op0=ALU.mult, op1=ALU.mult),
                         reads=[yg.key, st2.key, nrmc.key], writes=[yg.key])
                    pY = ps()
                    for j in range(4):
                        P.op("pe", lambda e, j=j, pY=pY: e.transpose(pY.v(j * 64, [[1, 64]]), yg.v(j * 128, [[1, 128]], Pn=64), c64("id")),
                             reads=[yg.key, "C64"], writes=[pY.key])
                    P.op("act", lambda e, c=c, pY=pY: e.activation(out=ytb.v(c * 64, [[BLK, 4], [1, 64]]), in_=pY.v(0, [[64, 4], [1, 64]]), func=AF.Copy),
                         reads=[pY.key], writes=[ytb.key])

            stage1(0)
            for c in range(CPB):
                A = P.capture(lambda: with_banks([0, 1, 2], lambda: stage2(c)))
                B = P.capture(lambda: with_banks([3, 4, 5, 6, 7], lambda: stage1(c + 1))) if c + 1 < CPB else []
                P.merge(A, B)
            store_yT(ytb, 8 + g * 4, 4, tok0, s)

    def mrg_load(slot, l, dc):
        base = l * D * P_IN
        for br in range(3):
            ld_w(slot, br * 1024, 128, dr["w_in"], base + O_GATE + br * 1024 + dc * 128, P_IN)
        ld_w(slot, 3072, 128, dr["w_branch_a"], l * 512 * D + dc * 128, D, nk=4)
        ld_w(slot, 3072 + 512, 128, dr["w_branch_b"], l * 512 * D + dc * 128, D, nk=4)
        ld_w(slot, 3072 + 1024, 128, dr["w_branch_c"], l * 1024 * D + dc * 128, D, nk=8)

    mrg_cnt = [0]

    def mrg_run(slot, l, dc, s):
        enter(M_)
        tA, tB = [M_.t[n] for n in ['tA', 'tB']]
        for b in range(NBLK):
            tok0 = b * BLK
            ytl = M_.t["ytl%d" % (mrg_cnt[0] % 2)]
            mrg_cnt[0] += 1
            P.dma("sp", lambda e, tok0=tok0: e.dma_start(out=ytl.v(0, [[BLK, 16], [1, BLK]]),
                                                         in_=dv(ytd, (s % 2) * 128 * 16 * T + tok0, [[16 * T, 128], [T, 16], [1, BLK]])),
                  reads=["ytd%d_%d_%d" % (s % 2, i, b) for i in range(16)], writes=[ytl.key], chan=ytl.key)
            first = True
            for br, (f0, nf, woff) in enumerate([(0, 4, 3072), (4, 4, 3072 + 512), (8, 8, 3072 + 1024)]):
                pg = ps()
                proj_fm(slot, br * 1024, 128, 0, tok0, BLK, pg)
                P.op("act", lambda e, pg=pg, br=br: e.activation(out=tA.v(0, [[1, BLK]]), in_=pg.v(0, [[1, BLK]]), func=AF.Sigmoid,
                                                                 bias=bgt.v(l * 24 + br * 8 + dc, [[1, 1]])), reads=[pg.key, "bgt"], writes=[tA.key])
                pb = ps()
                for k in range(nf):
                    P.op("pe", lambda e, k=k, pb=pb, woff=woff, f0=f0, nf=nf: e.matmul(pb.v(0, [[1, BLK]]), lhsT=WB[slot].v(woff + k * 128, [[1, 128]]),
                                                                                       rhs=ytl.v((f0 + k) * BLK, [[1, BLK]]), start=(k == 0), stop=(k == nf - 1)),
                         reads=["WB%d" % slot, ytl.key], writes=[pb.key])
                if first:
                    P.op("dve", lambda e, pb=pb: e.tensor_tensor(out=tB.v(0, [[1, BLK]]), in0=pb.v(0, [[1, BLK]]), in1=tA.v(0, [[1, BLK]]), op=ALU.mult),
                         reads=[pb.key, tA.key], writes=[tB.key])
                    first = False
                else:
                    P.op("dve", lambda e, pb=pb: e.tensor_tensor(out=tA.v(0, [[1, BLK]]), in0=pb.v(0, [[1, BLK]]), in1=tA.v(0, [[1, BLK]]), op=ALU.mult),
                         reads=[pb.key, tA.key], writes=[tA.key])
                    if br == 1:
                        P.op("dve", lambda e: e.tensor_tensor(out=tB.v(0, [[1, BLK]]), in0=tB.v(0, [[1, BLK]]), in1=tA.v(0, [[1, BLK]]), op=ALU.add),
                             reads=[tA.key, tB.key], writes=[tB.key])
                    else:
                        P.op("dve", lambda e, tok0=tok0: e.tensor_tensor(out=mrgT.v(dc * T + tok0, [[1, BLK]]), in0=tB.v(0, [[1, BLK]]),
                                                                         in1=tA.v(0, [[1, BLK]]), op=ALU.add),
                             reads=[tA.key, tB.key], writes=["mrg%d" % b])


    def ln_tile(ph, src_ap, src_keys, l, which, tile, s, final, do_router):
        par = str(tile % 2)
        lng, lnb = ph.t['lng'], ph.t['lnb']
        xn, bst, mv = ph.t['xn' + par], ph.t['bst' + par], ph.t['mv' + par]
        xTf = ph.t.get('xTf' + par)
        if WSTOP <= 1:
            return
        for hh in range(2):
            P.op("dve", lambda e, hh=hh: e.bn_stats(out=bst.v(hh * 6, [[1, 6]]), in_=src_ap[:, hh * 512:(hh + 1) * 512]), reads=src_keys, writes=[bst.key])
        P.op("dve", lambda e: e.bn_aggr(out=mv.v(0, [[1, 2]]), in_=bst.v(0, [[1, 12]])), reads=[bst.key], writes=[mv.key])
        P.op("act", lambda e: e.activation(out=mv.v(2, [[1, 1]]), in_=mv.v(1, [[1, 1]]), func=AF.Ln, bias=eps_ap, scale=1.0), reads=[mv.key, "epst"], writes=[mv.key])
        P.op("act", lambda e: e.activation(out=mv.v(2, [[1, 1]]), in_=mv.v(2, [[1, 1]]), func=AF.Exp, scale=-0.5), reads=[mv.key], writes=[mv.key])
        P.op("dve", lambda e: e.tensor_scalar(out=xn.v(0, [[1, 1024]]), in0=src_ap, scalar1=mv.v(0, [[1, 1]]), scalar2=mv.v(2, [[1, 1]]),
                                              op0=ALU.subtract, op1=ALU.mult), reads=list(src_keys) + [mv.key], writes=[xn.key])
        P.op("dve", lambda e: e.tensor_tensor(out=xn.v(0, [[1, 1024]]), in0=xn.v(0, [[1, 1024]]), in1=lng.v(0, [[1, 1024]]), op=ALU.mult),
             reads=[xn.key, lng.key], writes=[xn.key])
        P.op("dve", lambda e: e.tensor_tensor(out=xn.v(0, [[1, 1024]]), in0=xn.v(0, [[1, 1024]]), in1=lnb.v(0, [[1, 1024]]), op=ALU.add),
             reads=[xn.key, lnb.key], writes=[xn.key])
        if WSTOP <= 2:
            return
        if final:
            o = P.dma("sp", lambda e: e.dma_start(out=dv(yout, (s * T + tile * 128) * D, [[D, 128], [1, D]]), in_=xn.v(0, [[1, 1024]])),
                      reads=[xn.key], writes=["yout%d_%d" % (s, tile)], chan="out" + par)
            outs.append(o)
        else:
            P.dma("sp", lambda e: e.dma_start(out=dv(xres[which], tile * 128 * D, [[D, 128], [1, D]]), in_=xn.v(0, [[1, 1024]])),
                  reads=[xn.key], writes=["xres%d_p%s" % (which, par)], chan="xr%d_%s" % (which, par))
            if WSTOP <= 2.5:
                return
            for half in range(2):
                pt = ps()
                for k in range(4):
                    kc = half * 4 + k
                    P.op("pe", lambda e, k=k, kc=kc, pt=pt: e.transpose(pt.v(k * 128, [[1, 128]]), xn.v(kc * 128, [[1, 128]]), c128("id")),
                         reads=[xn.key, "C128"], writes=[pt.key])
                P.op("act", lambda e, half=half, pt=pt: e.activation(out=xT.v(half * 4 * T + tile * 128, [[T, 4], [1, 128]]), in_=pt.v(0, [[128, 4], [1, 128]]),
                                                                     func=AF.Copy), reads=[pt.key], writes=["xT%d" % tile])
                if do_router and WSTOP > 2.7:
                    P.op("dve", lambda e, half=half, pt=pt: e.tensor_copy(out=xTf.v(half * 512, [[1, 512]]), in_=pt.v(0, [[1, 512]])),
                         reads=[pt.key], writes=[xTf.key])
            if do_router and WSTOP > 3:
                router(ph, l, tile)

    outs = []

    def router(ph, l, tile):
        xTf, rt = ph.t['xTf' + str(tile % 2)], ph.t['rt' + str(tile % 2)]
        pr = ps()
        for kc in range(KC):
            P.op("pe", lambda e, kc=kc, pr=pr: e.matmul(pr.v(0, [[1, 36]]), lhsT=xTf.v(kc * 128, [[1, 128]]), rhs=wr.v(l * KC * 36 + kc * 36, [[1, 36]]),
                                                        start=(kc == 0), stop=(kc == KC - 1)), reads=[xTf.key, "wr"], writes=[pr.key])
        R = lambda o, n: rt.v(o, [[1, n]])
        P.op("dve", lambda e: e.tensor_tensor(out=R(0, 36), in0=pr.v(0, [[1, 36]]), in1=brt.v(l * 36, [[1, 36]]), op=ALU.add), reads=[pr.key, "brt"], writes=[rt.key])
        k = [rt.key]
        P.op("dve", lambda e: e.tensor_reduce(out=R(36, 1), in_=R(0, 4), axis=AX.X, op=ALU.max), reads=k, writes=k)
        P.op("dve", lambda e: e.tensor_scalar(out=R(37, 4), in0=R(0, 4), scalar1=R(36, 1), scalar2=None, op0=ALU.is_ge), reads=k, writes=k)
        P.op("dve", lambda e: e.tensor_scalar(out=R(41, 4), in0=R(0, 4), scalar1=R(36, 1), scalar2=None, op0=ALU.subtract), reads=k, writes=k)
        P.op("act", lambda e: e.activation(out=R(41, 4), in_=R(41, 4), func=AF.Exp), reads=k, writes=k)
        P.op("dve", lambda e: e.tensor_reduce(out=R(45, 1), in_=R(41, 4), axis=AX.X, op=ALU.add), reads=k, writes=k)
        P.op("dve", lambda e: e.reciprocal(out=R(46, 1), in_=R(45, 1)), reads=k, writes=k)
        P.op("dve", lambda e: e.tensor_scalar(out=R(41, 4), in0=R(37, 4), scalar1=-1.0, scalar2=BIGM, op0=ALU.add, op1=ALU.mult), reads=k, writes=k)
        P.op("dve", lambda e: e.tensor_tensor(out=rt.v(48, [[8, 4], [1, 8]]), in0=rt.v(4, [[8, 4], [1, 8]]), in1=rt.v(41, [[1, 4], [0, 8]]), op=ALU.add),
             reads=k, writes=k)
        P.op("dve", lambda e: e.tensor_reduce(out=R(80, 1), in_=R(48, 32), axis=AX.X, op=ALU.max), reads=k, writes=k)
        P.op("dve", lambda e: e.tensor_scalar(out=R(82, 32), in0=R(48, 32), scalar1=R(80, 1), scalar2=None, op0=ALU.is_ge), reads=k, writes=k)
        P.op("dve", lambda e: e.scalar_tensor_tensor(out=R(48, 32), in0=R(82, 32), scalar=-BIGM, in1=R(48, 32), op0=ALU.mult, op1=ALU.add), reads=k, writes=k)
        P.op("dve", lambda e: e.tensor_reduce(out=R(81, 1), in_=R(48, 32), axis=AX.X, op=ALU.max), reads=k, writes=k)
        P.op("dve", lambda e: e.tensor_scalar(out=R(114, 32), in0=R(48, 32), scalar1=R(81, 1), scalar2=None, op0=ALU.is_ge), reads=k, writes=k)
        P.op("dve", lambda e: e.tensor_tensor(out=R(146, 1), in0=R(81, 1), in1=R(80, 1), op=ALU.subtract), reads=k, writes=k)
        P.op("act", lambda e: e.activation(out=R(146, 1), in_=R(146, 1), func=AF.Exp), reads=k, writes=k)
        P.op("dve", lambda e: e.tensor_scalar(out=R(146, 1), in0=R(146, 1), scalar1=1.0, scalar2=None, op0=ALU.add), reads=k, writes=k)
        P.op("dve", lambda e: e.reciprocal(out=R(146, 1), in_=R(146, 1)), reads=k, writes=k)
        P.op("dve", lambda e: e.tensor_tensor(out=R(146, 1), in0=R(146, 1), in1=R(46, 1), op=ALU.mult), reads=k, writes=k)
        P.op("dve", lambda e: e.tensor_tensor(out=R(147, 1), in0=R(46, 1), in1=R(146, 1), op=ALU.subtract), reads=k, writes=k)
        P.op("dve", lambda e: e.tensor_scalar(out=R(82, 32), in0=R(82, 32), scalar1=R(146, 1), scalar2=None, op0=ALU.mult), reads=k, writes=k)
        P.op("dve", lambda e: e.scalar_tensor_tensor(out=comb.v(tile * 32, [[1, 32]]), in0=R(114, 32), scalar=R(147, 1), in1=R(82, 32),
                                                     op0=ALU.mult, op1=ALU.add), reads=k, writes=["comb%d" % tile])

    def load_ln(ph, l, which):
        lng, lnb = ph.t['lng'], ph.t['lnb']
        g, b_ = ("ln1_g", "ln1_b") if which == 1 else ("ln2_g", "ln2_b")
        P.dma("sp", lambda e: e.dma_start(out=lng.v(0, [[1, 1024]]), in_=dv(dr[g], l * D, [[0, 128], [1, D]])), writes=[lng.key], chan="lng")
        P.dma("sp", lambda e: e.dma_start(out=lnb.v(0, [[1, 1024]]), in_=dv(dr[b_], l * D, [[0, 128], [1, D]])), writes=[lnb.key], chan="lnb")

    def wout_load1(slot, l):
        ld_w(slot, 0, 512, dr["w_out"], l * D * D, D)

    def wout_load2(slot, l):
        ld_w(slot, 0, 512, dr["w_out"], l * D * D + 512, D)

    def wout_run(slot, l, s, src_dram_ap_fn, src_keys_fn):
        enter(W_)
        xl = [W_.t["xl0"], W_.t["xl1"]]
        slots = [1 - slot, slot]
        load_ln(W_, l, 1)
        def MM(tile):
            xo = xl[tile % 2]
            P.dma("sp", lambda e, tile=tile, xo=xo: e.dma_start(out=xo.v(0, [[1, 1024]]), in_=src_dram_ap_fn(tile)), reads=src_keys_fn(tile),
                  writes=[xo.key], chan="xl%d" % (tile % 2))
            for half in range(2):
                pm = ps()
                for kc in range(KC):
                    P.op("pe", lambda e, kc=kc, pm=pm, half=half, tile=tile: e.matmul(pm.v(0, [[1, 512]]), lhsT=mrgT.v(kc * T + tile * 128, [[1, 128]]),
                                                                                      rhs=WB[slots[half]].v(kc * 512, [[1, 512]]),
                                                                                      start=(kc == 0), stop=(kc == KC - 1)),
                         reads=["WB%d" % slots[half], "mrg%d" % (tile * 128 // BLK)], writes=[pm.key])
                P.op("dve", lambda e, pm=pm, half=half, xo=xo: e.scalar_tensor_tensor(out=xo.v(half * 512, [[1, 512]]), in0=xo.v(half * 512, [[1, 512]]),
                                                                                      scalar=ALPHA, in1=pm.v(0, [[1, 512]]), op0=ALU.mult, op1=ALU.add),
                     reads=[xo.key, pm.key], writes=[xo.key])

        MM(0)
        for tile in range(NT):
            if tile + 1 < NT:
                MM(tile + 1)
            xo = xl[tile % 2]
            ln_tile(W_, xo.v(0, [[1, 1024]]), [xo.key], l, 0, tile, s, False, True)

    def moe_load(slot, l, e_, hf):
        ld_w(slot, 0, 256, dr["w_gate_e"], ((l * NE + e_) * D) * DE + hf * 256, DE)
        ld_w(slot, 2048, 256, dr["w_up_e"], ((l * NE + e_) * D) * DE + hf * 256, DE)
        ld_w(slot, 4096, 1024, dr["w_down_e"], ((l * NE + e_) * DE + hf * 256) * D, D, nk=2)

    def moe_init(l, s):
        enter(E_)
        for tile in range(NT):
            P.dma("sp", lambda e, tile=tile: e.dma_start(out=yacc.v(tile * 1024, [[1, 1024]]), in_=dv(xres[0], tile * 128 * D, [[D, 128], [1, D]])),
                  reads=["xres0_p0", "xres0_p1"], writes=["yacc%d" % tile], chan="ya%d" % tile)
            P.op("pool", lambda e, tile=tile: e.tensor_scalar(out=yacc.v(tile * 1024, [[1, 1024]]), in0=yacc.v(tile * 1024, [[1, 1024]]), scalar1=ALPHA,
                                                               scalar2=None, op0=ALU.mult), reads=["yacc%d" % tile], writes=["yacc%d" % tile])

    def moe_run(slot, l, e_, hf, s):
        wk = "WB%d" % slot
        Hh = [E_.t["Hh0"], E_.t["Hh1"]]
        hs = [E_.t["hs0"], E_.t["hs1"]]
        def GU(b):
            tok0 = b * BLK
            H = Hh[b % 2]
            for k2 in range(2):
                pg = ps()
                pu = ps()
                for kc in range(KC):
                    P.op("pe", lambda e, kc=kc, pg=pg, k2=k2, tok0=tok0: e.matmul(pg.v(0, [[1, BLK]]), lhsT=WB[slot].v(kc * 256 + k2 * 128, [[1, 128]]),
                                                                                  rhs=xT.v(kc * T + tok0, [[1, BLK]]), start=(kc == 0), stop=(kc == KC - 1)),
                         reads=[wk] + xt_keys(tok0, BLK), writes=[pg.key])
                for kc in range(KC):
                    P.op("pe", lambda e, kc=kc, pu=pu, k2=k2, tok0=tok0: e.matmul(pu.v(0, [[1, BLK]]), lhsT=WB[slot].v(2048 + kc * 256 + k2 * 128, [[1, 128]]),
                                                                                  rhs=xT.v(kc * T + tok0, [[1, BLK]]), start=(kc == 0), stop=(kc == KC - 1)),
                         reads=[wk] + xt_keys(tok0, BLK), writes=[pu.key])
                hsx = hs[k2]
                P.op("act", lambda e, pg=pg, hsx=hsx: e.activation(out=hsx.v(0, [[1, BLK]]), in_=pg.v(0, [[1, BLK]]), func=AF.Silu), reads=[pg.key], writes=[hsx.key])
                P.op("dve", lambda e, pu=pu, hsx=hsx, H=H, k2=k2: e.tensor_tensor(out=H.v(k2 * BLK, [[1, BLK]]), in0=hsx.v(0, [[1, BLK]]), in1=pu.v(0, [[1, BLK]]),
                                                                                  op=ALU.mult), reads=[pu.key, hsx.key], writes=[H.key])
        def DN(b):
            H = Hh[b % 2]
            for t in range(TPB):
                tile = b * TPB + t
                for half in range(2):
                    py = ps()
                    for k2 in range(2):
                        P.op("pe", lambda e, k2=k2, py=py, t=t, half=half, H=H: e.matmul(py.v(0, [[1, 512]]), lhsT=H.v(k2 * BLK + t * 128, [[1, 128]]),
                                                                                         rhs=WB[slot].v(4096 + k2 * 1024 + half * 512, [[1, 512]]),
                                                                                         start=(k2 == 0), stop=(k2 == 1)), reads=[wk, H.key], writes=[py.key])
                    P.op("dve", lambda e, py=py, tile=tile, half=half: e.scalar_tensor_tensor(
                        out=yacc.v(tile * 1024 + half * 512, [[1, 512]]), in0=py.v(0, [[1, 512]]), scalar=comb.v(tile * 32 + e_, [[1, 1]]),
                        in1=yacc.v(tile * 1024 + half * 512, [[1, 512]]), op0=ALU.mult, op1=ALU.add),
                        reads=[py.key, "comb%d" % tile, "yacc%d" % tile], writes=["yacc%d" % tile])

        GU(0)
        for b in range(NBLK):
            if b + 1 < NBLK:
                GU(b + 1)
            DN(b)

    def ln2_run(l, s, final):
        enter(N_)
        load_ln(N_, l, 2)
        for tile in range(NT):
            ln_tile(N_, yacc.v(tile * 1024, [[1, 1024]]), ["yacc%d" % tile], l, 1, tile, s, final, False)

    def x0_run(s):
        enter(X_)
        xl = [X_.t["xl0"], X_.t["xl1"]]
        for tile in range(NT):
            xo = xl[tile % 2]
            P.dma("sp", lambda e, tile=tile, xo=xo: e.dma_start(out=xo.v(0, [[1, 1024]]), in_=dv(dr["x"], (s * T + tile * 128) * D, [[D, 128], [1, D]])),
                  writes=[xo.key], chan="xl%d" % (tile % 2))
            for half in range(2):
                pt = ps()
                for k in range(4):
                    kc = half * 4 + k
                    P.op("pe", lambda e, k=k, kc=kc, pt=pt, xo=xo: e.transpose(pt.v(k * 128, [[1, 128]]), xo.v(kc * 128, [[1, 128]]), c128("id")),
                         reads=[xo.key, "C128"], writes=[pt.key])
                P.op("act", lambda e, half=half, pt=pt, tile=tile: e.activation(out=xT.v(half * 4 * T + tile * 128, [[T, 4], [1, 128]]),
                                                                                in_=pt.v(0, [[128, 4], [1, 128]]), func=AF.Copy),
                     reads=[pt.key], writes=["xT%d" % tile])

    units = []
    for s in range(NSEQ):
        units.append((None, lambda slot, s=s: x0_run(s)))
        for l in range(L):
            for h in range(4):
                units.append((lambda slot, l=l, h=h: gdn_load(slot, l, h), lambda slot, l=l, h=h, s=s: gdn_run(slot, l, h, s)))
            for h in range(4):
                units.append((lambda slot, l=l, h=h: ret_load(slot, l, h), lambda slot, l=l, h=h, s=s: ret_run(slot, l, h, s)))
            for g in range(2):
                units.append((lambda slot, l=l, g=g: ssd_load1(slot, l, g), lambda slot: None))
                units.append((lambda slot, l=l, g=g: ssd_load2(slot, l, g), lambda slot, l=l, g=g, s=s: ssd_run(slot, l, g, s), True))
            for dc in range(8):
                units.append((lambda slot, l=l, dc=dc: mrg_load(slot, l, dc), lambda slot, l=l, dc=dc, s=s: mrg_run(slot, l, dc, s)))
            if l == 0:
                srcf = lambda tile, s=s: dv(dr["x"], (s * T + tile * 128) * D, [[D, 128], [1, D]])
                srck = lambda tile: []
            else:
                srcf = lambda tile: dv(xres[1], tile * 128 * D, [[D, 128], [1, D]])
                srck = lambda tile: ["xres1_p0", "xres1_p1"]
            units.append((lambda slot, l=l: wout_load1(slot, l), lambda slot: None))
            units.append((lambda slot, l=l: wout_load2(slot, l), lambda slot, l=l, s=s, srcf=srcf, srck=srck: wout_run(slot, l, s, srcf, srck), True))
            units.append((None, lambda slot, l=l, s=s: moe_init(l, s)))
            for e_ in range(NE):
                for hf in range(2):
                    units.append((lambda slot, l=l, e_=e_, hf=hf: moe_load(slot, l, e_, hf),
                                  lambda slot, l=l, e_=e_, hf=hf, s=s: moe_run(slot, l, e_, hf, s)))
            units.append((None, lambda slot, l=l, s=s: ln2_run(l, s, l == L - 1)))

    wl = [u for u in units if u[0] is not None]
    slot_of = {}
    k = 0
    for i, u in enumerate(units):
        if u[0] is not None:
            slot_of[i] = k % 2
            k += 1
    loaded = set()
    idxs = [i for i, u in enumerate(units) if u[0] is not None]

    def ensure_loaded(i):
        if i not in loaded:
            units[i][0](slot_of[i])
            loaded.add(i)

    kstop = int(os.environ.get("KSTOP", "100000"))
    for i, u in enumerate(units):
        if i >= kstop:
            break
        if u[0] is not None:
            ensure_loaded(i)
            nxt = [j for j in idxs if j > i]
            both = len(u) > 2
            if nxt and not both:
                ensure_loaded(nxt[0])
            u[1](slot_of[i])
            if nxt and both:
                ensure_loaded(nxt[0])
        else:
            u[1](None)

    P.emit(final_wait_ops=outs)
    st.close()
    return nc, (a64, a128, cs_np)


_CACHE = {}


def kernel(**inputs):
    NCORES = 8
    x = np.ascontiguousarray(inputs["x"], dtype=np.float32)
    Bt, T, _ = x.shape
    NSEQ = Bt // NCORES
    DEPTH = inputs["w_in"].shape[0]
    key = (NSEQ, T, DEPTH)
    if key not in _CACHE:
        _CACHE[key] = build(NSEQ, T, DEPTH, 512)
    nc, (a64, a128, cs_np) = _CACHE[key]
    shared = {k: np.ascontiguousarray(v, dtype=np.float32) for k, v in inputs.items() if k != "x"}
    shared["c64"] = a64
    shared["c128"] = a128
    shared["cs"] = cs_np
    in_maps = []
    for c in range(NCORES):
        m = dict(shared)
        m["x"] = np.ascontiguousarray(x[c * NSEQ:(c + 1) * NSEQ])
        in_maps.append(m)
    res = run_bass_kernel_spmd(nc, in_maps, core_ids=list(range(NCORES)))
    return np.concatenate([r["y"] for r in res.results], axis=0).astype(np.float32)
```

```python
import contextlib
import numpy as np
import concourse.bass as bass
import concourse.mybir as mybir
from concourse.bass_utils import run_bass_kernel_spmd

F32 = mybir.dt.float32
BF16 = mybir.dt.bfloat16
AF = mybir.ActivationFunctionType
ALU = mybir.AluOpType
AX = mybir.AxisListType

D = 1024
KC = 8
P_IN = 9752
O_AQKV, O_AZ, O_AA, O_AB = 0, 1536, 2048, 2052
O_BQ, O_BK, O_BV, O_BG = 2056, 2568, 3080, 3592
O_CZ, O_CXBC, O_CDT, O_GATE = 4104, 5128, 6664, 6680
EPS = 1e-6
ALPHA = 4.0 ** 0.25
NE = 32
DE = 512
BIGM = 30000.0
ENGS = ("pe", "act", "dve", "pool", "sp")


class _Rec:
    def __getattr__(self, name):
        def f(*a, **k):
            self.__dict__["call"] = (name, a, k)
            return self
        return f


class Prog:
    def __init__(self, nc):
        self.nc = nc
        self.ops = []
        self.last_w = {}
        self.readers = {}
        self.cap = None

    def _add(self, eng, fn, reads, writes, chan=None):
        rec = _Rec()
        fn(rec)
        call = rec.call
        if self.cap is not None:
            self.cap.append((eng, call, reads, writes, chan))
            return -1
        return self._commit(eng, call, reads, writes, chan)

    def capture(self, f):
        old = self.cap
        self.cap = []
        f()
        lst = self.cap
        self.cap = old
        return lst

    def merge(self, A, B):
        ia = ib = 0
        na, nb = len(A), len(B)
        while ia < na or ib < nb:
            if ib >= nb or (ia < na and ia * nb <= ib * na):
                self._commit(*A[ia]); ia += 1
            else:
                self._commit(*B[ib]); ib += 1

    def _commit(self, eng, call, reads, writes, chan=None):
        if self.cap is not None:
            self.cap.append((eng, call, reads, writes, chan))
            return -1
        fn = lambda e, call=call: getattr(e, call[0])(*call[1], **call[2])
        idx = len(self.ops)
        deps = set()
        for r in reads:
            w = self.last_w.get(r)
            if w is not None:
                deps.add(w)
            if isinstance(r, str) and r.startswith("pb"):
                for rd in self.readers.get(r, ()):
                    if self.ops[rd]["eng"] != eng:
                        deps.add(rd)
        for r in writes:
            w = self.last_w.get(r)
            if w is not None and not (chan is not None and self.ops[w]["chan"] == chan and self.ops[w]["eng"] == eng):
                deps.add(w)
            for rd in self.readers.get(r, ()):
                deps.add(rd)
        for r in reads:
            self.readers.setdefault(r, []).append(idx)
        for r in writes:
            self.last_w[r] = idx
            self.readers[r] = []
        deps.discard(idx)
        self.ops.append(dict(eng=eng, fn=fn, deps=deps, chan=chan, has_dep=False))
        return idx

    def op(self, eng, fn, reads=(), writes=()):
        return self._add(eng, fn, tuple(reads), tuple(writes))

    def dma(self, eng, fn, reads=(), writes=(), chan="d0"):
        return self._add(eng, fn, tuple(reads), tuple(writes), chan=chan)

    def emit(self, final_wait_ops=()):
        nc = self.nc
        ops = self.ops
        for i, o in enumerate(ops):
            nd = set()
            for d in o["deps"]:
                p = ops[d]
                if p["chan"] is None and o["chan"] is None and p["eng"] == "pe" and o["eng"] == "pe":
                    continue
                nd.add(d)
            o["deps"] = nd
            for d in nd:
                ops[d]["has_dep"] = True
        for d in final_wait_ops:
            ops[d]["has_dep"] = True
        eng_cnt = {e: 0 for e in ENGS}
        chan_cnt = {}
        chans = []
        for o in ops:
            if o["chan"] is not None:
                c = o["chan"]
                if c not in chan_cnt:
                    chan_cnt[c] = 0
                    chans.append(c)
                chan_cnt[c] += 16
                o["tok"] = (("chan", c), chan_cnt[c])
            elif o["has_dep"]:
                eng_cnt[o["eng"]] += 1
                o["tok"] = (("eng", o["eng"]), eng_cnt[o["eng"]])
            else:
                o["tok"] = None
        sem_keys = [("eng", e) for e in ENGS] + [("chan", c) for c in chans]
        with contextlib.ExitStack() as st:
            sems = {}
            for k in sem_keys:
                sems[k] = st.enter_context(nc.semaphore("s_%s_%s" % k))
            blk = st.enter_context(nc.Block())

            def run_engine(eng_name, eng_obj):
                waited = {}
                for i, o in enumerate(ops):
                    if o["eng"] != eng_name:
                        continue
                    need = {}
                    for d in o["deps"]:
                        k, v = ops[d]["tok"]
                        if need.get(k, 0) < v:
                            need[k] = v
                    for k, v in need.items():
                        if waited.get(k, 0) >= v:
                            continue
                        eng_obj.wait_ge(sems[k], v)
                        waited[k] = v
                    ins = o["fn"](eng_obj)
                    if o["tok"] is not None:
                        k, v = o["tok"]
                        ins.then_inc(sems[k], 16 if k[0] == "chan" else 1)
                if eng_name == "sp":
                    need = {}
                    for d in final_wait_ops:
                        k, v = ops[d]["tok"]
                        if need.get(k, 0) < v:
                            need[k] = v
                    for k, v in need.items():
                        eng_obj.wait_ge(sems[k], v)

            blk.sync(lambda e: run_engine("sp", e))
            blk.tensor(lambda e: run_engine("pe", e))
            blk.scalar(lambda e: run_engine("act", e))
            blk.vector(lambda e: run_engine("dve", e))
            blk.gpsimd(lambda e: run_engine("pool", e))


def host_consts(T, BLK):
    c64 = {}
    i = np.arange(64)
    c64["tri"] = (i[:, None] <= i[None, :]).astype(np.float32)
    c64["id"] = np.eye(64, dtype=np.float32)
    c64["ones"] = np.ones((64, 128), np.float32)
    c64["bigm"] = np.where(i[None, :] < i[:, None], 0.0, BIGM).astype(np.float32)
    lg = np.log1p(-np.exp2(-5.0 - np.arange(4, dtype=np.float32))).astype(np.float32)
    idx = i.astype(np.float32)
    dec = np.exp(lg[:, None, None] * np.abs(idx[:, None] - idx[None, :])).astype(np.float32)
    c64["rdec"] = np.transpose(dec, (1, 0, 2)).reshape(64, 256)
    c64["wd"] = np.exp(lg[None, :] * (63.0 - idx[:, None])).astype(np.float32)
    names64 = ["tri", "id", "ones", "bigm", "rdec", "wd"]
    off64 = {}
    o = 0
    for n in names64:
        off64[n] = o
        o += c64[n].shape[1]
    a64 = np.concatenate([c64[n] for n in names64], axis=1).astype(np.float32)
    c128 = {}
    c128["id"] = np.eye(128, dtype=np.float32)
    c128["ones"] = np.ones((128, 128), np.float32)
    rot = np.zeros((128, 128), np.float32)
    for m in range(64):
        rot[m + 64, m] = -1.0
    for m in range(64, 128):
        rot[m - 64, m] = 1.0
    c128["rot"] = rot
    rd = np.exp(lg[:, None] * (idx[None, :] + 1.0)).astype(np.float32)
    rdt = np.tile(rd, (1, BLK // 64))
    c128["rd"] = np.broadcast_to(rdt.reshape(1, 4 * BLK), (128, 4 * BLK)).astype(np.float32)
    cd = np.exp(lg * 64.0).astype(np.float32)
    names128 = ["id", "ones", "rot", "rd"]
    off128 = {}
    o = 0
    for n in names128:
        off128[n] = o
        o += c128[n].shape[1]
    a128 = np.concatenate([c128[n] for n in names128], axis=1).astype(np.float32)
    pos = np.arange(T, dtype=np.float32)
    inv_freq = (np.float32(10000.0) ** (-np.arange(0, 128, 2, dtype=np.float32) / np.float32(128))).astype(np.float32)
    ang = (pos[:, None] * inv_freq[None, :]).astype(np.float32)
    cos = np.cos(ang).astype(np.float32).T
    sin = np.sin(ang).astype(np.float32).T
    cs = np.concatenate([np.concatenate([cos, cos], 0), np.concatenate([sin, sin], 0)], axis=1).astype(np.float32)
    return a64, off64, a128, off128, cs, [float(x) for x in cd]


def build(NSEQ, T, DEPTH, BLK):
    import os
    SSTOP = float(os.environ.get("SSTOP", "100"))
    WSTOP = float(os.environ.get("WSTOP", "100"))
    NT = T // 128
    NBLK = T // BLK
    CPB = BLK // 64
    TPB = BLK // 128
    a64, off64, a128, off128, cs_np, cdec = host_consts(T, BLK)
    nc = bass.Bass("TRN2", target_bir_lowering=False)
    dr = {}

    def din(name, shape):
        dr[name] = nc.dram_tensor(name, list(shape), F32, kind="ExternalInput")
        return dr[name]

    din("x", [NSEQ, T, D])
    L = DEPTH
    specs = dict(w_in=[L, D, P_IN], conv_a=[L, 4, 1536], a_log_a=[L, 4], dt_bias_a=[L, 4], norm_a=[L, 128],
                 norm_b=[L, 512], conv_c=[L, 4, 1536], conv_bias_c=[L, 1536], dt_bias_c=[L, 16], a_log_c=[L, 16],
                 d_skip_c=[L, 16], norm_c=[L, 1024], b_gate=[L, 3, D], w_branch_a=[L, 512, D],
                 w_branch_b=[L, 512, D], w_branch_c=[L, 1024, D], w_out=[L, D, D], ln1_g=[L, D], ln1_b=[L, D],
                 w_router_group=[L, D, 4], b_router_group=[L, 4], w_router_expert=[L, D, 32],
                 b_router_expert=[L, 32], w_gate_e=[L, NE, D, DE], w_up_e=[L, NE, D, DE], w_down_e=[L, NE, DE, D],
                 ln2_g=[L, D], ln2_b=[L, D])
    for k, v in specs.items():
        din(k, v)
    din("c64", list(a64.shape))
    din("c128", list(a128.shape))
    din("cs", list(cs_np.shape))
    yout = nc.dram_tensor("y", [NSEQ, T, D], F32, kind="ExternalOutput")
    xres = [nc.dram_tensor("xres%d" % i, [T, D], F32, kind="Internal") for i in range(2)]
    ytd = nc.dram_tensor("ytd", [2, 128, 16, T], BF16, kind="Internal")

    P = Prog(nc)
    st = contextlib.ExitStack()

    class Tl:
        def __init__(self, h, shape, key, base=0, pstride=None):
            self.h = h
            self.shape = shape
            self.key = key
            self.base = base
            self.row = int(np.prod(shape[1:])) if pstride is None else pstride

        def v(self, off, dims, Pn=128, p0=0):
            return bass.AP(self.h, self.base + off + p0 * self.row, [[self.row, Pn]] + [list(d) for d in dims])

    def sb(name, shape, dt=F32):
        h = st.enter_context(nc.sbuf_tensor(name, list(shape), dt))
        return Tl(h, list(shape), name)

    def dv(t, off, dims):
        return bass.AP(t, off, [list(d) for d in dims])

    banks = [Tl(st.enter_context(nc.psum_tensor("pb%d" % i, [128, 512], F32)), [128, 512], "pb%d" % i)
             for i in range(8)]
    bank_i = [0]

    xT = sb("xT", [128, KC, T], BF16)
    WBSZ = 6208
    WB = [sb("WB%d" % i, [128, WBSZ], BF16) for i in range(2)]
    C64 = sb("C64", [64, a64.shape[1]])
    C128 = sb("C128", [128, 384])
    cwa = sb("cwa", [128, L, 12, 4])
    cwc = sb("cwc", [128, L, 12, 4])
    cbc = sb("cbc", [128, L, 12])
    bgt = sb("bgt", [128, L, 3, 8])
    pb_a = sb("pb_a", [128, L, 8])
    nrm_a = sb("nrm_a", [64, L, 128])
    pc = sb("pc", [64, L, 48])
    wr = sb("wr", [128, L, KC, 36])
    brt = sb("brt", [128, L, 36])
    comb = sb("comb", [128, NT, 32])
    nexa = sb("nexa", [128, L, 4])
    nac = sb("nac", [64, L, 16])
    epst = sb("epst", [128, 4])
    ARENA_W = 88 * 256
    ARh = st.enter_context(nc.sbuf_tensor("AR", [128, ARENA_W], F32))
    ARb = ARh.bitcast(BF16)

    class Phase:
        def __init__(self, name):
            self.name = name
            self.off = 0
            self.t = {}
            self.keys = []

        def a(self, nm, free, dt=F32):
            n = int(np.prod(free))
            sz = n * (4 if dt == F32 else 2)
            off = self.off
            self.off += (sz + 3) // 4 * 4
            assert self.off <= ARENA_W * 4, (self.name, nm, self.off)
            if dt == F32:
                tl = Tl(ARh, [128] + list(free), self.name + "_" + nm, base=off // 4, pstride=ARENA_W)
            else:
                tl = Tl(ARb, [128] + list(free), self.name + "_" + nm, base=off // 2, pstride=ARENA_W * 2)
            self.t[nm] = tl
            self.keys.append(tl.key)
            return tl

    PH = {}
    for nm in "GRSMWXEN":
        PH[nm] = Phase(nm)
    G_, R_, S_, M_, W_, X_, E_, N_ = [PH[k] for k in "GRSMWXEN"]
    for nm in ["fmq", "fmk", "fmv", "fmz", "tA", "tB"]:
        R_.a(nm, [BLK])
    for nm in ["tA", "tB"]:
        G_.a(nm, [BLK])
    for par in range(2):
        for nm in ["fmq", "fmk", "fmv", "fmz"]:
            G_.a(nm + str(par), [BLK])
        for nm in ["rhs_u", "rhs_w", "kend", "u_t"]:
            G_.a(nm + str(par), [CPB * 128])
        G_.a("wT" + str(par), [CPB * 64]); G_.a("decS" + str(par), [CPB])
    for nm in ["raw0", "raw1"]:
        G_.a(nm, [BLK + 3]); S_.a(nm, [BLK + 3])
    G_.a("car", [12]); S_.a("car", [24])
    G_.a("ytb", [BLK], BF16); R_.a("ytb", [BLK], BF16); S_.a("ytb", [4 * BLK], BF16)
    G_.a("S_a", [128])
    for nm in ["g_t", "beta", "gc", "egl", "bex", "st1", "st2"]:
        G_.a(nm, [CPB])
    G_.a("gbr", [CPB * 64])
    for nm in ["o_t", "sq_t"]:
        G_.a(nm, [CPB * 128])
    for nm in ["t1", "Pm", "Qm", "Pm2", "Qm2", "Rm"]:
        G_.a(nm, [CPB * 64])
    G_.a("delta", [128])
    R_.a("cst", [2 * BLK]); R_.a("rdt", [BLK]); R_.a("SM", [CPB * 64])
    for nm in ["v_tm", "kw", "o_t", "sq_t"]:
        R_.a(nm, [CPB * 128])
    R_.a("St", [(CPB + 1) * 128]); R_.a("st1", [CPB]); R_.a("st2", [CPB]); R_.a("nrmb", [128])
    S_.a("fx", [4 * BLK])
    for nm in ["fmq", "fmk", "tA"]:
        S_.a(nm, [BLK])
    S_.a("Sc", [512])
    for nm in ["dt_t", "dta", "lc", "wr_t", "elc", "decb"]:
        S_.a(nm, [CPB * 8])
    for nm in ["nrmc", "dsk"]:
        S_.a(nm, [512])
    S_.a("szb", [4 * BLK])
    for par in range(2):
        for nm in ["db", "e1", "x_tm", "xw", "yi", "yg"]:
            S_.a(nm + str(par), [512])
        S_.a("cbT" + str(par), [64]); S_.a("B_tm" + str(par), [128]); S_.a("st2" + str(par), [4])
    mrgT = M_.a("mrg", [8 * T], BF16)
    W_.t["mrg"] = mrgT; W_.off = M_.off
    mkeys = ["mrg%d" % i for i in range(NBLK)]
    M_.keys += mkeys; W_.keys += mkeys + [mrgT.key]
    M_.a("ytl0", [16 * BLK], BF16); M_.a("ytl1", [16 * BLK], BF16); M_.a("tA", [BLK]); M_.a("tB", [BLK])
    for ph in (W_, X_):
        ph.a("xl0", [1024]); ph.a("xl1", [1024])
    yacc = E_.a("yacc", [NT * 1024])
    N_.t["yacc"] = yacc; N_.off = E_.off
    ykeys = ["yacc%d" % i for i in range(NT)]
    E_.keys += ykeys; N_.keys += ykeys + [yacc.key]
    for ph in (W_, N_):
        for nm in ["lng", "lnb", "xn0", "xn1"]:
            ph.a(nm, [1024])
        for par in range(2):
            ph.a("bst%d" % par, [12]); ph.a("mv%d" % par, [4])
    for par in range(2):
        W_.a("xTf%d" % par, [1024])
    W_.a("rtb", [NT * 160])
    E_.a("hs0", [BLK]); E_.a("hs1", [BLK]); E_.a("Hh0", [2 * BLK], BF16); E_.a("Hh1", [2 * BLK], BF16)
    cur_ph = [None]

    def enter(ph):
        old = cur_ph[0]
        if old is ph:
            return
        cur_ph[0] = ph
        if old is None:
            return
        P.op("dve", lambda e: e.memset(epst.v(3, [[1, 1]]), 0.0), writes=list(old.keys) + list(ph.keys) + ["epst3"])

    def c64(name, w=None, p0=0, Pn=64, coff=0):
        w = w if w is not None else {"tri": 64, "id": 64, "ones": 128, "bigm": 64, "rdec": 256, "wd": 4}[name]
        return C64.v(off64[name] + coff, [[1, w]], Pn=Pn, p0=p0)

    def c128(name, w=128, coff=0):
        return C128.v(off128[name] + coff, [[1, w]])

    P.dma("sp", lambda e: e.dma_start(out=C64.h[:, :], in_=dr["c64"][:, :]), writes=["C64"], chan="i_c64")
    P.dma("sp", lambda e: e.dma_start(out=C128.h[:, :], in_=dr["c128"][:, 0:384]), writes=["C128"], chan="i_c128")

    def small(dst_ap, src_ap, key, chan=None):
        P.dma("sp", lambda e: e.dma_start(out=dst_ap, in_=src_ap, allow_slow_non_contiguous=True), writes=[key], chan="i_" + key)

    for l in range(L):
        for k in range(4):
            small(cwa.v(l * 48 + k, [[4, 12]]), dv(dr["conv_a"], l * 6144 + k * 1536, [[1, 128], [128, 12]]), "cwa")
            small(cwc.v(l * 48 + k, [[4, 12]]), dv(dr["conv_c"], l * 6144 + k * 1536, [[1, 128], [128, 12]]), "cwc")
        small(cbc.v(l * 12, [[1, 12]]), dv(dr["conv_bias_c"], l * 1536, [[1, 128], [128, 12]]), "cbc")
        for br in range(3):
            small(bgt.v(l * 24 + br * 8, [[1, 8]]), dv(dr["b_gate"], l * 3072 + br * 1024, [[1, 128], [128, 8]]), "bgt")
        small(pb_a.v(l * 8, [[1, 4]]), dv(dr["a_log_a"], l * 4, [[0, 128], [1, 4]]), "pb_a")
        small(pb_a.v(l * 8 + 4, [[1, 4]]), dv(dr["dt_bias_a"], l * 4, [[0, 128], [1, 4]]), "pb_a")
        small(nrm_a.v(l * 128, [[1, 128]], Pn=64), dv(dr["norm_a"], l * 128, [[0, 64], [1, 128]]), "nrm_a")
        small(pc.v(l * 48, [[1, 16]], Pn=64), dv(dr["dt_bias_c"], l * 16, [[0, 64], [1, 16]]), "pc")
        small(pc.v(l * 48 + 16, [[1, 16]], Pn=64), dv(dr["a_log_c"], l * 16, [[0, 64], [1, 16]]), "pc")
        small(pc.v(l * 48 + 32, [[1, 16]], Pn=64), dv(dr["d_skip_c"], l * 16, [[0, 64], [1, 16]]), "pc")
        small(wr.v(l * KC * 36, [[36, KC], [1, 4]]), dv(dr["w_router_group"], l * D * 4, [[4, 128], [512, KC], [1, 4]]), "wr")
        small(wr.v(l * KC * 36 + 4, [[36, KC], [1, 32]]), dv(dr["w_router_expert"], l * D * 32, [[32, 128], [4096, KC], [1, 32]]), "wr")
        small(brt.v(l * 36, [[1, 4]]), dv(dr["b_router_group"], l * 4, [[0, 128], [1, 4]]), "brt")
        small(brt.v(l * 36 + 4, [[1, 32]]), dv(dr["b_router_expert"], l * 32, [[0, 128], [1, 32]]), "brt")
    P.op("dve", lambda e: e.memset(epst.v(0, [[1, 1]]), EPS), writes=["epst"])
    P.op("dve", lambda e: e.memset(epst.v(1, [[1, 1]]), 1.0), reads=["epst"], writes=["epst"])
    P.op("dve", lambda e: e.memset(epst.v(2, [[1, 1]]), 0.0), reads=["epst"], writes=["epst"])
    eps_ap = epst.v(0, [[1, 1]])
    one_ap = epst.v(1, [[1, 1]])
    for l in range(L):
        P.op("act", lambda e, l=l: e.activation(out=nexa.v(l * 4, [[1, 4]]), in_=pb_a.v(l * 8, [[1, 4]]), func=AF.Exp),
             reads=["pb_a"], writes=["nexa"])
        P.op("dve", lambda e, l=l: e.tensor_scalar(out=nexa.v(l * 4, [[1, 4]]), in0=nexa.v(l * 4, [[1, 4]]), scalar1=-1.0,
                                                   scalar2=None, op0=ALU.mult), reads=["nexa"], writes=["nexa"])
        P.op("act", lambda e, l=l: e.activation(out=nac.v(l * 16, [[1, 16]], Pn=64), in_=pc.v(l * 48 + 16, [[1, 16]], Pn=64),
                                                func=AF.Exp), reads=["pc"], writes=["nac"])
        P.op("dve", lambda e, l=l: e.tensor_scalar(out=nac.v(l * 16, [[1, 16]], Pn=64), in0=nac.v(l * 16, [[1, 16]], Pn=64),
                                                   scalar1=-1.0, scalar2=None, op0=ALU.mult), reads=["nac"], writes=["nac"])

    pinned = set()
    bpool = [list(range(8))]

    def ps():
        while True:
            pool = bpool[0]
            b = banks[pool[bank_i[0] % len(pool)]]
            bank_i[0] += 1
            if b.key not in pinned:
                return b

    def with_banks(lst, f):
        old = bpool[0]
        bpool[0] = lst
        f()
        bpool[0] = old

    def ld_w(slot, off_el, ncols, src_t, src_off, row_stride, nk=KC, krows=128):
        P.dma("pool", lambda e: e.dma_start(out=WB[slot].v(off_el, [[ncols, nk], [1, ncols]]),
                                            in_=dv(src_t, src_off, [[row_stride, 128], [128 * row_stride, nk], [1, ncols]]),
                                            allow_slow_non_contiguous=True),
              writes=["WB%d" % slot], chan="w%d" % slot)

    def xt_keys(tok0, n):
        return ["xT%d" % i for i in range(tok0 // 128, (tok0 + n + 127) // 128)]

    def proj_fm(slot, woff, wcols, c0, tok0, n, pbank, pcol=0):
        for kc in range(KC):
            P.op("pe", lambda e, kc=kc: e.matmul(pbank.v(pcol, [[1, n]]),
                                                 lhsT=WB[slot].v(woff + kc * wcols + c0, [[1, 128]]),
                                                 rhs=xT.v(kc * T + tok0, [[1, n]]), start=(kc == 0), stop=(kc == KC - 1)),
                 reads=["WB%d" % slot] + xt_keys(tok0, n), writes=[pbank.key])

    def proj_tm(slot, woff, wcols, c0, ncol, tok0, pbank, pcol=0):
        for kc in range(KC):
            P.op("pe", lambda e, kc=kc: e.matmul(pbank.v(pcol, [[1, ncol]], Pn=64),
                                                 lhsT=xT.v(kc * T + tok0, [[1, 64]]),
                                                 rhs=WB[slot].v(woff + kc * wcols + c0, [[1, ncol]]),
                                                 start=(kc == 0), stop=(kc == KC - 1)),
                 reads=["WB%d" % slot] + xt_keys(tok0, 64), writes=[pbank.key])

    def l2norm_fm(ph, t, scale):
        tB = ph.t["tB"]
        P.op("act", lambda e: e.activation(out=tB.v(0, [[1, BLK]]), in_=t.v(0, [[1, BLK]]), func=AF.Square), reads=[t.key], writes=[tB.key])
        pb = ps()
        P.op("pe", lambda e: e.matmul(pb.v(0, [[1, BLK]]), lhsT=c128("ones"), rhs=tB.v(0, [[1, BLK]]), start=True, stop=True),
             reads=[tB.key, "C128"], writes=[pb.key])
        P.op("act", lambda e: e.activation(out=tB.v(0, [[1, BLK]]), in_=pb.v(0, [[1, BLK]]), func=AF.Ln, bias=eps_ap, scale=1.0),
             reads=[pb.key, "epst"], writes=[tB.key])
        P.op("act", lambda e: e.activation(out=tB.v(0, [[1, BLK]]), in_=tB.v(0, [[1, BLK]]), func=AF.Exp, scale=-0.5), reads=[tB.key], writes=[tB.key])
        P.op("dve", lambda e: e.scalar_tensor_tensor(out=t.v(0, [[1, BLK]]), in0=t.v(0, [[1, BLK]]), scalar=float(scale),
                                                     in1=tB.v(0, [[1, BLK]]), op0=ALU.mult, op1=ALU.mult),
             reads=[t.key, tB.key], writes=[t.key])

    def store_yT(ytb, fc0, nfc, tok0, s):
        P.dma("sp", lambda e: e.dma_start(out=dv(ytd, (s % 2) * 128 * 16 * T + fc0 * T + tok0, [[16 * T, 128], [T, nfc], [1, BLK]]),
                                          in_=ytb.v(0, [[BLK, nfc], [1, BLK]])),
              reads=[ytb.key], writes=["ytd%d_%d_%d" % (s % 2, fc0 + i, tok0 // BLK) for i in range(nfc)], chan="yt")

    def gdn_load(slot, l, h):
        base = l * D * P_IN
        for j, c0 in enumerate([O_AQKV + h * 128, O_AQKV + 512 + h * 128, O_AQKV + 1024 + h * 128, O_AZ + h * 128]):
            ld_w(slot, j * 1024, 128, dr["w_in"], base + c0, P_IN)
        ld_w(slot, 4096, 1, dr["w_in"], base + O_AA + h, P_IN)
        ld_w(slot, 4096 + 8, 1, dr["w_in"], base + O_AB + h, P_IN)

    def gdn_run(slot, l, h, s):
        wk = "WB%d" % slot
        enter(G_)
        ph = G_
        tA, tB, ytb, S_a, g_t, beta, gc, egl, bex, st1, st2, gbr, o_t, sq_t, t1, Pm, Qm, Pm2, Qm2, Rm, delta = [G_.t[n] for n in ['tA', 'tB', 'ytb', 'S_a', 'g_t', 'beta', 'gc', 'egl', 'bex', 'st1', 'st2', 'gbr', 'o_t', 'sq_t', 't1', 'Pm', 'Qm', 'Pm2', 'Qm2', 'Rm', 'delta']]
        def pre(b):
            tok0 = b * BLK
            fmq, fmk, fmv, fmz, rhs_u, rhs_w, kend, u_t, wT, decS = [G_.t[n + str(b % 2)] for n in ['fmq', 'fmk', 'fmv', 'fmz', 'rhs_u', 'rhs_w', 'kend', 'u_t', 'wT', 'decS']]
            for j, dst in enumerate([fmq, fmk, fmv]):
                pb = ps()
                proj_fm(slot, j * 1024, 128, 0, tok0, BLK, pb)
                cw = lambda k, j=j: cwa.v(l * 48 + (j * 4 + h) * 4 + k, [[1, 1]])
                conv_stream(ph, pb, j, b, cw, dst.v(0, [[1, BLK]]), dst.key)
            l2norm_fm(ph, fmq, 128.0 ** -0.5)
            l2norm_fm(ph, fmk, 1.0)
            pb = ps()
            proj_fm(slot, 3 * 1024, 128, 0, tok0, BLK, pb)
            P.op("act", lambda e, pb=pb: e.activation(out=fmz.v(0, [[1, BLK]]), in_=pb.v(0, [[1, BLK]]), func=AF.Silu),
                 reads=[pb.key], writes=[fmz.key])
            pb = ps()
            for c in range(CPB):
                proj_tm(slot, 4096, 1, 0, 1, tok0 + c * 64, pb, pcol=2 * c)
                proj_tm(slot, 4096 + 8, 1, 0, 1, tok0 + c * 64, pb, pcol=2 * c + 1)
            P.op("act", lambda e, pb=pb: e.activation(out=g_t.v(0, [[1, CPB]], Pn=64), in_=pb.v(0, [[2, CPB]], Pn=64), func=AF.Exp,
                                                      bias=pb_a.v(l * 8 + 4 + h, [[1, 1]], Pn=64)),
                 reads=[pb.key, "pb_a"], writes=[g_t.key])
            P.op("act", lambda e: e.activation(out=g_t.v(0, [[1, CPB]], Pn=64), in_=g_t.v(0, [[1, CPB]], Pn=64), func=AF.Ln,
                                               bias=one_ap[0:64, :]), reads=[g_t.key, "epst"], writes=[g_t.key])
            P.op("dve", lambda e: e.tensor_scalar(out=g_t.v(0, [[1, CPB]], Pn=64), in0=g_t.v(0, [[1, CPB]], Pn=64),
                                                  scalar1=nexa.v(l * 4 + h, [[1, 1]], Pn=64), scalar2=None, op0=ALU.mult),
                 reads=[g_t.key, "nexa"], writes=[g_t.key])
            P.op("act", lambda e, pb=pb: e.activation(out=beta.v(0, [[1, CPB]], Pn=64), in_=pb.v(1, [[2, CPB]], Pn=64),
                                                      func=AF.Exp, scale=-1.0), reads=[pb.key], writes=[beta.key])
            P.op("dve", lambda e: e.tensor_scalar(out=beta.v(0, [[1, CPB]], Pn=64), in0=beta.v(0, [[1, CPB]], Pn=64), scalar1=1.0, scalar2=None,
                                                  op0=ALU.add), reads=[beta.key], writes=[beta.key])
            P.op("dve", lambda e: e.reciprocal(out=beta.v(0, [[1, CPB]], Pn=64), in_=beta.v(0, [[1, CPB]], Pn=64)), reads=[beta.key], writes=[beta.key])
            pg = ps()
            P.op("pe", lambda e, pg=pg: e.matmul(pg.v(0, [[1, CPB]], Pn=64), lhsT=c64("tri"), rhs=g_t.v(0, [[1, CPB]], Pn=64),
                                                 start=True, stop=True), reads=[g_t.key, "C64"], writes=[pg.key])
            P.op("pe", lambda e, pg=pg: e.matmul(pg.v(64, [[1, CPB]]), lhsT=c64("ones"), rhs=g_t.v(0, [[1, CPB]], Pn=64),
                                                 start=True, stop=True), reads=[g_t.key, "C64"], writes=[pg.key])
            P.op("dve", lambda e, pg=pg: e.tensor_copy(out=gc.v(0, [[1, CPB]], Pn=64), in_=pg.v(0, [[1, CPB]], Pn=64)),
                 reads=[pg.key], writes=[gc.key])
            P.op("act", lambda e, pg=pg: e.activation(out=decS.v(0, [[1, CPB]]), in_=pg.v(64, [[1, CPB]]), func=AF.Exp),
                 reads=[pg.key], writes=[decS.key])
            P.op("dve", lambda e, pg=pg: e.tensor_tensor(out=egl.v(0, [[1, CPB]], Pn=64), in0=pg.v(64, [[1, CPB]], Pn=64),
                                                         in1=gc.v(0, [[1, CPB]], Pn=64), op=ALU.subtract),
                 reads=[pg.key, gc.key], writes=[egl.key])
            P.op("act", lambda e: e.activation(out=egl.v(0, [[1, CPB]], Pn=64), in_=egl.v(0, [[1, CPB]], Pn=64), func=AF.Exp),
                 reads=[egl.key], writes=[egl.key])
            P.op("act", lambda e: e.activation(out=bex.v(0, [[1, CPB]], Pn=64), in_=gc.v(0, [[1, CPB]], Pn=64), func=AF.Exp),
                 reads=[gc.key], writes=[bex.key])
            P.op("dve", lambda e: e.tensor_tensor(out=bex.v(0, [[1, CPB]], Pn=64), in0=bex.v(0, [[1, CPB]], Pn=64),
                                                  in1=beta.v(0, [[1, CPB]], Pn=64), op=ALU.mult), reads=[bex.key, beta.key], writes=[bex.key])
            P.op("dve", lambda e: e.tensor_copy(out=gbr.v(0, [[64, CPB], [1, 64]], Pn=64), in_=g_t.v(0, [[1, CPB], [0, 64]], Pn=64)),
                 reads=[g_t.key], writes=[gbr.key])
            pG = ps()
            pK = ps()
            for c in range(CPB):
                P.op("pe", lambda e, c=c, pG=pG: e.matmul(pG.v(c * 64, [[1, 64]], Pn=64), lhsT=gbr.v(c * 64, [[1, 64]], Pn=64),
                                                          rhs=c64("tri"), start=True, stop=True), reads=[gbr.key, "C64"], writes=[pG.key])
                P.op("pe", lambda e, c=c, pK=pK: e.matmul(pK.v(c * 64, [[1, 64]], Pn=64), lhsT=fmk.v(c * 64, [[1, 64]]),
                                                          rhs=fmk.v(c * 64, [[1, 64]]), start=True, stop=True), reads=[fmk.key], writes=[pK.key])
            P.op("dve", lambda e, pG=pG: e.tensor_tensor(out=t1.v(0, [[64, CPB], [1, 64]], Pn=64), in0=pG.v(0, [[64, CPB], [1, 64]], Pn=64),
                                                         in1=gc.v(0, [[1, CPB], [0, 64]], Pn=64), op=ALU.subtract),
                 reads=[pG.key, gc.key], writes=[t1.key])
            P.op("dve", lambda e: e.tensor_tensor(out=t1.v(0, [[64, CPB], [1, 64]], Pn=64), in0=t1.v(0, [[64, CPB], [1, 64]], Pn=64),
                                                  in1=C64.v(off64["bigm"], [[0, CPB], [1, 64]], Pn=64), op=ALU.max),
                 reads=[t1.key, "C64"], writes=[t1.key])
            P.op("act", lambda e: e.activation(out=t1.v(0, [[1, CPB * 64]], Pn=64), in_=t1.v(0, [[1, CPB * 64]], Pn=64), func=AF.Exp, scale=-1.0),
                 reads=[t1.key], writes=[t1.key])
            P.op("dve", lambda e, pK=pK: e.tensor_tensor(out=Qm.v(0, [[64, CPB], [1, 64]], Pn=64), in0=pK.v(0, [[64, CPB], [1, 64]], Pn=64),
                                                         in1=beta.v(0, [[1, CPB], [0, 64]], Pn=64), op=ALU.mult),
                 reads=[pK.key, beta.key], writes=[Qm.key])
            P.op("dve", lambda e: e.tensor_tensor(out=Qm.v(0, [[1, CPB * 64]], Pn=64), in0=Qm.v(0, [[1, CPB * 64]], Pn=64),
                                                  in1=t1.v(0, [[1, CPB * 64]], Pn=64), op=ALU.mult), reads=[Qm.key, t1.key], writes=[Qm.key])
            pT = [ps(), ps()]
            pV = [ps(), ps()]
            pB_ = ps()
            for c in range(CPB):
                P.op("pe", lambda e, c=c: e.transpose(pT[c // 4].v((c % 4) * 128, [[1, 128]], Pn=64), fmk.v(c * 64, [[1, 64]]), c128("id")),
                     reads=[fmk.key, "C128"], writes=[pT[c // 4].key])
                P.op("pe", lambda e, c=c: e.transpose(pV[c // 4].v((c % 4) * 128, [[1, 128]], Pn=64), fmv.v(c * 64, [[1, 64]]), c128("id")),
                     reads=[fmv.key, "C128"], writes=[pV[c // 4].key])
                P.op("pe", lambda e, c=c: e.transpose(pB_.v(c * 64, [[1, 64]], Pn=64), Qm.v(c * 64, [[1, 64]], Pn=64), c64("id")),
                     reads=[Qm.key, "C64"], writes=[pB_.key])
            for hb in range((CPB + 3) // 4):
                n = min(4, CPB - hb * 4)
                P.op("dve", lambda e, hb=hb, n=n: e.tensor_tensor(out=rhs_w.v(hb * 512, [[128, n], [1, 128]], Pn=64),
                                                                  in0=pT[hb].v(0, [[128, n], [1, 128]], Pn=64),
                                                                  in1=bex.v(hb * 4, [[1, n], [0, 128]], Pn=64), op=ALU.mult),
                     reads=[pT[hb].key, bex.key], writes=[rhs_w.key])
                P.op("dve", lambda e, hb=hb, n=n: e.tensor_tensor(out=kend.v(hb * 512, [[128, n], [1, 128]], Pn=64),
                                                                  in0=pT[hb].v(0, [[128, n], [1, 128]], Pn=64),
                                                                  in1=egl.v(hb * 4, [[1, n], [0, 128]], Pn=64), op=ALU.mult),
                     reads=[pT[hb].key, egl.key], writes=[kend.key])
                P.op("dve", lambda e, hb=hb, n=n: e.tensor_tensor(out=rhs_u.v(hb * 512, [[128, n], [1, 128]], Pn=64),
                                                                  in0=pV[hb].v(0, [[128, n], [1, 128]], Pn=64),
                                                                  in1=beta.v(hb * 4, [[1, n], [0, 128]], Pn=64), op=ALU.mult),
                     reads=[pV[hb].key, beta.key], writes=[rhs_u.key])
            P.op("act", lambda e: e.activation(out=Pm.v(0, [[1, CPB * 64]], Pn=64), in_=pB_.v(0, [[1, CPB * 64]], Pn=64), func=AF.Copy),
                 reads=[pB_.key], writes=[Pm.key])
            P.op("dve", lambda e: e.tensor_tensor(out=Rm.v(0, [[64, CPB], [1, 64]], Pn=64), in0=C64.v(off64["id"], [[0, CPB], [1, 64]], Pn=64),
                                                  in1=pB_.v(0, [[64, CPB], [1, 64]], Pn=64), op=ALU.subtract),
                 reads=[pB_.key, "C64"], writes=[Rm.key])
            Pc, Qc, Pn_, Qn_ = Pm, Qm, Pm2, Qm2
            for lev in range(5):
                last = lev == 4
                pq = ps()
                pp = ps() if not last else None
                for c in range(CPB):
                    P.op("pe", lambda e, c=c, pq=pq, Pc=Pc, Qc=Qc: e.matmul(pq.v(c * 64, [[1, 64]], Pn=64), lhsT=Pc.v(c * 64, [[1, 64]], Pn=64),
                                                                         rhs=Qc.v(c * 64, [[1, 64]], Pn=64), start=True, stop=True),
                         reads=[Pc.key, Qc.key], writes=[pq.key])
                    if not last:
                        P.op("pe", lambda e, c=c, pp=pp, Pc=Pc, Qc=Qc: e.matmul(pp.v(c * 64, [[1, 64]], Pn=64), lhsT=Qc.v(c * 64, [[1, 64]], Pn=64),
                                                                             rhs=Pc.v(c * 64, [[1, 64]], Pn=64), start=True, stop=True),
                             reads=[Pc.key, Qc.key], writes=[pp.key])
                P.op("act", lambda e, pq=pq, Qn_=Qn_: e.activation(out=Qn_.v(0, [[1, CPB * 64]], Pn=64), in_=pq.v(0, [[1, CPB * 64]], Pn=64), func=AF.Copy),
                     reads=[pq.key], writes=[Qn_.key])
                if not last:
                    P.op("dve", lambda e, pp=pp, Pn_=Pn_: e.tensor_copy(out=Pn_.v(0, [[1, CPB * 64]], Pn=64), in_=pp.v(0, [[1, CPB * 64]], Pn=64)),
                         reads=[pp.key], writes=[Pn_.key])
                pr = ps()
                for c in range(CPB):
                    P.op("pe", lambda e, c=c, pr=pr, Qn_=Qn_: e.matmul(pr.v(c * 64, [[1, 64]], Pn=64), lhsT=Qn_.v(c * 64, [[1, 64]], Pn=64),
                                                                      rhs=Rm.v(c * 64, [[1, 64]], Pn=64), start=True, stop=True),
                         reads=[Qn_.key, Rm.key], writes=[pr.key])
                P.op("dve", lambda e, pr=pr: e.tensor_tensor(out=Rm.v(0, [[1, CPB * 64]], Pn=64), in0=Rm.v(0, [[1, CPB * 64]], Pn=64),
                                                             in1=pr.v(0, [[1, CPB * 64]], Pn=64), op=ALU.add), reads=[pr.key, Rm.key], writes=[Rm.key])
                Pc, Qc, Pn_, Qn_ = Pn_, Qn_, Pc, Qc
            pU = [ps(), ps()]
            pW = ps()
            for c in range(CPB):
                P.op("pe", lambda e, c=c: e.matmul(pU[c // 4].v((c % 4) * 128, [[1, 128]], Pn=64), lhsT=Rm.v(c * 64, [[1, 64]], Pn=64),
                                                   rhs=rhs_u.v(c * 128, [[1, 128]], Pn=64), start=True, stop=True),
                     reads=[Rm.key, rhs_u.key], writes=[pU[c // 4].key])
                P.op("pe", lambda e, c=c: e.matmul(pW.v(c * 64, [[1, 64]]), lhsT=rhs_w.v(c * 128, [[1, 128]], Pn=64),
                                                   rhs=Rm.v(c * 64, [[1, 64]], Pn=64), start=True, stop=True),
                     reads=[Rm.key, rhs_w.key], writes=[pW.key])
            for hb in range((CPB + 3) // 4):
                n = min(4, CPB - hb * 4)
                P.op("act", lambda e, hb=hb, n=n: e.activation(out=u_t.v(hb * 512, [[1, n * 128]], Pn=64), in_=pU[hb].v(0, [[1, n * 128]], Pn=64),
                                                               func=AF.Copy), reads=[pU[hb].key], writes=[u_t.key])
            P.op("act", lambda e: e.activation(out=wT.v(0, [[1, CPB * 64]]), in_=pW.v(0, [[1, CPB * 64]]), func=AF.Copy),
                 reads=[pW.key], writes=[wT.key])

        def scanpost(b):
            tok0 = b * BLK
            fmq, fmk, fmv, fmz, rhs_u, rhs_w, kend, u_t, wT, decS = [G_.t[n + str(b % 2)] for n in ['fmq', 'fmk', 'fmv', 'fmz', 'rhs_u', 'rhs_w', 'kend', 'u_t', 'wT', 'decS']]
            if b == 0:
                P.op("dve", lambda e: e.memset(S_a.v(0, [[1, 128]]), 0.0), writes=[S_a.key])
            pO = [ps(), ps()]
            pinned.update([pO[0].key, pO[1].key])
            for c in range(CPB):
                p1 = ps()
                P.op("pe", lambda e, c=c, p1=p1: e.matmul(p1.v(0, [[1, 128]], Pn=64), lhsT=wT.v(c * 64, [[1, 64]]), rhs=S_a.v(0, [[1, 128]]),
                                                          start=True, stop=True), reads=[wT.key, S_a.key], writes=[p1.key])
                P.op("dve", lambda e, c=c, p1=p1: e.tensor_tensor(out=delta.v(0, [[1, 128]], Pn=64), in0=u_t.v(c * 128, [[1, 128]], Pn=64),
                                                                  in1=p1.v(0, [[1, 128]], Pn=64), op=ALU.subtract),
                     reads=[u_t.key, p1.key], writes=[delta.key])
                p2 = ps()
                P.op("pe", lambda e, c=c, p2=p2: e.matmul(p2.v(0, [[1, 128]]), lhsT=kend.v(c * 128, [[1, 128]], Pn=64),
                                                          rhs=delta.v(0, [[1, 128]], Pn=64), start=True, stop=True),
                     reads=[kend.key, delta.key], writes=[p2.key])
                P.op("dve", lambda e, c=c, p2=p2: e.scalar_tensor_tensor(out=S_a.v(0, [[1, 128]]), in0=S_a.v(0, [[1, 128]]),
                                                                         scalar=decS.v(c, [[1, 1]]), in1=p2.v(0, [[1, 128]]),
                                                                         op0=ALU.mult, op1=ALU.add), reads=[S_a.key, decS.key, p2.key], writes=[S_a.key])
                P.op("pe", lambda e, c=c: e.matmul(pO[c // 4].v((c % 4) * 128, [[1, 128]], Pn=64), lhsT=fmq.v(c * 64, [[1, 64]]),
                                                   rhs=S_a.v(0, [[1, 128]]), start=True, stop=True), reads=[fmq.key, S_a.key], writes=[pO[c // 4].key])
            pinned.difference_update([pO[0].key, pO[1].key])
            for hb in range((CPB + 3) // 4):
                n = min(4, CPB - hb * 4)
                P.op("act", lambda e, hb=hb, n=n: e.activation(out=o_t.v(hb * 512, [[1, n * 128]], Pn=64), in_=pO[hb].v(0, [[1, n * 128]], Pn=64),
                                                               func=AF.Copy), reads=[pO[hb].key], writes=[o_t.key])
            rms_tm(ph, o_t, CPB, 128, nrm_a.v(l * 128, [[0, CPB], [1, 128]], Pn=64), "nrm_a")
            pY = ps()
            for c in range(CPB):
                P.op("pe", lambda e, c=c, pY=pY: e.transpose(pY.v(c * 64, [[1, 64]]), o_t.v(c * 128, [[1, 128]], Pn=64), c64("id")),
                     reads=[o_t.key, "C64"], writes=[pY.key])
            P.op("dve", lambda e, pY=pY: e.tensor_tensor(out=ytb.v(0, [[1, BLK]]), in0=pY.v(0, [[1, BLK]]), in1=fmz.v(0, [[1, BLK]]), op=ALU.mult),
                 reads=[pY.key, fmz.key], writes=[ytb.key])
            store_yT(ytb, h, 1, tok0, s)


        pre(0)
        for b in range(NBLK):
            A = P.capture(lambda: with_banks([0, 1, 2], lambda: scanpost(b)))
            B = P.capture(lambda: with_banks([3, 4, 5, 6, 7], lambda: pre(b + 1))) if b + 1 < NBLK else []
            P.merge(A, B)

    conv_cnt = [0]

    def conv_stream(ph, pb, sid, b, cw, out_ap, out_key, bias_ap=None):
        r = ph.t["raw%d" % (conv_cnt[0] % 2)]
        conv_cnt[0] += 1
        car = ph.t["car"]
        tA = ph.t["tA"]
        if b == 0:
            P.op("dve", lambda e: e.memset(r.v(0, [[1, 3]]), 0.0), writes=[r.key])
        else:
            P.op("dve", lambda e: e.tensor_copy(out=r.v(0, [[1, 3]]), in_=car.v(sid * 3, [[1, 3]])), reads=[car.key], writes=[r.key])
        P.op("act", lambda e: e.activation(out=r.v(3, [[1, BLK]]), in_=pb.v(0, [[1, BLK]]), func=AF.Copy),
             reads=[pb.key, r.key], writes=[r.key])
        P.op("dve", lambda e: e.tensor_copy(out=car.v(sid * 3, [[1, 3]]), in_=r.v(BLK, [[1, 3]])), reads=[r.key, car.key], writes=[car.key])
        P.op("dve", lambda e: e.tensor_scalar(out=tA.v(0, [[1, BLK]]), in0=r.v(0, [[1, BLK]]), scalar1=cw(0), scalar2=None, op0=ALU.mult),
             reads=[r.key, "cwa", "cwc"], writes=[tA.key])
        for k in range(1, 4):
            P.op("dve", lambda e, k=k: e.scalar_tensor_tensor(out=tA.v(0, [[1, BLK]]), in0=r.v(k, [[1, BLK]]), scalar=cw(k),
                                                              in1=tA.v(0, [[1, BLK]]), op0=ALU.mult, op1=ALU.add),
                 reads=[r.key, tA.key, "cwa", "cwc"], writes=[tA.key])
        if bias_ap is None:
            P.op("act", lambda e: e.activation(out=out_ap, in_=tA.v(0, [[1, BLK]]), func=AF.Silu), reads=[tA.key], writes=[out_key])
        else:
            P.op("act", lambda e: e.activation(out=out_ap, in_=tA.v(0, [[1, BLK]]), func=AF.Silu, bias=bias_ap),
                 reads=[tA.key, "cbc"], writes=[out_key])

    def rms_tm(ph, t, nch, width, w_ap, wkey, center=False):
        st1, st2, sq_t = ph.t["st1"], ph.t["st2"], ph.t["sq_t"]
        full = t.v(0, [[width, nch], [1, width]], Pn=64)
        if center:
            P.op("dve", lambda e: e.tensor_reduce(out=st1.v(0, [[1, nch]], Pn=64), in_=full, axis=AX.X, op=ALU.add), reads=[t.key], writes=[st1.key])
            P.op("dve", lambda e: e.tensor_scalar(out=st1.v(0, [[1, nch]], Pn=64), in0=st1.v(0, [[1, nch]], Pn=64), scalar1=1.0 / width,
                                                  scalar2=None, op0=ALU.mult), reads=[st1.key], writes=[st1.key])
            P.op("dve", lambda e: e.tensor_tensor(out=full, in0=full, in1=st1.v(0, [[1, nch], [0, width]], Pn=64), op=ALU.subtract),
                 reads=[t.key, st1.key], writes=[t.key])
        P.op("act", lambda e: e.activation(out=sq_t.v(0, [[1, nch * width]], Pn=64),
                                           in_=t.v(0, [[1, nch * width]], Pn=64), func=AF.Square), reads=[t.key], writes=[sq_t.key])
        P.op("dve", lambda e: e.tensor_reduce(out=st2.v(0, [[1, nch]], Pn=64), in_=sq_t.v(0, [[width, nch], [1, width]], Pn=64), axis=AX.X, op=ALU.add),
             reads=[sq_t.key], writes=[st2.key])
        P.op("act", lambda e: e.activation(out=st2.v(0, [[1, nch]], Pn=64), in_=st2.v(0, [[1, nch]], Pn=64), func=AF.Ln,
                                           bias=eps_ap[0:64, :], scale=1.0 / width), reads=[st2.key, "epst"], writes=[st2.key])
        P.op("act", lambda e: e.activation(out=st2.v(0, [[1, nch]], Pn=64), in_=st2.v(0, [[1, nch]], Pn=64), func=AF.Exp, scale=-0.5),
             reads=[st2.key], writes=[st2.key])
        P.op("dve", lambda e: e.tensor_tensor(out=full, in0=full, in1=st2.v(0, [[1, nch], [0, width]], Pn=64), op=ALU.mult),
             reads=[t.key, st2.key], writes=[t.key])
        P.op("dve", lambda e: e.tensor_tensor(out=full, in0=full, in1=w_ap, op=ALU.mult), reads=[t.key, wkey], writes=[t.key])

    def ret_load(slot, l, h):
        base = l * D * P_IN
        for j, c0 in enumerate([O_BQ + h * 128, O_BK + h * 128, O_BV + h * 128, O_BG + h * 128]):
            ld_w(slot, j * 1024, 128, dr["w_in"], base + c0, P_IN)

    def ret_run(slot, l, h, s):
        enter(R_)
        ph = R_
        fmq, fmk, fmv, fmz, tA, tB, ytb, cst, rdt, SM, v_tm, kw, o_t, sq_t, St, st1, st2, nrmb = [R_.t[n] for n in ['fmq', 'fmk', 'fmv', 'fmz', 'tA', 'tB', 'ytb', 'cst', 'rdt', 'SM', 'v_tm', 'kw', 'o_t', 'sq_t', 'St', 'st1', 'st2', 'nrmb']]
        small(rdt.v(0, [[1, BLK]]), dv(dr["c128"], off128["rd"] + h * BLK, [[a128.shape[1], 128], [1, BLK]]), rdt.key, chan="rp")
        small(nrmb.v(0, [[1, 128]], Pn=64), dv(dr["norm_b"], l * 512 + h * 128, [[0, 64], [1, 128]]), nrmb.key, chan="rp")
        for b in range(NBLK):
            tok0 = b * BLK
            P.dma("sp", lambda e, tok0=tok0: e.dma_start(out=cst.v(0, [[BLK, 2], [1, BLK]]),
                                                         in_=dv(dr["cs"], tok0, [[2 * T, 128], [T, 2], [1, BLK]])), writes=[cst.key], chan="cs")
            for j, dst in enumerate([fmq, fmk]):
                pb = ps()
                proj_fm(slot, j * 1024, 128, 0, tok0, BLK, pb)
                P.op("act", lambda e, pb=pb: e.activation(out=tA.v(0, [[1, BLK]]), in_=pb.v(0, [[1, BLK]]), func=AF.Copy), reads=[pb.key], writes=[tA.key])
                pr = ps()
                P.op("pe", lambda e, pr=pr: e.matmul(pr.v(0, [[1, BLK]]), lhsT=c128("rot"), rhs=tA.v(0, [[1, BLK]]), start=True, stop=True),
                     reads=[tA.key, "C128"], writes=[pr.key])
                sc = 1.0 if j == 0 else 128.0 ** -0.5
                P.op("dve", lambda e, pr=pr, sc=sc: e.scalar_tensor_tensor(out=tB.v(0, [[1, BLK]]), in0=pr.v(0, [[1, BLK]]), scalar=sc,
                                                                           in1=cst.v(BLK, [[1, BLK]]), op0=ALU.mult, op1=ALU.mult),
                     reads=[pr.key, cst.key], writes=[tB.key])
                P.op("dve", lambda e, sc=sc: e.scalar_tensor_tensor(out=tA.v(0, [[1, BLK]]), in0=tA.v(0, [[1, BLK]]), scalar=sc,
                                                                    in1=cst.v(0, [[1, BLK]]), op0=ALU.mult, op1=ALU.mult),
                     reads=[tA.key, cst.key], writes=[tA.key])
                P.op("dve", lambda e, dst=dst: e.tensor_tensor(out=dst.v(0, [[1, BLK]]), in0=tA.v(0, [[1, BLK]]), in1=tB.v(0, [[1, BLK]]), op=ALU.add),
                     reads=[tA.key, tB.key], writes=[dst.key])
            pb = ps()
            proj_fm(slot, 3 * 1024, 128, 0, tok0, BLK, pb)
            P.op("act", lambda e, pb=pb: e.activation(out=fmz.v(0, [[1, BLK]]), in_=pb.v(0, [[1, BLK]]), func=AF.Silu), reads=[pb.key], writes=[fmz.key])
            P.op("dve", lambda e: e.tensor_tensor(out=fmv.v(0, [[1, BLK]]), in0=fmq.v(0, [[1, BLK]]), in1=rdt.v(0, [[1, BLK]]), op=ALU.mult),
                 reads=[fmq.key, rdt.key], writes=[fmv.key])
            pV = [ps(), ps()]
            pT = [ps(), ps()]
            pS = ps()
            for c in range(CPB):
                proj_tm(slot, 2 * 1024, 128, 0, 128, tok0 + c * 64, pV[c // 4], pcol=(c % 4) * 128)
                P.op("pe", lambda e, c=c: e.transpose(pT[c // 4].v((c % 4) * 128, [[1, 128]], Pn=64), fmk.v(c * 64, [[1, 64]]), c128("id")),
                     reads=[fmk.key, "C128"], writes=[pT[c // 4].key])
                P.op("pe", lambda e, c=c: e.matmul(pS.v(c * 64, [[1, 64]], Pn=64), lhsT=fmk.v(c * 64, [[1, 64]]), rhs=fmq.v(c * 64, [[1, 64]]),
                                                   start=True, stop=True), reads=[fmk.key, fmq.key], writes=[pS.key])
            for hb in range((CPB + 3) // 4):
                n = min(4, CPB - hb * 4)
                P.op("act", lambda e, hb=hb, n=n: e.activation(out=v_tm.v(hb * 512, [[1, n * 128]], Pn=64), in_=pV[hb].v(0, [[1, n * 128]], Pn=64),
                                                               func=AF.Copy), reads=[pV[hb].key], writes=[v_tm.key])
                P.op("dve", lambda e, hb=hb, n=n: e.tensor_scalar(out=kw.v(hb * 512, [[1, n * 128]], Pn=64), in0=pT[hb].v(0, [[1, n * 128]], Pn=64),
                                                                  scalar1=c64("wd", 1, coff=h), scalar2=None, op0=ALU.mult),
                     reads=[pT[hb].key, "C64"], writes=[kw.key])
            P.op("dve", lambda e: e.tensor_tensor(out=SM.v(0, [[64, CPB], [1, 64]], Pn=64), in0=pS.v(0, [[64, CPB], [1, 64]], Pn=64),
                                                  in1=C64.v(off64["rdec"] + h * 64, [[0, CPB], [1, 64]], Pn=64), op=ALU.mult),
                 reads=[pS.key, "C64"], writes=[SM.key])
            if b == 0:
                P.op("dve", lambda e: e.memset(St.v(0, [[1, 128]]), 0.0), reads=[St.key], writes=[St.key])
            else:
                P.op("dve", lambda e: e.tensor_copy(out=St.v(0, [[1, 128]]), in_=St.v(CPB * 128, [[1, 128]])), reads=[St.key], writes=[St.key])
            for c in range(CPB):
                pu = ps()
                P.op("pe", lambda e, c=c, pu=pu: e.matmul(pu.v(0, [[1, 128]]), lhsT=kw.v(c * 128, [[1, 128]], Pn=64),
                                                          rhs=v_tm.v(c * 128, [[1, 128]], Pn=64), start=True, stop=True),
                     reads=[kw.key, v_tm.key], writes=[pu.key])
                P.op("dve", lambda e, c=c, pu=pu: e.scalar_tensor_tensor(out=St.v((c + 1) * 128, [[1, 128]]), in0=St.v(c * 128, [[1, 128]]),
                                                                         scalar=cdec[h], in1=pu.v(0, [[1, 128]]), op0=ALU.mult, op1=ALU.add),
                     reads=[St.key, pu.key], writes=[St.key])
            pO = [ps(), ps()]
            for c in range(CPB):
                P.op("pe", lambda e, c=c: e.matmul(pO[c // 4].v((c % 4) * 128, [[1, 128]], Pn=64), lhsT=SM.v(c * 64, [[1, 64]], Pn=64),
                                                   rhs=v_tm.v(c * 128, [[1, 128]], Pn=64), start=True, stop=False),
                     reads=[SM.key, v_tm.key], writes=[pO[c // 4].key])
                P.op("pe", lambda e, c=c: e.matmul(pO[c // 4].v((c % 4) * 128, [[1, 128]], Pn=64), lhsT=fmv.v(c * 64, [[1, 64]]),
                                                   rhs=St.v(c * 128, [[1, 128]]), start=False, stop=True),
                     reads=[fmv.key, St.key], writes=[pO[c // 4].key])
            for hb in range((CPB + 3) // 4):
                n = min(4, CPB - hb * 4)
                P.op("act", lambda e, hb=hb, n=n: e.activation(out=o_t.v(hb * 512, [[1, n * 128]], Pn=64), in_=pO[hb].v(0, [[1, n * 128]], Pn=64),
                                                               func=AF.Copy), reads=[pO[hb].key], writes=[o_t.key])
            rms_tm(ph, o_t, CPB, 128, nrmb.v(0, [[0, CPB], [1, 128]], Pn=64), nrmb.key, center=True)
            pY = ps()
            for c in range(CPB):
                P.op("pe", lambda e, c=c, pY=pY: e.transpose(pY.v(c * 64, [[1, 64]]), o_t.v(c * 128, [[1, 128]], Pn=64), c64("id")),
                     reads=[o_t.key, "C64"], writes=[pY.key])
            P.op("dve", lambda e, pY=pY: e.tensor_tensor(out=ytb.v(0, [[1, BLK]]), in0=pY.v(0, [[1, BLK]]), in1=fmz.v(0, [[1, BLK]]), op=ALU.mult),
                 reads=[pY.key, fmz.key], writes=[ytb.key])
            store_yT(ytb, 4 + h, 1, tok0, s)

    def ssd_load1(slot, l, g):
        base = l * D * P_IN
        ld_w(slot, 0, 512, dr["w_in"], base + O_CXBC + g * 512, P_IN)
        ld_w(slot, 4096, 128, dr["w_in"], base + O_CXBC + 1024 + g * 128, P_IN)
        ld_w(slot, 5120, 128, dr["w_in"], base + O_CXBC + 1280 + g * 128, P_IN)
        ld_w(slot, 6144, 8, dr["w_in"], base + O_CDT + g * 8, P_IN)

    def ssd_load2(slot, l, g):
        ld_w(slot, 0, 512, dr["w_in"], l * D * P_IN + O_CZ + g * 512, P_IN)

    def ssd_run(slot, l, g, s):
        enter(S_)
        ph = S_
        zslot = slot
        slot = 1 - slot
        fx, fmq, fmk, tA, Sc, dt_t, dta, lc, wr_t, elc, decb, nrmc, dsk, ytb, szb = [S_.t[n] for n in ['fx', 'fmq', 'fmk', 'tA', 'Sc', 'dt_t', 'dta', 'lc', 'wr_t', 'elc', 'decb', 'nrmc', 'dsk', 'ytb', 'szb']]
        small(nrmc.v(0, [[1, 512]], Pn=64), dv(dr["norm_c"], l * 1024 + g * 512, [[0, 64], [1, 512]]), nrmc.key, chan="rp")
        P.op("dve", lambda e: e.tensor_tensor(out=dsk.v(0, [[64, 8], [1, 64]], Pn=64), in0=C64.v(off64["id"], [[0, 8], [1, 64]], Pn=64),
                                              in1=pc.v(l * 48 + 32 + g * 8, [[1, 8], [0, 64]], Pn=64), op=ALU.mult), reads=["C64", "pc"], writes=[dsk.key])
        for b in range(NBLK):
            tok0 = b * BLK
            for j in range(4):
                pb = ps()
                proj_fm(slot, 0, 512, j * 128, tok0, BLK, pb)
                ch = g * 4 + j
                conv_stream(ph, pb, j, b, lambda k, ch=ch: cwc.v(l * 48 + ch * 4 + k, [[1, 1]]), fx.v(j * BLK, [[1, BLK]]), fx.key,
                            bias_ap=cbc.v(l * 12 + ch, [[1, 1]]))
            if SSTOP <= 0.1:
                continue
            for j, (woff, ch, dst) in enumerate([(4096, 8 + g, fmk), (5120, 10 + g, fmq)]):
                pb = ps()
                proj_fm(slot, woff, 128, 0, tok0, BLK, pb)
                conv_stream(ph, pb, 4 + j, b, lambda k, ch=ch: cwc.v(l * 48 + ch * 4 + k, [[1, 1]]), dst.v(0, [[1, BLK]]), dst.key,
                            bias_ap=cbc.v(l * 12 + ch, [[1, 1]]))
            if SSTOP <= 0.2:
                continue
            for j in range(4):
                pZ = ps()
                proj_fm(zslot, 0, 512, j * 128, tok0, BLK, pZ)
                P.op("act", lambda e, pZ=pZ, j=j: e.activation(out=szb.v(j * BLK, [[1, BLK]]), in_=pZ.v(0, [[1, BLK]]), func=AF.Silu),
                     reads=[pZ.key], writes=[szb.key])
            pd = ps()
            for c in range(CPB):
                proj_tm(slot, 6144, 8, 0, 8, tok0 + c * 64, pd, pcol=c * 8)
            P.op("dve", lambda e, pd=pd: e.tensor_tensor(out=dt_t.v(0, [[8, CPB], [1, 8]], Pn=64), in0=pd.v(0, [[8, CPB], [1, 8]], Pn=64),
                                                         in1=pc.v(l * 48 + g * 8, [[0, CPB], [1, 8]], Pn=64), op=ALU.add),
                 reads=[pd.key, "pc"], writes=[dt_t.key])
            P.op("act", lambda e: e.activation(out=dt_t.v(0, [[1, CPB * 8]], Pn=64), in_=dt_t.v(0, [[1, CPB * 8]], Pn=64), func=AF.Exp),
                 reads=[dt_t.key], writes=[dt_t.key])
            P.op("act", lambda e: e.activation(out=dt_t.v(0, [[1, CPB * 8]], Pn=64), in_=dt_t.v(0, [[1, CPB * 8]], Pn=64), func=AF.Ln,
                                               bias=one_ap[0:64, :]), reads=[dt_t.key, "epst"], writes=[dt_t.key])
            P.op("dve", lambda e: e.tensor_tensor(out=dta.v(0, [[8, CPB], [1, 8]], Pn=64), in0=dt_t.v(0, [[8, CPB], [1, 8]], Pn=64),
                                                  in1=nac.v(l * 16 + g * 8, [[0, CPB], [1, 8]], Pn=64), op=ALU.mult),
                 reads=[dt_t.key, "nac"], writes=[dta.key])
            if SSTOP <= 0.3:
                continue
            pl = ps()
            P.op("pe", lambda e, pl=pl: e.matmul(pl.v(0, [[1, CPB * 8]], Pn=64), lhsT=c64("tri"), rhs=dta.v(0, [[1, CPB * 8]], Pn=64),
                                                 start=True, stop=True), reads=[dta.key, "C64"], writes=[pl.key])
            P.op("pe", lambda e, pl=pl: e.matmul(pl.v(128, [[1, CPB * 8]]), lhsT=c64("ones"), rhs=dta.v(0, [[1, CPB * 8]], Pn=64),
                                                 start=True, stop=True), reads=[dta.key, "C64"], writes=[pl.key])
            P.op("dve", lambda e, pl=pl: e.tensor_copy(out=lc.v(0, [[1, CPB * 8]], Pn=64), in_=pl.v(0, [[1, CPB * 8]], Pn=64)),
                 reads=[pl.key], writes=[lc.key])
            if SSTOP <= 0.45:
                continue
            P.op("dve", lambda e, pl=pl: e.tensor_copy(out=decb.v(0, [[1, CPB * 8]]), in_=pl.v(128, [[1, CPB * 8]])),
                 reads=[pl.key], writes=[decb.key])
            P.op("act", lambda e: e.activation(out=decb.v(0, [[1, CPB * 8]]), in_=decb.v(0, [[1, CPB * 8]]), func=AF.Exp),
                 reads=[decb.key], writes=[decb.key])
            if SSTOP <= 0.5:
                continue
            P.op("dve", lambda e, pl=pl: e.tensor_tensor(out=wr_t.v(0, [[1, CPB * 8]], Pn=64), in0=pl.v(128, [[1, CPB * 8]], Pn=64),
                                                         in1=lc.v(0, [[1, CPB * 8]], Pn=64), op=ALU.subtract), reads=[pl.key, lc.key], writes=[wr_t.key])
            P.op("act", lambda e: e.activation(out=wr_t.v(0, [[1, CPB * 8]], Pn=64), in_=wr_t.v(0, [[1, CPB * 8]], Pn=64), func=AF.Exp),
                 reads=[wr_t.key], writes=[wr_t.key])
            P.op("dve", lambda e: e.tensor_tensor(out=wr_t.v(0, [[1, CPB * 8]], Pn=64), in0=wr_t.v(0, [[1, CPB * 8]], Pn=64),
                                                  in1=dt_t.v(0, [[1, CPB * 8]], Pn=64), op=ALU.mult), reads=[wr_t.key, dt_t.key], writes=[wr_t.key])
            P.op("act", lambda e: e.activation(out=elc.v(0, [[1, CPB * 8]], Pn=64), in_=lc.v(0, [[1, CPB * 8]], Pn=64), func=AF.Exp),
                 reads=[lc.key], writes=[elc.key])
            if SSTOP <= 1:
                continue
            if b == 0:
                P.op("dve", lambda e: e.memset(Sc.v(0, [[1, 512]]), 0.0), reads=[Sc.key], writes=[Sc.key])
            def stage1(c):
                    ct = tok0 + c * 64
                    db, e1, x_tm, xw, yi, yg, cbT, B_tm, st2 = [S_.t[n + str(c % 2)] for n in ['db', 'e1', 'x_tm', 'xw', 'yi', 'yg', 'cbT', 'B_tm', 'st2']]
                    P.op("dve", lambda e, c=c: e.tensor_copy(out=db.v(0, [[64, 8], [1, 64]], Pn=64), in_=dta.v(c * 8, [[1, 8], [0, 64]], Pn=64)),
                         reads=[dta.key], writes=[db.key])
                    pL = ps()
                    for hh in range(8):
                        P.op("pe", lambda e, hh=hh, pL=pL: e.matmul(pL.v(hh * 64, [[1, 64]], Pn=64), lhsT=db.v(hh * 64, [[1, 64]], Pn=64),
                                                                    rhs=c64("tri"), start=True, stop=True), reads=[db.key, "C64"], writes=[pL.key])
                    P.op("dve", lambda e, c=c, pL=pL: e.tensor_tensor(out=e1.v(0, [[64, 8], [1, 64]], Pn=64), in0=pL.v(0, [[64, 8], [1, 64]], Pn=64),
                                                                      in1=lc.v(c * 8, [[1, 8], [0, 64]], Pn=64), op=ALU.subtract),
                         reads=[pL.key, lc.key], writes=[e1.key])
                    P.op("dve", lambda e: e.scalar_tensor_tensor(out=e1.v(0, [[1, 512]], Pn=64), in0=e1.v(0, [[1, 512]], Pn=64), scalar=-1.0,
                                                                 in1=e1.v(0, [[1, 512]], Pn=64), op0=ALU.mult, op1=ALU.max),
                         reads=[e1.key], writes=[e1.key])
                    P.op("act", lambda e: e.activation(out=e1.v(0, [[1, 512]], Pn=64), in_=e1.v(0, [[1, 512]], Pn=64), func=AF.Exp, scale=-1.0),
                         reads=[e1.key], writes=[e1.key])
                    if SSTOP <= 2:
                        return
                    pc_ = ps()
                    P.op("pe", lambda e, c=c, pc_=pc_: e.matmul(pc_.v(0, [[1, 64]], Pn=64), lhsT=fmk.v(c * 64, [[1, 64]]), rhs=fmq.v(c * 64, [[1, 64]]),
                                                                start=True, stop=True), reads=[fmk.key, fmq.key], writes=[pc_.key])
                    P.op("act", lambda e, pc_=pc_: e.activation(out=cbT.v(0, [[1, 64]], Pn=64), in_=pc_.v(0, [[1, 64]], Pn=64), func=AF.Copy),
                         reads=[pc_.key], writes=[cbT.key])
                    P.op("dve", lambda e: e.tensor_tensor(out=e1.v(0, [[64, 8], [1, 64]], Pn=64), in0=e1.v(0, [[64, 8], [1, 64]], Pn=64),
                                                          in1=cbT.v(0, [[0, 8], [1, 64]], Pn=64), op=ALU.mult), reads=[e1.key, cbT.key], writes=[e1.key])
                    P.op("dve", lambda e, c=c: e.tensor_tensor(out=e1.v(0, [[64, 8], [1, 64]], Pn=64), in0=e1.v(0, [[64, 8], [1, 64]], Pn=64),
                                                               in1=dt_t.v(c * 8, [[1, 8], [0, 64]], Pn=64), op=ALU.mult), reads=[e1.key, dt_t.key], writes=[e1.key])
                    P.op("dve", lambda e: e.tensor_tensor(out=e1.v(0, [[1, 512]], Pn=64), in0=e1.v(0, [[1, 512]], Pn=64),
                                                          in1=dsk.v(0, [[1, 512]], Pn=64), op=ALU.add), reads=[e1.key, dsk.key], writes=[e1.key])
                    if SSTOP <= 3:
                        return
                    pX = ps()
                    for j in range(4):
                        P.op("pe", lambda e, c=c, j=j, pX=pX: e.transpose(pX.v(j * 128, [[1, 128]], Pn=64), fx.v(j * BLK + c * 64, [[1, 64]]), c128("id")),
                             reads=[fx.key, "C128"], writes=[pX.key])
                    P.op("act", lambda e, pX=pX: e.activation(out=x_tm.v(0, [[1, 512]], Pn=64), in_=pX.v(0, [[1, 512]], Pn=64), func=AF.Copy),
                         reads=[pX.key], writes=[x_tm.key])
                    P.op("dve", lambda e, c=c, pX=pX: e.tensor_tensor(out=xw.v(0, [[64, 8], [1, 64]], Pn=64), in0=pX.v(0, [[64, 8], [1, 64]], Pn=64),
                                                                      in1=wr_t.v(c * 8, [[1, 8], [0, 64]], Pn=64), op=ALU.mult),
                         reads=[pX.key, wr_t.key], writes=[xw.key])
                    pBt = ps()
                    P.op("pe", lambda e, c=c, pBt=pBt: e.transpose(pBt.v(0, [[1, 128]], Pn=64), fmk.v(c * 64, [[1, 64]]), c128("id")),
                         reads=[fmk.key, "C128"], writes=[pBt.key])
                    P.op("act", lambda e, pBt=pBt: e.activation(out=B_tm.v(0, [[1, 128]], Pn=64), in_=pBt.v(0, [[1, 128]], Pn=64), func=AF.Copy),
                         reads=[pBt.key], writes=[B_tm.key])
                    pI = ps()
                    for hh in range(8):
                        P.op("pe", lambda e, hh=hh, pI=pI: e.matmul(pI.v(hh * 64, [[1, 64]], Pn=64), lhsT=e1.v(hh * 64, [[1, 64]], Pn=64),
                                                                    rhs=x_tm.v(hh * 64, [[1, 64]], Pn=64), start=True, stop=True),
                             reads=[e1.key, x_tm.key], writes=[pI.key])
                    pN = ps()
                    for qq in range(4):
                        P.op("pe", lambda e, c=c, pN=pN, qq=qq: e.matmul(pN.v(qq * 128, [[1, 128]], Pn=64), lhsT=fmq.v(c * 64, [[1, 64]]),
                                                                         rhs=Sc.v(qq * 128, [[1, 128]]), start=True, stop=True),
                             reads=[fmq.key, Sc.key], writes=[pN.key])
                    P.op("act", lambda e, pI=pI: e.activation(out=yi.v(0, [[1, 512]], Pn=64), in_=pI.v(0, [[1, 512]], Pn=64), func=AF.Copy),
                         reads=[pI.key], writes=[yi.key])
                    P.op("dve", lambda e, c=c, pN=pN: e.tensor_tensor(out=yg.v(0, [[64, 8], [1, 64]], Pn=64), in0=pN.v(0, [[64, 8], [1, 64]], Pn=64),
                                                                      in1=elc.v(c * 8, [[1, 8], [0, 64]], Pn=64), op=ALU.mult),
                         reads=[pN.key, elc.key], writes=[yg.key])
                    P.op("dve", lambda e: e.tensor_tensor(out=yg.v(0, [[1, 512]], Pn=64), in0=yg.v(0, [[1, 512]], Pn=64), in1=yi.v(0, [[1, 512]], Pn=64),
                                                          op=ALU.add), reads=[yg.key, yi.key], writes=[yg.key])
                    if SSTOP <= 4:
                        return
                    pS_ = ps()
                    for qq in range(4):
                        P.op("pe", lambda e, pS_=pS_, qq=qq: e.matmul(pS_.v(qq * 128, [[1, 128]]), lhsT=B_tm.v(0, [[1, 128]], Pn=64),
                                                                      rhs=xw.v(qq * 128, [[1, 128]], Pn=64), start=True, stop=True),
                             reads=[B_tm.key, xw.key], writes=[pS_.key])
                    P.op("dve", lambda e, c=c: e.tensor_tensor(out=Sc.v(0, [[64, 8], [1, 64]]), in0=Sc.v(0, [[64, 8], [1, 64]]),
                                                               in1=decb.v(c * 8, [[1, 8], [0, 64]]), op=ALU.mult), reads=[Sc.key, decb.key], writes=[Sc.key])
                    P.op("dve", lambda e, pS_=pS_: e.tensor_tensor(out=Sc.v(0, [[1, 512]]), in0=Sc.v(0, [[1, 512]]), in1=pS_.v(0, [[1, 512]]), op=ALU.add),
                         reads=[Sc.key, pS_.key], writes=[Sc.key])

            def stage2(c):
                    ct = tok0 + c * 64
                    db, e1, x_tm, xw, yi, yg, cbT, B_tm, st2 = [S_.t[n + str(c % 2)] for n in ['db', 'e1', 'x_tm', 'xw', 'yi', 'yg', 'cbT', 'B_tm', 'st2']]
                    pZ = ps()
                    for j in range(4):
                        P.op("pe", lambda e, j=j, pZ=pZ: e.transpose(pZ.v(j * 128, [[1, 128]], Pn=64), szb.v(j * BLK + c * 64, [[1, 64]]), c128("id")),
                             reads=[szb.key, "C128"], writes=[pZ.key])
                    P.op("dve", lambda e, pZ=pZ: e.tensor_tensor(out=yg.v(0, [[1, 512]], Pn=64), in0=yg.v(0, [[1, 512]], Pn=64), in1=pZ.v(0, [[1, 512]], Pn=64),
                                                                 op=ALU.mult), reads=[yg.key, pZ.key], writes=[yg.key])
                    P.op("act", lambda e: e.activation(out=yi.v(0, [[1, 512]], Pn=64), in_=yg.v(0, [[1, 512]], Pn=64), func=AF.Square),
                         reads=[yg.key], writes=[yi.key])
                    P.op("dve", lambda e: e.tensor_reduce(out=st2.v(0, [[1, 1]], Pn=64), in_=yi.v(0, [[1, 512]], Pn=64), axis=AX.X, op=ALU.add),
                         reads=[yi.key], writes=[st2.key])
                    P.op("act", lambda e: e.activation(out=st2.v(0, [[1, 1]], Pn=64), in_=st2.v(0, [[1, 1]], Pn=64), func=AF.Ln,
                                                       bias=eps_ap[0:64, :], scale=1.0 / 512), reads=[st2.key, "epst"], writes=[st2.key])
                    P.op("act", lambda e: e.activation(out=st2.v(0, [[1, 1]], Pn=64), in_=st2.v(0, [[1, 1]], Pn=64), func=AF.Exp, scale=-0.5),
                         reads=[st2.key], writes=[st2.key])
                    P.op("dve", lambda e: e.scalar_tensor_tensor(out=yg.v(0, [[1, 512]], Pn=64), in0=yg.v(0, [[1, 512]], Pn=64), scalar=st2.v(0, [[1, 1]], Pn=64),
                                                                 in1=nrmc.v(0, [[1, 512]], Pn=64), op0=ALU.mult, op1=ALU.mult),
                         reads=[yg.key, st2.key, nrmc.key], writes=[yg.key])
                    pY = ps()
                    for j in range(4):
                        P.op("pe", lambda e, j=j, pY=pY: e.transpose(pY.v(j * 64, [[1, 64]]), yg.v(j * 128, [[1, 128]], Pn=64), c64("id")),
                             reads=[yg.key, "C64"], writes=[pY.key])
                    P.op("act", lambda e, c=c, pY=pY: e.activation(out=ytb.v(c * 64, [[BLK, 4], [1, 64]]), in_=pY.v(0, [[64, 4], [1, 64]]), func=AF.Copy),
                         reads=[pY.key], writes=[ytb.key])

            stage1(0)
            for c in range(CPB):
                A = P.capture(lambda: with_banks([0, 1, 2], lambda: stage2(c)))
                B = P.capture(lambda: with_banks([3, 4, 5, 6, 7], lambda: stage1(c + 1))) if c + 1 < CPB else []
                P.merge(A, B)
            store_yT(ytb, 8 + g * 4, 4, tok0, s)

    def mrg_load(slot, l, dc):
        base = l * D * P_IN
        for br in range(3):
            ld_w(slot, br * 1024, 128, dr["w_in"], base + O_GATE + br * 1024 + dc * 128, P_IN)
        ld_w(slot, 3072, 128, dr["w_branch_a"], l * 512 * D + dc * 128, D, nk=4)
        ld_w(slot, 3072 + 512, 128, dr["w_branch_b"], l * 512 * D + dc * 128, D, nk=4)
        ld_w(slot, 3072 + 1024, 128, dr["w_branch_c"], l * 1024 * D + dc * 128, D, nk=8)

    mrg_cnt = [0]

    def mrg_run(slot, l, dc, s):
        enter(M_)
        tA, tB = [M_.t[n] for n in ['tA', 'tB']]
        for b in range(NBLK):
            tok0 = b * BLK
            ytl = M_.t["ytl%d" % (mrg_cnt[0] % 2)]
            mrg_cnt[0] += 1
            P.dma("sp", lambda e, tok0=tok0: e.dma_start(out=ytl.v(0, [[BLK, 16], [1, BLK]]),
                                                         in_=dv(ytd, (s % 2) * 128 * 16 * T + tok0, [[16 * T, 128], [T, 16], [1, BLK]])),
                  reads=["ytd%d_%d_%d" % (s % 2, i, b) for i in range(16)], writes=[ytl.key], chan=ytl.key)
            first = True
            for br, (f0, nf, woff) in enumerate([(0, 4, 3072), (4, 4, 3072 + 512), (8, 8, 3072 + 1024)]):
                pg = ps()
                proj_fm(slot, br * 1024, 128, 0, tok0, BLK, pg)
                P.op("act", lambda e, pg=pg, br=br: e.activation(out=tA.v(0, [[1, BLK]]), in_=pg.v(0, [[1, BLK]]), func=AF.Sigmoid,
                                                                 bias=bgt.v(l * 24 + br * 8 + dc, [[1, 1]])), reads=[pg.key, "bgt"], writes=[tA.key])
                pb = ps()
                for k in range(nf):
                    P.op("pe", lambda e, k=k, pb=pb, woff=woff, f0=f0, nf=nf: e.matmul(pb.v(0, [[1, BLK]]), lhsT=WB[slot].v(woff + k * 128, [[1, 128]]),
                                                                                       rhs=ytl.v((f0 + k) * BLK, [[1, BLK]]), start=(k == 0), stop=(k == nf - 1)),
                         reads=["WB%d" % slot, ytl.key], writes=[pb.key])
                if first:
                    P.op("dve", lambda e, pb=pb: e.tensor_tensor(out=tB.v(0, [[1, BLK]]), in0=pb.v(0, [[1, BLK]]), in1=tA.v(0, [[1, BLK]]), op=ALU.mult),
                         reads=[pb.key, tA.key], writes=[tB.key])
                    first = False
                else:
                    P.op("dve", lambda e, pb=pb: e.tensor_tensor(out=tA.v(0, [[1, BLK]]), in0=pb.v(0, [[1, BLK]]), in1=tA.v(0, [[1, BLK]]), op=ALU.mult),
                         reads=[pb.key, tA.key], writes=[tA.key])
                    if br == 1:
                        P.op("dve", lambda e: e.tensor_tensor(out=tB.v(0, [[1, BLK]]), in0=tB.v(0, [[1, BLK]]), in1=tA.v(0, [[1, BLK]]), op=ALU.add),
                             reads=[tA.key, tB.key], writes=[tB.key])
                    else:
                        P.op("dve", lambda e, tok0=tok0: e.tensor_tensor(out=mrgT.v(dc * T + tok0, [[1, BLK]]), in0=tB.v(0, [[1, BLK]]),
                                                                         in1=tA.v(0, [[1, BLK]]), op=ALU.add),
                             reads=[tA.key, tB.key], writes=["mrg%d" % b])


    def ln_tile(ph, src_ap, src_keys, l, which, tile, s, final, do_router):
        par = str(tile % 2)
        lng, lnb = ph.t['lng'], ph.t['lnb']
        xn, bst, mv = ph.t['xn' + par], ph.t['bst' + par], ph.t['mv' + par]
        xTf = ph.t.get('xTf' + par)
        if WSTOP <= 1:
            return
        for hh in range(2):
            P.op("dve", lambda e, hh=hh: e.bn_stats(out=bst.v(hh * 6, [[1, 6]]), in_=src_ap[:, hh * 512:(hh + 1) * 512]), reads=src_keys, writes=[bst.key])
        P.op("dve", lambda e: e.bn_aggr(out=mv.v(0, [[1, 2]]), in_=bst.v(0, [[1, 12]])), reads=[bst.key], writes=[mv.key])
        P.op("act", lambda e: e.activation(out=mv.v(2, [[1, 1]]), in_=mv.v(1, [[1, 1]]), func=AF.Ln, bias=eps_ap, scale=1.0), reads=[mv.key, "epst"], writes=[mv.key])
        P.op("act", lambda e: e.activation(out=mv.v(2, [[1, 1]]), in_=mv.v(2, [[1, 1]]), func=AF.Exp, scale=-0.5), reads=[mv.key], writes=[mv.key])
        P.op("dve", lambda e: e.tensor_scalar(out=xn.v(0, [[1, 1024]]), in0=src_ap, scalar1=mv.v(0, [[1, 1]]), scalar2=mv.v(2, [[1, 1]]),
                                              op0=ALU.subtract, op1=ALU.mult), reads=list(src_keys) + [mv.key], writes=[xn.key])
        P.op("dve", lambda e: e.tensor_tensor(out=xn.v(0, [[1, 1024]]), in0=xn.v(0, [[1, 1024]]), in1=lng.v(0, [[1, 1024]]), op=ALU.mult),
             reads=[xn.key, lng.key], writes=[xn.key])
        P.op("dve", lambda e: e.tensor_tensor(out=xn.v(0, [[1, 1024]]), in0=xn.v(0, [[1, 1024]]), in1=lnb.v(0, [[1, 1024]]), op=ALU.add),
             reads=[xn.key, lnb.key], writes=[xn.key])
        if WSTOP <= 2:
            return
        if final:
            o = P.dma("sp", lambda e: e.dma_start(out=dv(yout, (s * T + tile * 128) * D, [[D, 128], [1, D]]), in_=xn.v(0, [[1, 1024]])),
                      reads=[xn.key], writes=["yout%d_%d" % (s, tile)], chan="out" + par)
            outs.append(o)
        else:
            P.dma("sp", lambda e: e.dma_start(out=dv(xres[which], tile * 128 * D, [[D, 128], [1, D]]), in_=xn.v(0, [[1, 1024]])),
                  reads=[xn.key], writes=["xres%d_p%s" % (which, par)], chan="xr%d_%s" % (which, par))
            if WSTOP <= 2.5:
                return
            for half in range(2):
                pt = ps()
                for k in range(4):
                    kc = half * 4 + k
                    P.op("pe", lambda e, k=k, kc=kc, pt=pt: e.transpose(pt.v(k * 128, [[1, 128]]), xn.v(kc * 128, [[1, 128]]), c128("id")),
                         reads=[xn.key, "C128"], writes=[pt.key])
                P.op("act", lambda e, half=half, pt=pt: e.activation(out=xT.v(half * 4 * T + tile * 128, [[T, 4], [1, 128]]), in_=pt.v(0, [[128, 4], [1, 128]]),
                                                                     func=AF.Copy), reads=[pt.key], writes=["xT%d" % tile])
                if do_router and WSTOP > 2.7:
                    P.op("dve", lambda e, half=half, pt=pt: e.tensor_copy(out=xTf.v(half * 512, [[1, 512]]), in_=pt.v(0, [[1, 512]])),
                         reads=[pt.key], writes=[xTf.key])
            if do_router and WSTOP > 3:
                router(ph, l, tile)

    outs = []

    def router(ph, l, tile):
        xTf = ph.t['xTf' + str(tile % 2)]
        rtb = ph.t['rtb']
        pr = ps()
        for kc in range(KC):
            P.op("pe", lambda e, kc=kc, pr=pr: e.matmul(pr.v(0, [[1, 36]]), lhsT=xTf.v(kc * 128, [[1, 128]]), rhs=wr.v(l * KC * 36 + kc * 36, [[1, 36]]),
                                                        start=(kc == 0), stop=(kc == KC - 1)), reads=[xTf.key, "wr"], writes=[pr.key])
        P.op("dve", lambda e: e.tensor_tensor(out=rtb.v(tile * 160, [[1, 36]]), in0=pr.v(0, [[1, 36]]), in1=brt.v(l * 36, [[1, 36]]), op=ALU.add),
             reads=[pr.key, "brt"], writes=[rtb.key])

    def router_batch(ph):
        rtb = ph.t['rtb']
        k = [rtb.key]
        F = lambda o, n: rtb.v(o, [[160, NT], [1, n]])
        Bc = lambda o, n: rtb.v(o, [[160, NT], [0, n]])
        S1 = lambda o: rtb.v(o, [[160, NT]])
        P.op("dve", lambda e: e.tensor_reduce(out=S1(36), in_=F(0, 4), axis=AX.X, op=ALU.max), reads=k, writes=k)
        P.op("dve", lambda e: e.tensor_tensor(out=F(37, 4), in0=F(0, 4), in1=Bc(36, 4), op=ALU.is_ge), reads=k, writes=k)
        P.op("dve", lambda e: e.tensor_tensor(out=F(41, 4), in0=F(0, 4), in1=Bc(36, 4), op=ALU.subtract), reads=k, writes=k)
        P.op("act", lambda e: e.activation(out=F(41, 4), in_=F(41, 4), func=AF.Exp), reads=k, writes=k)
        P.op("dve", lambda e: e.tensor_reduce(out=S1(45), in_=F(41, 4), axis=AX.X, op=ALU.add), reads=k, writes=k)
        P.op("dve", lambda e: e.reciprocal(out=S1(46), in_=S1(45)), reads=k, writes=k)
        P.op("dve", lambda e: e.tensor_scalar(out=F(41, 4), in0=F(37, 4), scalar1=-1.0, scalar2=BIGM, op0=ALU.add, op1=ALU.mult), reads=k, writes=k)
        P.op("dve", lambda e: e.tensor_tensor(out=rtb.v(48, [[160, NT], [8, 4], [1, 8]]), in0=rtb.v(4, [[160, NT], [8, 4], [1, 8]]),
                                              in1=rtb.v(41, [[160, NT], [1, 4], [0, 8]]), op=ALU.add), reads=k, writes=k)
        P.op("dve", lambda e: e.tensor_reduce(out=S1(80), in_=F(48, 32), axis=AX.X, op=ALU.max), reads=k, writes=k)
        P.op("dve", lambda e: e.tensor_tensor(out=F(82, 32), in0=F(48, 32), in1=Bc(80, 32), op=ALU.is_ge), reads=k, writes=k)
        P.op("dve", lambda e: e.scalar_tensor_tensor(out=F(48, 32), in0=F(82, 32), scalar=-BIGM, in1=F(48, 32), op0=ALU.mult, op1=ALU.add), reads=k, writes=k)
        P.op("dve", lambda e: e.tensor_reduce(out=S1(81), in_=F(48, 32), axis=AX.X, op=ALU.max), reads=k, writes=k)
        P.op("dve", lambda e: e.tensor_tensor(out=F(114, 32), in0=F(48, 32), in1=Bc(81, 32), op=ALU.is_ge), reads=k, writes=k)
        P.op("dve", lambda e: e.tensor_tensor(out=S1(146), in0=S1(81), in1=S1(80), op=ALU.subtract), reads=k, writes=k)
        P.op("act", lambda e: e.activation(out=S1(146), in_=S1(146), func=AF.Exp), reads=k, writes=k)
        P.op("dve", lambda e: e.tensor_scalar(out=S1(146), in0=S1(146), scalar1=1.0, scalar2=None, op0=ALU.add), reads=k, writes=k)
        P.op("dve", lambda e: e.reciprocal(out=S1(146), in_=S1(146)), reads=k, writes=k)
        P.op("dve", lambda e: e.tensor_tensor(out=S1(146), in0=S1(146), in1=S1(46), op=ALU.mult), reads=k, writes=k)
        P.op("dve", lambda e: e.tensor_tensor(out=S1(147), in0=S1(46), in1=S1(146), op=ALU.subtract), reads=k, writes=k)
        P.op("dve", lambda e: e.tensor_tensor(out=F(82, 32), in0=F(82, 32), in1=Bc(146, 32), op=ALU.mult), reads=k, writes=k)
        P.op("dve", lambda e: e.tensor_tensor(out=F(114, 32), in0=F(114, 32), in1=Bc(147, 32), op=ALU.mult), reads=k, writes=k)
        P.op("dve", lambda e: e.tensor_tensor(out=comb.v(0, [[32, NT], [1, 32]]), in0=F(114, 32), in1=F(82, 32), op=ALU.add), reads=k,
             writes=["comb%d" % t for t in range(NT)])

    def load_ln(ph, l, which):
        lng, lnb = ph.t['lng'], ph.t['lnb']
        g, b_ = ("ln1_g", "ln1_b") if which == 1 else ("ln2_g", "ln2_b")
        P.dma("sp", lambda e: e.dma_start(out=lng.v(0, [[1, 1024]]), in_=dv(dr[g], l * D, [[0, 128], [1, D]])), writes=[lng.key], chan="lng")
        P.dma("sp", lambda e: e.dma_start(out=lnb.v(0, [[1, 1024]]), in_=dv(dr[b_], l * D, [[0, 128], [1, D]])), writes=[lnb.key], chan="lnb")

    def wout_load1(slot, l):
        ld_w(slot, 0, 512, dr["w_out"], l * D * D, D)

    def wout_load2(slot, l):
        ld_w(slot, 0, 512, dr["w_out"], l * D * D + 512, D)

    def wout_run(slot, l, s, src_dram_ap_fn, src_keys_fn):
        enter(W_)
        xl = [W_.t["xl0"], W_.t["xl1"]]
        slots = [1 - slot, slot]
        load_ln(W_, l, 1)
        def MM(tile):
            xo = xl[tile % 2]
            P.dma("sp", lambda e, tile=tile, xo=xo: e.dma_start(out=xo.v(0, [[1, 1024]]), in_=src_dram_ap_fn(tile)), reads=src_keys_fn(tile),
                  writes=[xo.key], chan="xl%d" % (tile % 2))
            for half in range(2):
                pm = ps()
                for kc in range(KC):
                    P.op("pe", lambda e, kc=kc, pm=pm, half=half, tile=tile: e.matmul(pm.v(0, [[1, 512]]), lhsT=mrgT.v(kc * T + tile * 128, [[1, 128]]),
                                                                                      rhs=WB[slots[half]].v(kc * 512, [[1, 512]]),
                                                                                      start=(kc == 0), stop=(kc == KC - 1)),
                         reads=["WB%d" % slots[half], "mrg%d" % (tile * 128 // BLK)], writes=[pm.key])
                P.op("dve", lambda e, pm=pm, half=half, xo=xo: e.scalar_tensor_tensor(out=xo.v(half * 512, [[1, 512]]), in0=xo.v(half * 512, [[1, 512]]),
                                                                                      scalar=ALPHA, in1=pm.v(0, [[1, 512]]), op0=ALU.mult, op1=ALU.add),
                     reads=[xo.key, pm.key], writes=[xo.key])

        MM(0)
        for tile in range(NT):
            if tile + 1 < NT:
                MM(tile + 1)
            xo = xl[tile % 2]
            ln_tile(W_, xo.v(0, [[1, 1024]]), [xo.key], l, 0, tile, s, False, True)
        router_batch(W_)

    def moe_load(slot, l, e_, hf):
        ld_w(slot, 0, 256, dr["w_gate_e"], ((l * NE + e_) * D) * DE + hf * 256, DE)
        ld_w(slot, 2048, 256, dr["w_up_e"], ((l * NE + e_) * D) * DE + hf * 256, DE)
        ld_w(slot, 4096, 1024, dr["w_down_e"], ((l * NE + e_) * DE + hf * 256) * D, D, nk=2)

    def moe_init(l, s):
        enter(E_)
        for tile in range(NT):
            P.dma("sp", lambda e, tile=tile: e.dma_start(out=yacc.v(tile * 1024, [[1, 1024]]), in_=dv(xres[0], tile * 128 * D, [[D, 128], [1, D]])),
                  reads=["xres0_p0", "xres0_p1"], writes=["yacc%d" % tile], chan="ya%d" % tile)
            P.op("pool", lambda e, tile=tile: e.tensor_scalar(out=yacc.v(tile * 1024, [[1, 1024]]), in0=yacc.v(tile * 1024, [[1, 1024]]), scalar1=ALPHA,
                                                               scalar2=None, op0=ALU.mult), reads=["yacc%d" % tile], writes=["yacc%d" % tile])

    def moe_run(slot, l, e_, hf, s):
        wk = "WB%d" % slot
        Hh = [E_.t["Hh0"], E_.t["Hh1"]]
        hs = [E_.t["hs0"], E_.t["hs1"]]
        def GU(b):
            tok0 = b * BLK
            H = Hh[b % 2]
            for k2 in range(2):
                pg = ps()
                pu = ps()
                for kc in range(KC):
                    P.op("pe", lambda e, kc=kc, pg=pg, k2=k2, tok0=tok0: e.matmul(pg.v(0, [[1, BLK]]), lhsT=WB[slot].v(kc * 256 + k2 * 128, [[1, 128]]),
                                                                                  rhs=xT.v(kc * T + tok0, [[1, BLK]]), start=(kc == 0), stop=(kc == KC - 1)),
                         reads=[wk] + xt_keys(tok0, BLK), writes=[pg.key])
                for kc in range(KC):
                    P.op("pe", lambda e, kc=kc, pu=pu, k2=k2, tok0=tok0: e.matmul(pu.v(0, [[1, BLK]]), lhsT=WB[slot].v(2048 + kc * 256 + k2 * 128, [[1, 128]]),
                                                                                  rhs=xT.v(kc * T + tok0, [[1, BLK]]), start=(kc == 0), stop=(kc == KC - 1)),
                         reads=[wk] + xt_keys(tok0, BLK), writes=[pu.key])
                hsx = hs[k2]
                P.op("act", lambda e, pg=pg, hsx=hsx: e.activation(out=hsx.v(0, [[1, BLK]]), in_=pg.v(0, [[1, BLK]]), func=AF.Silu), reads=[pg.key], writes=[hsx.key])
                P.op("dve", lambda e, pu=pu, hsx=hsx, H=H, k2=k2: e.tensor_tensor(out=H.v(k2 * BLK, [[1, BLK]]), in0=hsx.v(0, [[1, BLK]]), in1=pu.v(0, [[1, BLK]]),
                                                                                  op=ALU.mult), reads=[pu.key, hsx.key], writes=[H.key])
        def DN(b):
            H = Hh[b % 2]
            for t in range(TPB):
                tile = b * TPB + t
                for half in range(2):
                    py = ps()
                    for k2 in range(2):
                        P.op("pe", lambda e, k2=k2, py=py, t=t, half=half, H=H: e.matmul(py.v(0, [[1, 512]]), lhsT=H.v(k2 * BLK + t * 128, [[1, 128]]),
                                                                                         rhs=WB[slot].v(4096 + k2 * 1024 + half * 512, [[1, 512]]),
                                                                                         start=(k2 == 0), stop=(k2 == 1)), reads=[wk, H.key], writes=[py.key])
                    P.op("dve", lambda e, py=py, tile=tile, half=half: e.scalar_tensor_tensor(
                        out=yacc.v(tile * 1024 + half * 512, [[1, 512]]), in0=py.v(0, [[1, 512]]), scalar=comb.v(tile * 32 + e_, [[1, 1]]),
                        in1=yacc.v(tile * 1024 + half * 512, [[1, 512]]), op0=ALU.mult, op1=ALU.add),
                        reads=[py.key, "comb%d" % tile, "yacc%d" % tile], writes=["yacc%d" % tile])

        GU(0)
        for b in range(NBLK):
            if b + 1 < NBLK:
                GU(b + 1)
            DN(b)

    def ln2_run(l, s, final):
        enter(N_)
        load_ln(N_, l, 2)
        for tile in range(NT):
            ln_tile(N_, yacc.v(tile * 1024, [[1, 1024]]), ["yacc%d" % tile], l, 1, tile, s, final, False)

    def x0_run(s):
        enter(X_)
        xl = [X_.t["xl0"], X_.t["xl1"]]
        for tile in range(NT):
            xo = xl[tile % 2]
            P.dma("sp", lambda e, tile=tile, xo=xo: e.dma_start(out=xo.v(0, [[1, 1024]]), in_=dv(dr["x"], (s * T + tile * 128) * D, [[D, 128], [1, D]])),
                  writes=[xo.key], chan="xl%d" % (tile % 2))
            for half in range(2):
                pt = ps()
                for k in range(4):
                    kc = half * 4 + k
                    P.op("pe", lambda e, k=k, kc=kc, pt=pt, xo=xo: e.transpose(pt.v(k * 128, [[1, 128]]), xo.v(kc * 128, [[1, 128]]), c128("id")),
                         reads=[xo.key, "C128"], writes=[pt.key])
                P.op("act", lambda e, half=half, pt=pt, tile=tile: e.activation(out=xT.v(half * 4 * T + tile * 128, [[T, 4], [1, 128]]),
                                                                                in_=pt.v(0, [[128, 4], [1, 128]]), func=AF.Copy),
                     reads=[pt.key], writes=["xT%d" % tile])

    units = []
    for s in range(NSEQ):
        units.append((None, lambda slot, s=s: x0_run(s)))
        for l in range(L):
            for h in range(4):
                units.append((lambda slot, l=l, h=h: gdn_load(slot, l, h), lambda slot, l=l, h=h, s=s: gdn_run(slot, l, h, s)))
            for h in range(4):
                units.append((lambda slot, l=l, h=h: ret_load(slot, l, h), lambda slot, l=l, h=h, s=s: ret_run(slot, l, h, s)))
            for g in range(2):
                units.append((lambda slot, l=l, g=g: ssd_load1(slot, l, g), lambda slot: None))
                units.append((lambda slot, l=l, g=g: ssd_load2(slot, l, g), lambda slot, l=l, g=g, s=s: ssd_run(slot, l, g, s), True))
            for dc in range(8):
                units.append((lambda slot, l=l, dc=dc: mrg_load(slot, l, dc), lambda slot, l=l, dc=dc, s=s: mrg_run(slot, l, dc, s)))
            if l == 0:
                srcf = lambda tile, s=s: dv(dr["x"], (s * T + tile * 128) * D, [[D, 128], [1, D]])
                srck = lambda tile: []
            else:
                srcf = lambda tile: dv(xres[1], tile * 128 * D, [[D, 128], [1, D]])
                srck = lambda tile: ["xres1_p0", "xres1_p1"]
            units.append((lambda slot, l=l: wout_load1(slot, l), lambda slot: None))
            units.append((lambda slot, l=l: wout_load2(slot, l), lambda slot, l=l, s=s, srcf=srcf, srck=srck: wout_run(slot, l, s, srcf, srck), True))
            units.append((None, lambda slot, l=l, s=s: moe_init(l, s)))
            for e_ in range(NE):
                for hf in range(2):
                    units.append((lambda slot, l=l, e_=e_, hf=hf: moe_load(slot, l, e_, hf),
                                  lambda slot, l=l, e_=e_, hf=hf, s=s: moe_run(slot, l, e_, hf, s)))
            units.append((None, lambda slot, l=l, s=s: ln2_run(l, s, l == L - 1)))

    wl = [u for u in units if u[0] is not None]
    slot_of = {}
    k = 0
    for i, u in enumerate(units):
        if u[0] is not None:
            slot_of[i] = k % 2
            k += 1
    loaded = set()
    idxs = [i for i, u in enumerate(units) if u[0] is not None]

    def ensure_loaded(i):
        if i not in loaded:
            units[i][0](slot_of[i])
            loaded.add(i)

    kstop = int(os.environ.get("KSTOP", "100000"))
    for i, u in enumerate(units):
        if i >= kstop:
            break
        if u[0] is not None:
            ensure_loaded(i)
            nxt = [j for j in idxs if j > i]
            both = len(u) > 2
            if nxt and not both:
                ensure_loaded(nxt[0])
            u[1](slot_of[i])
            if nxt and both:
                ensure_loaded(nxt[0])
        else:
            u[1](None)

    P.emit(final_wait_ops=outs)
    st.close()
    return nc, (a64, a128, cs_np)


_CACHE = {}


def kernel(**inputs):
    NCORES = 8
    x = np.ascontiguousarray(inputs["x"], dtype=np.float32)
    Bt, T, _ = x.shape
    NSEQ = Bt // NCORES
    DEPTH = inputs["w_in"].shape[0]
    key = (NSEQ, T, DEPTH)
    if key not in _CACHE:
        _CACHE[key] = build(NSEQ, T, DEPTH, 512)
    nc, (a64, a128, cs_np) = _CACHE[key]
    shared = {k: np.ascontiguousarray(v, dtype=np.float32) for k, v in inputs.items() if k != "x"}
    shared["c64"] = a64
    shared["c128"] = a128
    shared["cs"] = cs_np
    in_maps = []
    for c in range(NCORES):
        m = dict(shared)
        m["x"] = np.ascontiguousarray(x[c * NSEQ:(c + 1) * NSEQ])
        in_maps.append(m)
    res = run_bass_kernel_spmd(nc, in_maps, core_ids=list(range(NCORES)))
    return np.concatenate([r["y"] for r in res.results], axis=0).astype(np.float32)
```
